# Optimizing a Trainium2 kernel written in Bass

```python
import math
import jax, jax.numpy as jnp
from jax import lax
import numpy as np

D_MODEL = 1024
BATCH = 2
SEQ = 8192
DEPTH = 2

RET_HEADS = 4
RET_HEAD_DIM = 64
RET_WIDTH = RET_HEADS * RET_HEAD_DIM
RET_CHUNK = 128
SSM_GROUP_CH = 16
SSM_GROUPS = 16
SSM_WIDTH = SSM_GROUPS * SSM_GROUP_CH
SSM_STATE = 64
MLA_HEADS = 4
MLA_NOPE = 128
MLA_ROPE = 64
MLA_V = 128
MLA_QK = MLA_NOPE + MLA_ROPE
MLA_WIDTH = MLA_HEADS * MLA_V
MLA_Q_RANK = 256
MLA_KV_RANK = 128
ATTN_BLOCK = 128

D_MIX = RET_WIDTH + SSM_WIDTH + MLA_WIDTH
ROPE_DIM = 64
ROPE_BASE = 10000.0

D_FF = 3584
N_EXPERTS = 8
TOP_K = 2
MOE_BLOCK = 128
N_DENSE = (DEPTH + 1) // 2
N_MOE = DEPTH // 2
EPS = 1e-6
NEG_INF = -1e30

IN_SPLITS = (RET_WIDTH, RET_WIDTH, RET_WIDTH, RET_WIDTH, SSM_WIDTH, MLA_Q_RANK, MLA_KV_RANK, MLA_ROPE)
IN_COLS = sum(IN_SPLITS)

kernel_name = 'hybrid_retnet_s5_mla_moe_block'


def rms_norm(x, g):
    xf = x.astype(jnp.float32)
    y = xf * lax.rsqrt(jnp.mean(xf * xf, axis=-1, keepdims=True) + EPS)
    return (y * g.astype(jnp.float32)).astype(x.dtype)


def rope_tables(positions):
    inv = ROPE_BASE ** (-jnp.arange(0, ROPE_DIM, 2, dtype=jnp.float32) / ROPE_DIM)
    ang = positions.astype(jnp.float32)[..., None] * inv
    return jnp.cos(ang), jnp.sin(ang)


def apply_rope(x, cos, sin):
    shp = cos.shape[:2] + (1,) * (x.ndim - 3) + cos.shape[2:]
    cos = cos.reshape(shp)
    sin = sin.reshape(shp)
    x1, x2 = jnp.split(x.astype(jnp.float32), 2, axis=-1)
    return jnp.concatenate([x1 * cos - x2 * sin, x2 * cos + x1 * sin], axis=-1).astype(x.dtype)


def retention(q, k, v, cos, sin):
    bsz, L, H, dk = q.shape
    dv = v.shape[-1]
    C = RET_CHUNK
    N = L // C
    q = apply_rope(q, cos, sin)
    k = apply_rope(k, cos, sin) * (dk ** -0.5)
    log_g = jnp.log1p(-(2.0 ** (-5.0 - jnp.arange(H, dtype=jnp.float32))))
    i = jnp.arange(C, dtype=jnp.float32)
    diff = i[:, None] - i[None, :]
    dmask = jnp.where(diff >= 0, jnp.exp(log_g[:, None, None] * jnp.maximum(diff, 0.0)), 0.0)
    qc = q.reshape(bsz, N, C, H, dk)
    kc = k.reshape(bsz, N, C, H, dk)
    vc = v.reshape(bsz, N, C, H, dv)
    s = jnp.einsum('bnihd,bnjhd->bnhij', qc, kc) * dmask
    inner = jnp.einsum('bnhij,bnjhe->bnihe', s, vc)
    k_dec = jnp.exp(log_g[None, :] * (C - 1 - i)[:, None])
    kv = jnp.einsum('bnjhd,bnjhe->nbhde', kc * k_dec[:, :, None], vc)
    chunk_decay = jnp.exp(log_g * C)[:, None, None]

    def step(state, kv_n):
        return chunk_decay * state + kv_n, state

    _, states = lax.scan(step, jnp.zeros((bsz, H, dk, dv), kv.dtype), kv)
    q_dec = jnp.exp(log_g[None, :] * (i + 1.0)[:, None])
    cross = jnp.einsum('bnihd,nbhde->bnihe', qc * q_dec[:, :, None], states)
    return (inner + cross).reshape(bsz, L, H, dv).astype(v.dtype)


def s5_ssm(u, a_re, a_im, b_re, b_im, c_re, c_im, d, log_dt, glu_w, glu_b):
    bsz, L, _ = u.shape
    uf = u.astype(jnp.float32).reshape(bsz, L, SSM_GROUPS, SSM_GROUP_CH)
    lam = lax.complex(a_re.astype(jnp.float32), a_im.astype(jnp.float32))
    dt = jnp.exp(log_dt.astype(jnp.float32))[:, None]
    lam_bar = jnp.exp(lam * dt)
    b = lax.complex(b_re.astype(jnp.float32), b_im.astype(jnp.float32))
    b_bar = ((lam_bar - 1.0) / lam)[..., None] * b
    bu = jnp.einsum('blgh,gph->blgp', uf.astype(jnp.complex64), b_bar)
    a = jnp.broadcast_to(lam_bar, bu.shape)

    def combine(e1, e2):
        a1, b1 = e1
        a2, b2 = e2
        return a1 * a2, a2 * b1 + b2

    _, states = lax.associative_scan(combine, (a, bu), axis=1)
    cm = lax.complex(c_re.astype(jnp.float32), c_im.astype(jnp.float32))
    y = jnp.real(jnp.einsum('blgp,ghp->blgh', states, cm)) + d.astype(jnp.float32) * uf
    y = jax.nn.gelu(y.reshape(bsz, L, SSM_WIDTH))
    y = y * jax.nn.sigmoid(y @ glu_w.astype(jnp.float32) + glu_b.astype(jnp.float32))
    return y.astype(u.dtype)


def mla(c_q, c_kv, k_rope, q_norm, w_uq, kv_norm, w_ukv, cos, sin):
    bsz, L, _ = c_q.shape
    q = (rms_norm(c_q, q_norm) @ w_uq).reshape(bsz, L, MLA_HEADS, MLA_QK)
    q = jnp.concatenate([q[..., :MLA_NOPE], apply_rope(q[..., MLA_NOPE:], cos, sin)], axis=-1)
    kv = (rms_norm(c_kv, kv_norm) @ w_ukv).reshape(bsz, L, MLA_HEADS, MLA_NOPE + MLA_V)
    k_pe = apply_rope(k_rope, cos, sin)
    k = jnp.concatenate([kv[..., :MLA_NOPE], jnp.broadcast_to(k_pe[:, :, None, :], (bsz, L, MLA_HEADS, MLA_ROPE))], axis=-1)
    v = kv[..., MLA_NOPE:]
    scale = MLA_QK ** -0.5
    nb = L // ATTN_BLOCK
    q_blocks = q.reshape(bsz, nb, ATTN_BLOCK, MLA_HEADS, MLA_QK).transpose(1, 0, 2, 3, 4)
    k_pos = jnp.arange(L)

    def attend(args):
        qb, n = args
        s = jnp.einsum('bqhd,bkhd->bhqk', qb, k, preferred_element_type=jnp.float32) * scale
        q_pos = n * ATTN_BLOCK + jnp.arange(ATTN_BLOCK)
        s = jnp.where(k_pos[None, :] <= q_pos[:, None], s, NEG_INF)
        p = jax.nn.softmax(s, axis=-1).astype(v.dtype)
        return jnp.einsum('bhqk,bkhd->bqhd', p, v)

    o = lax.map(attend, (q_blocks, jnp.arange(nb)))
    return o.transpose(1, 0, 2, 3, 4).reshape(bsz, L, MLA_WIDTH)


def hybrid_mixer(h, cos, sin, w_in, ret_norm, a_re, a_im, b_re, b_im, c_re, c_im, d, log_dt,
                 glu_w, glu_b, ssm_norm, q_norm, w_uq, kv_norm, w_ukv, mla_norm, w_out):
    bsz, L, _ = h.shape
    offsets = np.cumsum(IN_SPLITS)[:-1].tolist()
    rq, rk, rv, rg, u, cq, ckv, kr = jnp.split(h @ w_in, offsets, axis=-1)
    ret = retention(rq.reshape(bsz, L, RET_HEADS, RET_HEAD_DIM), rk.reshape(bsz, L, RET_HEADS, RET_HEAD_DIM),
                    rv.reshape(bsz, L, RET_HEADS, RET_HEAD_DIM), cos, sin)
    ret = rms_norm(ret, ret_norm.reshape(RET_HEADS, RET_HEAD_DIM)).reshape(bsz, L, RET_WIDTH)
    ret = jax.nn.silu(rg) * ret
    ssm = rms_norm(s5_ssm(u, a_re, a_im, b_re, b_im, c_re, c_im, d, log_dt, glu_w, glu_b), ssm_norm)
    att = rms_norm(mla(cq, ckv, kr, q_norm, w_uq, kv_norm, w_ukv, cos, sin), mla_norm)
    return jnp.concatenate([ret, ssm, att], axis=-1) @ w_out


def swiglu(h, wg, wu, wd):
    return (jax.nn.silu(h @ wg) * (h @ wu)) @ wd


def routed_ffn(h, router, wg, wu, wd):
    bsz, L, D = h.shape
    t = h.reshape(-1, D)
    T = t.shape[0]
    A = T * TOP_K
    logits = (t @ router).astype(jnp.float32)
    top_v, top_i = lax.top_k(logits, TOP_K)
    top_w = jax.nn.softmax(top_v, axis=-1)
    expert = top_i.reshape(-1)
    tok = jnp.repeat(jnp.arange(T), TOP_K)
    order = jnp.argsort(expert)
    e_sorted = expert[order]
    tok_sorted = tok[order]
    w_sorted = top_w.reshape(-1)[order]
    counts = jnp.bincount(expert, length=N_EXPERTS)
    padded = ((counts + MOE_BLOCK - 1) // MOE_BLOCK) * MOE_BLOCK
    pad_end = jnp.cumsum(padded)
    pad_start = pad_end - padded
    raw_start = jnp.cumsum(counts) - counts
    dest = pad_start[e_sorted] + (jnp.arange(A) - raw_start[e_sorted])
    n_rows = A + N_EXPERTS * MOE_BLOCK
    n_blocks = n_rows // MOE_BLOCK
    x_buf = jnp.zeros((n_rows, D), t.dtype).at[dest].set(t[tok_sorted])
    block_start = jnp.arange(n_blocks) * MOE_BLOCK
    block_expert = jnp.clip(jnp.searchsorted(pad_end, block_start, side='right'), 0, N_EXPERTS - 1)

    def expert_block(args):
        xb, e = args
        return swiglu(xb, wg[e], wu[e], wd[e])

    y_buf = lax.map(expert_block, (x_buf.reshape(n_blocks, MOE_BLOCK, D), block_expert)).reshape(n_rows, D)
    y = jnp.zeros_like(t).at[tok_sorted].add(y_buf[dest] * w_sorted[:, None].astype(t.dtype))
    return y.reshape(bsz, L, D)


def setup_inputs(seed: int = 0) -> dict:
    key = jax.random.key(seed)
    ks = iter(jax.random.split(key, 64))

    def nrm(shape, scale):
        return jax.random.normal(next(ks), shape, jnp.float32) * scale

    def gain(shape):
        return 1.0 + 0.02 * jax.random.normal(next(ks), shape, jnp.float32)

    G, P, Hg = SSM_GROUPS, SSM_STATE, SSM_GROUP_CH
    x = nrm((BATCH, SEQ, D_MODEL), 1.0)
    c = nrm((BATCH, D_MODEL), 1.0)
    positions = jnp.broadcast_to(jnp.arange(SEQ, dtype=jnp.int32), (BATCH, SEQ))
    ada_w = nrm((DEPTH, D_MODEL, 6 * D_MODEL), 0.5 * D_MODEL ** -0.5)
    ada_b = nrm((DEPTH, 6 * D_MODEL), 0.01)
    norm_pre_mix = gain((DEPTH, D_MODEL))
    norm_post_mix = gain((DEPTH, D_MODEL))
    norm_pre_ffn = gain((DEPTH, D_MODEL))
    norm_post_ffn = gain((DEPTH, D_MODEL))
    w_in = nrm((DEPTH, D_MODEL, IN_COLS), D_MODEL ** -0.5)
    ret_norm = gain((DEPTH, RET_WIDTH))
    ssm_a_re = -0.5 + 0.01 * jax.random.normal(next(ks), (DEPTH, G, P), jnp.float32)
    ssm_a_im = math.pi * jnp.arange(P, dtype=jnp.float32) + 0.01 * jax.random.normal(next(ks), (DEPTH, G, P), jnp.float32)
    ssm_b_re = nrm((DEPTH, G, P, Hg), (2.0 * Hg) ** -0.5)
    ssm_b_im = nrm((DEPTH, G, P, Hg), (2.0 * Hg) ** -0.5)
    ssm_c_re = nrm((DEPTH, G, Hg, P), (2.0 * P) ** -0.5)
    ssm_c_im = nrm((DEPTH, G, Hg, P), (2.0 * P) ** -0.5)
    ssm_d = nrm((DEPTH, G, Hg), 1.0)
    ssm_log_dt = jax.random.uniform(next(ks), (DEPTH, G), jnp.float32, minval=math.log(1e-3), maxval=math.log(1e-1))
    ssm_glu_w = nrm((DEPTH, SSM_WIDTH, SSM_WIDTH), SSM_WIDTH ** -0.5)
    ssm_glu_b = nrm((DEPTH, SSM_WIDTH), 0.01)
    ssm_norm = gain((DEPTH, SSM_WIDTH))
    mla_q_norm = gain((DEPTH, MLA_Q_RANK))
    mla_w_uq = nrm((DEPTH, MLA_Q_RANK, MLA_HEADS * MLA_QK), MLA_Q_RANK ** -0.5)
    mla_kv_norm = gain((DEPTH, MLA_KV_RANK))
    mla_w_ukv = nrm((DEPTH, MLA_KV_RANK, MLA_HEADS * (MLA_NOPE + MLA_V)), MLA_KV_RANK ** -0.5)
    mla_norm = gain((DEPTH, MLA_WIDTH))
    w_out = nrm((DEPTH, D_MIX, D_MODEL), D_MIX ** -0.5)
    ffn_w_gate = nrm((N_DENSE, D_MODEL, D_FF), D_MODEL ** -0.5)
    ffn_w_up = nrm((N_DENSE, D_MODEL, D_FF), D_MODEL ** -0.5)
    ffn_w_down = nrm((N_DENSE, D_FF, D_MODEL), D_FF ** -0.5)
    moe_router = nrm((N_MOE, D_MODEL, N_EXPERTS), D_MODEL ** -0.5)
    moe_w_gate = nrm((N_MOE, N_EXPERTS, D_MODEL, D_FF), D_MODEL ** -0.5)
    moe_w_up = nrm((N_MOE, N_EXPERTS, D_MODEL, D_FF), D_MODEL ** -0.5)
    moe_w_down = nrm((N_MOE, N_EXPERTS, D_FF, D_MODEL), D_FF ** -0.5)
    return {'x': x, 'c': c, 'positions': positions, 'ada_w': ada_w, 'ada_b': ada_b,
            'norm_pre_mix': norm_pre_mix, 'norm_post_mix': norm_post_mix,
            'norm_pre_ffn': norm_pre_ffn, 'norm_post_ffn': norm_post_ffn,
            'w_in': w_in, 'ret_norm': ret_norm,
            'ssm_a_re': ssm_a_re, 'ssm_a_im': ssm_a_im, 'ssm_b_re': ssm_b_re, 'ssm_b_im': ssm_b_im,
            'ssm_c_re': ssm_c_re, 'ssm_c_im': ssm_c_im, 'ssm_d': ssm_d, 'ssm_log_dt': ssm_log_dt,
            'ssm_glu_w': ssm_glu_w, 'ssm_glu_b': ssm_glu_b, 'ssm_norm': ssm_norm,
            'mla_q_norm': mla_q_norm, 'mla_w_uq': mla_w_uq, 'mla_kv_norm': mla_kv_norm,
            'mla_w_ukv': mla_w_ukv, 'mla_norm': mla_norm, 'w_out': w_out,
            'ffn_w_gate': ffn_w_gate, 'ffn_w_up': ffn_w_up, 'ffn_w_down': ffn_w_down,
            'moe_router': moe_router, 'moe_w_gate': moe_w_gate, 'moe_w_up': moe_w_up,
            'moe_w_down': moe_w_down}


def reference(x, c, positions, ada_w, ada_b, norm_pre_mix, norm_post_mix, norm_pre_ffn, norm_post_ffn,
              w_in, ret_norm, ssm_a_re, ssm_a_im, ssm_b_re, ssm_b_im, ssm_c_re, ssm_c_im, ssm_d,
              ssm_log_dt, ssm_glu_w, ssm_glu_b, ssm_norm, mla_q_norm, mla_w_uq, mla_kv_norm,
              mla_w_ukv, mla_norm, w_out, ffn_w_gate, ffn_w_up, ffn_w_down, moe_router,
              moe_w_gate, moe_w_up, moe_w_down):
    cos, sin = rope_tables(positions)
    cond = jax.nn.silu(c)
    for layer in range(DEPTH):
        mod = (cond @ ada_w[layer] + ada_b[layer])[:, None, :]
        sh_m, sc_m, gt_m, sh_f, sc_f, gt_f = jnp.split(mod, 6, axis=-1)
        h = rms_norm(x, norm_pre_mix[layer]) * (1.0 + sc_m) + sh_m
        y = hybrid_mixer(h, cos, sin, w_in[layer], ret_norm[layer], ssm_a_re[layer], ssm_a_im[layer],
                         ssm_b_re[layer], ssm_b_im[layer], ssm_c_re[layer], ssm_c_im[layer], ssm_d[layer],
                         ssm_log_dt[layer], ssm_glu_w[layer], ssm_glu_b[layer], ssm_norm[layer],
                         mla_q_norm[layer], mla_w_uq[layer], mla_kv_norm[layer], mla_w_ukv[layer],
                         mla_norm[layer], w_out[layer])
        x = x + gt_m * rms_norm(y, norm_post_mix[layer])
        h = rms_norm(x, norm_pre_ffn[layer]) * (1.0 + sc_f) + sh_f
        j = layer // 2
        if layer % 2 == 0:
            y = swiglu(h, ffn_w_gate[j], ffn_w_up[j], ffn_w_down[j])
        else:
            y = routed_ffn(h, moe_router[j], moe_w_gate[j], moe_w_up[j], moe_w_down[j])
        x = x + gt_f * rms_norm(y, norm_post_ffn[layer])
    return x
```

```python
import math
from contextlib import ExitStack

import numpy as np
import concourse.bass as bass
import concourse.mybir as mybir
from concourse.bass_utils import run_bass_kernel_spmd

F32 = mybir.dt.float32
BF16 = mybir.dt.bfloat16
I32 = mybir.dt.int32
ALU = mybir.AluOpType
AF = mybir.ActivationFunctionType
AX = mybir.AxisListType

D = 1024
L = 8192
NT = 64
EPS = 1e-6
TWO_PI = 2.0 * math.pi
D_FF = 3584
NFC = 28
N_EXP = 8


class T:
    def __init__(self, t, name, root=None):
        self.t = t
        self.name = name
        self.lastw = None
        self.readers = {}
        self.root = self if root is None else root.root
        self.excl = False

    def __getitem__(self, idx):
        return self.t[idx]


class Prog:
    def __init__(self, nc, es, ndma_sems=8):
        self.nc = nc
        self.es = es
        self.engs = {'pe': nc.tensor, 'act': nc.scalar, 'dve': nc.vector, 'pool': nc.gpsimd, 'sp': nc.sync}
        self.sem = {k: es.enter_context(nc.semaphore('s_' + k)) for k in self.engs}
        self.cnt = {k: 0 for k in self.engs}
        self.waited = {k: {} for k in self.engs}
        self.dma_sems = {}
        for q in ('sp', 'pool'):
            self.dma_sems[q] = [[es.enter_context(nc.semaphore('d_%s%d' % (q, i))), 0] for i in range(ndma_sems)]
        self.dma_i = {q: 0 for q in self.dma_sems}
        self.cc_sem = es.enter_context(nc.semaphore('cc_sem'))
        self.cc_n = 0
        self.nbuf = 0
        self.tes = None
        self.pre = {}

    def sb(self, shape, dt, name=None, keep=False):
        if keep and name in self.pre:
            assert list(self.pre[name].t.shape) == list(shape), name
            return self.pre[name]
        assert not (keep and self.tes is not None), "persistent buffer %s must be pre-allocated" % name
        self.nbuf += 1
        uname = (name or 'b') + '_%d' % self.nbuf
        es = self.es if (keep or self.tes is None) else self.tes
        t = T(es.enter_context(self.nc.sbuf_tensor(uname, shape, dt)), uname)
        if keep:
            self.pre[name] = t
        return t

    def ps(self, shape, dt, name=None):
        self.nbuf += 1
        name = name or ('p%d' % self.nbuf)
        t = T(self.es.enter_context(self.nc.psum_tensor(name, shape, dt)), name)
        t.excl = True
        return t

    def view(self, ap, name='v', parent=None):
        return T(ap, name, root=parent)

    def _wait(self, engname, ev):
        sem, val, src = ev
        if src == 'pe' and engname == 'pe':
            return
        key = id(sem)
        w = self.waited[engname]
        if w.get(key, 0) >= val:
            return
        w[key] = val
        self.engs[engname].wait_ge(sem, val)

    def _deps(self, engname, r, w):
        for b in r:
            b = b.root
            if b.lastw is not None:
                self._wait(engname, b.lastw)
        for b in w:
            b = b.root
            if b.lastw is not None:
                self._wait(engname, b.lastw)
            for ev in b.readers.values():
                self._wait(engname, ev)

    def _commit(self, ev, r, w):
        for b in r:
            b = b.root
            old = b.readers.get(id(ev[0]))
            if old is None or old[1] < ev[1]:
                b.readers[id(ev[0])] = ev
        for b in w:
            b = b.root
            b.lastw = ev
            b.readers = {}

    def op(self, engname, fn, r=(), w=()):
        xr = [b for b in r if b.root.excl]
        if xr:
            r = [b for b in r if not b.root.excl]
            w = list(w) + xr
        self._deps(engname, r, w)
        ins = fn(self.engs[engname])
        self.cnt[engname] += 1
        ins.then_inc(self.sem[engname], 1)
        ev = (self.sem[engname], self.cnt[engname], engname)
        self._commit(ev, r, w)
        return ev

    def dma(self, q, out, in_, r=(), w=(), **kw):
        if out.dtype != in_.dtype:
            q = 'pool'
        self._deps(q, r, w)
        slot = self.dma_sems[q][self.dma_i[q] % len(self.dma_sems[q])]
        self.dma_i[q] += 1
        sem, n = slot
        if n > 0:
            self._wait(q, (sem, 16 * n, 'dma'))
        slot[1] = n + 1
        self.engs[q].dma_start(out=out, in_=in_, **kw).then_inc(sem, 16)
        ev = (sem, 16 * (n + 1), 'dma')
        self._commit(ev, r, w)
        return ev

    def coll(self, kind, src_ap, dst, dst_ap, groups):
        self.nc.gpsimd.collective_compute(kind, ALU.bypass, replica_groups=groups, ins=[src_ap], outs=[dst_ap]).then_inc(self.cc_sem, 1)
        self.cc_n += 1
        ev = (self.cc_sem, self.cc_n, 'cc')
        self._commit(ev, [], [dst])
        return ev

    def barrier(self):
        for e in self.engs:
            for o in self.engs:
                if o != e and self.cnt[o] > 0:
                    self._wait(e, (self.sem[o], self.cnt[o], 'x'))
            for q in self.dma_sems:
                for sem, n in self.dma_sems[q]:
                    if n > 0:
                        self._wait(e, (sem, 16 * n, 'dma'))
            if self.cc_n > 0:
                self._wait(e, (self.cc_sem, self.cc_n, 'cc'))

    def finish(self):
        self.barrier()
        for q in self.dma_sems:
            for sem, n in self.dma_sems[q]:
                if n > 0:
                    self._wait('sp', (sem, 16 * n, 'dma'))

    def mm(self, out, oap, lhsT, lap, rhs, rap, start=True, stop=True):
        return self.op('pe', lambda e: e.matmul(oap, lhsT=lap, rhs=rap, start=start, stop=stop),
                       r=[lhsT, rhs], w=[out])

    def tr(self, out, oap, src, sap, ident):
        n = sap.shape[0]
        return self.op('pe', lambda e: e.transpose(oap, sap, ident[0:n, 0:n]), r=[src, ident], w=[out])

    def tt(self, eng, out, oap, a, aap, b, bap, op):
        return self.op(eng, lambda e: e.tensor_tensor(out=oap, in0=aap, in1=bap, op=op), r=[a, b], w=[out])

    def ts(self, eng, out, oap, a, aap, s1, op0, s2=None, op1=None, extra_r=()):
        if op1 is None:
            return self.op(eng, lambda e: e.tensor_scalar(out=oap, in0=aap, scalar1=s1, scalar2=None, op0=op0),
                           r=[a] + list(extra_r), w=[out])
        return self.op(eng, lambda e: e.tensor_scalar(out=oap, in0=aap, scalar1=s1, scalar2=s2, op0=op0, op1=op1),
                       r=[a] + list(extra_r), w=[out])

    def act(self, out, oap, a, aap, func, extra_r=(), extra_w=(), **kw):
        return self.op('act', lambda e: e.activation(out=oap, in_=aap, func=func, **kw),
                       r=[a] + list(extra_r), w=[out] + list(extra_w))

    def cp(self, eng, out, oap, a, aap):
        if eng == 'act':
            return self.act(out, oap, a, aap, AF.Copy)
        return self.op(eng, lambda e: e.tensor_copy(out=oap, in_=aap), r=[a], w=[out])

    def rstd(self, ss, n, tmp):
        self.ts('dve', ss, ss[:], ss, ss[:], 1.0 / n, ALU.mult, EPS, ALU.add)
        self.act(ss, ss[:], ss, ss[:], AF.Sqrt)
        self.op('dve', lambda e: e.reciprocal(out=ss[:], in_=ss[:]), r=[ss], w=[ss])

    def range_reduce(self, out, oap, src, sap, kf, kfap, ki, kiap, shift=0.0):
        self.ts('dve', kf, kfap, src, sap, 1.0 / TWO_PI, ALU.mult, shift / TWO_PI, ALU.add)
        self.cp('dve', ki, kiap, kf, kfap)
        self.cp('dve', kf, kfap, ki, kiap)
        self.op('dve', lambda e: e.scalar_tensor_tensor(out=oap, in0=kfap, scalar=-TWO_PI, in1=sap,
                                                        op0=ALU.mult, op1=ALU.add), r=[kf, src], w=[out])
        if shift != 0.0:
            self.ts('dve', out, oap, out, oap, shift, ALU.add)
        self.ts('dve', out, oap, out, oap, -math.pi, ALU.max, math.pi, ALU.min)


def make_stager(P, nstage=4, cols=1024):
    stg = [P.sb([128, cols], F32, 'stage%d' % i) for i in range(nstage)]
    cnt = [0]
    engs = ('dve', 'act', 'pool')

    def load(dst, dst_ap, src_ap):
        n = src_ap.shape[-1]
        sg = stg[cnt[0] % nstage]
        eng = engs[cnt[0] % 3]
        cnt[0] += 1
        P.dma('sp', sg[:, 0:n], src_ap, w=[sg])
        P.cp(eng, dst, dst_ap, sg, sg[:, 0:n])
    return load


def make_ident(P):
    identf = P.sb([128, 128], F32, 'identf')
    ident = P.sb([128, 128], BF16, 'ident', keep=True)
    P.op('pool', lambda e: e.memset(identf[:], 1.0), w=[identf])
    P.op('pool', lambda e: e.affine_select(out=identf[:], in_=identf[:], pattern=[[-1, 128]],
                                           compare_op=ALU.is_equal, fill=0.0, base=0, channel_multiplier=1),
         r=[identf], w=[identf])
    P.cp('dve', ident, ident[:], identf, identf[:])
    return ident, identf


def adaln_mod(P, nc, cvec, adaw, adab, ncol_chunks, modps, name, stager):
    cv = P.sb([128, 8], F32, name + '_cv')
    cvb = P.sb([128, 8], BF16, name + '_cvb')
    P.dma('sp', cv[:], cvec[:, :], w=[cv])
    P.act(cvb, cvb[:], cv, cv[:], AF.Silu)
    ncols = ncol_chunks * 128
    mod = P.sb([128, ncol_chunks], F32, name + '_mod', keep=True)
    ab = P.sb([128, ncol_chunks], F32, name + '_ab')
    P.dma('sp', ab[:], adab[:, :], w=[ab])
    cw = 1024
    wts = [P.sb([128, 8, cw], BF16, name + '_w%d' % i) for i in range(2)]
    for ci in range(ncols // cw):
        wt = wts[ci % 2]
        for kc in range(8):
            stager(wt, wt[:, kc, :], adaw[:, kc, ci * cw:(ci + 1) * cw])
        for cc in range(cw // 128):
            ch = ci * (cw // 128) + cc
            for kc in range(8):
                P.mm(modps, modps[:, ch:ch + 1], wt, wt[:, kc, cc * 128:(cc + 1) * 128], cvb, cvb[:, kc:kc + 1],
                     start=(kc == 0), stop=(kc == 7))
    P.tt('dve', mod, mod[:], modps, modps[:, 0:ncol_chunks], ab, ab[:], ALU.add)
    return mod


import os
STOP_AT = float(os.environ.get('STOP_AT', '99'))


def decl_A(nc, sfx):
    io = {}
    io['cvec'] = nc.dram_tensor('cvec' + sfx, [128, 8], F32, kind='ExternalInput').ap()
    io['pos'] = nc.dram_tensor('pos' + sfx, [128, NT], I32, kind='ExternalInput').ap()
    io['adaw'] = nc.dram_tensor('adaw' + sfx, [128, 8, 2048], F32, kind='ExternalInput').ap()
    io['adab'] = nc.dram_tensor('adab' + sfx, [128, 16], F32, kind='ExternalInput').ap()
    io['gpre'] = nc.dram_tensor('gpre' + sfx, [128, 8], F32, kind='ExternalInput').ap()
    io['wsel'] = nc.dram_tensor('wsel' + sfx, [128, 8, 768], F32, kind='ExternalInput').ap()
    io['retg'] = nc.dram_tensor('retg' + sfx, [1, 64], F32, kind='ExternalInput').ap()
    io['invf'] = nc.dram_tensor('invf' + sfx, [1, 32], F32, kind='ExternalInput').ap()
    io['dmaskT_d'] = nc.dram_tensor('dmaskT' + sfx, [128, 128], F32, kind='ExternalInput').ap()
    io['tri_d'] = nc.dram_tensor('tri' + sfx, [128, 128], F32, kind='ExternalInput').ap()
    io['qdec_d'] = nc.dram_tensor('qdec' + sfx, [64, 128], F32, kind='ExternalInput').ap()
    io['kdec_d'] = nc.dram_tensor('kdec' + sfx, [128, 1], F32, kind='ExternalInput').ap()
    io['dcy_d'] = nc.dram_tensor('dcy' + sfx, [64, 1], F32, kind='ExternalInput').ap()
    io['cmask_d'] = nc.dram_tensor('cmask' + sfx, [128, 128], F32, kind='ExternalInput').ap()
    io['jp1_d'] = nc.dram_tensor('jp1' + sfx, [128, 1], F32, kind='ExternalInput').ap()
    io['irow1_d'] = nc.dram_tensor('irow1' + sfx, [128, 128], F32, kind='ExternalInput').ap()
    io['are_tm_d'] = nc.dram_tensor('are_tm' + sfx, [1, 512], F32, kind='ExternalInput').ap()
    io['aim_tm_d'] = nc.dram_tensor('aim_tm' + sfx, [1, 512], F32, kind='ExternalInput').ap()
    io['ldt_tm_d'] = nc.dram_tensor('ldt_tm' + sfx, [1, 512], F32, kind='ExternalInput').ap()
    io['BR_d'] = nc.dram_tensor('BR' + sfx, [64, 512], F32, kind='ExternalInput').ap()
    io['BI_d'] = nc.dram_tensor('BI' + sfx, [64, 512], F32, kind='ExternalInput').ap()
    io['are_sm_d'] = nc.dram_tensor('are_sm' + sfx, [128, 2], F32, kind='ExternalInput').ap()
    io['aim_sm_d'] = nc.dram_tensor('aim_sm' + sfx, [128, 2], F32, kind='ExternalInput').ap()
    io['ldt_sm_d'] = nc.dram_tensor('ldt_sm' + sfx, [128, 2], F32, kind='ExternalInput').ap()
    io['CR_d'] = nc.dram_tensor('CR' + sfx, [128, 2, 64], F32, kind='ExternalInput').ap()
    io['CI_d'] = nc.dram_tensor('CI' + sfx, [128, 2, 64], F32, kind='ExternalInput').ap()
    io['DD_d'] = nc.dram_tensor('DD' + sfx, [64, 64], F32, kind='ExternalInput').ap()
    io['qng_d'] = nc.dram_tensor('qng' + sfx, [128, 2], F32, kind='ExternalInput').ap()
    io['wuq_d'] = nc.dram_tensor('wuq' + sfx, [128, 2, 192], F32, kind='ExternalInput').ap()
    io['kvng_d'] = nc.dram_tensor('kvng' + sfx, [128, 1], F32, kind='ExternalInput').ap()
    io['wukv_d'] = nc.dram_tensor('wukv' + sfx, [128, 256], F32, kind='ExternalInput').ap()
    return io


def emit_A(P, nc, banks, io, xrow, hx, ntiles=NT, xdep=None, on_chunk=None):
    cvec = io['cvec']
    pos = io['pos']
    adaw = io['adaw']
    adab = io['adab']
    gpre = io['gpre']
    wsel = io['wsel']
    retg = io['retg']
    invf = io['invf']
    dmaskT_d = io['dmaskT_d']
    tri_d = io['tri_d']
    qdec_d = io['qdec_d']
    kdec_d = io['kdec_d']
    dcy_d = io['dcy_d']
    cmask_d = io['cmask_d']
    jp1_d = io['jp1_d']
    irow1_d = io['irow1_d']
    are_tm_d = io['are_tm_d']
    aim_tm_d = io['aim_tm_d']
    ldt_tm_d = io['ldt_tm_d']
    BR_d = io['BR_d']
    BI_d = io['BI_d']
    are_sm_d = io['are_sm_d']
    aim_sm_d = io['aim_sm_d']
    ldt_sm_d = io['ldt_sm_d']
    CR_d = io['CR_d']
    CI_d = io['CI_d']
    DD_d = io['DD_d']
    qng_d = io['qng_d']
    wuq_d = io['wuq_d']
    kvng_d = io['kvng_d']
    wukv_d = io['wukv_d']
    st = ExitStack()
    old_es = P.es
    P.es = st
    P.pre = {}
    b7 = banks[7][:, :].bitcast(BF16)
    TB = [P.view(b7[:, 0:512], 'TB', parent=banks[7])]
    TS = [P.view(b7[:, 256 + 128 * k:384 + 128 * k], 'TS%d' % k, parent=banks[7]) for k in range(2)]
    PJ0 = P.view(banks[0][:, :], 'PJ0', parent=banks[0])
    PJ1 = P.view(banks[1][:, 0:256], 'PJ1', parent=banks[1])
    RS = P.view(banks[1][:, 256:384], 'RS', parent=banks[1])
    RO = P.view(banks[1][:, 384:448], 'RO', parent=banks[1])
    RKV = P.view(banks[1][0:64, 448:512], 'RKV', parent=banks[1])
    SA = P.view(banks[2][:, :], 'SA', parent=banks[2])
    SB = P.view(banks[3][:, :], 'SB', parent=banks[3])
    QTN = P.view(banks[4][:, 0:128], 'QTN', parent=banks[4])
    KTN = P.view(banks[4][:, 128:256], 'KTN', parent=banks[4])
    VP = P.view(banks[4][:, 256:384], 'VP', parent=banks[4])
    QRP = P.view(banks[4][:, 384:448], 'QRP', parent=banks[4])
    SY = P.view(banks[4][:, 448:512], 'SY', parent=banks[4])
    SC = [P.view(banks[5][:, :], 'SC0', parent=banks[5]), P.view(banks[6][:, :], 'SC1', parent=banks[6])]

    for nm, shp, dt_ in [('ident', [128, 128], BF16), ('ada_mod', [128, 16], F32), ('scale1', [128, 8], F32),
                         ('wselb', [128, 8, 768], BF16), ('retg_t', [128, 64], F32), ('dmaskT', [128, 128], F32),
                         ('tri', [128, 128], BF16), ('qdec', [64, 128], F32), ('kdec', [128, 1], F32),
                         ('dcy', [64, 1], F32), ('cmask', [128, 128], F32), ('SINT', [128, NT * 32], F32),
                         ('COST', [128, NT * 32], F32), ('NSINT', [128, NT * 32], F32), ('EA', [128, 512], F32),
                         ('EB', [128, 512], F32), ('Bmat', [64, 512], BF16), ('Bswp', [64, 512], BF16),
                         ('EA2', [128, 512], F32), ('EB2', [128, 512], F32), ('Cmat', [128, 4, 64], BF16),
                         ('DDb', [64, 64], BF16), ('xprev', [128, 4], F32), ('xprevs', [128, 4], F32),
                         ('wuqb', [128, 2, 192], BF16), ('wukvb', [128, 256], BF16)]:
        P.sb(shp, dt_, nm, keep=True)
    tes = ExitStack()
    P.tes = tes
    ident, identf = make_ident(P)

    stager = make_stager(P)
    mod = adaln_mod(P, nc, cvec, adaw, adab, 16, SA, 'ada', stager)
    gp = P.sb([128, 8], F32, 'gp')
    P.dma('sp', gp[:], gpre[:, :], w=[gp])
    scale1 = P.sb([128, 8], F32, 'scale1', keep=True)
    P.ts('dve', scale1, scale1[:], mod, mod[:, 8:16], 1.0, ALU.add)
    P.tt('dve', scale1, scale1[:], scale1, scale1[:], gp, gp[:], ALU.mult)
    shm = P.view(mod[:, 0:8], 'shm', parent=mod)

    wselb = P.sb([128, 8, 768], BF16, 'wselb', keep=True)
    for kc in range(8):
        stager(wselb, wselb[:, kc, :], wsel[:, kc, :])

    def load(name, src, shape, dt=F32, q='sp', bcast=None, keep=False):
        t = P.sb(shape, dt, name, keep=keep)
        P.dma(q, t[:], src if bcast is None else src.broadcast_to(bcast), w=[t])
        return t

    retg_t = load('retg_t', retg[0:1, :], [128, 64], bcast=[128, 64], keep=True)
    invf_t = load('invf_t', invf[0:1, :], [128, 32], bcast=[128, 32])
    dmaskT = load('dmaskT', dmaskT_d[:, :], [128, 128], keep=True)
    tri_f = load('tri_f', tri_d[:, :], [128, 128])
    tri = P.sb([128, 128], BF16, 'tri', keep=True)
    P.cp('dve', tri, tri[:], tri_f, tri_f[:])
    qdec = load('qdec', qdec_d[:, :], [64, 128], keep=True)
    kdec = load('kdec', kdec_d[:, :], [128, 1], keep=True)
    dcy = load('dcy', dcy_d[:, :], [64, 1], keep=True)
    cmask = load('cmask', cmask_d[:, :], [128, 128], keep=True)
    jp1 = load('jp1', jp1_d[:, :], [128, 1])
    irow1 = load('irow1', irow1_d[:, :], [128, 128])

    posi = load('posi', pos[:, :], [128, NT], I32)
    posf = P.sb([128, NT], F32, 'posf')
    P.cp('dve', posf, posf[:], posi, posi[:])
    NR = NT * 32
    ang = P.sb([128, NR], F32, 'ang')
    P.tt('dve', ang, ang[:].rearrange("p (t f) -> p t f", f=32), posf,
         posf[:].unsqueeze(2).broadcast_to([128, NT, 32]), invf_t,
         invf_t[:].unsqueeze(1).broadcast_to([128, NT, 32]), ALU.mult)
    SINT = P.sb([128, NR], F32, 'SINT', keep=True)
    COST = P.sb([128, NR], F32, 'COST', keep=True)
    NSINT = P.sb([128, NR], F32, 'NSINT', keep=True)
    rkf = P.sb([128, NR], F32, 'rkf')
    rki = P.sb([128, NR], I32, 'rki')
    P.range_reduce(SINT, SINT[:], ang, ang[:], rkf, rkf[:], rki, rki[:])
    P.act(SINT, SINT[:], SINT, SINT[:], AF.Sin)
    P.range_reduce(COST, COST[:], ang, ang[:], rkf, rkf[:], rki, rki[:], shift=math.pi / 2)
    P.act(COST, COST[:], COST, COST[:], AF.Sin)
    P.ts('dve', NSINT, NSINT[:], SINT, SINT[:], -1.0, ALU.mult)

    are_tm = load('are_tm', are_tm_d[0:1, :], [128, 512], bcast=[128, 512])
    aim_tm = load('aim_tm', aim_tm_d[0:1, :], [128, 512], bcast=[128, 512])
    dt_tm = load('dt_tm', ldt_tm_d[0:1, :], [128, 512], bcast=[128, 512])
    P.act(dt_tm, dt_tm[:], dt_tm, dt_tm[:], AF.Exp)
    rho = P.sb([128, 512], F32, 'rho')
    tht = P.sb([128, 512], F32, 'tht')
    P.tt('dve', rho, rho[:], are_tm, are_tm[:], dt_tm, dt_tm[:], ALU.mult)
    P.tt('dve', tht, tht[:], aim_tm, aim_tm[:], dt_tm, dt_tm[:], ALU.mult)
    s5a = P.sb([128, 512], F32, 's5a')
    s5b = P.sb([128, 512], F32, 's5b')
    s5c = P.sb([128, 512], F32, 's5c')
    s5k = P.sb([128, 512], F32, 's5k')
    s5i = P.sb([128, 512], I32, 's5i')
    EA = P.sb([128, 512], F32, 'EA', keep=True)
    EB = P.sb([128, 512], F32, 'EB', keep=True)
    njp1 = P.sb([128, 1], F32, 'njp1')
    P.ts('dve', njp1, njp1[:], jp1, jp1[:], -1.0, ALU.mult)
    P.ts('dve', s5a, s5a[:], rho, rho[:], njp1[:, 0:1], ALU.mult, extra_r=[njp1])
    P.act(s5a, s5a[:], s5a, s5a[:], AF.Exp)
    P.ts('dve', s5b, s5b[:], tht, tht[:], jp1[:, 0:1], ALU.mult, extra_r=[jp1])
    P.range_reduce(s5c, s5c[:], s5b, s5b[:], s5k, s5k[:], s5i, s5i[:], shift=math.pi / 2)
    P.act(s5c, s5c[:], s5c, s5c[:], AF.Sin)
    P.tt('dve', EA, EA[:], s5a, s5a[:], s5c, s5c[:], ALU.mult)
    P.range_reduce(s5c, s5c[:], s5b, s5b[:], s5k, s5k[:], s5i, s5i[:])
    P.act(s5c, s5c[:], s5c, s5c[:], AF.Sin)
    P.tt('dve', EB, EB[:], s5a, s5a[:], s5c, s5c[:], ALU.mult)
    P.ts('dve', EB, EB[:], EB, EB[:], -1.0, ALU.mult)
    Lre = P.sb([128, 512], F32, 'Lre')
    Lim = P.sb([128, 512], F32, 'Lim')
    P.act(s5a, s5a[:], rho, rho[:], AF.Exp)
    P.range_reduce(s5c, s5c[:], tht, tht[:], s5k, s5k[:], s5i, s5i[:], shift=math.pi / 2)
    P.act(s5c, s5c[:], s5c, s5c[:], AF.Sin)
    P.tt('dve', Lre, Lre[:], s5a, s5a[:], s5c, s5c[:], ALU.mult)
    P.range_reduce(s5c, s5c[:], tht, tht[:], s5k, s5k[:], s5i, s5i[:])
    P.act(s5c, s5c[:], s5c, s5c[:], AF.Sin)
    P.tt('dve', Lim, Lim[:], s5a, s5a[:], s5c, s5c[:], ALU.mult)
    P.ts('dve', Lre, Lre[:], Lre, Lre[:], -1.0, ALU.add)
    P.tt('dve', s5a, s5a[:], are_tm, are_tm[:], are_tm, are_tm[:], ALU.mult)
    P.tt('dve', s5b, s5b[:], aim_tm, aim_tm[:], aim_tm, aim_tm[:], ALU.mult)
    P.tt('dve', s5a, s5a[:], s5a, s5a[:], s5b, s5b[:], ALU.add)
    P.op('dve', lambda e: e.reciprocal(out=s5a[:], in_=s5a[:]), r=[s5a], w=[s5a])
    Fre = P.sb([128, 512], F32, 'Fre')
    Fim = P.sb([128, 512], F32, 'Fim')
    P.tt('dve', s5b, s5b[:], Lre, Lre[:], are_tm, are_tm[:], ALU.mult)
    P.tt('dve', s5c, s5c[:], Lim, Lim[:], aim_tm, aim_tm[:], ALU.mult)
    P.tt('dve', s5b, s5b[:], s5b, s5b[:], s5c, s5c[:], ALU.add)
    P.tt('dve', Fre, Fre[:], s5b, s5b[:], s5a, s5a[:], ALU.mult)
    P.tt('dve', s5b, s5b[:], Lim, Lim[:], are_tm, are_tm[:], ALU.mult)
    P.tt('dve', s5c, s5c[:], Lre, Lre[:], aim_tm, aim_tm[:], ALU.mult)
    P.tt('dve', s5b, s5b[:], s5b, s5b[:], s5c, s5c[:], ALU.subtract)
    P.tt('dve', Fim, Fim[:], s5b, s5b[:], s5a, s5a[:], ALU.mult)
    BRt = load('BRt', BR_d[:, :], [64, 512])
    BIt = load('BIt', BI_d[:, :], [64, 512])
    bre = P.sb([64, 512], F32, 'bre')
    bim = P.sb([64, 512], F32, 'bim')
    btmp = P.sb([64, 512], F32, 'btmp')
    P.tt('dve', bre, bre[:], Fre, Fre[0:64, :], BRt, BRt[:], ALU.mult)
    P.tt('dve', btmp, btmp[:], Fim, Fim[0:64, :], BIt, BIt[:], ALU.mult)
    P.tt('dve', bre, bre[:], bre, bre[:], btmp, btmp[:], ALU.subtract)
    P.tt('dve', bim, bim[:], Fre, Fre[0:64, :], BIt, BIt[:], ALU.mult)
    P.tt('dve', btmp, btmp[:], Fim, Fim[0:64, :], BRt, BRt[:], ALU.mult)
    P.tt('dve', bim, bim[:], bim, bim[:], btmp, btmp[:], ALU.add)
    Bmat = P.sb([64, 512], BF16, 'Bmat', keep=True)
    Bswp = P.sb([64, 512], BF16, 'Bswp', keep=True)

    def v4(t, rows=128):
        return t[0:rows, :].rearrange("p (a r q) -> p a r q", a=2, r=2)

    P.cp('dve', Bmat, v4(Bmat, 64)[:, :, 0, :], bre, v4(bre, 64)[:, :, 0, :])
    P.cp('dve', Bmat, v4(Bmat, 64)[:, :, 1, :], bim, v4(bim, 64)[:, :, 1, :])
    P.ts('dve', Bswp, v4(Bswp, 64)[:, :, 0, :], bim, v4(bim, 64)[:, :, 0, :], -1.0, ALU.mult)
    P.cp('dve', Bswp, v4(Bswp, 64)[:, :, 1, :], bre, v4(bre, 64)[:, :, 1, :])
    are_sm = load('are_sm', are_sm_d[:, :], [128, 2])
    aim_sm = load('aim_sm', aim_sm_d[:, :], [128, 2])
    dt_sm = load('dt_sm', ldt_sm_d[:, :], [128, 2])
    P.act(dt_sm, dt_sm[:], dt_sm, dt_sm[:], AF.Exp)
    rho_sm = P.sb([128, 2], F32, 'rho_sm')
    tht_sm = P.sb([128, 2], F32, 'tht_sm')
    P.tt('dve', rho_sm, rho_sm[:], are_sm, are_sm[:], dt_sm, dt_sm[:], ALU.mult)
    P.tt('dve', tht_sm, tht_sm[:], aim_sm, aim_sm[:], dt_sm, dt_sm[:], ALU.mult)
    EA2 = P.sb([128, 512], F32, 'EA2', keep=True)
    EB2 = P.sb([128, 512], F32, 'EB2', keep=True)
    pm = P.sb([128, 256], F32, 'pm')
    pa = P.sb([128, 256], F32, 'pa')
    pc = P.sb([128, 256], F32, 'pc')
    pk = P.sb([128, 256], F32, 'pk')
    pki = P.sb([128, 256], I32, 'pki')
    for gp_ in range(2):
        sl = slice(gp_ * 128, (gp_ + 1) * 128)
        P.ts('dve', pm, pm[:, sl], irow1, irow1[:], rho_sm[:, gp_:gp_ + 1], ALU.mult, extra_r=[rho_sm])
        P.ts('dve', pa, pa[:, sl], irow1, irow1[:], tht_sm[:, gp_:gp_ + 1], ALU.mult, extra_r=[tht_sm])
    P.act(pm, pm[:], pm, pm[:], AF.Exp)
    P.range_reduce(pc, pc[:], pa, pa[:], pk, pk[:], pki, pki[:], shift=math.pi / 2)
    P.act(pc, pc[:], pc, pc[:], AF.Sin)
    P.tt('dve', pc, pc[:], pc, pc[:], pm, pm[:], ALU.mult)
    pc3 = pc[:].rearrange("p (a i) -> p a i", a=2)
    P.cp('dve', EA2, v4(EA2)[:, :, 0, :], pc, pc3)
    P.cp('dve', EA2, v4(EA2)[:, :, 1, :], pc, pc3)
    P.range_reduce(pc, pc[:], pa, pa[:], pk, pk[:], pki, pki[:])
    P.act(pc, pc[:], pc, pc[:], AF.Sin)
    P.tt('dve', pc, pc[:], pc, pc[:], pm, pm[:], ALU.mult)
    P.ts('dve', EB2, v4(EB2)[:, :, 0, :], pc, pc3, -1.0, ALU.mult)
    P.cp('dve', EB2, v4(EB2)[:, :, 1, :], pc, pc3)
    CRt = load('CRt', CR_d[:, :, :], [128, 2, 64])
    CIt = load('CIt', CI_d[:, :, :], [128, 2, 64])
    Cmat = P.sb([128, 4, 64], BF16, 'Cmat', keep=True)
    for gp_ in range(2):
        P.cp('dve', Cmat, Cmat[:, gp_ * 2, :], CRt, CRt[:, gp_, :])
        P.ts('dve', Cmat, Cmat[:, gp_ * 2 + 1, :], CIt, CIt[:, gp_, :], -1.0, ALU.mult)
    DDt = load('DDt', DD_d[:, :], [64, 64])
    DDb = P.sb([64, 64], BF16, 'DDb', keep=True)
    P.cp('dve', DDb, DDb[:], DDt, DDt[:])
    xprev = P.sb([128, 4], F32, 'xprev', keep=True)
    xprevs = P.sb([128, 4], F32, 'xprevs', keep=True)
    P.op('pool', lambda e: e.memset(xprev[:], 0.0), w=[xprev])
    P.op('pool', lambda e: e.memset(xprevs[:], 0.0), w=[xprevs])

    qng = load('qng', qng_d[:, :], [128, 2])
    wuq_f = load('wuq_f', wuq_d[:, :, :], [128, 2, 192])
    wuqb = P.sb([128, 2, 192], BF16, 'wuqb', keep=True)
    for kc in range(2):
        P.ts('dve', wuqb, wuqb[:, kc, :], wuq_f, wuq_f[:, kc, :], qng[:, kc:kc + 1], ALU.mult, extra_r=[qng])
    kvng = load('kvng', kvng_d[:, :], [128, 1])
    wukv_f = load('wukv_f', wukv_d[:, :], [128, 256])
    wukvb = P.sb([128, 256], BF16, 'wukvb', keep=True)
    P.ts('dve', wukvb, wukvb[:], wukv_f, wukv_f[:], kvng[:, 0:1], ALU.mult, extra_r=[kvng])

    P.barrier()
    tes.close()
    P.tes = None
    KTNP_t = P.es.enter_context(nc.sbuf_tensor('KTNP_%d' % id(P.es), [128, L], BF16))
    KTR_t = P.es.enter_context(nc.sbuf_tensor('KTR_%d' % id(P.es), [64, L], BF16))
    VV_t = P.es.enter_context(nc.sbuf_tensor('VV_%d' % id(P.es), [128, NT, 128], BF16))
    KTNP = [P.view(KTNP_t[:, t * 128:(t + 1) * 128], 'ktn%d' % t) for t in range(NT)]
    KTR = [P.view(KTR_t[:, t * 128:(t + 1) * 128], 'ktr%d' % t) for t in range(NT)]
    VV = [P.view(VV_t[:, t, :], 'vv%d' % t) for t in range(NT)]
    SROW_t = P.es.enter_context(nc.sbuf_tensor('SROW_%d' % id(P.es), [128, L], F32))
    SROW = [P.view(SROW_t, 'srow0'), P.view(SROW_t, 'srow1')]
    PB = P.sb([128, L], BF16, 'PB')
    state = P.sb([64, 64], F32, 'state')
    state_bf = P.sb([64, 64], BF16, 'state_bf')
    P.op('pool', lambda e: e.memset(state[:], 0.0), w=[state])
    P.op('pool', lambda e: e.memset(state_bf[:], 0.0), w=[state_bf])

    def dbl(shape, dt, name):
        return [P.sb(shape, dt, name + '%d' % i) for i in range(2)]

    xt = dbl([128, D], F32, 'xt')
    xnb = dbl([128, D], BF16, 'xnb')
    junk = P.sb([128, D], BF16, 'junk')
    ssx = dbl([128, 1], F32, 'ssx')
    hT = dbl([128, 8, 128], BF16, 'hT')
    ropeA = P.sb([128, 192], F32, 'ropeA')
    ropeB = P.sb([128, 192], F32, 'ropeB')
    rqk = dbl([128, 192], BF16, 'rqk')
    QTs = dbl([64, 128], BF16, 'QTs')
    KTs = dbl([64, 128], BF16, 'KTs')
    QTd = dbl([64, 128], BF16, 'QTd')
    vb = dbl([128, 64], BF16, 'vb')
    vdec = dbl([128, 64], BF16, 'vdec')
    smask = dbl([128, 128], BF16, 'smask')
    ssr = dbl([128, 1], F32, 'ssr')
    gs = dbl([128, 64], F32, 'gs')
    ro = dbl([128, 64], F32, 'ro')
    junk64 = P.sb([128, 256], F32, 'junk64')
    ub = dbl([128, 64], BF16, 'ub')
    UTs = dbl([64, 128], BF16, 'UTs')
    T1 = P.sb([128, 512], F32, 'T1')
    T2 = P.sb([128, 512], F32, 'T2')
    Wb = dbl([128, 512], BF16, 'Wb')
    Zf = T1
    Zsf = T2
    Xf = P.sb([128, 512], F32, 'Xf')
    Xb = dbl([128, 512], BF16, 'Xb')
    g1 = P.sb([128, 64], F32, 'g1')
    g2 = P.sb([128, 64], F32, 'g2')
    so = dbl([128, 64], F32, 'so')
    ssq = dbl([128, 1], F32, 'ssq')
    sskv = dbl([128, 1], F32, 'sskv')
    cqn = dbl([128, 256], BF16, 'cqn')
    ckvn = dbl([128, 128], BF16, 'ckvn')
    cqnT = dbl([128, 2, 128], BF16, 'cqnT')
    ckvnT = dbl([128, 128], BF16, 'ckvnT')
    QTNs = dbl([128, 128], BF16, 'QTNs')
    qrA = P.sb([128, 64], F32, 'qrA')
    qrB = P.sb([128, 64], F32, 'qrB')
    qrb = dbl([128, 64], BF16, 'qrb')
    QrTs = dbl([64, 128], BF16, 'QrTs')
    mrow = dbl([128, 1], F32, 'mrow')
    acc4 = dbl([128, 4], F32, 'acc4')
    rsum = dbl([128, 1], F32, 'rsum')
    PT = dbl([128, 512], BF16, 'PT')
    ao = dbl([128, 128], F32, 'ao')
    SM_SCALE = 192.0 ** -0.5
    ts_i = [0]

    def ts_slot():
        s = TS[ts_i[0] % 2]
        ts_i[0] += 1
        return s

    tb_i = [0]
    ev_i = [0]

    def evac_eng():
        ev_i[0] += 1
        return 'act' if ev_i[0] % 2 == 0 else 'dve'

    PVO = P.view(banks[5][:, 0:128], 'PVO', parent=banks[5])
    TBF = [P.view(banks[7][:, :].bitcast(BF16), 'TBF0', parent=banks[7]), P.view(banks[6][:, :].bitcast(BF16), 'TBF1', parent=banks[6])]
    PT8 = dbl([128, 1024], BF16, 'PT8')

    def frontend(t):
        p = t % 2
        P.dma('sp', xt[p][:], xrow(t), r=([xdep] if xdep is not None else []), w=[xt[p]])
        P.op('dve', lambda e: e.memset(ssx[p][:], 0.0), w=[ssx[p]])
        P.act(junk, junk[:], xt[p], xt[p][:], AF.Square, extra_w=[ssx[p]], accum_out=ssx[p][:])
        yield
        P.rstd(ssx[p], D, None)
        P.ts('dve', xnb[p], xnb[p][:], xt[p], xt[p][:], ssx[p][:, 0:1], ALU.mult, extra_r=[ssx[p]])
        yield
        for half in range(2):
            tb = TB[0]
            for c4 in range(4):
                c = half * 4 + c4
                P.tr(tb, tb[:, c4 * 128:(c4 + 1) * 128], xnb[p], xnb[p][:, c * 128:(c + 1) * 128], ident)
            for c4 in range(4):
                c = half * 4 + c4
                if c4 % 2 == 0:
                    P.act(hT[p], hT[p][:, c, :], tb, tb[:, c4 * 128:(c4 + 1) * 128], AF.Identity,
                          extra_r=[scale1, shm], scale=scale1[:, c:c + 1], bias=shm[:, c:c + 1])
                else:
                    P.ts('dve', hT[p], hT[p][:, c, :], tb, tb[:, c4 * 128:(c4 + 1) * 128],
                         scale1[:, c:c + 1], ALU.mult, shm[:, c:c + 1], ALU.add, extra_r=[scale1, shm])
            yield
        projection(t)
        yield
        for _ in heads(t):
            yield

    def projection(t):
        p = t % 2
        for kc in range(8):
            P.mm(PJ0, PJ0[:, :], hT[p], hT[p][:, kc, :], wselb, wselb[:, kc, 0:512], start=(kc == 0), stop=(kc == 7))
        for kc in range(8):
            P.mm(PJ1, PJ1[:, :], hT[p], hT[p][:, kc, :], wselb, wselb[:, kc, 512:768], start=(kc == 0), stop=(kc == 7))

    def heads(t):
        p = t % 2
        cos_t = COST[:, t * 32:(t + 1) * 32]
        sin_t = SINT[:, t * 32:(t + 1) * 32]
        nsin_t = NSINT[:, t * 32:(t + 1) * 32]
        src4 = PJ0[:, 0:192].rearrange("p (a h f) -> p a h f", a=3, h=2)
        A4 = ropeA[:].rearrange("p (a h f) -> p a h f", a=3, h=2)
        B4 = ropeB[:].rearrange("p (a h f) -> p a h f", a=3, h=2)
        P.tt('dve', ropeA, A4, PJ0, src4, COST, cos_t.unsqueeze(1).unsqueeze(1).broadcast_to([128, 3, 2, 32]), ALU.mult)
        P.tt('dve', ropeB, B4[:, :, 0, :], PJ0, src4[:, :, 1, :], NSINT, nsin_t.unsqueeze(1).broadcast_to([128, 3, 32]), ALU.mult)
        P.tt('dve', ropeB, B4[:, :, 1, :], PJ0, src4[:, :, 0, :], SINT, sin_t.unsqueeze(1).broadcast_to([128, 3, 32]), ALU.mult)
        yield
        P.cp('act', vb[p], vb[p][:], PJ0, PJ0[:, 192:256])
        P.ts('dve', vdec[p], vdec[p][:], PJ0, PJ0[:, 192:256], kdec[:, 0:1], ALU.mult, extra_r=[kdec])
        P.act(gs[p], gs[p][:], PJ0, PJ0[:, 256:320], AF.Silu)
        P.cp('act', ub[p], ub[p][:], PJ0, PJ0[:, 320:384])
        P.op('dve', lambda e: e.memset(ssq[p][:], 0.0), w=[ssq[p]])
        P.op('dve', lambda e: e.memset(sskv[p][:], 0.0), w=[sskv[p]])
        P.act(junk64, junk64[:, 0:256], PJ1, PJ1[:, :], AF.Square, extra_w=[ssq[p]], accum_out=ssq[p][:])
        P.act(junk64, junk64[:, 0:128], PJ0, PJ0[:, 384:512], AF.Square, extra_w=[sskv[p]], accum_out=sskv[p][:])
        yield
        P.rstd(ssq[p], 256, None)
        yield
        P.rstd(sskv[p], 128, None)
        yield
        P.ts('dve', cqn[p], cqn[p][:], PJ1, PJ1[:, :], ssq[p][:, 0:1], ALU.mult, extra_r=[ssq[p]])
        P.ts('dve', ckvn[p], ckvn[p][:], PJ0, PJ0[:, 384:512], sskv[p][:, 0:1], ALU.mult, extra_r=[sskv[p]])
        P.tt('dve', rqk[p], rqk[p][:], ropeA, ropeA[:], ropeB, ropeB[:], ALU.add)
        yield

    def ret_chain(t):
        p = t % 2
        tok = slice(t * 128, (t + 1) * 128)
        s0 = ts_slot()
        P.tr(s0, s0[0:64, :], rqk[p], rqk[p][:, 0:64], ident)
        P.cp('act', QTs[p], QTs[p][:], s0, s0[0:64, :])
        yield
        P.tt('dve', QTd[p], QTd[p][:], QTs[p], QTs[p][:], qdec, qdec[:], ALU.mult)
        s1 = ts_slot()
        P.tr(s1, s1[0:64, :], rqk[p], rqk[p][:, 64:128], ident)
        P.cp('act', KTs[p], KTs[p][:], s1, s1[0:64, :])
        yield
        P.tt('dve', gs[p], gs[p][:], gs[p], gs[p][:], retg_t, retg_t[:], ALU.mult)
        P.mm(RS, RS[:, :], KTs[p], KTs[p][:], QTs[p], QTs[p][:])
        yield
        P.tt('dve', smask[p], smask[p][:], RS, RS[:, :], dmaskT, dmaskT[:], ALU.mult)
        yield
        P.mm(RO, RO[:, :], smask[p], smask[p][:], vb[p], vb[p][:], start=True, stop=False)
        P.mm(RO, RO[:, :], QTd[p], QTd[p][:], state_bf, state_bf[:], start=False, stop=True)
        P.mm(RKV, RKV[:, :], rqk[p], rqk[p][:, 64:128], vdec[p], vdec[p][:])
        yield
        P.op('dve', lambda e: e.scalar_tensor_tensor(out=state[:], in0=state[:], scalar=dcy[:, 0:1], in1=RKV[:, :],
                                                     op0=ALU.mult, op1=ALU.add), r=[state, dcy, RKV], w=[state])
        P.cp('dve', state_bf, state_bf[:], state, state[:])
        P.op('dve', lambda e: e.memset(ssr[p][:], 0.0), w=[ssr[p]])
        P.act(junk64, junk64[:, 0:64], RO, RO[:, :], AF.Square, extra_w=[ssr[p]], accum_out=ssr[p][:])
        yield
        P.rstd(ssr[p], 64, None)
        yield
        P.op('dve', lambda e: e.scalar_tensor_tensor(out=ro[p][:], in0=RO[:, :], scalar=ssr[p][:, 0:1], in1=gs[p][:],
                                                     op0=ALU.mult, op1=ALU.mult), r=[RO, ssr[p], gs[p]], w=[ro[p]])
        out_evs.append(P.dma('sp', hx[tok, 0:64], ro[p][:], r=[ro[p]]))
        yield

    def s5_chain(t):
        p = t % 2
        tok = slice(t * 128, (t + 1) * 128)
        s3 = ts_slot()
        P.tr(s3, s3[0:64, :], ub[p], ub[p][:], ident)
        P.cp('dve', UTs[p], UTs[p][:], s3, s3[0:64, :])
        yield
        P.mm(SA, SA[:, :], UTs[p], UTs[p][:], Bmat, Bmat[:])
        P.mm(SB, SB[:, :], UTs[p], UTs[p][:], Bswp, Bswp[:])
        yield
        P.tt('dve', T1, T1[:], SA, SA[:, :], EA, EA[:], ALU.mult)
        yield
        P.tt('dve', T2, T2[:], SB, SB[:, :], EB, EB[:], ALU.mult)
        yield
        P.tt('dve', Wb[p], Wb[p][:], T1, T1[:], T2, T2[:], ALU.add)
        yield
        for blk in range(4):
            P.mm(SA, SA[:, blk * 128:(blk + 1) * 128], Wb[p], Wb[p][:, blk * 128:(blk + 1) * 128], tri, tri[:])
        for blk in range(4):
            b2 = blk ^ 1
            P.mm(SB, SB[:, blk * 128:(blk + 1) * 128], Wb[p], Wb[p][:, b2 * 128:(b2 + 1) * 128], tri, tri[:])
        yield
        P.tt('dve', Zf, Zf[:].rearrange("p (a i) -> p a i", a=4), SA, SA[:, :].rearrange("p (a i) -> p a i", a=4),
             xprev, xprev[:].unsqueeze(2).broadcast_to([128, 4, 128]), ALU.add)
        yield
        P.tt('dve', Zsf, Zsf[:].rearrange("p (a i) -> p a i", a=4), SB, SB[:, :].rearrange("p (a i) -> p a i", a=4),
             xprevs, xprevs[:].unsqueeze(2).broadcast_to([128, 4, 128]), ALU.add)
        yield
        P.tt('dve', Zf, Zf[:], Zf, Zf[:], EA2, EA2[:], ALU.mult)
        yield
        P.tt('dve', Zsf, Zsf[:], Zsf, Zsf[:], EB2, EB2[:], ALU.mult)
        yield
        P.tt('dve', Xf, Xf[:], Zf, Zf[:], Zsf, Zsf[:], ALU.add)
        yield
        P.cp('act', Xb[p], Xb[p][:], Xf, Xf[:])
        X3 = Xf[:].rearrange("p (a i) -> p a i", a=4)
        P.cp('dve', xprev, xprev[:], Xf, X3[:, :, 127])
        xp3 = xprev[:].rearrange("p (a r) -> p a r", r=2)
        xs3 = xprevs[:].rearrange("p (a r) -> p a r", r=2)
        P.cp('dve', xprevs, xs3[:, :, 0], xprev, xp3[:, :, 1])
        P.cp('dve', xprevs, xs3[:, :, 1], xprev, xp3[:, :, 0])
        yield
        for blk in range(4):
            P.mm(SY, SY[:, :], Xb[p], Xb[p][:, blk * 128:(blk + 1) * 128], Cmat, Cmat[:, blk, :], start=(blk == 0), stop=False)
        P.mm(SY, SY[:, :], UTs[p], UTs[p][:], DDb, DDb[:], start=False, stop=True)
        yield
        P.act(g1, g1[:], SY, SY[:, :], AF.Square)
        yield
        P.ts('dve', g1, g1[:], g1, g1[:], 0.044715, ALU.mult, 1.0, ALU.add)
        P.tt('dve', g1, g1[:], g1, g1[:], SY, SY[:, :], ALU.mult)
        yield
        P.act(g2, g2[:], g1, g1[:], AF.Sigmoid, scale=1.5957691216057308)
        yield
        P.tt('dve', so[p], so[p][:], g2, g2[:], SY, SY[:, :], ALU.mult)
        out_evs.append(P.dma('sp', hx[tok, 64:128], so[p][:], r=[so[p]]))
        yield

    def mla_chain(t):
        p = t % 2
        tok = slice(t * 128, (t + 1) * 128)
        cos_t = COST[:, t * 32:(t + 1) * 32]
        sin_t = SINT[:, t * 32:(t + 1) * 32]
        nsin_t = NSINT[:, t * 32:(t + 1) * 32]
        s2 = ts_slot()
        P.tr(s2, s2[0:64, :], rqk[p], rqk[p][:, 128:192], ident)
        P.cp('act', KTR[t], KTR[t][:], s2, s2[0:64, :])
        yield
        for kc in range(2):
            s = ts_slot()
            P.tr(s, s[:, :], cqn[p], cqn[p][:, kc * 128:(kc + 1) * 128], ident)
            P.cp(evac_eng(), cqnT[p], cqnT[p][:, kc, :], s, s[:, :])
            yield
        s = ts_slot()
        P.tr(s, s[:, :], ckvn[p], ckvn[p][:], ident)
        P.cp(evac_eng(), ckvnT[p], ckvnT[p][:], s, s[:, :])
        yield
        for kc in range(2):
            P.mm(QTN, QTN[:, :], wuqb, wuqb[:, kc, 0:128], cqnT[p], cqnT[p][:, kc, :], start=(kc == 0), stop=(kc == 1))
        for kc in range(2):
            P.mm(QRP, QRP[:, :], cqnT[p], cqnT[p][:, kc, :], wuqb, wuqb[:, kc, 128:192], start=(kc == 0), stop=(kc == 1))
        P.mm(KTN, KTN[:, :], wukvb, wukvb[:, 0:128], ckvnT[p], ckvnT[p][:])
        P.mm(VP, VP[:, :], ckvnT[p], ckvnT[p][:], wukvb, wukvb[:, 128:256])
        yield
        P.act(QTNs[p], QTNs[p][:], QTN, QTN[:, :], AF.Copy, scale=SM_SCALE)
        q3 = QRP[:, :].rearrange("p (h f) -> p h f", h=2)
        a3 = qrA[:].rearrange("p (h f) -> p h f", h=2)
        b3 = qrB[:].rearrange("p (h f) -> p h f", h=2)
        P.tt('dve', qrA, a3, QRP, q3, COST, cos_t.unsqueeze(1).broadcast_to([128, 2, 32]), ALU.mult)
        P.tt('dve', qrB, b3[:, 0, :], QRP, q3[:, 1, :], NSINT, nsin_t, ALU.mult)
        P.tt('dve', qrB, b3[:, 1, :], QRP, q3[:, 0, :], SINT, sin_t, ALU.mult)
        P.cp('act', KTNP[t], KTNP[t][:], KTN, KTN[:, :])
        P.cp('act', VV[t], VV[t][:], VP, VP[:, :])
        yield
        P.tt('dve', qrb[p], qrb[p][:], qrA, qrA[:], qrB, qrB[:], ALU.add)
        yield
        s = ts_slot()
        P.tr(s, s[0:64, :], qrb[p], qrb[p][:], ident)
        P.act(QrTs[p], QrTs[p][:], s, s[0:64, :], AF.Copy, scale=SM_SCALE)
        yield
        Lk = (t + 1) * 128
        nkb = (Lk + 511) // 512
        for kb in range(nkb):
            n = min(512, Lk - kb * 512)
            sc = SC[kb % 2]
            kts = [KTNP[4 * kb + i_] for i_ in range(n // 128)]
            krs = [KTR[4 * kb + i_] for i_ in range(n // 128)]
            P.op('pe', lambda e: e.matmul(sc[:, 0:n], lhsT=QTNs[p][:], rhs=KTNP_t[:, kb * 512:kb * 512 + n],
                                          start=True, stop=False), r=[QTNs[p]] + kts, w=[sc])
            P.op('pe', lambda e: e.matmul(sc[:, 0:n], lhsT=QrTs[p][:], rhs=KTR_t[:, kb * 512:kb * 512 + n],
                                          start=False, stop=True), r=[QrTs[p]] + krs, w=[sc])
            yield
            srow = SROW[kb % 2]
            if kb == nkb - 1:
                if n > 128:
                    P.cp('act', srow, SROW_t[:, kb * 512:kb * 512 + n - 128], sc, sc[:, 0:n - 128])
                P.tt('dve', srow, SROW_t[:, Lk - 128:Lk], sc, sc[:, n - 128:n], cmask, cmask[:], ALU.add)
            else:
                P.cp(evac_eng(), srow, SROW_t[:, kb * 512:kb * 512 + n], sc, sc[:, 0:n])
            yield
        P.op('dve', lambda e: e.reduce_max(out=mrow[p][:], in_=SROW_t[:, 0:Lk], axis=AX.X), r=[SROW[0], SROW[1]], w=[mrow[p]])
        P.op('dve', lambda e: e.memset(acc4[p][:], 0.0), w=[acc4[p]])
        yield
        P.ts('dve', mrow[p], mrow[p][:], mrow[p], mrow[p][:], -1.0, ALU.mult)
        yield
        nch = (Lk + 2047) // 2048
        for ci in range(nch):
            c0 = ci * 2048
            c1 = min(Lk, c0 + 2048)
            P.op('act', lambda e: e.activation(out=PB[:, c0:c1], in_=SROW_t[:, c0:c1], func=AF.Exp,
                                               bias=mrow[p][:, 0:1], scale=1.0, accum_out=acc4[p][:, ci:ci + 1]),
                 r=[SROW[0], SROW[1], mrow[p]], w=[PB, acc4[p]])
            yield
        P.op('dve', lambda e: e.reduce_sum(out=rsum[p][:], in_=acc4[p][:, 0:nch], axis=AX.X), r=[acc4[p]], w=[rsum[p]])
        yield
        P.op('dve', lambda e: e.reciprocal(out=rsum[p][:], in_=rsum[p][:]), r=[rsum[p]], w=[rsum[p]])
        ng = (t + 1 + 7) // 8

        def tr_round(g):
            tbf = TBF[g % 2]
            pt = PT8[g % 2]
            blks = list(range(g * 8, min(t + 1, g * 8 + 8)))
            for blk in blks:
                P.tr(tbf, tbf[:, (blk % 8) * 128:(blk % 8 + 1) * 128], PB, PB[:, blk * 128:(blk + 1) * 128], ident)
            w_ = len(blks) * 128
            P.cp(evac_eng(), pt, pt[:, 0:w_], tbf, tbf[:, 0:w_])
            return blks, pt

        cur = tr_round(0)
        yield
        for g in range(ng):
            nxt = tr_round(g + 1) if g + 1 < ng else None
            blks, pt = cur
            for blk in blks:
                P.mm(PVO, PVO[:, :], pt, pt[:, (blk % 8) * 128:(blk % 8 + 1) * 128], VV[blk], VV[blk][:],
                     start=(blk == 0), stop=(blk == t))
            cur = nxt
            yield
        P.ts('dve', ao[p], ao[p][:], PVO, PVO[:, :], rsum[p][:, 0:1], ALU.mult, extra_r=[rsum[p]])
        out_evs.append(P.dma('sp', hx[tok, 128:256], ao[p][:], r=[ao[p]]))
        yield

    def drain(g):
        for _ in g:
            pass

    def interleave(gens):
        gens = [[g, 2 if i == 0 else 1] for i, g in enumerate(gens)]
        while gens:
            alive = []
            for g, n_ in gens:
                ok = True
                for _ in range(n_):
                    try:
                        next(g)
                    except StopIteration:
                        ok = False
                        break
                if ok:
                    alive.append([g, n_])
            gens = alive

    out_evs = []
    if ntiles > 0:
        drain(frontend(0))
    for t in range(ntiles):
        chains = [mla_chain(t), ret_chain(t), s5_chain(t)]
        if t + 1 < ntiles:
            chains.append(frontend(t + 1))
        interleave(chains)
        if on_chunk is not None and (t + 1) % 8 == 0:
            on_chunk(t // 8, out_evs)
            out_evs = []
    P.barrier()
    st.close()
    P.es = old_es
    P.pre = {}


RET_GAMMA = [1.0 - 2.0 ** (-5.0 - h) for h in range(4)]


def consts_A(hg):
    g = RET_GAMMA[hg]
    lg = math.log1p(-(2.0 ** (-5.0 - hg)))
    i = np.arange(128, dtype=np.float64)
    diff = i[None, :] - i[:, None]
    dmaskT = np.where(diff >= 0, np.exp(lg * np.maximum(diff, 0.0)), 0.0) * 0.125
    tri = (diff >= 0).astype(np.float32)
    qdec = np.broadcast_to(np.exp(lg * (i + 1.0))[None, :], (64, 128))
    kdec = (np.exp(lg * (127.0 - i)) * 0.125)[:, None]
    dcy = np.full((64, 1), math.exp(lg * 128.0))
    cmask = np.where(i[None, :] <= i[:, None], 0.0, -1e30)
    inv = 10000.0 ** (-np.arange(0, 64, 2, dtype=np.float32) / 64.0)
    f = lambda a: np.ascontiguousarray(a, dtype=np.float32)
    return dict(dmaskT=f(dmaskT), tri=f(tri), qdec=f(qdec), kdec=f(kdec), dcy=f(dcy), cmask=f(cmask),
                jp1=f((i + 1.0)[:, None]), irow1=f(np.broadcast_to((i + 1.0)[None, :], (128, 128))),
                invf=f(inv[None, :]))


def chunkT(v, nch):
    return np.ascontiguousarray(v.reshape(nch, 128).T)


def inputs_A(inp, layer, xcur):
    f = lambda a: np.ascontiguousarray(a, dtype=np.float32)
    maps = []
    w_in = inp['w_in'][layer]
    for core in range(8):
        b, hg = core // 4, core % 4
        m = dict(consts_A(hg))
        m['x'] = f(xcur[b])
        m['cvec'] = f(inp['c'][b].reshape(128, 8))
        m['pos'] = np.ascontiguousarray(inp['positions'][b].reshape(NT, 128).T.astype(np.int32))
        m['adaw'] = f(inp['ada_w'][layer][:, 0:2048].reshape(128, 8, 2048))
        m['adab'] = chunkT(f(inp['ada_b'][layer][0:2048]), 16)
        m['gpre'] = chunkT(f(inp['norm_pre_mix'][layer]), 8)
        h64 = slice(hg * 64, (hg + 1) * 64)
        cols = np.concatenate([
            np.arange(0, 256)[h64], np.arange(256, 512)[h64], np.arange(1664, 1728),
            np.arange(512, 768)[h64], np.arange(768, 1024)[h64], np.arange(1024, 1280)[h64],
            np.arange(1536, 1664), np.arange(1280, 1536)])
        ws = w_in[:, cols]
        m['wsel'] = f(ws.reshape(8, 128, 768).transpose(1, 0, 2))
        m['retg'] = f(inp['ret_norm'][layer][h64][None, :])
        gs_ = slice(4 * hg, 4 * hg + 4)

        def tm(a):
            a = a.reshape(2, 1, 128)
            return f(np.broadcast_to(a, (2, 2, 128)).reshape(1, 512))

        def sm(a):
            return f(a.reshape(2, 128).T)

        are = inp['ssm_a_re'][layer][gs_]
        aim = inp['ssm_a_im'][layer][gs_]
        ldt = np.broadcast_to(inp['ssm_log_dt'][layer][gs_][:, None], (4, 64))
        m['are_tm'], m['aim_tm'], m['ldt_tm'] = tm(are), tm(aim), tm(ldt)
        m['are_sm'], m['aim_sm'], m['ldt_sm'] = sm(are), sm(aim), sm(ldt)
        BR = np.zeros((64, 2, 2, 2, 64), np.float32)
        BI = np.zeros((64, 2, 2, 2, 64), np.float32)
        CR = np.zeros((2, 64, 2, 64), np.float32)
        CI = np.zeros((2, 64, 2, 64), np.float32)
        for gl in range(4):
            gp_, g2_ = gl // 2, gl % 2
            br = inp['ssm_b_re'][layer][4 * hg + gl]
            bi = inp['ssm_b_im'][layer][4 * hg + gl]
            for ri in range(2):
                BR[gl * 16:(gl + 1) * 16, gp_, ri, g2_, :] = br.T
                BI[gl * 16:(gl + 1) * 16, gp_, ri, g2_, :] = bi.T
            cr = inp['ssm_c_re'][layer][4 * hg + gl]
            ci = inp['ssm_c_im'][layer][4 * hg + gl]
            CR[g2_, :, gp_, gl * 16:(gl + 1) * 16] = cr.T
            CI[g2_, :, gp_, gl * 16:(gl + 1) * 16] = ci.T
        m['BR'] = f(BR.reshape(64, 512))
        m['BI'] = f(BI.reshape(64, 512))
        m['CR'] = f(CR.reshape(128, 2, 64))
        m['CI'] = f(CI.reshape(128, 2, 64))
        DD = np.zeros((64, 64), np.float32)
        DD[np.arange(64), np.arange(64)] = inp['ssm_d'][layer][gs_].reshape(64)
        m['DD'] = DD
        m['qng'] = chunkT(f(inp['mla_q_norm'][layer]), 2)
        wq = inp['mla_w_uq'][layer][:, hg * 192:(hg + 1) * 192]
        m['wuq'] = f(wq.reshape(2, 128, 192).transpose(1, 0, 2))
        m['kvng'] = f(inp['mla_kv_norm'][layer][:, None])
        m['wukv'] = f(inp['mla_w_ukv'][layer][:, hg * 256:(hg + 1) * 256])
        maps.append(m)
    return maps


_CACHE = {}


TPC = 2048
GT = 4
NG = TPC // (GT * 128)


def decl_B(nc, sfx, moe):
    n_exp = N_EXP if moe else 1
    io = {}
    io['cvec'] = nc.dram_tensor('cvec' + sfx, [128, 8], F32, kind='ExternalInput').ap()
    io['adaw'] = nc.dram_tensor('adaw' + sfx, [128, 8, 4096], F32, kind='ExternalInput').ap()
    io['adab_row'] = nc.dram_tensor('adab_row' + sfx, [1, 4096], F32, kind='ExternalInput').ap()
    io['adab_col'] = nc.dram_tensor('adab_col' + sfx, [128, 32], F32, kind='ExternalInput').ap()
    io['gpost_m'] = nc.dram_tensor('gpost_m' + sfx, [1, D], F32, kind='ExternalInput').ap()
    io['gpre_f'] = nc.dram_tensor('gpre_f' + sfx, [128, 8], F32, kind='ExternalInput').ap()
    io['gpost_f'] = nc.dram_tensor('gpost_f' + sfx, [1, D], F32, kind='ExternalInput').ap()
    io['gluw_d'] = nc.dram_tensor('gluw' + sfx, [128, 2, 256], F32, kind='ExternalInput').ap()
    io['glub_d'] = nc.dram_tensor('glub' + sfx, [1, 256], F32, kind='ExternalInput').ap()
    io['ssmg_d'] = nc.dram_tensor('ssmg' + sfx, [1, 256], F32, kind='ExternalInput').ap()
    io['mlag_d'] = nc.dram_tensor('mlag' + sfx, [1, 512], F32, kind='ExternalInput').ap()
    io['wout_d'] = nc.dram_tensor('wout' + sfx, [128, 8, D], F32, kind='ExternalInput').ap()
    io['wg_d'] = nc.dram_tensor('wg' + sfx, [n_exp * NFC, 128, 1024], F32, kind='ExternalInput').ap()
    io['wu_d'] = nc.dram_tensor('wu' + sfx, [n_exp * NFC, 128, 1024], F32, kind='ExternalInput').ap()
    io['wd_d'] = nc.dram_tensor('wd' + sfx, [n_exp * NFC, 128, 1024], F32, kind='ExternalInput').ap()
    if moe:
        io['router_d'] = nc.dram_tensor('router' + sfx, [128, 8, 8], F32, kind='ExternalInput').ap()
    return io


def emit_B(P, nc, banks, io, x, hxall, HXALL, mixloc, out, rank256, moe, ngroups=NG, xdep=None, on_group=None):
    n_exp = N_EXP if moe else 1
    cvec = io['cvec']
    adaw = io['adaw']
    adab_row = io['adab_row']
    adab_col = io['adab_col']
    gpost_m = io['gpost_m']
    gpre_f = io['gpre_f']
    gpost_f = io['gpost_f']
    gluw_d = io['gluw_d']
    glub_d = io['glub_d']
    ssmg_d = io['ssmg_d']
    mlag_d = io['mlag_d']
    wout_d = io['wout_d']
    wg_d = io['wg_d']
    wu_d = io['wu_d']
    wd_d = io['wd_d']
    router_d = io.get('router_d')
    st = ExitStack()
    old_es = P.es
    P.es = st
    P.pre = {}
    b0 = banks[0][:, :].bitcast(BF16)
    TB = P.view(b0[:, 0:512], 'TB', parent=banks[0])
    ZP = P.view(banks[1][:, 0:256], 'ZP', parent=banks[1])
    LG = P.view(banks[1][:, 256:264], 'LG', parent=banks[1])
    MC = P.view(banks[1][:, 272:288], 'MC', parent=banks[1])
    YB = [P.view(banks[2][:, :], 'YA', parent=banks[2]), P.view(banks[3][:, :], 'YB', parent=banks[3])]
    GB = [P.view(banks[4][:, :], 'G0', parent=banks[4]), P.view(banks[5][:, :], 'G1', parent=banks[5])]
    UB = [P.view(banks[6][:, :], 'U0', parent=banks[6]), P.view(banks[7][:, :], 'U1', parent=banks[7])]

    ident = P.sb([128, 128], BF16, 'ident', keep=True)
    vecm = P.sb([128, D], F32, 'vecm', keep=True)
    vecf = P.sb([128, D], F32, 'vecf', keep=True)
    modc = P.sb([128, 16], F32, 'modc', keep=True)
    scale2 = P.sb([128, 8], F32, 'scale2', keep=True)
    woutb = P.sb([128, 8, D], BF16, 'woutb', keep=True)
    gluwb = P.sb([128, 2, 256], BF16, 'gluwb', keep=True)
    glub_t = P.sb([128, 256], F32, 'glub_t', keep=True)
    ssmg_t = P.sb([128, 256], F32, 'ssmg_t', keep=True)
    mlag_t = P.sb([128, 512], F32, 'mlag_t', keep=True)
    if moe:
        routerb = P.sb([128, 8, 8], BF16, 'routerb', keep=True)
    X1 = [P.sb([128, D], F32, 'X1_%d' % i, keep=True) for i in range(GT)]
    hT2 = P.sb([128, 8, GT * 128], BF16, 'hT2', keep=True)
    yacc = [P.sb([128, D], F32, 'yacc%d' % i, keep=True) for i in range(GT)]
    gatew = P.sb([128, GT, 8], F32, 'gatew', keep=True)

    tes = ExitStack()
    P.tes = tes
    make_ident(P)
    cv = P.sb([128, 8], F32, 'cv')
    cvb = P.sb([128, 8], BF16, 'cvb')
    cvbb = P.sb([128, 8, 128], BF16, 'cvbb')
    P.dma('sp', cv[:], cvec[:, :], w=[cv])
    P.act(cvb, cvb[:], cv, cv[:], AF.Silu)
    P.cp('dve', cvbb, cvbb[:], cvb, cvb[:].unsqueeze(2).broadcast_to([128, 8, 128]))
    abc = P.sb([128, 32], F32, 'abc')
    P.dma('sp', abc[:], adab_col[:, :], w=[abc])
    wts = [P.sb([128, 8, 1024], BF16, 'adw%d' % i) for i in range(2)]
    stager = make_stager(P)
    rowb = P.sb([128, D], F32, 'rowb')
    for ci in range(4):
        wt = wts[ci % 2]
        for kc in range(8):
            stager(wt, wt[:, kc, :], adaw[:, kc, ci * 1024:(ci + 1) * 1024])
        if ci in (0, 3):
            vec = vecm if ci == 0 else vecf
            gsrc = gpost_m if ci == 0 else gpost_f
            P.dma('sp', rowb[:], adab_row[0:1, ci * 1024:(ci + 1) * 1024].broadcast_to([128, 1024]), w=[rowb])
            for half in range(2):
                yb = YB[half]
                for kc in range(8):
                    P.mm(yb, yb[:, :], cvbb, cvbb[:, kc, :], wt, wt[:, kc, half * 512:(half + 1) * 512],
                         start=(kc == 0), stop=(kc == 7))
                P.tt('dve', vec, vec[:, half * 512:(half + 1) * 512], yb, yb[:, :], rowb, rowb[:, half * 512:(half + 1) * 512], ALU.add)
            P.dma('sp', rowb[:], gsrc[0:1, :].broadcast_to([128, 1024]), w=[rowb])
            P.tt('dve', vec, vec[:], vec, vec[:], rowb, rowb[:], ALU.mult)
        else:
            for cc in range(8):
                ch = (ci - 1) * 8 + cc
                for kc in range(8):
                    P.mm(MC, MC[:, ch:ch + 1], wt, wt[:, kc, cc * 128:(cc + 1) * 128], cvb, cvb[:, kc:kc + 1],
                         start=(kc == 0), stop=(kc == 7))
    P.tt('dve', modc, modc[:], MC, MC[:, 0:16], abc, abc[:, 8:24], ALU.add)
    gpf = P.sb([128, 8], F32, 'gpf')
    P.dma('sp', gpf[:], gpre_f[:, :], w=[gpf])
    P.ts('dve', scale2, scale2[:], modc, modc[:, 8:16], 1.0, ALU.add)
    P.tt('dve', scale2, scale2[:], scale2, scale2[:], gpf, gpf[:], ALU.mult)
    sh2 = P.view(modc[:, 0:8], 'sh2', parent=modc)
    for kc in range(8):
        stager(woutb, woutb[:, kc, :], wout_d[:, kc, :])
    P.dma('pool', gluwb[:], gluw_d[:, :, :], w=[gluwb])
    P.dma('sp', glub_t[:], glub_d[0:1, :].broadcast_to([128, 256]), w=[glub_t])
    P.dma('sp', ssmg_t[:], ssmg_d[0:1, :].broadcast_to([128, 256]), w=[ssmg_t])
    P.dma('sp', mlag_t[:], mlag_d[0:1, :].broadcast_to([128, 512]), w=[mlag_t])
    if moe:
        P.dma('pool', routerb[:], router_d[:, :, :], w=[routerb])
    P.barrier()
    tes.close()
    P.tes = None

    MIXLOC = P.view(mixloc, 'MIXLOC')
    for kk in range(2):
        src_ = hxall.rearrange("(a b) c -> a (b c)", b=32)[bass.ds(rank256 + kk * 128, 128), :]
        P.dma('sp', mixloc[kk].rearrange("r n c -> (r n) c").rearrange("(a b) c -> a (b c)", b=32), src_, r=[HXALL], w=[MIXLOC])
    ev_i = [0]

    def evac_eng():
        ev_i[0] += 1
        return 'act' if ev_i[0] % 2 == 0 else 'dve'

    for g in range(ngroups):
        ph = ExitStack()
        P.tes = ph
        junk = P.sb([128, D], BF16, 'junk')

        def p1_bufs(i):
            return dict(xt=P.sb([128, D], F32, 'xt%d' % i), mt=P.sb([128, D], F32, 'mt%d' % i),
                        hxt=P.sb([128, 4, 256], F32, 'hxt%d' % i), ysb=P.sb([128, 256], BF16, 'ysb%d' % i),
                        ysT=P.sb([128, 2, 128], BF16, 'ysT%d' % i), zz=P.sb([128, 256], F32, 'zz%d' % i),
                        s2=P.sb([128, 256], F32, 's2%d' % i), catb=P.sb([128, D], BF16, 'catb%d' % i),
                        catT=P.sb([128, 8, 128], BF16, 'catT%d' % i), tmp=P.sb([128, D], F32, 'tmp%d' % i),
                        xn2=P.sb([128, D], BF16, 'xn2%d' % i), ss=P.sb([128, 4], F32, 'ss%d' % i),
                        lg=P.sb([128, 8], F32, 'lg%d' % i), lg2=P.sb([128, 8], F32, 'lg2%d' % i),
                        mk1=P.sb([128, 8], F32, 'mk1%d' % i), mk2=P.sb([128, 8], F32, 'mk2%d' % i),
                        m12=P.sb([128, 4], F32, 'm12%d' % i))

        p1sets = [p1_bufs(0), p1_bufs(1)]

        def p1_tile(ti, B_, YK):
            xt, mt, hxt, ysb, ysT, zz, s2 = B_['xt'], B_['mt'], B_['hxt'], B_['ysb'], B_['ysT'], B_['zz'], B_['s2']
            catb, catT, tmp, xn2, ss = B_['catb'], B_['catT'], B_['tmp'], B_['xn2'], B_['ss']
            lg, lg2, mk1, mk2, m12 = B_['lg'], B_['lg2'], B_['mk1'], B_['mk2'], B_['m12']
            tok = slice((g * GT + ti) * 128, (g * GT + ti + 1) * 128)
            P.dma('sp', xt[:], x[tok, :], r=([xdep] if xdep is not None else []), w=[xt])
            row0 = (g * GT + ti) * 128
            P.dma('sp', hxt[:], mixloc[row0 // 1024, :, row0 % 1024:row0 % 1024 + 128, :].rearrange("r n c -> n r c"), r=[MIXLOC], w=[hxt])
            for (c0, w_, d0) in ((0, 64, 0), (64, 64, 256), (128, 128, 512)):
                P.cp('pool', mt, mt[:, d0:d0 + 4 * w_].rearrange("p (r c) -> p r c", r=4), hxt, hxt[:, :, c0:c0 + w_])
            yield
            P.cp('dve', ysb, ysb[:], mt, mt[:, 256:512])
            yield
            for kc in range(2):
                P.tr(TB, TB[:, kc * 128:(kc + 1) * 128], ysb, ysb[:, kc * 128:(kc + 1) * 128], ident)
            P.cp('act', ysT, ysT[:].rearrange("p a b -> p (a b)"), TB, TB[:, 0:256])
            yield
            for kc in range(2):
                P.mm(ZP, ZP[:, :], ysT, ysT[:, kc, :], gluwb, gluwb[:, kc, :], start=(kc == 0), stop=(kc == 1))
            P.tt('dve', zz, zz[:], ZP, ZP[:, :], glub_t, glub_t[:], ALU.add)
            yield
            P.act(zz, zz[:], zz, zz[:], AF.Sigmoid)
            P.op('pool', lambda e: e.memset(ss[:], 0.0), w=[ss])
            yield
            P.tt('dve', s2, s2[:], zz, zz[:], mt, mt[:, 256:512], ALU.mult)
            P.act(junk, junk[:, 0:512], mt, mt[:, 512:1024], AF.Square, extra_w=[ss], accum_out=ss[:, 1:2])
            yield
            P.act(junk, junk[:, 0:256], s2, s2[:], AF.Square, extra_w=[ss], accum_out=ss[:, 0:1])
            P.cp('act', catb, catb[:, 0:256], mt, mt[:, 0:256])
            yield
            P.ts('dve', ss, ss[:, 0:1], ss, ss[:, 0:1], 1.0 / 256, ALU.mult, EPS, ALU.add)
            P.ts('dve', ss, ss[:, 1:2], ss, ss[:, 1:2], 1.0 / 512, ALU.mult, EPS, ALU.add)
            yield
            P.act(ss, ss[:, 0:2], ss, ss[:, 0:2], AF.Sqrt)
            yield
            P.op('dve', lambda e: e.reciprocal(out=ss[:, 0:2], in_=ss[:, 0:2]), r=[ss], w=[ss])
            yield
            P.op('dve', lambda e: e.scalar_tensor_tensor(out=catb[:, 256:512], in0=s2[:], scalar=ss[:, 0:1], in1=ssmg_t[:],
                                                         op0=ALU.mult, op1=ALU.mult), r=[s2, ss, ssmg_t], w=[catb])
            P.op('dve', lambda e: e.scalar_tensor_tensor(out=catb[:, 512:1024], in0=mt[:, 512:1024], scalar=ss[:, 1:2], in1=mlag_t[:],
                                                         op0=ALU.mult, op1=ALU.mult), r=[mt, ss, mlag_t], w=[catb])
            yield
            for half in range(2):
                for c4 in range(4):
                    c = half * 4 + c4
                    P.tr(TB, TB[:, c4 * 128:(c4 + 1) * 128], catb, catb[:, c * 128:(c + 1) * 128], ident)
                P.cp(evac_eng(), catT, catT[:, half * 4:(half + 1) * 4, :].rearrange("p a b -> p (a b)"), TB, TB[:, :])
                yield
            for half in range(2):
                for kc in range(8):
                    P.mm(YK[half], YK[half][:, :], catT, catT[:, kc, :], woutb, woutb[:, kc, half * 512:(half + 1) * 512],
                         start=(kc == 0), stop=(kc == 7))
            P.op('pool', lambda e: e.memset(ss[:, 2:4], 0.0), w=[ss])
            yield
            for half in range(2):
                P.act(junk, junk[:, 0:512], YK[half], YK[half][:, :], AF.Square, extra_w=[ss], accum_out=ss[:, 2 + half:3 + half])
            yield
            P.tt('dve', ss, ss[:, 2:3], ss, ss[:, 2:3], ss, ss[:, 3:4], ALU.add)
            P.ts('dve', ss, ss[:, 2:3], ss, ss[:, 2:3], 1.0 / D, ALU.mult, EPS, ALU.add)
            yield
            P.act(ss, ss[:, 2:3], ss, ss[:, 2:3], AF.Sqrt)
            yield
            P.op('dve', lambda e: e.reciprocal(out=ss[:, 2:3], in_=ss[:, 2:3]), r=[ss], w=[ss])
            yield
            for half in range(2):
                hs = slice(half * 512, (half + 1) * 512)
                P.op('dve', lambda e: e.scalar_tensor_tensor(out=tmp[:, hs], in0=YK[half][:, :], scalar=ss[:, 2:3], in1=vecm[:, hs],
                                                             op0=ALU.mult, op1=ALU.mult), r=[YK[half], ss, vecm], w=[tmp])
            yield
            P.tt('dve', X1[ti], X1[ti][:], tmp, tmp[:], xt, xt[:], ALU.add)
            P.op('pool', lambda e: e.memset(ss[:, 3:4], 0.0), w=[ss])
            yield
            P.act(junk, junk[:], X1[ti], X1[ti][:], AF.Square, extra_w=[ss], accum_out=ss[:, 3:4])
            yield
            P.ts('dve', ss, ss[:, 3:4], ss, ss[:, 3:4], 1.0 / D, ALU.mult, EPS, ALU.add)
            yield
            P.act(ss, ss[:, 3:4], ss, ss[:, 3:4], AF.Sqrt)
            yield
            P.op('dve', lambda e: e.reciprocal(out=ss[:, 3:4], in_=ss[:, 3:4]), r=[ss], w=[ss])
            yield
            P.ts('dve', xn2, xn2[:], X1[ti], X1[ti][:], ss[:, 3:4], ALU.mult, extra_r=[ss])
            yield
            for half in range(2):
                for c4 in range(4):
                    c = half * 4 + c4
                    P.tr(TB, TB[:, c4 * 128:(c4 + 1) * 128], xn2, xn2[:, c * 128:(c + 1) * 128], ident)
                for c4 in range(4):
                    c = half * 4 + c4
                    if c4 % 2 == 0:
                        P.act(hT2, hT2[:, c, ti * 128:(ti + 1) * 128], TB, TB[:, c4 * 128:(c4 + 1) * 128], AF.Identity,
                              extra_r=[scale2, sh2], scale=scale2[:, c:c + 1], bias=sh2[:, c:c + 1])
                    else:
                        P.ts('dve', hT2, hT2[:, c, ti * 128:(ti + 1) * 128], TB, TB[:, c4 * 128:(c4 + 1) * 128],
                             scale2[:, c:c + 1], ALU.mult, sh2[:, c:c + 1], ALU.add, extra_r=[scale2, sh2])
                yield
            if moe:
                for kc in range(8):
                    P.mm(LG, LG[:, :], hT2, hT2[:, kc, ti * 128:(ti + 1) * 128], routerb, routerb[:, kc, :],
                         start=(kc == 0), stop=(kc == 7))
                P.cp('dve', lg, lg[:], LG, LG[:, :])
                yield
                P.op('dve', lambda e: e.reduce_max(out=m12[:, 0:1], in_=lg[:], axis=AX.X), r=[lg], w=[m12])
                yield
                P.ts('dve', mk1, mk1[:], lg, lg[:], m12[:, 0:1], ALU.is_equal, extra_r=[m12])
                yield
                P.op('dve', lambda e: e.scalar_tensor_tensor(out=lg2[:], in0=mk1[:], scalar=-1e30, in1=lg[:],
                                                             op0=ALU.mult, op1=ALU.add), r=[mk1, lg], w=[lg2])
                yield
                P.op('dve', lambda e: e.reduce_max(out=m12[:, 1:2], in_=lg2[:], axis=AX.X), r=[lg2], w=[m12])
                yield
                P.ts('dve', mk2, mk2[:], lg2, lg2[:], m12[:, 1:2], ALU.is_equal, extra_r=[m12])
                P.tt('dve', m12, m12[:, 2:3], m12, m12[:, 1:2], m12, m12[:, 0:1], ALU.subtract)
                yield
                P.act(m12, m12[:, 2:3], m12, m12[:, 2:3], AF.Exp)
                yield
                P.ts('dve', m12, m12[:, 3:4], m12, m12[:, 2:3], 1.0, ALU.add)
                yield
                P.op('dve', lambda e: e.reciprocal(out=m12[:, 3:4], in_=m12[:, 3:4]), r=[m12], w=[m12])
                yield
                P.tt('dve', m12, m12[:, 2:3], m12, m12[:, 2:3], m12, m12[:, 3:4], ALU.mult)
                P.ts('dve', mk1, mk1[:], mk1, mk1[:], m12[:, 3:4], ALU.mult, extra_r=[m12])
                yield
                P.op('dve', lambda e: e.scalar_tensor_tensor(out=gatew[:, ti, :], in0=mk2[:], scalar=m12[:, 2:3], in1=mk1[:],
                                                             op0=ALU.mult, op1=ALU.add), r=[mk2, m12, mk1], w=[gatew])
                yield

        for pair in range(GT // 2):
            gens = [p1_tile(2 * pair, p1sets[0], YB), p1_tile(2 * pair + 1, p1sets[1], GB)]
            while gens:
                alive = []
                for gen_ in gens:
                    try:
                        next(gen_)
                        alive.append(gen_)
                    except StopIteration:
                        pass
                gens = alive
        P.barrier()
        ph.close()
        ph = ExitStack()
        P.tes = ph
        actT = P.sb([128, NFC, GT * 128], BF16, 'actT')
        wdb = P.sb([128, NFC, D], BF16, 'wdb')
        stg = [P.sb([128, 1024], F32, 'stg%d' % i) for i in range(4)]
        wgb = [P.sb([128, 8, 128], BF16, 'wgb%d' % i) for i in range(2)]
        wub = [P.sb([128, 8, 128], BF16, 'wub%d' % i) for i in range(2)]
        gsil = [P.sb([128, GT * 128], F32, 'gsil%d' % i) for i in range(2)]
        ftmp = P.sb([128, D], F32, 'ftmp')
        fjunk = P.sb([128, D], BF16, 'fjunk')
        fss = P.sb([128, 1], F32, 'fss')
        st_i = [0]

        def load_cast(dst, dst_ap, src_ap, eng):
            sg = stg[st_i[0] % 4]
            st_i[0] += 1
            P.dma('sp', sg[:], src_ap, w=[sg])
            P.cp(eng, dst, dst_ap, sg, sg[:])

        for e_ in range(n_exp):
            for fc in range(NFC):
                b2 = fc % 2
                load_cast(wgb[b2], wgb[b2][:].rearrange("p a b -> p (a b)"), wg_d[e_ * NFC + fc, :, :], 'dve')
                load_cast(wub[b2], wub[b2][:].rearrange("p a b -> p (a b)"), wu_d[e_ * NFC + fc, :, :], 'act')
                load_cast(wdb, wdb[:, fc, :], wd_d[e_ * NFC + fc, :, :], 'pool')
                for kc in range(8):
                    P.mm(GB[b2], GB[b2][:, :], wgb[b2], wgb[b2][:, kc, :], hT2, hT2[:, kc, :], start=(kc == 0), stop=(kc == 7))
                for kc in range(8):
                    P.mm(UB[b2], UB[b2][:, :], wub[b2], wub[b2][:, kc, :], hT2, hT2[:, kc, :], start=(kc == 0), stop=(kc == 7))
                P.act(gsil[b2], gsil[b2][:], GB[b2], GB[b2][:, :], AF.Silu)
                P.tt('dve', actT, actT[:, fc, :], UB[b2], UB[b2][:, :], gsil[b2], gsil[b2][:], ALU.mult)
            for ti in range(GT):
                for half in range(2):
                    hs = slice(half * 512, (half + 1) * 512)
                    yb = YB[(ti * 2 + half) % 2]
                    for fc in range(NFC):
                        P.mm(yb, yb[:, :], actT, actT[:, fc, ti * 128:(ti + 1) * 128], wdb, wdb[:, fc, hs],
                             start=(fc == 0), stop=(fc == NFC - 1))
                    if not moe:
                        P.cp(evac_eng(), yacc[ti], yacc[ti][:, hs], yb, yb[:, :])
                    elif e_ == 0:
                        P.ts('dve', yacc[ti], yacc[ti][:, hs], yb, yb[:, :], gatew[:, ti, 0:1], ALU.mult, extra_r=[gatew])
                    else:
                        P.op('dve', lambda e: e.scalar_tensor_tensor(out=yacc[ti][:, hs], in0=yb[:, :], scalar=gatew[:, ti, e_:e_ + 1],
                                                                     in1=yacc[ti][:, hs], op0=ALU.mult, op1=ALU.add),
                             r=[yb, gatew, yacc[ti]], w=[yacc[ti]])
        for ti in range(GT):
            tok = slice((g * GT + ti) * 128, (g * GT + ti + 1) * 128)
            P.op('pool', lambda e: e.memset(fss[:], 0.0), w=[fss])
            P.act(fjunk, fjunk[:], yacc[ti], yacc[ti][:], AF.Square, extra_w=[fss], accum_out=fss[:, 0:1])
            P.rstd(fss, D, None)
            P.op('dve', lambda e: e.scalar_tensor_tensor(out=ftmp[:], in0=yacc[ti][:], scalar=fss[:, 0:1], in1=vecf[:],
                                                         op0=ALU.mult, op1=ALU.mult), r=[yacc[ti], fss, vecf], w=[ftmp])
            P.tt('dve', ftmp, ftmp[:], ftmp, ftmp[:], X1[ti], X1[ti][:], ALU.add)
            P.dma('sp', out[tok, :], ftmp[:], r=[ftmp])
        P.barrier()
        ph.close()
        P.tes = None
        if on_group is not None:
            on_group(g)
    st.close()
    P.es = old_es
    P.pre = {}


def inputs_B(inp, layer, xcur, mix):
    f = lambda a: np.ascontiguousarray(a, dtype=np.float32)
    moe = (layer % 2 == 1)
    j = layer // 2
    xf = xcur.reshape(-1, D)
    mf = mix.reshape(-1, D)
    aw = f(inp['ada_w'][layer][:, 2048:6144].reshape(128, 8, 4096))
    ab = inp['ada_b'][layer]
    shared = dict(
        adaw=aw, adab_row=f(ab[2048:6144][None, :]), adab_col=chunkT(f(ab[2048:6144]), 32),
        gpost_m=f(inp['norm_post_mix'][layer][None, :]), gpre_f=chunkT(f(inp['norm_pre_ffn'][layer]), 8),
        gpost_f=f(inp['norm_post_ffn'][layer][None, :]),
        gluw=f(inp['ssm_glu_w'][layer].reshape(2, 128, 256).transpose(1, 0, 2)),
        glub=f(inp['ssm_glu_b'][layer][None, :]), ssmg=f(inp['ssm_norm'][layer][None, :]),
        mlag=f(inp['mla_norm'][layer][None, :]),
        wout=f(inp['w_out'][layer].reshape(8, 128, D).transpose(1, 0, 2)))
    if moe:
        wg, wu, wd = inp['moe_w_gate'][j], inp['moe_w_up'][j], inp['moe_w_down'][j]
        shared['router'] = f(inp['moe_router'][j].reshape(8, 128, 8).transpose(1, 0, 2))
    else:
        wg, wu, wd = inp['ffn_w_gate'][j][None], inp['ffn_w_up'][j][None], inp['ffn_w_down'][j][None]
    ne = wg.shape[0]

    def gu(w):
        return f(w.reshape(ne, 8, 128, NFC, 128).transpose(0, 3, 2, 1, 4).reshape(ne * NFC, 128, 1024))

    shared['wg'] = gu(wg)
    shared['wu'] = gu(wu)
    shared['wd'] = f(wd.reshape(ne * NFC, 128, D))
    maps = []
    for core in range(8):
        b = core // 4
        m = dict(shared)
        m['x'] = f(xf[core * TPC:(core + 1) * TPC])
        m['mix'] = f(mf[core * TPC:(core + 1) * TPC])
        m['cvec'] = f(inp['c'][b].reshape(128, 8))
        maps.append(m)
    return maps


RG4 = [[0, 1, 2, 3], [4, 5, 6, 7]]


def build_fused(ntiles=NT, ngroups=NG):
    nc = bass.Bass("TRN2", target_bir_lowering=False)
    ioA = [decl_A(nc, '_a%d' % l) for l in range(2)]
    ioB = [decl_B(nc, '_b%d' % l, moe=(l == 1)) for l in range(2)]
    x = nc.dram_tensor("x", [L, D], F32, kind="ExternalInput").ap()
    xsl = nc.dram_tensor("xsl", [TPC, D], F32, kind="ExternalInput").ap()
    out = nc.dram_tensor("out", [TPC, D], F32, kind="ExternalOutput").ap()
    hx = [nc.dram_tensor("hx%d" % l, [L, 256], F32).ap() for l in range(2)]
    hxall = [nc.dram_tensor("hxall%d" % l, [8 * 4 * 1024, 256], F32).ap() for l in range(2)]
    mixloc = [nc.dram_tensor("mixloc%d" % l, [2, 4, 1024, 256], F32).ap() for l in range(2)]
    xs = nc.dram_tensor("xs", [TPC, D], F32).ap()
    xall = nc.dram_tensor("xall", [8 * 4 * 256, D], F32).ap()

    def xrow1(t):
        tok0 = t * 128
        r_, j_, i0 = tok0 // TPC, (tok0 % TPC) // 256, tok0 % 256
        return xall[(j_ * 4 + r_) * 256 + i0:(j_ * 4 + r_) * 256 + i0 + 128, :]

    with ExitStack() as es:
        P = Prog(nc, es)
        banks = [P.ps([128, 512], F32, 'bank%d' % i) for i in range(8)]
        rank256 = nc.sync.snap((nc.sync.partition_id() % 4) * 256, min_val=0, max_val=768)

        H = [P.view(hxall[l], 'HXALL%d' % l) for l in range(2)]
        XALL = P.view(xall, 'XALL')

        def hx_chunk(l):
            def f(k, evs):
                for ev in evs:
                    P._wait('pool', ev)
                P.coll('AllGather', hx[l][k * 1024:(k + 1) * 1024, :], H[l], hxall[l][k * 4096:(k + 1) * 4096, :], RG4)
            return f

        def xs_group(g):
            for j in (2 * g, 2 * g + 1):
                P.coll('AllGather', xs[j * 256:(j + 1) * 256, :], XALL, xall[j * 1024:(j + 1) * 1024, :], RG4)

        emit_A(P, nc, banks, ioA[0], lambda t: x[t * 128:(t + 1) * 128, :], hx[0], ntiles, on_chunk=hx_chunk(0))
        emit_B(P, nc, banks, ioB[0], xsl, hxall[0], H[0], mixloc[0], xs, rank256, False, ngroups, on_group=xs_group)
        emit_A(P, nc, banks, ioA[1], xrow1, hx[1], ntiles, xdep=XALL, on_chunk=hx_chunk(1))
        emit_B(P, nc, banks, ioB[1], xs, hxall[1], H[1], mixloc[1], out, rank256, True, ngroups)
        P.finish()
    return nc


def fused_inputs(inp):
    x = np.ascontiguousarray(inp['x'], dtype=np.float32)
    dummy = np.zeros((2, L, D), np.float32)
    maps = [dict() for _ in range(8)]
    for layer in range(2):
        ma = inputs_A(inp, layer, x)
        mb = inputs_B(inp, layer, x, dummy)
        for c in range(8):
            for k, v in ma[c].items():
                if k != 'x':
                    maps[c][k + '_a%d' % layer] = v
            for k, v in mb[c].items():
                if k not in ('x', 'mix'):
                    maps[c][k + '_b%d' % layer] = v
    xf = x.reshape(-1, D)
    for c in range(8):
        maps[c]['x'] = x[c // 4]
        maps[c]['xsl'] = np.ascontiguousarray(xf[c * TPC:(c + 1) * TPC])
    return maps


def kernel(**inputs):
    inp = {k: np.asarray(v) for k, v in inputs.items()}
    if 'F' not in _CACHE:
        _CACHE['F'] = build_fused()
    res = run_bass_kernel_spmd(_CACHE['F'], fused_inputs(inp), core_ids=list(range(8)))
    return np.concatenate([res.results[c]['out'] for c in range(8)], axis=0).reshape(2, L, D).astype(np.float32)
```

```python
import math
from contextlib import ExitStack

import numpy as np
import concourse.bass as bass
import concourse.mybir as mybir
from concourse.bass_utils import run_bass_kernel_spmd

F32 = mybir.dt.float32
BF16 = mybir.dt.bfloat16
I32 = mybir.dt.int32
ALU = mybir.AluOpType
AF = mybir.ActivationFunctionType
AX = mybir.AxisListType

D = 1024
L = 8192
NT = 64
EPS = 1e-6
TWO_PI = 2.0 * math.pi
D_FF = 3584
NFC = 28
N_EXP = 8


class T:
    def __init__(self, t, name, root=None):
        self.t = t
        self.name = name
        self.lastw = None
        self.readers = {}
        self.root = self if root is None else root.root
        self.excl = False

    def __getitem__(self, idx):
        return self.t[idx]


class Prog:
    def __init__(self, nc, es, ndma_sems=8):
        self.nc = nc
        self.es = es
        self.engs = {'pe': nc.tensor, 'act': nc.scalar, 'dve': nc.vector, 'pool': nc.gpsimd, 'sp': nc.sync}
        self.sem = {k: es.enter_context(nc.semaphore('s_' + k)) for k in self.engs}
        self.cnt = {k: 0 for k in self.engs}
        self.waited = {k: {} for k in self.engs}
        self.dma_sems = {}
        for q in ('sp', 'pool'):
            self.dma_sems[q] = [[es.enter_context(nc.semaphore('d_%s%d' % (q, i))), 0] for i in range(ndma_sems)]
        self.dma_i = {q: 0 for q in self.dma_sems}
        self.cc_sem = es.enter_context(nc.semaphore('cc_sem'))
        self.cc_n = 0
        self.nbuf = 0
        self.tes = None
        self.pre = {}

    def sb(self, shape, dt, name=None, keep=False):
        if keep and name in self.pre:
            assert list(self.pre[name].t.shape) == list(shape), name
            return self.pre[name]
        assert not (keep and self.tes is not None), "persistent buffer %s must be pre-allocated" % name
        self.nbuf += 1
        uname = (name or 'b') + '_%d' % self.nbuf
        es = self.es if (keep or self.tes is None) else self.tes
        t = T(es.enter_context(self.nc.sbuf_tensor(uname, shape, dt)), uname)
        if keep:
            self.pre[name] = t
        return t

    def ps(self, shape, dt, name=None):
        self.nbuf += 1
        name = name or ('p%d' % self.nbuf)
        t = T(self.es.enter_context(self.nc.psum_tensor(name, shape, dt)), name)
        t.excl = True
        return t

    def view(self, ap, name='v', parent=None):
        return T(ap, name, root=parent)

    def _wait(self, engname, ev):
        sem, val, src = ev
        if src == 'pe' and engname == 'pe':
            return
        key = id(sem)
        w = self.waited[engname]
        if w.get(key, 0) >= val:
            return
        w[key] = val
        self.engs[engname].wait_ge(sem, val)

    def _deps(self, engname, r, w):
        for b in r:
            b = b.root
            if b.lastw is not None:
                self._wait(engname, b.lastw)
        for b in w:
            b = b.root
            if b.lastw is not None:
                self._wait(engname, b.lastw)
            for ev in b.readers.values():
                self._wait(engname, ev)

    def _commit(self, ev, r, w):
        for b in r:
            b = b.root
            old = b.readers.get(id(ev[0]))
            if old is None or old[1] < ev[1]:
                b.readers[id(ev[0])] = ev
        for b in w:
            b = b.root
            b.lastw = ev
            b.readers = {}

    def op(self, engname, fn, r=(), w=()):
        xr = [b for b in r if b.root.excl]
        if xr:
            r = [b for b in r if not b.root.excl]
            w = list(w) + xr
        self._deps(engname, r, w)
        ins = fn(self.engs[engname])
        self.cnt[engname] += 1
        ins.then_inc(self.sem[engname], 1)
        ev = (self.sem[engname], self.cnt[engname], engname)
        self._commit(ev, r, w)
        return ev

    def dma(self, q, out, in_, r=(), w=(), **kw):
        if out.dtype != in_.dtype:
            q = 'pool'
        self._deps(q, r, w)
        slot = self.dma_sems[q][self.dma_i[q] % len(self.dma_sems[q])]
        self.dma_i[q] += 1
        sem, n = slot
        if n > 0:
            self._wait(q, (sem, 16 * n, 'dma'))
        slot[1] = n + 1
        self.engs[q].dma_start(out=out, in_=in_, **kw).then_inc(sem, 16)
        ev = (sem, 16 * (n + 1), 'dma')
        self._commit(ev, r, w)
        return ev

    def coll(self, kind, src_ap, dst, dst_ap, groups):
        self.nc.gpsimd.collective_compute(kind, ALU.bypass, replica_groups=groups, ins=[src_ap], outs=[dst_ap]).then_inc(self.cc_sem, 1)
        self.cc_n += 1
        ev = (self.cc_sem, self.cc_n, 'cc')
        self._commit(ev, [], [dst])
        return ev

    def barrier(self):
        for e in self.engs:
            for o in self.engs:
                if o != e and self.cnt[o] > 0:
                    self._wait(e, (self.sem[o], self.cnt[o], 'x'))
            for q in self.dma_sems:
                for sem, n in self.dma_sems[q]:
                    if n > 0:
                        self._wait(e, (sem, 16 * n, 'dma'))
            if self.cc_n > 0:
                self._wait(e, (self.cc_sem, self.cc_n, 'cc'))

    def finish(self):
        self.barrier()
        for q in self.dma_sems:
            for sem, n in self.dma_sems[q]:
                if n > 0:
                    self._wait('sp', (sem, 16 * n, 'dma'))

    def mm(self, out, oap, lhsT, lap, rhs, rap, start=True, stop=True):
        return self.op('pe', lambda e: e.matmul(oap, lhsT=lap, rhs=rap, start=start, stop=stop),
                       r=[lhsT, rhs], w=[out])

    def tr(self, out, oap, src, sap, ident):
        n = sap.shape[0]
        return self.op('pe', lambda e: e.transpose(oap, sap, ident[0:n, 0:n]), r=[src, ident], w=[out])

    def tt(self, eng, out, oap, a, aap, b, bap, op):
        return self.op(eng, lambda e: e.tensor_tensor(out=oap, in0=aap, in1=bap, op=op), r=[a, b], w=[out])

    def ts(self, eng, out, oap, a, aap, s1, op0, s2=None, op1=None, extra_r=()):
        if op1 is None:
            return self.op(eng, lambda e: e.tensor_scalar(out=oap, in0=aap, scalar1=s1, scalar2=None, op0=op0),
                           r=[a] + list(extra_r), w=[out])
        return self.op(eng, lambda e: e.tensor_scalar(out=oap, in0=aap, scalar1=s1, scalar2=s2, op0=op0, op1=op1),
                       r=[a] + list(extra_r), w=[out])

    def act(self, out, oap, a, aap, func, extra_r=(), extra_w=(), **kw):
        return self.op('act', lambda e: e.activation(out=oap, in_=aap, func=func, **kw),
                       r=[a] + list(extra_r), w=[out] + list(extra_w))

    def cp(self, eng, out, oap, a, aap):
        if eng == 'act':
            return self.act(out, oap, a, aap, AF.Copy)
        return self.op(eng, lambda e: e.tensor_copy(out=oap, in_=aap), r=[a], w=[out])

    def rstd(self, ss, n, tmp):
        self.ts('dve', ss, ss[:], ss, ss[:], 1.0 / n, ALU.mult, EPS, ALU.add)
        self.act(ss, ss[:], ss, ss[:], AF.Sqrt)
        self.op('dve', lambda e: e.reciprocal(out=ss[:], in_=ss[:]), r=[ss], w=[ss])

    def range_reduce(self, out, oap, src, sap, kf, kfap, ki, kiap, shift=0.0):
        self.ts('dve', kf, kfap, src, sap, 1.0 / TWO_PI, ALU.mult, shift / TWO_PI, ALU.add)
        self.cp('dve', ki, kiap, kf, kfap)
        self.cp('dve', kf, kfap, ki, kiap)
        self.op('dve', lambda e: e.scalar_tensor_tensor(out=oap, in0=kfap, scalar=-TWO_PI, in1=sap,
                                                        op0=ALU.mult, op1=ALU.add), r=[kf, src], w=[out])
        if shift != 0.0:
            self.ts('dve', out, oap, out, oap, shift, ALU.add)
        self.ts('dve', out, oap, out, oap, -math.pi, ALU.max, math.pi, ALU.min)


def make_stager(P, nstage=4, cols=1024):
    stg = [P.sb([128, cols], F32, 'stage%d' % i) for i in range(nstage)]
    cnt = [0]
    engs = ('dve', 'act', 'pool')

    def load(dst, dst_ap, src_ap):
        n = src_ap.shape[-1]
        sg = stg[cnt[0] % nstage]
        eng = engs[cnt[0] % 3]
        cnt[0] += 1
        P.dma('sp', sg[:, 0:n], src_ap, w=[sg])
        P.cp(eng, dst, dst_ap, sg, sg[:, 0:n])
    return load


def make_ident(P):
    identf = P.sb([128, 128], F32, 'identf')
    ident = P.sb([128, 128], BF16, 'ident', keep=True)
    P.op('pool', lambda e: e.memset(identf[:], 1.0), w=[identf])
    P.op('pool', lambda e: e.affine_select(out=identf[:], in_=identf[:], pattern=[[-1, 128]],
                                           compare_op=ALU.is_equal, fill=0.0, base=0, channel_multiplier=1),
         r=[identf], w=[identf])
    P.cp('dve', ident, ident[:], identf, identf[:])
    return ident, identf


def adaln_mod(P, nc, cvec, adaw, adab, ncol_chunks, modps, name, stager):
    cv = P.sb([128, 8], F32, name + '_cv')
    cvb = P.sb([128, 8], BF16, name + '_cvb')
    P.dma('sp', cv[:], cvec[:, :], w=[cv])
    P.act(cvb, cvb[:], cv, cv[:], AF.Silu)
    ncols = ncol_chunks * 128
    mod = P.sb([128, ncol_chunks], F32, name + '_mod', keep=True)
    ab = P.sb([128, ncol_chunks], F32, name + '_ab')
    P.dma('sp', ab[:], adab[:, :], w=[ab])
    cw = 1024
    wts = [P.sb([128, 8, cw], BF16, name + '_w%d' % i) for i in range(2)]
    for ci in range(ncols // cw):
        wt = wts[ci % 2]
        for kc in range(8):
            stager(wt, wt[:, kc, :], adaw[:, kc, ci * cw:(ci + 1) * cw])
        for cc in range(cw // 128):
            ch = ci * (cw // 128) + cc
            for kc in range(8):
                P.mm(modps, modps[:, ch:ch + 1], wt, wt[:, kc, cc * 128:(cc + 1) * 128], cvb, cvb[:, kc:kc + 1],
                     start=(kc == 0), stop=(kc == 7))
    P.tt('dve', mod, mod[:], modps, modps[:, 0:ncol_chunks], ab, ab[:], ALU.add)
    return mod


import os
STOP_AT = float(os.environ.get('STOP_AT', '99'))


def decl_A(nc, sfx):
    io = {}
    io['cvec'] = nc.dram_tensor('cvec' + sfx, [128, 8], F32, kind='ExternalInput').ap()
    io['pos'] = nc.dram_tensor('pos' + sfx, [128, NT], I32, kind='ExternalInput').ap()
    io['adaw'] = nc.dram_tensor('adaw' + sfx, [128, 8, 2048], F32, kind='ExternalInput').ap()
    io['adab'] = nc.dram_tensor('adab' + sfx, [128, 16], F32, kind='ExternalInput').ap()
    io['gpre'] = nc.dram_tensor('gpre' + sfx, [128, 8], F32, kind='ExternalInput').ap()
    io['wsel'] = nc.dram_tensor('wsel' + sfx, [128, 8, 768], F32, kind='ExternalInput').ap()
    io['retg'] = nc.dram_tensor('retg' + sfx, [1, 64], F32, kind='ExternalInput').ap()
    io['invf'] = nc.dram_tensor('invf' + sfx, [1, 32], F32, kind='ExternalInput').ap()
    io['dmaskT_d'] = nc.dram_tensor('dmaskT' + sfx, [128, 128], F32, kind='ExternalInput').ap()
    io['tri_d'] = nc.dram_tensor('tri' + sfx, [128, 128], F32, kind='ExternalInput').ap()
    io['qdec_d'] = nc.dram_tensor('qdec' + sfx, [64, 128], F32, kind='ExternalInput').ap()
    io['kdec_d'] = nc.dram_tensor('kdec' + sfx, [128, 1], F32, kind='ExternalInput').ap()
    io['dcy_d'] = nc.dram_tensor('dcy' + sfx, [64, 1], F32, kind='ExternalInput').ap()
    io['cmask_d'] = nc.dram_tensor('cmask' + sfx, [128, 128], F32, kind='ExternalInput').ap()
    io['jp1_d'] = nc.dram_tensor('jp1' + sfx, [128, 1], F32, kind='ExternalInput').ap()
    io['irow1_d'] = nc.dram_tensor('irow1' + sfx, [128, 128], F32, kind='ExternalInput').ap()
    io['are_tm_d'] = nc.dram_tensor('are_tm' + sfx, [1, 512], F32, kind='ExternalInput').ap()
    io['aim_tm_d'] = nc.dram_tensor('aim_tm' + sfx, [1, 512], F32, kind='ExternalInput').ap()
    io['ldt_tm_d'] = nc.dram_tensor('ldt_tm' + sfx, [1, 512], F32, kind='ExternalInput').ap()
    io['BR_d'] = nc.dram_tensor('BR' + sfx, [64, 512], F32, kind='ExternalInput').ap()
    io['BI_d'] = nc.dram_tensor('BI' + sfx, [64, 512], F32, kind='ExternalInput').ap()
    io['are_sm_d'] = nc.dram_tensor('are_sm' + sfx, [128, 2], F32, kind='ExternalInput').ap()
    io['aim_sm_d'] = nc.dram_tensor('aim_sm' + sfx, [128, 2], F32, kind='ExternalInput').ap()
    io['ldt_sm_d'] = nc.dram_tensor('ldt_sm' + sfx, [128, 2], F32, kind='ExternalInput').ap()
    io['CR_d'] = nc.dram_tensor('CR' + sfx, [128, 2, 64], F32, kind='ExternalInput').ap()
    io['CI_d'] = nc.dram_tensor('CI' + sfx, [128, 2, 64], F32, kind='ExternalInput').ap()
    io['DD_d'] = nc.dram_tensor('DD' + sfx, [64, 64], F32, kind='ExternalInput').ap()
    io['qng_d'] = nc.dram_tensor('qng' + sfx, [128, 2], F32, kind='ExternalInput').ap()
    io['wuq_d'] = nc.dram_tensor('wuq' + sfx, [128, 2, 192], F32, kind='ExternalInput').ap()
    io['kvng_d'] = nc.dram_tensor('kvng' + sfx, [128, 1], F32, kind='ExternalInput').ap()
    io['wukv_d'] = nc.dram_tensor('wukv' + sfx, [128, 256], F32, kind='ExternalInput').ap()
    return io


def emit_A(P, nc, banks, io, xrow, hx, ntiles=NT, xdep=None, on_chunk=None):
    cvec = io['cvec']
    pos = io['pos']
    adaw = io['adaw']
    adab = io['adab']
    gpre = io['gpre']
    wsel = io['wsel']
    retg = io['retg']
    invf = io['invf']
    dmaskT_d = io['dmaskT_d']
    tri_d = io['tri_d']
    qdec_d = io['qdec_d']
    kdec_d = io['kdec_d']
    dcy_d = io['dcy_d']
    cmask_d = io['cmask_d']
    jp1_d = io['jp1_d']
    irow1_d = io['irow1_d']
    are_tm_d = io['are_tm_d']
    aim_tm_d = io['aim_tm_d']
    ldt_tm_d = io['ldt_tm_d']
    BR_d = io['BR_d']
    BI_d = io['BI_d']
    are_sm_d = io['are_sm_d']
    aim_sm_d = io['aim_sm_d']
    ldt_sm_d = io['ldt_sm_d']
    CR_d = io['CR_d']
    CI_d = io['CI_d']
    DD_d = io['DD_d']
    qng_d = io['qng_d']
    wuq_d = io['wuq_d']
    kvng_d = io['kvng_d']
    wukv_d = io['wukv_d']
    st = ExitStack()
    old_es = P.es
    P.es = st
    P.pre = {}
    b7 = banks[7][:, :].bitcast(BF16)
    TB = [P.view(b7[:, 0:512], 'TB', parent=banks[7])]
    TS = [P.view(b7[:, 256 + 128 * k:384 + 128 * k], 'TS%d' % k, parent=banks[7]) for k in range(2)]
    PJ0 = P.view(banks[0][:, :], 'PJ0', parent=banks[0])
    PJ1 = P.view(banks[1][:, 0:256], 'PJ1', parent=banks[1])
    RS = P.view(banks[1][:, 256:384], 'RS', parent=banks[1])
    RO = P.view(banks[1][:, 384:448], 'RO', parent=banks[1])
    RKV = P.view(banks[1][0:64, 448:512], 'RKV', parent=banks[1])
    SA = P.view(banks[2][:, :], 'SA', parent=banks[2])
    SB = P.view(banks[3][:, :], 'SB', parent=banks[3])
    QTN = P.view(banks[4][:, 0:128], 'QTN', parent=banks[4])
    KTN = P.view(banks[4][:, 128:256], 'KTN', parent=banks[4])
    VP = P.view(banks[4][:, 256:384], 'VP', parent=banks[4])
    QRP = P.view(banks[4][:, 384:448], 'QRP', parent=banks[4])
    SY = P.view(banks[4][:, 448:512], 'SY', parent=banks[4])
    SC = [P.view(banks[5][:, :], 'SC0', parent=banks[5]), P.view(banks[6][:, :], 'SC1', parent=banks[6])]

    for nm, shp, dt_ in [('ident', [128, 128], BF16), ('ada_mod', [128, 16], F32), ('scale1', [128, 8], F32),
                         ('wselb', [128, 8, 768], BF16), ('retg_t', [128, 64], F32), ('dmaskT', [128, 128], F32),
                         ('tri', [128, 128], BF16), ('qdec', [64, 128], F32), ('kdec', [128, 1], F32),
                         ('dcy', [64, 1], F32), ('cmask', [128, 128], F32), ('SINT', [128, NT * 32], F32),
                         ('COST', [128, NT * 32], F32), ('NSINT', [128, NT * 32], F32), ('EA', [128, 512], F32),
                         ('EB', [128, 512], F32), ('Bmat', [64, 512], BF16), ('Bswp', [64, 512], BF16),
                         ('EA2', [128, 512], F32), ('EB2', [128, 512], F32), ('Cmat', [128, 4, 64], BF16),
                         ('DDb', [64, 64], BF16), ('xprev', [128, 4], F32), ('xprevs', [128, 4], F32),
                         ('wuqb', [128, 2, 192], BF16), ('wukvb', [128, 256], BF16)]:
        P.sb(shp, dt_, nm, keep=True)
    tes = ExitStack()
    P.tes = tes
    ident, identf = make_ident(P)

    stager = make_stager(P)
    mod = adaln_mod(P, nc, cvec, adaw, adab, 16, SA, 'ada', stager)
    gp = P.sb([128, 8], F32, 'gp')
    P.dma('sp', gp[:], gpre[:, :], w=[gp])
    scale1 = P.sb([128, 8], F32, 'scale1', keep=True)
    P.ts('dve', scale1, scale1[:], mod, mod[:, 8:16], 1.0, ALU.add)
    P.tt('dve', scale1, scale1[:], scale1, scale1[:], gp, gp[:], ALU.mult)
    shm = P.view(mod[:, 0:8], 'shm', parent=mod)

    wselb = P.sb([128, 8, 768], BF16, 'wselb', keep=True)
    for kc in range(8):
        stager(wselb, wselb[:, kc, :], wsel[:, kc, :])

    def load(name, src, shape, dt=F32, q='sp', bcast=None, keep=False):
        t = P.sb(shape, dt, name, keep=keep)
        P.dma(q, t[:], src if bcast is None else src.broadcast_to(bcast), w=[t])
        return t

    retg_t = load('retg_t', retg[0:1, :], [128, 64], bcast=[128, 64], keep=True)
    invf_t = load('invf_t', invf[0:1, :], [128, 32], bcast=[128, 32])
    dmaskT = load('dmaskT', dmaskT_d[:, :], [128, 128], keep=True)
    tri_f = load('tri_f', tri_d[:, :], [128, 128])
    tri = P.sb([128, 128], BF16, 'tri', keep=True)
    P.cp('dve', tri, tri[:], tri_f, tri_f[:])
    qdec = load('qdec', qdec_d[:, :], [64, 128], keep=True)
    kdec = load('kdec', kdec_d[:, :], [128, 1], keep=True)
    dcy = load('dcy', dcy_d[:, :], [64, 1], keep=True)
    cmask = load('cmask', cmask_d[:, :], [128, 128], keep=True)
    jp1 = load('jp1', jp1_d[:, :], [128, 1])
    irow1 = load('irow1', irow1_d[:, :], [128, 128])

    posi = load('posi', pos[:, :], [128, NT], I32)
    posf = P.sb([128, NT], F32, 'posf')
    P.cp('dve', posf, posf[:], posi, posi[:])
    NR = NT * 32
    ang = P.sb([128, NR], F32, 'ang')
    P.tt('dve', ang, ang[:].rearrange("p (t f) -> p t f", f=32), posf,
         posf[:].unsqueeze(2).broadcast_to([128, NT, 32]), invf_t,
         invf_t[:].unsqueeze(1).broadcast_to([128, NT, 32]), ALU.mult)
    SINT = P.sb([128, NR], F32, 'SINT', keep=True)
    COST = P.sb([128, NR], F32, 'COST', keep=True)
    NSINT = P.sb([128, NR], F32, 'NSINT', keep=True)
    rkf = P.sb([128, NR], F32, 'rkf')
    rki = P.sb([128, NR], I32, 'rki')
    P.range_reduce(SINT, SINT[:], ang, ang[:], rkf, rkf[:], rki, rki[:])
    P.act(SINT, SINT[:], SINT, SINT[:], AF.Sin)
    P.range_reduce(COST, COST[:], ang, ang[:], rkf, rkf[:], rki, rki[:], shift=math.pi / 2)
    P.act(COST, COST[:], COST, COST[:], AF.Sin)
    P.ts('dve', NSINT, NSINT[:], SINT, SINT[:], -1.0, ALU.mult)

    are_tm = load('are_tm', are_tm_d[0:1, :], [128, 512], bcast=[128, 512])
    aim_tm = load('aim_tm', aim_tm_d[0:1, :], [128, 512], bcast=[128, 512])
    dt_tm = load('dt_tm', ldt_tm_d[0:1, :], [128, 512], bcast=[128, 512])
    P.act(dt_tm, dt_tm[:], dt_tm, dt_tm[:], AF.Exp)
    rho = P.sb([128, 512], F32, 'rho')
    tht = P.sb([128, 512], F32, 'tht')
    P.tt('dve', rho, rho[:], are_tm, are_tm[:], dt_tm, dt_tm[:], ALU.mult)
    P.tt('dve', tht, tht[:], aim_tm, aim_tm[:], dt_tm, dt_tm[:], ALU.mult)
    s5a = P.sb([128, 512], F32, 's5a')
    s5b = P.sb([128, 512], F32, 's5b')
    s5c = P.sb([128, 512], F32, 's5c')
    s5k = P.sb([128, 512], F32, 's5k')
    s5i = P.sb([128, 512], I32, 's5i')
    EA = P.sb([128, 512], F32, 'EA', keep=True)
    EB = P.sb([128, 512], F32, 'EB', keep=True)
    njp1 = P.sb([128, 1], F32, 'njp1')
    P.ts('dve', njp1, njp1[:], jp1, jp1[:], -1.0, ALU.mult)
    P.ts('dve', s5a, s5a[:], rho, rho[:], njp1[:, 0:1], ALU.mult, extra_r=[njp1])
    P.act(s5a, s5a[:], s5a, s5a[:], AF.Exp)
    P.ts('dve', s5b, s5b[:], tht, tht[:], jp1[:, 0:1], ALU.mult, extra_r=[jp1])
    P.range_reduce(s5c, s5c[:], s5b, s5b[:], s5k, s5k[:], s5i, s5i[:], shift=math.pi / 2)
    P.act(s5c, s5c[:], s5c, s5c[:], AF.Sin)
    P.tt('dve', EA, EA[:], s5a, s5a[:], s5c, s5c[:], ALU.mult)
    P.range_reduce(s5c, s5c[:], s5b, s5b[:], s5k, s5k[:], s5i, s5i[:])
    P.act(s5c, s5c[:], s5c, s5c[:], AF.Sin)
    P.tt('dve', EB, EB[:], s5a, s5a[:], s5c, s5c[:], ALU.mult)
    P.ts('dve', EB, EB[:], EB, EB[:], -1.0, ALU.mult)
    Lre = P.sb([128, 512], F32, 'Lre')
    Lim = P.sb([128, 512], F32, 'Lim')
    P.act(s5a, s5a[:], rho, rho[:], AF.Exp)
    P.range_reduce(s5c, s5c[:], tht, tht[:], s5k, s5k[:], s5i, s5i[:], shift=math.pi / 2)
    P.act(s5c, s5c[:], s5c, s5c[:], AF.Sin)
    P.tt('dve', Lre, Lre[:], s5a, s5a[:], s5c, s5c[:], ALU.mult)
    P.range_reduce(s5c, s5c[:], tht, tht[:], s5k, s5k[:], s5i, s5i[:])
    P.act(s5c, s5c[:], s5c, s5c[:], AF.Sin)
    P.tt('dve', Lim, Lim[:], s5a, s5a[:], s5c, s5c[:], ALU.mult)
    P.ts('dve', Lre, Lre[:], Lre, Lre[:], -1.0, ALU.add)
    P.tt('dve', s5a, s5a[:], are_tm, are_tm[:], are_tm, are_tm[:], ALU.mult)
    P.tt('dve', s5b, s5b[:], aim_tm, aim_tm[:], aim_tm, aim_tm[:], ALU.mult)
    P.tt('dve', s5a, s5a[:], s5a, s5a[:], s5b, s5b[:], ALU.add)
    P.op('dve', lambda e: e.reciprocal(out=s5a[:], in_=s5a[:]), r=[s5a], w=[s5a])
    Fre = P.sb([128, 512], F32, 'Fre')
    Fim = P.sb([128, 512], F32, 'Fim')
    P.tt('dve', s5b, s5b[:], Lre, Lre[:], are_tm, are_tm[:], ALU.mult)
    P.tt('dve', s5c, s5c[:], Lim, Lim[:], aim_tm, aim_tm[:], ALU.mult)
    P.tt('dve', s5b, s5b[:], s5b, s5b[:], s5c, s5c[:], ALU.add)
    P.tt('dve', Fre, Fre[:], s5b, s5b[:], s5a, s5a[:], ALU.mult)
    P.tt('dve', s5b, s5b[:], Lim, Lim[:], are_tm, are_tm[:], ALU.mult)
    P.tt('dve', s5c, s5c[:], Lre, Lre[:], aim_tm, aim_tm[:], ALU.mult)
    P.tt('dve', s5b, s5b[:], s5b, s5b[:], s5c, s5c[:], ALU.subtract)
    P.tt('dve', Fim, Fim[:], s5b, s5b[:], s5a, s5a[:], ALU.mult)
    BRt = load('BRt', BR_d[:, :], [64, 512])
    BIt = load('BIt', BI_d[:, :], [64, 512])
    bre = P.sb([64, 512], F32, 'bre')
    bim = P.sb([64, 512], F32, 'bim')
    btmp = P.sb([64, 512], F32, 'btmp')
    P.tt('dve', bre, bre[:], Fre, Fre[0:64, :], BRt, BRt[:], ALU.mult)
    P.tt('dve', btmp, btmp[:], Fim, Fim[0:64, :], BIt, BIt[:], ALU.mult)
    P.tt('dve', bre, bre[:], bre, bre[:], btmp, btmp[:], ALU.subtract)
    P.tt('dve', bim, bim[:], Fre, Fre[0:64, :], BIt, BIt[:], ALU.mult)
    P.tt('dve', btmp, btmp[:], Fim, Fim[0:64, :], BRt, BRt[:], ALU.mult)
    P.tt('dve', bim, bim[:], bim, bim[:], btmp, btmp[:], ALU.add)
    Bmat = P.sb([64, 512], BF16, 'Bmat', keep=True)
    Bswp = P.sb([64, 512], BF16, 'Bswp', keep=True)

    def v4(t, rows=128):
        return t[0:rows, :].rearrange("p (a r q) -> p a r q", a=2, r=2)

    P.cp('dve', Bmat, v4(Bmat, 64)[:, :, 0, :], bre, v4(bre, 64)[:, :, 0, :])
    P.cp('dve', Bmat, v4(Bmat, 64)[:, :, 1, :], bim, v4(bim, 64)[:, :, 1, :])
    P.ts('dve', Bswp, v4(Bswp, 64)[:, :, 0, :], bim, v4(bim, 64)[:, :, 0, :], -1.0, ALU.mult)
    P.cp('dve', Bswp, v4(Bswp, 64)[:, :, 1, :], bre, v4(bre, 64)[:, :, 1, :])
    are_sm = load('are_sm', are_sm_d[:, :], [128, 2])
    aim_sm = load('aim_sm', aim_sm_d[:, :], [128, 2])
    dt_sm = load('dt_sm', ldt_sm_d[:, :], [128, 2])
    P.act(dt_sm, dt_sm[:], dt_sm, dt_sm[:], AF.Exp)
    rho_sm = P.sb([128, 2], F32, 'rho_sm')
    tht_sm = P.sb([128, 2], F32, 'tht_sm')
    P.tt('dve', rho_sm, rho_sm[:], are_sm, are_sm[:], dt_sm, dt_sm[:], ALU.mult)
    P.tt('dve', tht_sm, tht_sm[:], aim_sm, aim_sm[:], dt_sm, dt_sm[:], ALU.mult)
    EA2 = P.sb([128, 512], F32, 'EA2', keep=True)
    EB2 = P.sb([128, 512], F32, 'EB2', keep=True)
    pm = P.sb([128, 256], F32, 'pm')
    pa = P.sb([128, 256], F32, 'pa')
    pc = P.sb([128, 256], F32, 'pc')
    pk = P.sb([128, 256], F32, 'pk')
    pki = P.sb([128, 256], I32, 'pki')
    for gp_ in range(2):
        sl = slice(gp_ * 128, (gp_ + 1) * 128)
        P.ts('dve', pm, pm[:, sl], irow1, irow1[:], rho_sm[:, gp_:gp_ + 1], ALU.mult, extra_r=[rho_sm])
        P.ts('dve', pa, pa[:, sl], irow1, irow1[:], tht_sm[:, gp_:gp_ + 1], ALU.mult, extra_r=[tht_sm])
    P.act(pm, pm[:], pm, pm[:], AF.Exp)
    P.range_reduce(pc, pc[:], pa, pa[:], pk, pk[:], pki, pki[:], shift=math.pi / 2)
    P.act(pc, pc[:], pc, pc[:], AF.Sin)
    P.tt('dve', pc, pc[:], pc, pc[:], pm, pm[:], ALU.mult)
    pc3 = pc[:].rearrange("p (a i) -> p a i", a=2)
    P.cp('dve', EA2, v4(EA2)[:, :, 0, :], pc, pc3)
    P.cp('dve', EA2, v4(EA2)[:, :, 1, :], pc, pc3)
    P.range_reduce(pc, pc[:], pa, pa[:], pk, pk[:], pki, pki[:])
    P.act(pc, pc[:], pc, pc[:], AF.Sin)
    P.tt('dve', pc, pc[:], pc, pc[:], pm, pm[:], ALU.mult)
    P.ts('dve', EB2, v4(EB2)[:, :, 0, :], pc, pc3, -1.0, ALU.mult)
    P.cp('dve', EB2, v4(EB2)[:, :, 1, :], pc, pc3)
    CRt = load('CRt', CR_d[:, :, :], [128, 2, 64])
    CIt = load('CIt', CI_d[:, :, :], [128, 2, 64])
    Cmat = P.sb([128, 4, 64], BF16, 'Cmat', keep=True)
    for gp_ in range(2):
        P.cp('dve', Cmat, Cmat[:, gp_ * 2, :], CRt, CRt[:, gp_, :])
        P.ts('dve', Cmat, Cmat[:, gp_ * 2 + 1, :], CIt, CIt[:, gp_, :], -1.0, ALU.mult)
    DDt = load('DDt', DD_d[:, :], [64, 64])
    DDb = P.sb([64, 64], BF16, 'DDb', keep=True)
    P.cp('dve', DDb, DDb[:], DDt, DDt[:])
    xprev = P.sb([128, 4], F32, 'xprev', keep=True)
    xprevs = P.sb([128, 4], F32, 'xprevs', keep=True)
    P.op('pool', lambda e: e.memset(xprev[:], 0.0), w=[xprev])
    P.op('pool', lambda e: e.memset(xprevs[:], 0.0), w=[xprevs])

    qng = load('qng', qng_d[:, :], [128, 2])
    wuq_f = load('wuq_f', wuq_d[:, :, :], [128, 2, 192])
    wuqb = P.sb([128, 2, 192], BF16, 'wuqb', keep=True)
    for kc in range(2):
        P.ts('dve', wuqb, wuqb[:, kc, :], wuq_f, wuq_f[:, kc, :], qng[:, kc:kc + 1], ALU.mult, extra_r=[qng])
    kvng = load('kvng', kvng_d[:, :], [128, 1])
    wukv_f = load('wukv_f', wukv_d[:, :], [128, 256])
    wukvb = P.sb([128, 256], BF16, 'wukvb', keep=True)
    P.ts('dve', wukvb, wukvb[:], wukv_f, wukv_f[:], kvng[:, 0:1], ALU.mult, extra_r=[kvng])

    P.barrier()
    tes.close()
    P.tes = None
    KTNP_t = P.es.enter_context(nc.sbuf_tensor('KTNP_%d' % id(P.es), [128, L], BF16))
    KTR_t = P.es.enter_context(nc.sbuf_tensor('KTR_%d' % id(P.es), [64, L], BF16))
    VV_t = P.es.enter_context(nc.sbuf_tensor('VV_%d' % id(P.es), [128, NT, 128], BF16))
    KTNP = [P.view(KTNP_t[:, t * 128:(t + 1) * 128], 'ktn%d' % t) for t in range(NT)]
    KTR = [P.view(KTR_t[:, t * 128:(t + 1) * 128], 'ktr%d' % t) for t in range(NT)]
    VV = [P.view(VV_t[:, t, :], 'vv%d' % t) for t in range(NT)]
    SROW_t = P.es.enter_context(nc.sbuf_tensor('SROW_%d' % id(P.es), [128, L], F32))
    SROW = [P.view(SROW_t, 'srow0'), P.view(SROW_t, 'srow1')]
    PB = P.sb([128, L], BF16, 'PB')
    state = P.sb([64, 64], F32, 'state')
    state_bf = P.sb([64, 64], BF16, 'state_bf')
    P.op('pool', lambda e: e.memset(state[:], 0.0), w=[state])
    P.op('pool', lambda e: e.memset(state_bf[:], 0.0), w=[state_bf])

    def dbl(shape, dt, name):
        return [P.sb(shape, dt, name + '%d' % i) for i in range(2)]

    xt = dbl([128, D], F32, 'xt')
    xnb = dbl([128, D], BF16, 'xnb')
    junk = P.sb([128, D], BF16, 'junk')
    ssx = dbl([128, 1], F32, 'ssx')
    hT = dbl([128, 8, 128], BF16, 'hT')
    ropeA = P.sb([128, 192], F32, 'ropeA')
    ropeB = P.sb([128, 192], F32, 'ropeB')
    rqk = dbl([128, 192], BF16, 'rqk')
    QTs = dbl([64, 128], BF16, 'QTs')
    KTs = dbl([64, 128], BF16, 'KTs')
    QTd = dbl([64, 128], BF16, 'QTd')
    vb = dbl([128, 64], BF16, 'vb')
    vdec = dbl([128, 64], BF16, 'vdec')
    smask = dbl([128, 128], BF16, 'smask')
    ssr = dbl([128, 1], F32, 'ssr')
    gs = dbl([128, 64], F32, 'gs')
    ro = dbl([128, 64], F32, 'ro')
    junk64 = P.sb([128, 256], F32, 'junk64')
    ub = dbl([128, 64], BF16, 'ub')
    UTs = dbl([64, 128], BF16, 'UTs')
    T1 = P.sb([128, 512], F32, 'T1')
    T2 = P.sb([128, 512], F32, 'T2')
    Wb = dbl([128, 512], BF16, 'Wb')
    Zf = T1
    Zsf = T2
    Xf = P.sb([128, 512], F32, 'Xf')
    Xb = dbl([128, 512], BF16, 'Xb')
    g1 = P.sb([128, 64], F32, 'g1')
    g2 = P.sb([128, 64], F32, 'g2')
    so = dbl([128, 64], F32, 'so')
    ssq = dbl([128, 1], F32, 'ssq')
    sskv = dbl([128, 1], F32, 'sskv')
    cqn = dbl([128, 256], BF16, 'cqn')
    ckvn = dbl([128, 128], BF16, 'ckvn')
    cqnT = dbl([128, 2, 128], BF16, 'cqnT')
    ckvnT = dbl([128, 128], BF16, 'ckvnT')
    QTNs = dbl([128, 128], BF16, 'QTNs')
    qrA = P.sb([128, 64], F32, 'qrA')
    qrB = P.sb([128, 64], F32, 'qrB')
    qrb = dbl([128, 64], BF16, 'qrb')
    QrTs = dbl([64, 128], BF16, 'QrTs')
    mrow = dbl([128, 1], F32, 'mrow')
    acc4 = dbl([128, 4], F32, 'acc4')
    rsum = dbl([128, 1], F32, 'rsum')
    PT = dbl([128, 512], BF16, 'PT')
    ao = dbl([128, 128], F32, 'ao')
    SM_SCALE = 192.0 ** -0.5
    ts_i = [0]

    def ts_slot():
        s = TS[ts_i[0] % 2]
        ts_i[0] += 1
        return s

    tb_i = [0]
    ev_i = [0]

    def evac_eng():
        ev_i[0] += 1
        return 'act' if ev_i[0] % 2 == 0 else 'dve'

    PVO = P.view(banks[5][:, 0:128], 'PVO', parent=banks[5])
    TBF = [P.view(banks[7][:, :].bitcast(BF16), 'TBF0', parent=banks[7]), P.view(banks[6][:, :].bitcast(BF16), 'TBF1', parent=banks[6])]
    PT8 = dbl([128, 1024], BF16, 'PT8')

    def frontend(t):
        p = t % 2
        P.dma('sp', xt[p][:], xrow(t), r=([xdep] if xdep is not None else []), w=[xt[p]])
        P.op('dve', lambda e: e.memset(ssx[p][:], 0.0), w=[ssx[p]])
        P.act(junk, junk[:], xt[p], xt[p][:], AF.Square, extra_w=[ssx[p]], accum_out=ssx[p][:])
        yield
        P.rstd(ssx[p], D, None)
        P.ts('dve', xnb[p], xnb[p][:], xt[p], xt[p][:], ssx[p][:, 0:1], ALU.mult, extra_r=[ssx[p]])
        yield
        for half in range(2):
            tb = TB[0]
            for c4 in range(4):
                c = half * 4 + c4
                P.tr(tb, tb[:, c4 * 128:(c4 + 1) * 128], xnb[p], xnb[p][:, c * 128:(c + 1) * 128], ident)
            for c4 in range(4):
                c = half * 4 + c4
                if c4 % 2 == 0:
                    P.act(hT[p], hT[p][:, c, :], tb, tb[:, c4 * 128:(c4 + 1) * 128], AF.Identity,
                          extra_r=[scale1, shm], scale=scale1[:, c:c + 1], bias=shm[:, c:c + 1])
                else:
                    P.ts('dve', hT[p], hT[p][:, c, :], tb, tb[:, c4 * 128:(c4 + 1) * 128],
                         scale1[:, c:c + 1], ALU.mult, shm[:, c:c + 1], ALU.add, extra_r=[scale1, shm])
            yield

    def projheads(t):
        projection(t)
        yield
        for _ in heads(t):
            yield

    def projection(t):
        p = t % 2
        for kc in range(8):
            P.mm(PJ0, PJ0[:, :], hT[p], hT[p][:, kc, :], wselb, wselb[:, kc, 0:512], start=(kc == 0), stop=(kc == 7))
        for kc in range(8):
            P.mm(PJ1, PJ1[:, :], hT[p], hT[p][:, kc, :], wselb, wselb[:, kc, 512:768], start=(kc == 0), stop=(kc == 7))

    def heads(t):
        p = t % 2
        cos_t = COST[:, t * 32:(t + 1) * 32]
        sin_t = SINT[:, t * 32:(t + 1) * 32]
        nsin_t = NSINT[:, t * 32:(t + 1) * 32]
        src4 = PJ0[:, 0:192].rearrange("p (a h f) -> p a h f", a=3, h=2)
        A4 = ropeA[:].rearrange("p (a h f) -> p a h f", a=3, h=2)
        B4 = ropeB[:].rearrange("p (a h f) -> p a h f", a=3, h=2)
        P.tt('dve', ropeA, A4, PJ0, src4, COST, cos_t.unsqueeze(1).unsqueeze(1).broadcast_to([128, 3, 2, 32]), ALU.mult)
        P.tt('dve', ropeB, B4[:, :, 0, :], PJ0, src4[:, :, 1, :], NSINT, nsin_t.unsqueeze(1).broadcast_to([128, 3, 32]), ALU.mult)
        P.tt('dve', ropeB, B4[:, :, 1, :], PJ0, src4[:, :, 0, :], SINT, sin_t.unsqueeze(1).broadcast_to([128, 3, 32]), ALU.mult)
        yield
        P.cp('act', vb[p], vb[p][:], PJ0, PJ0[:, 192:256])
        P.ts('dve', vdec[p], vdec[p][:], PJ0, PJ0[:, 192:256], kdec[:, 0:1], ALU.mult, extra_r=[kdec])
        P.act(gs[p], gs[p][:], PJ0, PJ0[:, 256:320], AF.Silu)
        P.cp('act', ub[p], ub[p][:], PJ0, PJ0[:, 320:384])
        P.op('dve', lambda e: e.memset(ssq[p][:], 0.0), w=[ssq[p]])
        P.op('dve', lambda e: e.memset(sskv[p][:], 0.0), w=[sskv[p]])
        P.act(junk64, junk64[:, 0:256], PJ1, PJ1[:, :], AF.Square, extra_w=[ssq[p]], accum_out=ssq[p][:])
        P.act(junk64, junk64[:, 0:128], PJ0, PJ0[:, 384:512], AF.Square, extra_w=[sskv[p]], accum_out=sskv[p][:])
        yield
        P.rstd(ssq[p], 256, None)
        yield
        P.rstd(sskv[p], 128, None)
        yield
        P.ts('dve', cqn[p], cqn[p][:], PJ1, PJ1[:, :], ssq[p][:, 0:1], ALU.mult, extra_r=[ssq[p]])
        P.ts('dve', ckvn[p], ckvn[p][:], PJ0, PJ0[:, 384:512], sskv[p][:, 0:1], ALU.mult, extra_r=[sskv[p]])
        P.tt('dve', rqk[p], rqk[p][:], ropeA, ropeA[:], ropeB, ropeB[:], ALU.add)
        yield

    def ret_chain(t):
        p = t % 2
        tok = slice(t * 128, (t + 1) * 128)
        s0 = ts_slot()
        P.tr(s0, s0[0:64, :], rqk[p], rqk[p][:, 0:64], ident)
        P.cp('act', QTs[p], QTs[p][:], s0, s0[0:64, :])
        yield
        P.tt('dve', QTd[p], QTd[p][:], QTs[p], QTs[p][:], qdec, qdec[:], ALU.mult)
        s1 = ts_slot()
        P.tr(s1, s1[0:64, :], rqk[p], rqk[p][:, 64:128], ident)
        P.cp('act', KTs[p], KTs[p][:], s1, s1[0:64, :])
        yield
        P.tt('dve', gs[p], gs[p][:], gs[p], gs[p][:], retg_t, retg_t[:], ALU.mult)
        P.mm(RS, RS[:, :], KTs[p], KTs[p][:], QTs[p], QTs[p][:])
        yield
        P.tt('dve', smask[p], smask[p][:], RS, RS[:, :], dmaskT, dmaskT[:], ALU.mult)
        yield
        P.mm(RO, RO[:, :], smask[p], smask[p][:], vb[p], vb[p][:], start=True, stop=False)
        P.mm(RO, RO[:, :], QTd[p], QTd[p][:], state_bf, state_bf[:], start=False, stop=True)
        P.mm(RKV, RKV[:, :], rqk[p], rqk[p][:, 64:128], vdec[p], vdec[p][:])
        yield
        P.op('dve', lambda e: e.scalar_tensor_tensor(out=state[:], in0=state[:], scalar=dcy[:, 0:1], in1=RKV[:, :],
                                                     op0=ALU.mult, op1=ALU.add), r=[state, dcy, RKV], w=[state])
        P.cp('dve', state_bf, state_bf[:], state, state[:])
        P.op('dve', lambda e: e.memset(ssr[p][:], 0.0), w=[ssr[p]])
        P.act(junk64, junk64[:, 0:64], RO, RO[:, :], AF.Square, extra_w=[ssr[p]], accum_out=ssr[p][:])
        yield
        P.rstd(ssr[p], 64, None)
        yield
        P.op('dve', lambda e: e.scalar_tensor_tensor(out=ro[p][:], in0=RO[:, :], scalar=ssr[p][:, 0:1], in1=gs[p][:],
                                                     op0=ALU.mult, op1=ALU.mult), r=[RO, ssr[p], gs[p]], w=[ro[p]])
        out_evs.append(P.dma('sp', hx[tok, 0:64], ro[p][:], r=[ro[p]]))
        yield

    def s5_chain(t):
        p = t % 2
        tok = slice(t * 128, (t + 1) * 128)
        s3 = ts_slot()
        P.tr(s3, s3[0:64, :], ub[p], ub[p][:], ident)
        P.cp('dve', UTs[p], UTs[p][:], s3, s3[0:64, :])
        yield
        P.mm(SA, SA[:, :], UTs[p], UTs[p][:], Bmat, Bmat[:])
        P.mm(SB, SB[:, :], UTs[p], UTs[p][:], Bswp, Bswp[:])
        yield
        P.tt('dve', T1, T1[:], SA, SA[:, :], EA, EA[:], ALU.mult)
        yield
        P.tt('dve', T2, T2[:], SB, SB[:, :], EB, EB[:], ALU.mult)
        yield
        P.tt('dve', Wb[p], Wb[p][:], T1, T1[:], T2, T2[:], ALU.add)
        yield
        for blk in range(4):
            P.mm(SA, SA[:, blk * 128:(blk + 1) * 128], Wb[p], Wb[p][:, blk * 128:(blk + 1) * 128], tri, tri[:])
        for blk in range(4):
            b2 = blk ^ 1
            P.mm(SB, SB[:, blk * 128:(blk + 1) * 128], Wb[p], Wb[p][:, b2 * 128:(b2 + 1) * 128], tri, tri[:])
        yield
        P.tt('dve', Zf, Zf[:].rearrange("p (a i) -> p a i", a=4), SA, SA[:, :].rearrange("p (a i) -> p a i", a=4),
             xprev, xprev[:].unsqueeze(2).broadcast_to([128, 4, 128]), ALU.add)
        yield
        P.tt('dve', Zsf, Zsf[:].rearrange("p (a i) -> p a i", a=4), SB, SB[:, :].rearrange("p (a i) -> p a i", a=4),
             xprevs, xprevs[:].unsqueeze(2).broadcast_to([128, 4, 128]), ALU.add)
        yield
        P.tt('dve', Zf, Zf[:], Zf, Zf[:], EA2, EA2[:], ALU.mult)
        yield
        P.tt('dve', Zsf, Zsf[:], Zsf, Zsf[:], EB2, EB2[:], ALU.mult)
        yield
        P.tt('dve', Xf, Xf[:], Zf, Zf[:], Zsf, Zsf[:], ALU.add)
        yield
        P.cp('act', Xb[p], Xb[p][:], Xf, Xf[:])
        X3 = Xf[:].rearrange("p (a i) -> p a i", a=4)
        P.cp('dve', xprev, xprev[:], Xf, X3[:, :, 127])
        xp3 = xprev[:].rearrange("p (a r) -> p a r", r=2)
        xs3 = xprevs[:].rearrange("p (a r) -> p a r", r=2)
        P.cp('dve', xprevs, xs3[:, :, 0], xprev, xp3[:, :, 1])
        P.cp('dve', xprevs, xs3[:, :, 1], xprev, xp3[:, :, 0])
        yield
        for blk in range(4):
            P.mm(SY, SY[:, :], Xb[p], Xb[p][:, blk * 128:(blk + 1) * 128], Cmat, Cmat[:, blk, :], start=(blk == 0), stop=False)
        P.mm(SY, SY[:, :], UTs[p], UTs[p][:], DDb, DDb[:], start=False, stop=True)
        yield
        P.act(g1, g1[:], SY, SY[:, :], AF.Square)
        yield
        P.ts('dve', g1, g1[:], g1, g1[:], 0.044715, ALU.mult, 1.0, ALU.add)
        P.tt('dve', g1, g1[:], g1, g1[:], SY, SY[:, :], ALU.mult)
        yield
        P.act(g2, g2[:], g1, g1[:], AF.Sigmoid, scale=1.5957691216057308)
        yield
        P.tt('dve', so[p], so[p][:], g2, g2[:], SY, SY[:, :], ALU.mult)
        out_evs.append(P.dma('sp', hx[tok, 64:128], so[p][:], r=[so[p]]))
        yield

    def mla_chain(t):
        p = t % 2
        tok = slice(t * 128, (t + 1) * 128)
        cos_t = COST[:, t * 32:(t + 1) * 32]
        sin_t = SINT[:, t * 32:(t + 1) * 32]
        nsin_t = NSINT[:, t * 32:(t + 1) * 32]
        s2 = ts_slot()
        P.tr(s2, s2[0:64, :], rqk[p], rqk[p][:, 128:192], ident)
        P.cp('act', KTR[t], KTR[t][:], s2, s2[0:64, :])
        yield
        for kc in range(2):
            s = ts_slot()
            P.tr(s, s[:, :], cqn[p], cqn[p][:, kc * 128:(kc + 1) * 128], ident)
            P.cp(evac_eng(), cqnT[p], cqnT[p][:, kc, :], s, s[:, :])
            yield
        s = ts_slot()
        P.tr(s, s[:, :], ckvn[p], ckvn[p][:], ident)
        P.cp(evac_eng(), ckvnT[p], ckvnT[p][:], s, s[:, :])
        yield
        for kc in range(2):
            P.mm(QTN, QTN[:, :], wuqb, wuqb[:, kc, 0:128], cqnT[p], cqnT[p][:, kc, :], start=(kc == 0), stop=(kc == 1))
        for kc in range(2):
            P.mm(QRP, QRP[:, :], cqnT[p], cqnT[p][:, kc, :], wuqb, wuqb[:, kc, 128:192], start=(kc == 0), stop=(kc == 1))
        P.mm(KTN, KTN[:, :], wukvb, wukvb[:, 0:128], ckvnT[p], ckvnT[p][:])
        P.mm(VP, VP[:, :], ckvnT[p], ckvnT[p][:], wukvb, wukvb[:, 128:256])
        yield
        P.act(QTNs[p], QTNs[p][:], QTN, QTN[:, :], AF.Copy, scale=SM_SCALE)
        q3 = QRP[:, :].rearrange("p (h f) -> p h f", h=2)
        a3 = qrA[:].rearrange("p (h f) -> p h f", h=2)
        b3 = qrB[:].rearrange("p (h f) -> p h f", h=2)
        P.tt('dve', qrA, a3, QRP, q3, COST, cos_t.unsqueeze(1).broadcast_to([128, 2, 32]), ALU.mult)
        P.tt('dve', qrB, b3[:, 0, :], QRP, q3[:, 1, :], NSINT, nsin_t, ALU.mult)
        P.tt('dve', qrB, b3[:, 1, :], QRP, q3[:, 0, :], SINT, sin_t, ALU.mult)
        P.cp('act', KTNP[t], KTNP[t][:], KTN, KTN[:, :])
        P.cp('act', VV[t], VV[t][:], VP, VP[:, :])
        yield
        P.tt('dve', qrb[p], qrb[p][:], qrA, qrA[:], qrB, qrB[:], ALU.add)
        yield
        s = ts_slot()
        P.tr(s, s[0:64, :], qrb[p], qrb[p][:], ident)
        P.act(QrTs[p], QrTs[p][:], s, s[0:64, :], AF.Copy, scale=SM_SCALE)
        yield
        Lk = (t + 1) * 128
        nkb = (Lk + 511) // 512
        for kb in range(nkb):
            n = min(512, Lk - kb * 512)
            sc = SC[kb % 2]
            kts = [KTNP[4 * kb + i_] for i_ in range(n // 128)]
            krs = [KTR[4 * kb + i_] for i_ in range(n // 128)]
            P.op('pe', lambda e: e.matmul(sc[:, 0:n], lhsT=QTNs[p][:], rhs=KTNP_t[:, kb * 512:kb * 512 + n],
                                          start=True, stop=False), r=[QTNs[p]] + kts, w=[sc])
            P.op('pe', lambda e: e.matmul(sc[:, 0:n], lhsT=QrTs[p][:], rhs=KTR_t[:, kb * 512:kb * 512 + n],
                                          start=False, stop=True), r=[QrTs[p]] + krs, w=[sc])
            yield
            srow = SROW[kb % 2]
            if kb == nkb - 1:
                if n > 128:
                    P.cp('act', srow, SROW_t[:, kb * 512:kb * 512 + n - 128], sc, sc[:, 0:n - 128])
                P.tt('dve', srow, SROW_t[:, Lk - 128:Lk], sc, sc[:, n - 128:n], cmask, cmask[:], ALU.add)
            else:
                P.cp(evac_eng(), srow, SROW_t[:, kb * 512:kb * 512 + n], sc, sc[:, 0:n])
            yield
        P.op('dve', lambda e: e.reduce_max(out=mrow[p][:], in_=SROW_t[:, 0:Lk], axis=AX.X), r=[SROW[0], SROW[1]], w=[mrow[p]])
        P.op('dve', lambda e: e.memset(acc4[p][:], 0.0), w=[acc4[p]])
        yield
        P.ts('dve', mrow[p], mrow[p][:], mrow[p], mrow[p][:], -1.0, ALU.mult)
        yield
        nch = (Lk + 2047) // 2048
        for ci in range(nch):
            c0 = ci * 2048
            c1 = min(Lk, c0 + 2048)
            P.op('act', lambda e: e.activation(out=PB[:, c0:c1], in_=SROW_t[:, c0:c1], func=AF.Exp,
                                               bias=mrow[p][:, 0:1], scale=1.0, accum_out=acc4[p][:, ci:ci + 1]),
                 r=[SROW[0], SROW[1], mrow[p]], w=[PB, acc4[p]])
            yield
        P.op('dve', lambda e: e.reduce_sum(out=rsum[p][:], in_=acc4[p][:, 0:nch], axis=AX.X), r=[acc4[p]], w=[rsum[p]])
        yield
        P.op('dve', lambda e: e.reciprocal(out=rsum[p][:], in_=rsum[p][:]), r=[rsum[p]], w=[rsum[p]])
        ng = (t + 1 + 7) // 8

        def tr_round(g):
            tbf = TBF[g % 2]
            pt = PT8[g % 2]
            blks = list(range(g * 8, min(t + 1, g * 8 + 8)))
            for blk in blks:
                P.tr(tbf, tbf[:, (blk % 8) * 128:(blk % 8 + 1) * 128], PB, PB[:, blk * 128:(blk + 1) * 128], ident)
            w_ = len(blks) * 128
            P.cp(evac_eng(), pt, pt[:, 0:w_], tbf, tbf[:, 0:w_])
            return blks, pt

        cur = tr_round(0)
        yield
        for g in range(ng):
            nxt = tr_round(g + 1) if g + 1 < ng else None
            blks, pt = cur
            for blk in blks:
                P.mm(PVO, PVO[:, :], pt, pt[:, (blk % 8) * 128:(blk % 8 + 1) * 128], VV[blk], VV[blk][:],
                     start=(blk == 0), stop=(blk == t))
            cur = nxt
            yield
        P.ts('dve', ao[p], ao[p][:], PVO, PVO[:, :], rsum[p][:, 0:1], ALU.mult, extra_r=[rsum[p]])
        out_evs.append(P.dma('sp', hx[tok, 128:256], ao[p][:], r=[ao[p]]))
        yield

    def drain(g):
        for _ in g:
            pass

    def interleave(gens):
        gens = [[g, 2 if i == 0 else 1] for i, g in enumerate(gens)]
        while gens:
            alive = []
            for g, n_ in gens:
                ok = True
                for _ in range(n_):
                    try:
                        next(g)
                    except StopIteration:
                        ok = False
                        break
                if ok:
                    alive.append([g, n_])
            gens = alive

    def step(g, n_=1):
        for _ in range(n_):
            try:
                next(g)
            except StopIteration:
                return False
        return True

    out_evs = []
    if ntiles > 0:
        drain(frontend(0))
        drain(projheads(0))
    for t in range(ntiles):
        mla = mla_chain(t)
        mla_alive = True
        others = [ret_chain(t), s5_chain(t)]
        if t + 1 < ntiles:
            others.append(frontend(t + 1))
        while others:
            if mla_alive:
                mla_alive = step(mla, 2)
            others = [g for g in others if step(g)]
        nxt = projheads(t + 1) if t + 1 < ntiles else None
        nxt_alive = nxt is not None
        while mla_alive or nxt_alive:
            if mla_alive:
                mla_alive = step(mla, 2)
            if nxt_alive:
                nxt_alive = step(nxt)
        if on_chunk is not None and (t + 1) % 8 == 0:
            on_chunk(t // 8, out_evs)
            out_evs = []
    P.barrier()
    st.close()
    P.es = old_es
    P.pre = {}


RET_GAMMA = [1.0 - 2.0 ** (-5.0 - h) for h in range(4)]


def consts_A(hg):
    g = RET_GAMMA[hg]
    lg = math.log1p(-(2.0 ** (-5.0 - hg)))
    i = np.arange(128, dtype=np.float64)
    diff = i[None, :] - i[:, None]
    dmaskT = np.where(diff >= 0, np.exp(lg * np.maximum(diff, 0.0)), 0.0) * 0.125
    tri = (diff >= 0).astype(np.float32)
    qdec = np.broadcast_to(np.exp(lg * (i + 1.0))[None, :], (64, 128))
    kdec = (np.exp(lg * (127.0 - i)) * 0.125)[:, None]
    dcy = np.full((64, 1), math.exp(lg * 128.0))
    cmask = np.where(i[None, :] <= i[:, None], 0.0, -1e30)
    inv = 10000.0 ** (-np.arange(0, 64, 2, dtype=np.float32) / 64.0)
    f = lambda a: np.ascontiguousarray(a, dtype=np.float32)
    return dict(dmaskT=f(dmaskT), tri=f(tri), qdec=f(qdec), kdec=f(kdec), dcy=f(dcy), cmask=f(cmask),
                jp1=f((i + 1.0)[:, None]), irow1=f(np.broadcast_to((i + 1.0)[None, :], (128, 128))),
                invf=f(inv[None, :]))


def chunkT(v, nch):
    return np.ascontiguousarray(v.reshape(nch, 128).T)


def inputs_A(inp, layer, xcur):
    f = lambda a: np.ascontiguousarray(a, dtype=np.float32)
    maps = []
    w_in = inp['w_in'][layer]
    for core in range(8):
        b, hg = core // 4, core % 4
        m = dict(consts_A(hg))
        m['x'] = f(xcur[b])
        m['cvec'] = f(inp['c'][b].reshape(128, 8))
        m['pos'] = np.ascontiguousarray(inp['positions'][b].reshape(NT, 128).T.astype(np.int32))
        m['adaw'] = f(inp['ada_w'][layer][:, 0:2048].reshape(128, 8, 2048))
        m['adab'] = chunkT(f(inp['ada_b'][layer][0:2048]), 16)
        m['gpre'] = chunkT(f(inp['norm_pre_mix'][layer]), 8)
        h64 = slice(hg * 64, (hg + 1) * 64)
        cols = np.concatenate([
            np.arange(0, 256)[h64], np.arange(256, 512)[h64], np.arange(1664, 1728),
            np.arange(512, 768)[h64], np.arange(768, 1024)[h64], np.arange(1024, 1280)[h64],
            np.arange(1536, 1664), np.arange(1280, 1536)])
        ws = w_in[:, cols]
        m['wsel'] = f(ws.reshape(8, 128, 768).transpose(1, 0, 2))
        m['retg'] = f(inp['ret_norm'][layer][h64][None, :])
        gs_ = slice(4 * hg, 4 * hg + 4)

        def tm(a):
            a = a.reshape(2, 1, 128)
            return f(np.broadcast_to(a, (2, 2, 128)).reshape(1, 512))

        def sm(a):
            return f(a.reshape(2, 128).T)

        are = inp['ssm_a_re'][layer][gs_]
        aim = inp['ssm_a_im'][layer][gs_]
        ldt = np.broadcast_to(inp['ssm_log_dt'][layer][gs_][:, None], (4, 64))
        m['are_tm'], m['aim_tm'], m['ldt_tm'] = tm(are), tm(aim), tm(ldt)
        m['are_sm'], m['aim_sm'], m['ldt_sm'] = sm(are), sm(aim), sm(ldt)
        BR = np.zeros((64, 2, 2, 2, 64), np.float32)
        BI = np.zeros((64, 2, 2, 2, 64), np.float32)
        CR = np.zeros((2, 64, 2, 64), np.float32)
        CI = np.zeros((2, 64, 2, 64), np.float32)
        for gl in range(4):
            gp_, g2_ = gl // 2, gl % 2
            br = inp['ssm_b_re'][layer][4 * hg + gl]
            bi = inp['ssm_b_im'][layer][4 * hg + gl]
            for ri in range(2):
                BR[gl * 16:(gl + 1) * 16, gp_, ri, g2_, :] = br.T
                BI[gl * 16:(gl + 1) * 16, gp_, ri, g2_, :] = bi.T
            cr = inp['ssm_c_re'][layer][4 * hg + gl]
            ci = inp['ssm_c_im'][layer][4 * hg + gl]
            CR[g2_, :, gp_, gl * 16:(gl + 1) * 16] = cr.T
            CI[g2_, :, gp_, gl * 16:(gl + 1) * 16] = ci.T
        m['BR'] = f(BR.reshape(64, 512))
        m['BI'] = f(BI.reshape(64, 512))
        m['CR'] = f(CR.reshape(128, 2, 64))
        m['CI'] = f(CI.reshape(128, 2, 64))
        DD = np.zeros((64, 64), np.float32)
        DD[np.arange(64), np.arange(64)] = inp['ssm_d'][layer][gs_].reshape(64)
        m['DD'] = DD
        m['qng'] = chunkT(f(inp['mla_q_norm'][layer]), 2)
        wq = inp['mla_w_uq'][layer][:, hg * 192:(hg + 1) * 192]
        m['wuq'] = f(wq.reshape(2, 128, 192).transpose(1, 0, 2))
        m['kvng'] = f(inp['mla_kv_norm'][layer][:, None])
        m['wukv'] = f(inp['mla_w_ukv'][layer][:, hg * 256:(hg + 1) * 256])
        maps.append(m)
    return maps


_CACHE = {}


TPC = 2048
GT = 4
NG = TPC // (GT * 128)


def decl_B(nc, sfx, moe):
    n_exp = N_EXP if moe else 1
    io = {}
    io['cvec'] = nc.dram_tensor('cvec' + sfx, [128, 8], F32, kind='ExternalInput').ap()
    io['adaw'] = nc.dram_tensor('adaw' + sfx, [128, 8, 4096], F32, kind='ExternalInput').ap()
    io['adab_row'] = nc.dram_tensor('adab_row' + sfx, [1, 4096], F32, kind='ExternalInput').ap()
    io['adab_col'] = nc.dram_tensor('adab_col' + sfx, [128, 32], F32, kind='ExternalInput').ap()
    io['gpost_m'] = nc.dram_tensor('gpost_m' + sfx, [1, D], F32, kind='ExternalInput').ap()
    io['gpre_f'] = nc.dram_tensor('gpre_f' + sfx, [128, 8], F32, kind='ExternalInput').ap()
    io['gpost_f'] = nc.dram_tensor('gpost_f' + sfx, [1, D], F32, kind='ExternalInput').ap()
    io['gluw_d'] = nc.dram_tensor('gluw' + sfx, [128, 2, 256], F32, kind='ExternalInput').ap()
    io['glub_d'] = nc.dram_tensor('glub' + sfx, [1, 256], F32, kind='ExternalInput').ap()
    io['ssmg_d'] = nc.dram_tensor('ssmg' + sfx, [1, 256], F32, kind='ExternalInput').ap()
    io['mlag_d'] = nc.dram_tensor('mlag' + sfx, [1, 512], F32, kind='ExternalInput').ap()
    io['wout_d'] = nc.dram_tensor('wout' + sfx, [128, 8, D], F32, kind='ExternalInput').ap()
    io['wg_d'] = nc.dram_tensor('wg' + sfx, [n_exp * NFC, 128, 1024], F32, kind='ExternalInput').ap()
    io['wu_d'] = nc.dram_tensor('wu' + sfx, [n_exp * NFC, 128, 1024], F32, kind='ExternalInput').ap()
    io['wd_d'] = nc.dram_tensor('wd' + sfx, [n_exp * NFC, 128, 1024], F32, kind='ExternalInput').ap()
    if moe:
        io['router_d'] = nc.dram_tensor('router' + sfx, [128, 8, 8], F32, kind='ExternalInput').ap()
    return io


def emit_B(P, nc, banks, io, x, hxall, HXALL, mixloc, out, rank256, moe, ngroups=NG, xdep=None, on_group=None):
    n_exp = N_EXP if moe else 1
    cvec = io['cvec']
    adaw = io['adaw']
    adab_row = io['adab_row']
    adab_col = io['adab_col']
    gpost_m = io['gpost_m']
    gpre_f = io['gpre_f']
    gpost_f = io['gpost_f']
    gluw_d = io['gluw_d']
    glub_d = io['glub_d']
    ssmg_d = io['ssmg_d']
    mlag_d = io['mlag_d']
    wout_d = io['wout_d']
    wg_d = io['wg_d']
    wu_d = io['wu_d']
    wd_d = io['wd_d']
    router_d = io.get('router_d')
    st = ExitStack()
    old_es = P.es
    P.es = st
    P.pre = {}
    b0 = banks[0][:, :].bitcast(BF16)
    TB = P.view(b0[:, 0:512], 'TB', parent=banks[0])
    ZP = P.view(banks[1][:, 0:256], 'ZP', parent=banks[1])
    LG = P.view(banks[1][:, 256:264], 'LG', parent=banks[1])
    MC = P.view(banks[1][:, 272:288], 'MC', parent=banks[1])
    YB = [P.view(banks[2][:, :], 'YA', parent=banks[2]), P.view(banks[3][:, :], 'YB', parent=banks[3])]
    GB = [P.view(banks[4][:, :], 'G0', parent=banks[4]), P.view(banks[5][:, :], 'G1', parent=banks[5])]
    UB = [P.view(banks[6][:, :], 'U0', parent=banks[6]), P.view(banks[7][:, :], 'U1', parent=banks[7])]

    ident = P.sb([128, 128], BF16, 'ident', keep=True)
    vecm = P.sb([128, D], F32, 'vecm', keep=True)
    vecf = P.sb([128, D], F32, 'vecf', keep=True)
    modc = P.sb([128, 16], F32, 'modc', keep=True)
    scale2 = P.sb([128, 8], F32, 'scale2', keep=True)
    woutb = P.sb([128, 8, D], BF16, 'woutb', keep=True)
    gluwb = P.sb([128, 2, 256], BF16, 'gluwb', keep=True)
    glub_t = P.sb([128, 256], F32, 'glub_t', keep=True)
    ssmg_t = P.sb([128, 256], F32, 'ssmg_t', keep=True)
    mlag_t = P.sb([128, 512], F32, 'mlag_t', keep=True)
    if moe:
        routerb = P.sb([128, 8, 8], BF16, 'routerb', keep=True)
    X1 = [P.sb([128, D], F32, 'X1_%d' % i, keep=True) for i in range(GT)]
    hT2 = P.sb([128, 8, GT * 128], BF16, 'hT2', keep=True)
    yacc = [P.sb([128, D], F32, 'yacc%d' % i, keep=True) for i in range(GT)]
    gatew = P.sb([128, GT, 8], F32, 'gatew', keep=True)

    tes = ExitStack()
    P.tes = tes
    make_ident(P)
    cv = P.sb([128, 8], F32, 'cv')
    cvb = P.sb([128, 8], BF16, 'cvb')
    cvbb = P.sb([128, 8, 128], BF16, 'cvbb')
    P.dma('sp', cv[:], cvec[:, :], w=[cv])
    P.act(cvb, cvb[:], cv, cv[:], AF.Silu)
    P.cp('dve', cvbb, cvbb[:], cvb, cvb[:].unsqueeze(2).broadcast_to([128, 8, 128]))
    abc = P.sb([128, 32], F32, 'abc')
    P.dma('sp', abc[:], adab_col[:, :], w=[abc])
    wts = [P.sb([128, 8, 1024], BF16, 'adw%d' % i) for i in range(2)]
    stager = make_stager(P)
    rowb = P.sb([128, D], F32, 'rowb')
    for ci in range(4):
        wt = wts[ci % 2]
        for kc in range(8):
            stager(wt, wt[:, kc, :], adaw[:, kc, ci * 1024:(ci + 1) * 1024])
        if ci in (0, 3):
            vec = vecm if ci == 0 else vecf
            gsrc = gpost_m if ci == 0 else gpost_f
            P.dma('sp', rowb[:], adab_row[0:1, ci * 1024:(ci + 1) * 1024].broadcast_to([128, 1024]), w=[rowb])
            for half in range(2):
                yb = YB[half]
                for kc in range(8):
                    P.mm(yb, yb[:, :], cvbb, cvbb[:, kc, :], wt, wt[:, kc, half * 512:(half + 1) * 512],
                         start=(kc == 0), stop=(kc == 7))
                P.tt('dve', vec, vec[:, half * 512:(half + 1) * 512], yb, yb[:, :], rowb, rowb[:, half * 512:(half + 1) * 512], ALU.add)
            P.dma('sp', rowb[:], gsrc[0:1, :].broadcast_to([128, 1024]), w=[rowb])
            P.tt('dve', vec, vec[:], vec, vec[:], rowb, rowb[:], ALU.mult)
        else:
            for cc in range(8):
                ch = (ci - 1) * 8 + cc
                for kc in range(8):
                    P.mm(MC, MC[:, ch:ch + 1], wt, wt[:, kc, cc * 128:(cc + 1) * 128], cvb, cvb[:, kc:kc + 1],
                         start=(kc == 0), stop=(kc == 7))
    P.tt('dve', modc, modc[:], MC, MC[:, 0:16], abc, abc[:, 8:24], ALU.add)
    gpf = P.sb([128, 8], F32, 'gpf')
    P.dma('sp', gpf[:], gpre_f[:, :], w=[gpf])
    P.ts('dve', scale2, scale2[:], modc, modc[:, 8:16], 1.0, ALU.add)
    P.tt('dve', scale2, scale2[:], scale2, scale2[:], gpf, gpf[:], ALU.mult)
    sh2 = P.view(modc[:, 0:8], 'sh2', parent=modc)
    for kc in range(8):
        stager(woutb, woutb[:, kc, :], wout_d[:, kc, :])
    P.dma('pool', gluwb[:], gluw_d[:, :, :], w=[gluwb])
    P.dma('sp', glub_t[:], glub_d[0:1, :].broadcast_to([128, 256]), w=[glub_t])
    P.dma('sp', ssmg_t[:], ssmg_d[0:1, :].broadcast_to([128, 256]), w=[ssmg_t])
    P.dma('sp', mlag_t[:], mlag_d[0:1, :].broadcast_to([128, 512]), w=[mlag_t])
    if moe:
        P.dma('pool', routerb[:], router_d[:, :, :], w=[routerb])
    P.barrier()
    tes.close()
    P.tes = None

    MIXLOC = P.view(mixloc, 'MIXLOC')
    for kk in range(2):
        src_ = hxall.rearrange("(a b) c -> a (b c)", b=32)[bass.ds(rank256 + kk * 128, 128), :]
        P.dma('sp', mixloc[kk].rearrange("r n c -> (r n) c").rearrange("(a b) c -> a (b c)", b=32), src_, r=[HXALL], w=[MIXLOC])
    ev_i = [0]

    def evac_eng():
        ev_i[0] += 1
        return 'act' if ev_i[0] % 2 == 0 else 'dve'

    for g in range(ngroups):
        ph = ExitStack()
        P.tes = ph
        junk = P.sb([128, D], BF16, 'junk')

        def p1_bufs(i):
            return dict(xt=P.sb([128, D], F32, 'xt%d' % i), mt=P.sb([128, D], F32, 'mt%d' % i),
                        hxt=P.sb([128, 4, 256], F32, 'hxt%d' % i), ysb=P.sb([128, 256], BF16, 'ysb%d' % i),
                        ysT=P.sb([128, 2, 128], BF16, 'ysT%d' % i), zz=P.sb([128, 256], F32, 'zz%d' % i),
                        s2=P.sb([128, 256], F32, 's2%d' % i), catb=P.sb([128, D], BF16, 'catb%d' % i),
                        catT=P.sb([128, 8, 128], BF16, 'catT%d' % i), tmp=P.sb([128, D], F32, 'tmp%d' % i),
                        xn2=P.sb([128, D], BF16, 'xn2%d' % i), ss=P.sb([128, 4], F32, 'ss%d' % i),
                        lg=P.sb([128, 8], F32, 'lg%d' % i), lg2=P.sb([128, 8], F32, 'lg2%d' % i),
                        mk1=P.sb([128, 8], F32, 'mk1%d' % i), mk2=P.sb([128, 8], F32, 'mk2%d' % i),
                        m12=P.sb([128, 4], F32, 'm12%d' % i))

        p1sets = [p1_bufs(0), p1_bufs(1)]

        def p1_tile(ti, B_, YK):
            xt, mt, hxt, ysb, ysT, zz, s2 = B_['xt'], B_['mt'], B_['hxt'], B_['ysb'], B_['ysT'], B_['zz'], B_['s2']
            catb, catT, tmp, xn2, ss = B_['catb'], B_['catT'], B_['tmp'], B_['xn2'], B_['ss']
            lg, lg2, mk1, mk2, m12 = B_['lg'], B_['lg2'], B_['mk1'], B_['mk2'], B_['m12']
            tok = slice((g * GT + ti) * 128, (g * GT + ti + 1) * 128)
            P.dma('sp', xt[:], x[tok, :], r=([xdep] if xdep is not None else []), w=[xt])
            row0 = (g * GT + ti) * 128
            P.dma('sp', hxt[:], mixloc[row0 // 1024, :, row0 % 1024:row0 % 1024 + 128, :].rearrange("r n c -> n r c"), r=[MIXLOC], w=[hxt])
            for (c0, w_, d0) in ((0, 64, 0), (64, 64, 256), (128, 128, 512)):
                P.cp('pool', mt, mt[:, d0:d0 + 4 * w_].rearrange("p (r c) -> p r c", r=4), hxt, hxt[:, :, c0:c0 + w_])
            yield
            P.cp('dve', ysb, ysb[:], mt, mt[:, 256:512])
            yield
            for kc in range(2):
                P.tr(TB, TB[:, kc * 128:(kc + 1) * 128], ysb, ysb[:, kc * 128:(kc + 1) * 128], ident)
            P.cp('act', ysT, ysT[:].rearrange("p a b -> p (a b)"), TB, TB[:, 0:256])
            yield
            for kc in range(2):
                P.mm(ZP, ZP[:, :], ysT, ysT[:, kc, :], gluwb, gluwb[:, kc, :], start=(kc == 0), stop=(kc == 1))
            P.tt('dve', zz, zz[:], ZP, ZP[:, :], glub_t, glub_t[:], ALU.add)
            yield
            P.act(zz, zz[:], zz, zz[:], AF.Sigmoid)
            P.op('pool', lambda e: e.memset(ss[:], 0.0), w=[ss])
            yield
            P.tt('dve', s2, s2[:], zz, zz[:], mt, mt[:, 256:512], ALU.mult)
            P.act(junk, junk[:, 0:512], mt, mt[:, 512:1024], AF.Square, extra_w=[ss], accum_out=ss[:, 1:2])
            yield
            P.act(junk, junk[:, 0:256], s2, s2[:], AF.Square, extra_w=[ss], accum_out=ss[:, 0:1])
            P.cp('act', catb, catb[:, 0:256], mt, mt[:, 0:256])
            yield
            P.ts('dve', ss, ss[:, 0:1], ss, ss[:, 0:1], 1.0 / 256, ALU.mult, EPS, ALU.add)
            P.ts('dve', ss, ss[:, 1:2], ss, ss[:, 1:2], 1.0 / 512, ALU.mult, EPS, ALU.add)
            yield
            P.act(ss, ss[:, 0:2], ss, ss[:, 0:2], AF.Sqrt)
            yield
            P.op('dve', lambda e: e.reciprocal(out=ss[:, 0:2], in_=ss[:, 0:2]), r=[ss], w=[ss])
            yield
            P.op('dve', lambda e: e.scalar_tensor_tensor(out=catb[:, 256:512], in0=s2[:], scalar=ss[:, 0:1], in1=ssmg_t[:],
                                                         op0=ALU.mult, op1=ALU.mult), r=[s2, ss, ssmg_t], w=[catb])
            P.op('dve', lambda e: e.scalar_tensor_tensor(out=catb[:, 512:1024], in0=mt[:, 512:1024], scalar=ss[:, 1:2], in1=mlag_t[:],
                                                         op0=ALU.mult, op1=ALU.mult), r=[mt, ss, mlag_t], w=[catb])
            yield
            for half in range(2):
                for c4 in range(4):
                    c = half * 4 + c4
                    P.tr(TB, TB[:, c4 * 128:(c4 + 1) * 128], catb, catb[:, c * 128:(c + 1) * 128], ident)
                P.cp(evac_eng(), catT, catT[:, half * 4:(half + 1) * 4, :].rearrange("p a b -> p (a b)"), TB, TB[:, :])
                yield
            for half in range(2):
                for kc in range(8):
                    P.mm(YK[half], YK[half][:, :], catT, catT[:, kc, :], woutb, woutb[:, kc, half * 512:(half + 1) * 512],
                         start=(kc == 0), stop=(kc == 7))
            P.op('pool', lambda e: e.memset(ss[:, 2:4], 0.0), w=[ss])
            yield
            for half in range(2):
                P.act(junk, junk[:, 0:512], YK[half], YK[half][:, :], AF.Square, extra_w=[ss], accum_out=ss[:, 2 + half:3 + half])
            yield
            P.tt('dve', ss, ss[:, 2:3], ss, ss[:, 2:3], ss, ss[:, 3:4], ALU.add)
            P.ts('dve', ss, ss[:, 2:3], ss, ss[:, 2:3], 1.0 / D, ALU.mult, EPS, ALU.add)
            yield
            P.act(ss, ss[:, 2:3], ss, ss[:, 2:3], AF.Sqrt)
            yield
            P.op('dve', lambda e: e.reciprocal(out=ss[:, 2:3], in_=ss[:, 2:3]), r=[ss], w=[ss])
            yield
            for half in range(2):
                hs = slice(half * 512, (half + 1) * 512)
                P.op('dve', lambda e: e.scalar_tensor_tensor(out=tmp[:, hs], in0=YK[half][:, :], scalar=ss[:, 2:3], in1=vecm[:, hs],
                                                             op0=ALU.mult, op1=ALU.mult), r=[YK[half], ss, vecm], w=[tmp])
            yield
            P.tt('dve', X1[ti], X1[ti][:], tmp, tmp[:], xt, xt[:], ALU.add)
            P.op('pool', lambda e: e.memset(ss[:, 3:4], 0.0), w=[ss])
            yield
            P.act(junk, junk[:], X1[ti], X1[ti][:], AF.Square, extra_w=[ss], accum_out=ss[:, 3:4])
            yield
            P.ts('dve', ss, ss[:, 3:4], ss, ss[:, 3:4], 1.0 / D, ALU.mult, EPS, ALU.add)
            yield
            P.act(ss, ss[:, 3:4], ss, ss[:, 3:4], AF.Sqrt)
            yield
            P.op('dve', lambda e: e.reciprocal(out=ss[:, 3:4], in_=ss[:, 3:4]), r=[ss], w=[ss])
            yield
            P.ts('dve', xn2, xn2[:], X1[ti], X1[ti][:], ss[:, 3:4], ALU.mult, extra_r=[ss])
            yield
            for half in range(2):
                for c4 in range(4):
                    c = half * 4 + c4
                    P.tr(TB, TB[:, c4 * 128:(c4 + 1) * 128], xn2, xn2[:, c * 128:(c + 1) * 128], ident)
                for c4 in range(4):
                    c = half * 4 + c4
                    if c4 % 2 == 0:
                        P.act(hT2, hT2[:, c, ti * 128:(ti + 1) * 128], TB, TB[:, c4 * 128:(c4 + 1) * 128], AF.Identity,
                              extra_r=[scale2, sh2], scale=scale2[:, c:c + 1], bias=sh2[:, c:c + 1])
                    else:
                        P.ts('dve', hT2, hT2[:, c, ti * 128:(ti + 1) * 128], TB, TB[:, c4 * 128:(c4 + 1) * 128],
                             scale2[:, c:c + 1], ALU.mult, sh2[:, c:c + 1], ALU.add, extra_r=[scale2, sh2])
                yield
            if moe:
                for kc in range(8):
                    P.mm(LG, LG[:, :], hT2, hT2[:, kc, ti * 128:(ti + 1) * 128], routerb, routerb[:, kc, :],
                         start=(kc == 0), stop=(kc == 7))
                P.cp('dve', lg, lg[:], LG, LG[:, :])
                yield
                P.op('dve', lambda e: e.reduce_max(out=m12[:, 0:1], in_=lg[:], axis=AX.X), r=[lg], w=[m12])
                yield
                P.ts('dve', mk1, mk1[:], lg, lg[:], m12[:, 0:1], ALU.is_equal, extra_r=[m12])
                yield
                P.op('dve', lambda e: e.scalar_tensor_tensor(out=lg2[:], in0=mk1[:], scalar=-1e30, in1=lg[:],
                                                             op0=ALU.mult, op1=ALU.add), r=[mk1, lg], w=[lg2])
                yield
                P.op('dve', lambda e: e.reduce_max(out=m12[:, 1:2], in_=lg2[:], axis=AX.X), r=[lg2], w=[m12])
                yield
                P.ts('dve', mk2, mk2[:], lg2, lg2[:], m12[:, 1:2], ALU.is_equal, extra_r=[m12])
                P.tt('dve', m12, m12[:, 2:3], m12, m12[:, 1:2], m12, m12[:, 0:1], ALU.subtract)
                yield
                P.act(m12, m12[:, 2:3], m12, m12[:, 2:3], AF.Exp)
                yield
                P.ts('dve', m12, m12[:, 3:4], m12, m12[:, 2:3], 1.0, ALU.add)
                yield
                P.op('dve', lambda e: e.reciprocal(out=m12[:, 3:4], in_=m12[:, 3:4]), r=[m12], w=[m12])
                yield
                P.tt('dve', m12, m12[:, 2:3], m12, m12[:, 2:3], m12, m12[:, 3:4], ALU.mult)
                P.ts('dve', mk1, mk1[:], mk1, mk1[:], m12[:, 3:4], ALU.mult, extra_r=[m12])
                yield
                P.op('dve', lambda e: e.scalar_tensor_tensor(out=gatew[:, ti, :], in0=mk2[:], scalar=m12[:, 2:3], in1=mk1[:],
                                                             op0=ALU.mult, op1=ALU.add), r=[mk2, m12, mk1], w=[gatew])
                yield

        for pair in range(GT // 2):
            gens = [p1_tile(2 * pair, p1sets[0], YB), p1_tile(2 * pair + 1, p1sets[1], GB)]
            while gens:
                alive = []
                for gen_ in gens:
                    try:
                        next(gen_)
                        alive.append(gen_)
                    except StopIteration:
                        pass
                gens = alive
        P.barrier()
        ph.close()
        ph = ExitStack()
        P.tes = ph
        actT = P.sb([128, NFC, GT * 128], BF16, 'actT')
        wdb = P.sb([128, NFC, D], BF16, 'wdb')
        stg = [P.sb([128, 1024], F32, 'stg%d' % i) for i in range(4)]
        wgb = [P.sb([128, 8, 128], BF16, 'wgb%d' % i) for i in range(2)]
        wub = [P.sb([128, 8, 128], BF16, 'wub%d' % i) for i in range(2)]
        gsil = [P.sb([128, GT * 128], F32, 'gsil%d' % i) for i in range(2)]
        ftmp = P.sb([128, D], F32, 'ftmp')
        fjunk = P.sb([128, D], BF16, 'fjunk')
        fss = P.sb([128, 1], F32, 'fss')
        st_i = [0]

        def load_cast(dst, dst_ap, src_ap, eng):
            sg = stg[st_i[0] % 4]
            st_i[0] += 1
            P.dma('sp', sg[:], src_ap, w=[sg])
            P.cp(eng, dst, dst_ap, sg, sg[:])

        for e_ in range(n_exp):
            for fc in range(NFC):
                b2 = fc % 2
                load_cast(wgb[b2], wgb[b2][:].rearrange("p a b -> p (a b)"), wg_d[e_ * NFC + fc, :, :], 'dve')
                load_cast(wub[b2], wub[b2][:].rearrange("p a b -> p (a b)"), wu_d[e_ * NFC + fc, :, :], 'act')
                load_cast(wdb, wdb[:, fc, :], wd_d[e_ * NFC + fc, :, :], 'pool')
                for kc in range(8):
                    P.mm(GB[b2], GB[b2][:, :], wgb[b2], wgb[b2][:, kc, :], hT2, hT2[:, kc, :], start=(kc == 0), stop=(kc == 7))
                for kc in range(8):
                    P.mm(UB[b2], UB[b2][:, :], wub[b2], wub[b2][:, kc, :], hT2, hT2[:, kc, :], start=(kc == 0), stop=(kc == 7))
                P.act(gsil[b2], gsil[b2][:], GB[b2], GB[b2][:, :], AF.Silu)
                P.tt('dve', actT, actT[:, fc, :], UB[b2], UB[b2][:, :], gsil[b2], gsil[b2][:], ALU.mult)
            for ti in range(GT):
                for half in range(2):
                    hs = slice(half * 512, (half + 1) * 512)
                    yb = YB[(ti * 2 + half) % 2]
                    for fc in range(NFC):
                        P.mm(yb, yb[:, :], actT, actT[:, fc, ti * 128:(ti + 1) * 128], wdb, wdb[:, fc, hs],
                             start=(fc == 0), stop=(fc == NFC - 1))
                    if not moe:
                        P.cp(evac_eng(), yacc[ti], yacc[ti][:, hs], yb, yb[:, :])
                    elif e_ == 0:
                        P.ts('dve', yacc[ti], yacc[ti][:, hs], yb, yb[:, :], gatew[:, ti, 0:1], ALU.mult, extra_r=[gatew])
                    else:
                        P.op('dve', lambda e: e.scalar_tensor_tensor(out=yacc[ti][:, hs], in0=yb[:, :], scalar=gatew[:, ti, e_:e_ + 1],
                                                                     in1=yacc[ti][:, hs], op0=ALU.mult, op1=ALU.add),
                             r=[yb, gatew, yacc[ti]], w=[yacc[ti]])
        for ti in range(GT):
            tok = slice((g * GT + ti) * 128, (g * GT + ti + 1) * 128)
            P.op('pool', lambda e: e.memset(fss[:], 0.0), w=[fss])
            P.act(fjunk, fjunk[:], yacc[ti], yacc[ti][:], AF.Square, extra_w=[fss], accum_out=fss[:, 0:1])
            P.rstd(fss, D, None)
            P.op('dve', lambda e: e.scalar_tensor_tensor(out=ftmp[:], in0=yacc[ti][:], scalar=fss[:, 0:1], in1=vecf[:],
                                                         op0=ALU.mult, op1=ALU.mult), r=[yacc[ti], fss, vecf], w=[ftmp])
            P.tt('dve', ftmp, ftmp[:], ftmp, ftmp[:], X1[ti], X1[ti][:], ALU.add)
            P.dma('sp', out[tok, :], ftmp[:], r=[ftmp])
        P.barrier()
        ph.close()
        P.tes = None
        if on_group is not None:
            on_group(g)
    st.close()
    P.es = old_es
    P.pre = {}


def inputs_B(inp, layer, xcur, mix):
    f = lambda a: np.ascontiguousarray(a, dtype=np.float32)
    moe = (layer % 2 == 1)
    j = layer // 2
    xf = xcur.reshape(-1, D)
    mf = mix.reshape(-1, D)
    aw = f(inp['ada_w'][layer][:, 2048:6144].reshape(128, 8, 4096))
    ab = inp['ada_b'][layer]
    shared = dict(
        adaw=aw, adab_row=f(ab[2048:6144][None, :]), adab_col=chunkT(f(ab[2048:6144]), 32),
        gpost_m=f(inp['norm_post_mix'][layer][None, :]), gpre_f=chunkT(f(inp['norm_pre_ffn'][layer]), 8),
        gpost_f=f(inp['norm_post_ffn'][layer][None, :]),
        gluw=f(inp['ssm_glu_w'][layer].reshape(2, 128, 256).transpose(1, 0, 2)),
        glub=f(inp['ssm_glu_b'][layer][None, :]), ssmg=f(inp['ssm_norm'][layer][None, :]),
        mlag=f(inp['mla_norm'][layer][None, :]),
        wout=f(inp['w_out'][layer].reshape(8, 128, D).transpose(1, 0, 2)))
    if moe:
        wg, wu, wd = inp['moe_w_gate'][j], inp['moe_w_up'][j], inp['moe_w_down'][j]
        shared['router'] = f(inp['moe_router'][j].reshape(8, 128, 8).transpose(1, 0, 2))
    else:
        wg, wu, wd = inp['ffn_w_gate'][j][None], inp['ffn_w_up'][j][None], inp['ffn_w_down'][j][None]
    ne = wg.shape[0]

    def gu(w):
        return f(w.reshape(ne, 8, 128, NFC, 128).transpose(0, 3, 2, 1, 4).reshape(ne * NFC, 128, 1024))

    shared['wg'] = gu(wg)
    shared['wu'] = gu(wu)
    shared['wd'] = f(wd.reshape(ne * NFC, 128, D))
    maps = []
    for core in range(8):
        b = core // 4
        m = dict(shared)
        m['x'] = f(xf[core * TPC:(core + 1) * TPC])
        m['mix'] = f(mf[core * TPC:(core + 1) * TPC])
        m['cvec'] = f(inp['c'][b].reshape(128, 8))
        maps.append(m)
    return maps


RG4 = [[0, 1, 2, 3], [4, 5, 6, 7]]


def build_fused(ntiles=NT, ngroups=NG):
    nc = bass.Bass("TRN2", target_bir_lowering=False)
    ioA = [decl_A(nc, '_a%d' % l) for l in range(2)]
    ioB = [decl_B(nc, '_b%d' % l, moe=(l == 1)) for l in range(2)]
    x = nc.dram_tensor("x", [L, D], F32, kind="ExternalInput").ap()
    xsl = nc.dram_tensor("xsl", [TPC, D], F32, kind="ExternalInput").ap()
    out = nc.dram_tensor("out", [TPC, D], F32, kind="ExternalOutput").ap()
    hx = [nc.dram_tensor("hx%d" % l, [L, 256], F32).ap() for l in range(2)]
    hxall = [nc.dram_tensor("hxall%d" % l, [8 * 4 * 1024, 256], F32).ap() for l in range(2)]
    mixloc = [nc.dram_tensor("mixloc%d" % l, [2, 4, 1024, 256], F32).ap() for l in range(2)]
    xs = nc.dram_tensor("xs", [TPC, D], F32).ap()
    xall = nc.dram_tensor("xall", [8 * 4 * 256, D], F32).ap()

    def xrow1(t):
        tok0 = t * 128
        r_, j_, i0 = tok0 // TPC, (tok0 % TPC) // 256, tok0 % 256
        return xall[(j_ * 4 + r_) * 256 + i0:(j_ * 4 + r_) * 256 + i0 + 128, :]

    with ExitStack() as es:
        P = Prog(nc, es)
        banks = [P.ps([128, 512], F32, 'bank%d' % i) for i in range(8)]
        rank256 = nc.sync.snap((nc.sync.partition_id() % 4) * 256, min_val=0, max_val=768)

        H = [P.view(hxall[l], 'HXALL%d' % l) for l in range(2)]
        XALL = P.view(xall, 'XALL')

        def hx_chunk(l):
            def f(k, evs):
                for ev in evs:
                    P._wait('pool', ev)
                P.coll('AllGather', hx[l][k * 1024:(k + 1) * 1024, :], H[l], hxall[l][k * 4096:(k + 1) * 4096, :], RG4)
            return f

        def xs_group(g):
            for j in (2 * g, 2 * g + 1):
                P.coll('AllGather', xs[j * 256:(j + 1) * 256, :], XALL, xall[j * 1024:(j + 1) * 1024, :], RG4)

        emit_A(P, nc, banks, ioA[0], lambda t: x[t * 128:(t + 1) * 128, :], hx[0], ntiles, on_chunk=hx_chunk(0))
        emit_B(P, nc, banks, ioB[0], xsl, hxall[0], H[0], mixloc[0], xs, rank256, False, ngroups, on_group=xs_group)
        emit_A(P, nc, banks, ioA[1], xrow1, hx[1], ntiles, xdep=XALL, on_chunk=hx_chunk(1))
        emit_B(P, nc, banks, ioB[1], xs, hxall[1], H[1], mixloc[1], out, rank256, True, ngroups)
        P.finish()
    return nc


def fused_inputs(inp):
    x = np.ascontiguousarray(inp['x'], dtype=np.float32)
    dummy = np.zeros((2, L, D), np.float32)
    maps = [dict() for _ in range(8)]
    for layer in range(2):
        ma = inputs_A(inp, layer, x)
        mb = inputs_B(inp, layer, x, dummy)
        for c in range(8):
            for k, v in ma[c].items():
                if k != 'x':
                    maps[c][k + '_a%d' % layer] = v
            for k, v in mb[c].items():
                if k not in ('x', 'mix'):
                    maps[c][k + '_b%d' % layer] = v
    xf = x.reshape(-1, D)
    for c in range(8):
        maps[c]['x'] = x[c // 4]
        maps[c]['xsl'] = np.ascontiguousarray(xf[c * TPC:(c + 1) * TPC])
    return maps


def kernel(**inputs):
    inp = {k: np.asarray(v) for k, v in inputs.items()}
    if 'F' not in _CACHE:
        _CACHE['F'] = build_fused()
    res = run_bass_kernel_spmd(_CACHE['F'], fused_inputs(inp), core_ids=list(range(8)))
    return np.concatenate([res.results[c]['out'] for c in range(8)], axis=0).reshape(2, L, D).astype(np.float32)
```

```python
import math
from contextlib import ExitStack

import numpy as np
import concourse.bass as bass
import concourse.mybir as mybir
from concourse.bass_utils import run_bass_kernel_spmd

F32 = mybir.dt.float32
BF16 = mybir.dt.bfloat16
I32 = mybir.dt.int32
ALU = mybir.AluOpType
AF = mybir.ActivationFunctionType
AX = mybir.AxisListType

D = 1024
L = 8192
NT = 64
EPS = 1e-6
TWO_PI = 2.0 * math.pi
D_FF = 3584
NFC = 28
N_EXP = 8


class T:
    def __init__(self, t, name, root=None):
        self.t = t
        self.name = name
        self.lastw = None
        self.readers = {}
        self.root = self if root is None else root.root
        self.excl = False

    def __getitem__(self, idx):
        return self.t[idx]


class Prog:
    def __init__(self, nc, es, ndma_sems=8):
        self.nc = nc
        self.es = es
        self.engs = {'pe': nc.tensor, 'act': nc.scalar, 'dve': nc.vector, 'pool': nc.gpsimd, 'sp': nc.sync}
        self.sem = {k: es.enter_context(nc.semaphore('s_' + k)) for k in self.engs}
        self.cnt = {k: 0 for k in self.engs}
        self.waited = {k: {} for k in self.engs}
        self.dma_sems = {}
        for q in ('sp', 'pool'):
            self.dma_sems[q] = [[es.enter_context(nc.semaphore('d_%s%d' % (q, i))), 0] for i in range(ndma_sems)]
        self.dma_i = {q: 0 for q in self.dma_sems}
        self.cc_sem = es.enter_context(nc.semaphore('cc_sem'))
        self.cc_n = 0
        self.nbuf = 0
        self.tes = None
        self.pre = {}

    def sb(self, shape, dt, name=None, keep=False):
        if keep and name in self.pre:
            assert list(self.pre[name].t.shape) == list(shape), name
            return self.pre[name]
        assert not (keep and self.tes is not None), "persistent buffer %s must be pre-allocated" % name
        self.nbuf += 1
        uname = (name or 'b') + '_%d' % self.nbuf
        es = self.es if (keep or self.tes is None) else self.tes
        t = T(es.enter_context(self.nc.sbuf_tensor(uname, shape, dt)), uname)
        if keep:
            self.pre[name] = t
        return t

    def ps(self, shape, dt, name=None):
        self.nbuf += 1
        name = name or ('p%d' % self.nbuf)
        t = T(self.es.enter_context(self.nc.psum_tensor(name, shape, dt)), name)
        t.excl = True
        return t

    def view(self, ap, name='v', parent=None):
        return T(ap, name, root=parent)

    def _wait(self, engname, ev):
        sem, val, src = ev
        if src == 'pe' and engname == 'pe':
            return
        key = id(sem)
        w = self.waited[engname]
        if w.get(key, 0) >= val:
            return
        w[key] = val
        self.engs[engname].wait_ge(sem, val)

    def _deps(self, engname, r, w):
        for b in r:
            b = b.root
            if b.lastw is not None:
                self._wait(engname, b.lastw)
        for b in w:
            b = b.root
            if b.lastw is not None:
                self._wait(engname, b.lastw)
            for ev in b.readers.values():
                self._wait(engname, ev)

    def _commit(self, ev, r, w):
        for b in r:
            b = b.root
            old = b.readers.get(id(ev[0]))
            if old is None or old[1] < ev[1]:
                b.readers[id(ev[0])] = ev
        for b in w:
            b = b.root
            b.lastw = ev
            b.readers = {}

    def op(self, engname, fn, r=(), w=()):
        xr = [b for b in r if b.root.excl]
        if xr:
            r = [b for b in r if not b.root.excl]
            w = list(w) + xr
        self._deps(engname, r, w)
        ins = fn(self.engs[engname])
        self.cnt[engname] += 1
        ins.then_inc(self.sem[engname], 1)
        ev = (self.sem[engname], self.cnt[engname], engname)
        self._commit(ev, r, w)
        return ev

    def dma(self, q, out, in_, r=(), w=(), **kw):
        if out.dtype != in_.dtype:
            q = 'pool'
        self._deps(q, r, w)
        slot = self.dma_sems[q][self.dma_i[q] % len(self.dma_sems[q])]
        self.dma_i[q] += 1
        sem, n = slot
        if n > 0:
            self._wait(q, (sem, 16 * n, 'dma'))
        slot[1] = n + 1
        self.engs[q].dma_start(out=out, in_=in_, **kw).then_inc(sem, 16)
        ev = (sem, 16 * (n + 1), 'dma')
        self._commit(ev, r, w)
        return ev

    def coll(self, kind, src_ap, dst, dst_ap, groups):
        self.nc.gpsimd.collective_compute(kind, ALU.bypass, replica_groups=groups, ins=[src_ap], outs=[dst_ap]).then_inc(self.cc_sem, 1)
        self.cc_n += 1
        ev = (self.cc_sem, self.cc_n, 'cc')
        self._commit(ev, [], [dst])
        return ev

    def barrier(self):
        for e in self.engs:
            for o in self.engs:
                if o != e and self.cnt[o] > 0:
                    self._wait(e, (self.sem[o], self.cnt[o], 'x'))
            for q in self.dma_sems:
                for sem, n in self.dma_sems[q]:
                    if n > 0:
                        self._wait(e, (sem, 16 * n, 'dma'))
            if self.cc_n > 0:
                self._wait(e, (self.cc_sem, self.cc_n, 'cc'))

    def finish(self):
        self.barrier()
        for q in self.dma_sems:
            for sem, n in self.dma_sems[q]:
                if n > 0:
                    self._wait('sp', (sem, 16 * n, 'dma'))

    def mm(self, out, oap, lhsT, lap, rhs, rap, start=True, stop=True):
        return self.op('pe', lambda e: e.matmul(oap, lhsT=lap, rhs=rap, start=start, stop=stop),
                       r=[lhsT, rhs], w=[out])

    def tr(self, out, oap, src, sap, ident):
        n = sap.shape[0]
        return self.op('pe', lambda e: e.transpose(oap, sap, ident[0:n, 0:n]), r=[src, ident], w=[out])

    def tt(self, eng, out, oap, a, aap, b, bap, op):
        return self.op(eng, lambda e: e.tensor_tensor(out=oap, in0=aap, in1=bap, op=op), r=[a, b], w=[out])

    def ts(self, eng, out, oap, a, aap, s1, op0, s2=None, op1=None, extra_r=()):
        if op1 is None:
            return self.op(eng, lambda e: e.tensor_scalar(out=oap, in0=aap, scalar1=s1, scalar2=None, op0=op0),
                           r=[a] + list(extra_r), w=[out])
        return self.op(eng, lambda e: e.tensor_scalar(out=oap, in0=aap, scalar1=s1, scalar2=s2, op0=op0, op1=op1),
                       r=[a] + list(extra_r), w=[out])

    def act(self, out, oap, a, aap, func, extra_r=(), extra_w=(), **kw):
        return self.op('act', lambda e: e.activation(out=oap, in_=aap, func=func, **kw),
                       r=[a] + list(extra_r), w=[out] + list(extra_w))

    def cp(self, eng, out, oap, a, aap):
        if eng == 'act':
            return self.act(out, oap, a, aap, AF.Copy)
        return self.op(eng, lambda e: e.tensor_copy(out=oap, in_=aap), r=[a], w=[out])

    def rstd(self, ss, n, tmp):
        self.ts('dve', ss, ss[:], ss, ss[:], 1.0 / n, ALU.mult, EPS, ALU.add)
        self.act(ss, ss[:], ss, ss[:], AF.Sqrt)
        self.op('dve', lambda e: e.reciprocal(out=ss[:], in_=ss[:]), r=[ss], w=[ss])

    def range_reduce(self, out, oap, src, sap, kf, kfap, ki, kiap, shift=0.0):
        self.ts('dve', kf, kfap, src, sap, 1.0 / TWO_PI, ALU.mult, shift / TWO_PI, ALU.add)
        self.cp('dve', ki, kiap, kf, kfap)
        self.cp('dve', kf, kfap, ki, kiap)
        self.op('dve', lambda e: e.scalar_tensor_tensor(out=oap, in0=kfap, scalar=-TWO_PI, in1=sap,
                                                        op0=ALU.mult, op1=ALU.add), r=[kf, src], w=[out])
        if shift != 0.0:
            self.ts('dve', out, oap, out, oap, shift, ALU.add)
        self.ts('dve', out, oap, out, oap, -math.pi, ALU.max, math.pi, ALU.min)


def make_stager(P, nstage=4, cols=1024):
    stg = [P.sb([128, cols], F32, 'stage%d' % i) for i in range(nstage)]
    cnt = [0]
    engs = ('dve', 'act', 'pool')

    def load(dst, dst_ap, src_ap):
        n = src_ap.shape[-1]
        sg = stg[cnt[0] % nstage]
        eng = engs[cnt[0] % 3]
        cnt[0] += 1
        P.dma('sp', sg[:, 0:n], src_ap, w=[sg])
        P.cp(eng, dst, dst_ap, sg, sg[:, 0:n])
    return load


def make_ident(P):
    identf = P.sb([128, 128], F32, 'identf')
    ident = P.sb([128, 128], BF16, 'ident', keep=True)
    P.op('pool', lambda e: e.memset(identf[:], 1.0), w=[identf])
    P.op('pool', lambda e: e.affine_select(out=identf[:], in_=identf[:], pattern=[[-1, 128]],
                                           compare_op=ALU.is_equal, fill=0.0, base=0, channel_multiplier=1),
         r=[identf], w=[identf])
    P.cp('dve', ident, ident[:], identf, identf[:])
    return ident, identf


def adaln_mod(P, nc, cvec, adaw, adab, ncol_chunks, modps, name, stager):
    cv = P.sb([128, 8], F32, name + '_cv')
    cvb = P.sb([128, 8], BF16, name + '_cvb')
    P.dma('sp', cv[:], cvec[:, :], w=[cv])
    P.act(cvb, cvb[:], cv, cv[:], AF.Silu)
    ncols = ncol_chunks * 128
    mod = P.sb([128, ncol_chunks], F32, name + '_mod', keep=True)
    ab = P.sb([128, ncol_chunks], F32, name + '_ab')
    P.dma('sp', ab[:], adab[:, :], w=[ab])
    cw = 1024
    wts = [P.sb([128, 8, cw], BF16, name + '_w%d' % i) for i in range(2)]
    for ci in range(ncols // cw):
        wt = wts[ci % 2]
        for kc in range(8):
            stager(wt, wt[:, kc, :], adaw[:, kc, ci * cw:(ci + 1) * cw])
        for cc in range(cw // 128):
            ch = ci * (cw // 128) + cc
            for kc in range(8):
                P.mm(modps, modps[:, ch:ch + 1], wt, wt[:, kc, cc * 128:(cc + 1) * 128], cvb, cvb[:, kc:kc + 1],
                     start=(kc == 0), stop=(kc == 7))
    P.tt('dve', mod, mod[:], modps, modps[:, 0:ncol_chunks], ab, ab[:], ALU.add)
    return mod


import os
STOP_AT = float(os.environ.get('STOP_AT', '99'))


def decl_A(nc, sfx):
    io = {}
    io['cvec'] = nc.dram_tensor('cvec' + sfx, [128, 8], F32, kind='ExternalInput').ap()
    io['pos'] = nc.dram_tensor('pos' + sfx, [128, NT], I32, kind='ExternalInput').ap()
    io['adaw'] = nc.dram_tensor('adaw' + sfx, [128, 8, 2048], F32, kind='ExternalInput').ap()
    io['adab'] = nc.dram_tensor('adab' + sfx, [128, 16], F32, kind='ExternalInput').ap()
    io['gpre'] = nc.dram_tensor('gpre' + sfx, [128, 8], F32, kind='ExternalInput').ap()
    io['wsel'] = nc.dram_tensor('wsel' + sfx, [128, 8, 768], F32, kind='ExternalInput').ap()
    io['retg'] = nc.dram_tensor('retg' + sfx, [1, 64], F32, kind='ExternalInput').ap()
    io['invf'] = nc.dram_tensor('invf' + sfx, [1, 32], F32, kind='ExternalInput').ap()
    io['dmaskT_d'] = nc.dram_tensor('dmaskT' + sfx, [128, 128], F32, kind='ExternalInput').ap()
    io['tri_d'] = nc.dram_tensor('tri' + sfx, [128, 128], F32, kind='ExternalInput').ap()
    io['qdec_d'] = nc.dram_tensor('qdec' + sfx, [64, 128], F32, kind='ExternalInput').ap()
    io['kdec_d'] = nc.dram_tensor('kdec' + sfx, [128, 1], F32, kind='ExternalInput').ap()
    io['dcy_d'] = nc.dram_tensor('dcy' + sfx, [64, 1], F32, kind='ExternalInput').ap()
    io['cmask_d'] = nc.dram_tensor('cmask' + sfx, [128, 128], F32, kind='ExternalInput').ap()
    io['jp1_d'] = nc.dram_tensor('jp1' + sfx, [128, 1], F32, kind='ExternalInput').ap()
    io['irow1_d'] = nc.dram_tensor('irow1' + sfx, [128, 128], F32, kind='ExternalInput').ap()
    io['are_tm_d'] = nc.dram_tensor('are_tm' + sfx, [1, 512], F32, kind='ExternalInput').ap()
    io['aim_tm_d'] = nc.dram_tensor('aim_tm' + sfx, [1, 512], F32, kind='ExternalInput').ap()
    io['ldt_tm_d'] = nc.dram_tensor('ldt_tm' + sfx, [1, 512], F32, kind='ExternalInput').ap()
    io['BR_d'] = nc.dram_tensor('BR' + sfx, [64, 512], F32, kind='ExternalInput').ap()
    io['BI_d'] = nc.dram_tensor('BI' + sfx, [64, 512], F32, kind='ExternalInput').ap()
    io['are_sm_d'] = nc.dram_tensor('are_sm' + sfx, [128, 2], F32, kind='ExternalInput').ap()
    io['aim_sm_d'] = nc.dram_tensor('aim_sm' + sfx, [128, 2], F32, kind='ExternalInput').ap()
    io['ldt_sm_d'] = nc.dram_tensor('ldt_sm' + sfx, [128, 2], F32, kind='ExternalInput').ap()
    io['CR_d'] = nc.dram_tensor('CR' + sfx, [128, 2, 64], F32, kind='ExternalInput').ap()
    io['CI_d'] = nc.dram_tensor('CI' + sfx, [128, 2, 64], F32, kind='ExternalInput').ap()
    io['DD_d'] = nc.dram_tensor('DD' + sfx, [64, 64], F32, kind='ExternalInput').ap()
    io['qng_d'] = nc.dram_tensor('qng' + sfx, [128, 2], F32, kind='ExternalInput').ap()
    io['wuq_d'] = nc.dram_tensor('wuq' + sfx, [128, 2, 192], F32, kind='ExternalInput').ap()
    io['kvng_d'] = nc.dram_tensor('kvng' + sfx, [128, 1], F32, kind='ExternalInput').ap()
    io['wukv_d'] = nc.dram_tensor('wukv' + sfx, [128, 256], F32, kind='ExternalInput').ap()
    return io


def emit_A(P, nc, banks, io, xrow, hx, ntiles=NT, xdep=None, on_chunk=None):
    cvec = io['cvec']
    pos = io['pos']
    adaw = io['adaw']
    adab = io['adab']
    gpre = io['gpre']
    wsel = io['wsel']
    retg = io['retg']
    invf = io['invf']
    dmaskT_d = io['dmaskT_d']
    tri_d = io['tri_d']
    qdec_d = io['qdec_d']
    kdec_d = io['kdec_d']
    dcy_d = io['dcy_d']
    cmask_d = io['cmask_d']
    jp1_d = io['jp1_d']
    irow1_d = io['irow1_d']
    are_tm_d = io['are_tm_d']
    aim_tm_d = io['aim_tm_d']
    ldt_tm_d = io['ldt_tm_d']
    BR_d = io['BR_d']
    BI_d = io['BI_d']
    are_sm_d = io['are_sm_d']
    aim_sm_d = io['aim_sm_d']
    ldt_sm_d = io['ldt_sm_d']
    CR_d = io['CR_d']
    CI_d = io['CI_d']
    DD_d = io['DD_d']
    qng_d = io['qng_d']
    wuq_d = io['wuq_d']
    kvng_d = io['kvng_d']
    wukv_d = io['wukv_d']
    st = ExitStack()
    old_es = P.es
    P.es = st
    P.pre = {}
    b7 = banks[7][:, :].bitcast(BF16)
    TB = [P.view(b7[:, 0:512], 'TB', parent=banks[7])]
    TS = [P.view(b7[:, 256 + 128 * k:384 + 128 * k], 'TS%d' % k, parent=banks[7]) for k in range(2)]
    PJ0 = P.view(banks[0][:, :], 'PJ0', parent=banks[0])
    PJ1 = P.view(banks[1][:, 0:256], 'PJ1', parent=banks[1])
    RS = P.view(banks[1][:, 256:384], 'RS', parent=banks[1])
    RO = P.view(banks[1][:, 384:448], 'RO', parent=banks[1])
    RKV = P.view(banks[1][0:64, 448:512], 'RKV', parent=banks[1])
    SA = P.view(banks[2][:, :], 'SA', parent=banks[2])
    SB = P.view(banks[3][:, :], 'SB', parent=banks[3])
    QTN = P.view(banks[4][:, 0:128], 'QTN', parent=banks[4])
    KTN = P.view(banks[4][:, 128:256], 'KTN', parent=banks[4])
    VP = P.view(banks[4][:, 256:384], 'VP', parent=banks[4])
    QRP = P.view(banks[4][:, 384:448], 'QRP', parent=banks[4])
    SY = P.view(banks[4][:, 448:512], 'SY', parent=banks[4])
    SC = [P.view(banks[5][:, :], 'SC0', parent=banks[5]), P.view(banks[6][:, :], 'SC1', parent=banks[6])]

    for nm, shp, dt_ in [('ident', [128, 128], BF16), ('ada_mod', [128, 16], F32), ('scale1', [128, 8], F32),
                         ('wselb', [128, 8, 768], BF16), ('retg_t', [128, 64], F32), ('dmaskT', [128, 128], F32),
                         ('tri', [128, 128], BF16), ('qdec', [64, 128], F32), ('kdec', [128, 1], F32),
                         ('dcy', [64, 1], F32), ('cmask', [128, 128], F32), ('SINT', [128, NT * 32], F32),
                         ('COST', [128, NT * 32], F32), ('NSINT', [128, NT * 32], F32), ('EA', [128, 512], F32),
                         ('EB', [128, 512], F32), ('Bmat', [64, 512], BF16), ('Bswp', [64, 512], BF16),
                         ('EA2', [128, 512], F32), ('EB2', [128, 512], F32), ('Cmat', [128, 4, 64], BF16),
                         ('DDb', [64, 64], BF16), ('xprev', [128, 4], F32), ('xprevs', [128, 4], F32),
                         ('wuqb', [128, 2, 192], BF16), ('wukvb', [128, 256], BF16)]:
        P.sb(shp, dt_, nm, keep=True)
    tes = ExitStack()
    P.tes = tes
    ident, identf = make_ident(P)

    stager = make_stager(P)
    mod = adaln_mod(P, nc, cvec, adaw, adab, 16, SA, 'ada', stager)
    gp = P.sb([128, 8], F32, 'gp')
    P.dma('sp', gp[:], gpre[:, :], w=[gp])
    scale1 = P.sb([128, 8], F32, 'scale1', keep=True)
    P.ts('dve', scale1, scale1[:], mod, mod[:, 8:16], 1.0, ALU.add)
    P.tt('dve', scale1, scale1[:], scale1, scale1[:], gp, gp[:], ALU.mult)
    shm = P.view(mod[:, 0:8], 'shm', parent=mod)

    wselb = P.sb([128, 8, 768], BF16, 'wselb', keep=True)
    for kc in range(8):
        stager(wselb, wselb[:, kc, :], wsel[:, kc, :])

    def load(name, src, shape, dt=F32, q='sp', bcast=None, keep=False):
        t = P.sb(shape, dt, name, keep=keep)
        P.dma(q, t[:], src if bcast is None else src.broadcast_to(bcast), w=[t])
        return t

    retg_t = load('retg_t', retg[0:1, :], [128, 64], bcast=[128, 64], keep=True)
    invf_t = load('invf_t', invf[0:1, :], [128, 32], bcast=[128, 32])
    dmaskT = load('dmaskT', dmaskT_d[:, :], [128, 128], keep=True)
    tri_f = load('tri_f', tri_d[:, :], [128, 128])
    tri = P.sb([128, 128], BF16, 'tri', keep=True)
    P.cp('dve', tri, tri[:], tri_f, tri_f[:])
    qdec = load('qdec', qdec_d[:, :], [64, 128], keep=True)
    kdec = load('kdec', kdec_d[:, :], [128, 1], keep=True)
    dcy = load('dcy', dcy_d[:, :], [64, 1], keep=True)
    cmask = load('cmask', cmask_d[:, :], [128, 128], keep=True)
    jp1 = load('jp1', jp1_d[:, :], [128, 1])
    irow1 = load('irow1', irow1_d[:, :], [128, 128])

    posi = load('posi', pos[:, :], [128, NT], I32)
    posf = P.sb([128, NT], F32, 'posf')
    P.cp('dve', posf, posf[:], posi, posi[:])
    NR = NT * 32
    ang = P.sb([128, NR], F32, 'ang')
    P.tt('dve', ang, ang[:].rearrange("p (t f) -> p t f", f=32), posf,
         posf[:].unsqueeze(2).broadcast_to([128, NT, 32]), invf_t,
         invf_t[:].unsqueeze(1).broadcast_to([128, NT, 32]), ALU.mult)
    SINT = P.sb([128, NR], F32, 'SINT', keep=True)
    COST = P.sb([128, NR], F32, 'COST', keep=True)
    NSINT = P.sb([128, NR], F32, 'NSINT', keep=True)
    rkf = P.sb([128, NR], F32, 'rkf')
    rki = P.sb([128, NR], I32, 'rki')
    P.range_reduce(SINT, SINT[:], ang, ang[:], rkf, rkf[:], rki, rki[:])
    P.act(SINT, SINT[:], SINT, SINT[:], AF.Sin)
    P.range_reduce(COST, COST[:], ang, ang[:], rkf, rkf[:], rki, rki[:], shift=math.pi / 2)
    P.act(COST, COST[:], COST, COST[:], AF.Sin)
    P.ts('dve', NSINT, NSINT[:], SINT, SINT[:], -1.0, ALU.mult)

    are_tm = load('are_tm', are_tm_d[0:1, :], [128, 512], bcast=[128, 512])
    aim_tm = load('aim_tm', aim_tm_d[0:1, :], [128, 512], bcast=[128, 512])
    dt_tm = load('dt_tm', ldt_tm_d[0:1, :], [128, 512], bcast=[128, 512])
    P.act(dt_tm, dt_tm[:], dt_tm, dt_tm[:], AF.Exp)
    rho = P.sb([128, 512], F32, 'rho')
    tht = P.sb([128, 512], F32, 'tht')
    P.tt('dve', rho, rho[:], are_tm, are_tm[:], dt_tm, dt_tm[:], ALU.mult)
    P.tt('dve', tht, tht[:], aim_tm, aim_tm[:], dt_tm, dt_tm[:], ALU.mult)
    s5a = P.sb([128, 512], F32, 's5a')
    s5b = P.sb([128, 512], F32, 's5b')
    s5c = P.sb([128, 512], F32, 's5c')
    s5k = P.sb([128, 512], F32, 's5k')
    s5i = P.sb([128, 512], I32, 's5i')
    EA = P.sb([128, 512], F32, 'EA', keep=True)
    EB = P.sb([128, 512], F32, 'EB', keep=True)
    njp1 = P.sb([128, 1], F32, 'njp1')
    P.ts('dve', njp1, njp1[:], jp1, jp1[:], -1.0, ALU.mult)
    P.ts('dve', s5a, s5a[:], rho, rho[:], njp1[:, 0:1], ALU.mult, extra_r=[njp1])
    P.act(s5a, s5a[:], s5a, s5a[:], AF.Exp)
    P.ts('dve', s5b, s5b[:], tht, tht[:], jp1[:, 0:1], ALU.mult, extra_r=[jp1])
    P.range_reduce(s5c, s5c[:], s5b, s5b[:], s5k, s5k[:], s5i, s5i[:], shift=math.pi / 2)
    P.act(s5c, s5c[:], s5c, s5c[:], AF.Sin)
    P.tt('dve', EA, EA[:], s5a, s5a[:], s5c, s5c[:], ALU.mult)
    P.range_reduce(s5c, s5c[:], s5b, s5b[:], s5k, s5k[:], s5i, s5i[:])
    P.act(s5c, s5c[:], s5c, s5c[:], AF.Sin)
    P.tt('dve', EB, EB[:], s5a, s5a[:], s5c, s5c[:], ALU.mult)
    P.ts('dve', EB, EB[:], EB, EB[:], -1.0, ALU.mult)
    Lre = P.sb([128, 512], F32, 'Lre')
    Lim = P.sb([128, 512], F32, 'Lim')
    P.act(s5a, s5a[:], rho, rho[:], AF.Exp)
    P.range_reduce(s5c, s5c[:], tht, tht[:], s5k, s5k[:], s5i, s5i[:], shift=math.pi / 2)
    P.act(s5c, s5c[:], s5c, s5c[:], AF.Sin)
    P.tt('dve', Lre, Lre[:], s5a, s5a[:], s5c, s5c[:], ALU.mult)
    P.range_reduce(s5c, s5c[:], tht, tht[:], s5k, s5k[:], s5i, s5i[:])
    P.act(s5c, s5c[:], s5c, s5c[:], AF.Sin)
    P.tt('dve', Lim, Lim[:], s5a, s5a[:], s5c, s5c[:], ALU.mult)
    P.ts('dve', Lre, Lre[:], Lre, Lre[:], -1.0, ALU.add)
    P.tt('dve', s5a, s5a[:], are_tm, are_tm[:], are_tm, are_tm[:], ALU.mult)
    P.tt('dve', s5b, s5b[:], aim_tm, aim_tm[:], aim_tm, aim_tm[:], ALU.mult)
    P.tt('dve', s5a, s5a[:], s5a, s5a[:], s5b, s5b[:], ALU.add)
    P.op('dve', lambda e: e.reciprocal(out=s5a[:], in_=s5a[:]), r=[s5a], w=[s5a])
    Fre = P.sb([128, 512], F32, 'Fre')
    Fim = P.sb([128, 512], F32, 'Fim')
    P.tt('dve', s5b, s5b[:], Lre, Lre[:], are_tm, are_tm[:], ALU.mult)
    P.tt('dve', s5c, s5c[:], Lim, Lim[:], aim_tm, aim_tm[:], ALU.mult)
    P.tt('dve', s5b, s5b[:], s5b, s5b[:], s5c, s5c[:], ALU.add)
    P.tt('dve', Fre, Fre[:], s5b, s5b[:], s5a, s5a[:], ALU.mult)
    P.tt('dve', s5b, s5b[:], Lim, Lim[:], are_tm, are_tm[:], ALU.mult)
    P.tt('dve', s5c, s5c[:], Lre, Lre[:], aim_tm, aim_tm[:], ALU.mult)
    P.tt('dve', s5b, s5b[:], s5b, s5b[:], s5c, s5c[:], ALU.subtract)
    P.tt('dve', Fim, Fim[:], s5b, s5b[:], s5a, s5a[:], ALU.mult)
    BRt = load('BRt', BR_d[:, :], [64, 512])
    BIt = load('BIt', BI_d[:, :], [64, 512])
    bre = P.sb([64, 512], F32, 'bre')
    bim = P.sb([64, 512], F32, 'bim')
    btmp = P.sb([64, 512], F32, 'btmp')
    P.tt('dve', bre, bre[:], Fre, Fre[0:64, :], BRt, BRt[:], ALU.mult)
    P.tt('dve', btmp, btmp[:], Fim, Fim[0:64, :], BIt, BIt[:], ALU.mult)
    P.tt('dve', bre, bre[:], bre, bre[:], btmp, btmp[:], ALU.subtract)
    P.tt('dve', bim, bim[:], Fre, Fre[0:64, :], BIt, BIt[:], ALU.mult)
    P.tt('dve', btmp, btmp[:], Fim, Fim[0:64, :], BRt, BRt[:], ALU.mult)
    P.tt('dve', bim, bim[:], bim, bim[:], btmp, btmp[:], ALU.add)
    Bmat = P.sb([64, 512], BF16, 'Bmat', keep=True)
    Bswp = P.sb([64, 512], BF16, 'Bswp', keep=True)

    def v4(t, rows=128):
        return t[0:rows, :].rearrange("p (a r q) -> p a r q", a=2, r=2)

    P.cp('dve', Bmat, v4(Bmat, 64)[:, :, 0, :], bre, v4(bre, 64)[:, :, 0, :])
    P.cp('dve', Bmat, v4(Bmat, 64)[:, :, 1, :], bim, v4(bim, 64)[:, :, 1, :])
    P.ts('dve', Bswp, v4(Bswp, 64)[:, :, 0, :], bim, v4(bim, 64)[:, :, 0, :], -1.0, ALU.mult)
    P.cp('dve', Bswp, v4(Bswp, 64)[:, :, 1, :], bre, v4(bre, 64)[:, :, 1, :])
    are_sm = load('are_sm', are_sm_d[:, :], [128, 2])
    aim_sm = load('aim_sm', aim_sm_d[:, :], [128, 2])
    dt_sm = load('dt_sm', ldt_sm_d[:, :], [128, 2])
    P.act(dt_sm, dt_sm[:], dt_sm, dt_sm[:], AF.Exp)
    rho_sm = P.sb([128, 2], F32, 'rho_sm')
    tht_sm = P.sb([128, 2], F32, 'tht_sm')
    P.tt('dve', rho_sm, rho_sm[:], are_sm, are_sm[:], dt_sm, dt_sm[:], ALU.mult)
    P.tt('dve', tht_sm, tht_sm[:], aim_sm, aim_sm[:], dt_sm, dt_sm[:], ALU.mult)
    EA2 = P.sb([128, 512], F32, 'EA2', keep=True)
    EB2 = P.sb([128, 512], F32, 'EB2', keep=True)
    pm = P.sb([128, 256], F32, 'pm')
    pa = P.sb([128, 256], F32, 'pa')
    pc = P.sb([128, 256], F32, 'pc')
    pk = P.sb([128, 256], F32, 'pk')
    pki = P.sb([128, 256], I32, 'pki')
    for gp_ in range(2):
        sl = slice(gp_ * 128, (gp_ + 1) * 128)
        P.ts('dve', pm, pm[:, sl], irow1, irow1[:], rho_sm[:, gp_:gp_ + 1], ALU.mult, extra_r=[rho_sm])
        P.ts('dve', pa, pa[:, sl], irow1, irow1[:], tht_sm[:, gp_:gp_ + 1], ALU.mult, extra_r=[tht_sm])
    P.act(pm, pm[:], pm, pm[:], AF.Exp)
    P.range_reduce(pc, pc[:], pa, pa[:], pk, pk[:], pki, pki[:], shift=math.pi / 2)
    P.act(pc, pc[:], pc, pc[:], AF.Sin)
    P.tt('dve', pc, pc[:], pc, pc[:], pm, pm[:], ALU.mult)
    pc3 = pc[:].rearrange("p (a i) -> p a i", a=2)
    P.cp('dve', EA2, v4(EA2)[:, :, 0, :], pc, pc3)
    P.cp('dve', EA2, v4(EA2)[:, :, 1, :], pc, pc3)
    P.range_reduce(pc, pc[:], pa, pa[:], pk, pk[:], pki, pki[:])
    P.act(pc, pc[:], pc, pc[:], AF.Sin)
    P.tt('dve', pc, pc[:], pc, pc[:], pm, pm[:], ALU.mult)
    P.ts('dve', EB2, v4(EB2)[:, :, 0, :], pc, pc3, -1.0, ALU.mult)
    P.cp('dve', EB2, v4(EB2)[:, :, 1, :], pc, pc3)
    CRt = load('CRt', CR_d[:, :, :], [128, 2, 64])
    CIt = load('CIt', CI_d[:, :, :], [128, 2, 64])
    Cmat = P.sb([128, 4, 64], BF16, 'Cmat', keep=True)
    for gp_ in range(2):
        P.cp('dve', Cmat, Cmat[:, gp_ * 2, :], CRt, CRt[:, gp_, :])
        P.ts('dve', Cmat, Cmat[:, gp_ * 2 + 1, :], CIt, CIt[:, gp_, :], -1.0, ALU.mult)
    DDt = load('DDt', DD_d[:, :], [64, 64])
    DDb = P.sb([64, 64], BF16, 'DDb', keep=True)
    P.cp('dve', DDb, DDb[:], DDt, DDt[:])
    xprev = P.sb([128, 4], F32, 'xprev', keep=True)
    xprevs = P.sb([128, 4], F32, 'xprevs', keep=True)
    P.op('pool', lambda e: e.memset(xprev[:], 0.0), w=[xprev])
    P.op('pool', lambda e: e.memset(xprevs[:], 0.0), w=[xprevs])

    qng = load('qng', qng_d[:, :], [128, 2])
    wuq_f = load('wuq_f', wuq_d[:, :, :], [128, 2, 192])
    wuqb = P.sb([128, 2, 192], BF16, 'wuqb', keep=True)
    for kc in range(2):
        P.ts('dve', wuqb, wuqb[:, kc, :], wuq_f, wuq_f[:, kc, :], qng[:, kc:kc + 1], ALU.mult, extra_r=[qng])
    kvng = load('kvng', kvng_d[:, :], [128, 1])
    wukv_f = load('wukv_f', wukv_d[:, :], [128, 256])
    wukvb = P.sb([128, 256], BF16, 'wukvb', keep=True)
    P.ts('dve', wukvb, wukvb[:], wukv_f, wukv_f[:], kvng[:, 0:1], ALU.mult, extra_r=[kvng])

    P.barrier()
    tes.close()
    P.tes = None
    KTNP_t = P.es.enter_context(nc.sbuf_tensor('KTNP_%d' % id(P.es), [128, L], BF16))
    KTR_t = P.es.enter_context(nc.sbuf_tensor('KTR_%d' % id(P.es), [64, L], BF16))
    VV_t = P.es.enter_context(nc.sbuf_tensor('VV_%d' % id(P.es), [128, NT, 128], BF16))
    KTNP = [P.view(KTNP_t[:, t * 128:(t + 1) * 128], 'ktn%d' % t) for t in range(NT)]
    KTR = [P.view(KTR_t[:, t * 128:(t + 1) * 128], 'ktr%d' % t) for t in range(NT)]
    VV = [P.view(VV_t[:, t, :], 'vv%d' % t) for t in range(NT)]
    SROW_t = P.es.enter_context(nc.sbuf_tensor('SROW_%d' % id(P.es), [128, L], F32))
    SROW = [P.view(SROW_t, 'srow0'), P.view(SROW_t, 'srow1')]
    PB = P.sb([128, L], BF16, 'PB')
    state = P.sb([64, 64], F32, 'state')
    state_bf = P.sb([64, 64], BF16, 'state_bf')
    P.op('pool', lambda e: e.memset(state[:], 0.0), w=[state])
    P.op('pool', lambda e: e.memset(state_bf[:], 0.0), w=[state_bf])

    def dbl(shape, dt, name):
        return [P.sb(shape, dt, name + '%d' % i) for i in range(2)]

    xt = dbl([128, D], F32, 'xt')
    xnb = dbl([128, D], BF16, 'xnb')
    junk = P.sb([128, D], BF16, 'junk')
    ssx = dbl([128, 1], F32, 'ssx')
    hT = dbl([128, 8, 128], BF16, 'hT')
    ropeA = P.sb([128, 192], F32, 'ropeA')
    ropeB = P.sb([128, 192], F32, 'ropeB')
    rqk = dbl([128, 192], BF16, 'rqk')
    QTs = dbl([64, 128], BF16, 'QTs')
    KTs = dbl([64, 128], BF16, 'KTs')
    QTd = dbl([64, 128], BF16, 'QTd')
    vb = dbl([128, 64], BF16, 'vb')
    vdec = dbl([128, 64], BF16, 'vdec')
    smask = dbl([128, 128], BF16, 'smask')
    ssr = dbl([128, 1], F32, 'ssr')
    gs = dbl([128, 64], F32, 'gs')
    ro = dbl([128, 64], F32, 'ro')
    junk64 = P.sb([128, 256], F32, 'junk64')
    ub = dbl([128, 64], BF16, 'ub')
    UTs = dbl([64, 128], BF16, 'UTs')
    T1 = P.sb([128, 512], F32, 'T1')
    T2 = P.sb([128, 512], F32, 'T2')
    Wb = dbl([128, 512], BF16, 'Wb')
    Zf = T1
    Zsf = T2
    Xf = P.sb([128, 512], F32, 'Xf')
    Xb = dbl([128, 512], BF16, 'Xb')
    g1 = P.sb([128, 64], F32, 'g1')
    g2 = P.sb([128, 64], F32, 'g2')
    so = dbl([128, 64], F32, 'so')
    ssq = dbl([128, 1], F32, 'ssq')
    sskv = dbl([128, 1], F32, 'sskv')
    cqn = dbl([128, 256], BF16, 'cqn')
    ckvn = dbl([128, 128], BF16, 'ckvn')
    cqnT = dbl([128, 2, 128], BF16, 'cqnT')
    ckvnT = dbl([128, 128], BF16, 'ckvnT')
    QTNs = dbl([128, 128], BF16, 'QTNs')
    qrA = P.sb([128, 64], F32, 'qrA')
    qrB = P.sb([128, 64], F32, 'qrB')
    qrb = dbl([128, 64], BF16, 'qrb')
    QrTs = dbl([64, 128], BF16, 'QrTs')
    mrow = dbl([128, 1], F32, 'mrow')
    acc4 = dbl([128, 4], F32, 'acc4')
    rsum = dbl([128, 1], F32, 'rsum')
    PT = dbl([128, 512], BF16, 'PT')
    ao = dbl([128, 128], F32, 'ao')
    SM_SCALE = 192.0 ** -0.5
    ts_i = [0]

    def ts_slot():
        s = TS[ts_i[0] % 2]
        ts_i[0] += 1
        return s

    tb_i = [0]
    ev_i = [0]

    def evac_eng():
        ev_i[0] += 1
        return 'act' if ev_i[0] % 2 == 0 else 'dve'

    PVO = P.view(banks[5][:, 0:128], 'PVO', parent=banks[5])
    TBF = [P.view(banks[7][:, :].bitcast(BF16), 'TBF0', parent=banks[7]), P.view(banks[6][:, :].bitcast(BF16), 'TBF1', parent=banks[6])]
    PT8 = dbl([128, 1024], BF16, 'PT8')

    def frontend(t):
        p = t % 2
        P.dma('sp', xt[p][:], xrow(t), r=([xdep] if xdep is not None else []), w=[xt[p]])
        P.op('dve', lambda e: e.memset(ssx[p][:], 0.0), w=[ssx[p]])
        P.act(junk, junk[:], xt[p], xt[p][:], AF.Square, extra_w=[ssx[p]], accum_out=ssx[p][:])
        yield
        P.rstd(ssx[p], D, None)
        P.ts('dve', xnb[p], xnb[p][:], xt[p], xt[p][:], ssx[p][:, 0:1], ALU.mult, extra_r=[ssx[p]])
        yield
        for half in range(2):
            tb = TB[0]
            for c4 in range(4):
                c = half * 4 + c4
                P.tr(tb, tb[:, c4 * 128:(c4 + 1) * 128], xnb[p], xnb[p][:, c * 128:(c + 1) * 128], ident)
            for c4 in range(4):
                c = half * 4 + c4
                if c4 % 2 == 0:
                    P.act(hT[p], hT[p][:, c, :], tb, tb[:, c4 * 128:(c4 + 1) * 128], AF.Identity,
                          extra_r=[scale1, shm], scale=scale1[:, c:c + 1], bias=shm[:, c:c + 1])
                else:
                    P.ts('dve', hT[p], hT[p][:, c, :], tb, tb[:, c4 * 128:(c4 + 1) * 128],
                         scale1[:, c:c + 1], ALU.mult, shm[:, c:c + 1], ALU.add, extra_r=[scale1, shm])
            yield

    def projheads(t):
        projection(t)
        yield
        for _ in heads(t):
            yield

    def projection(t):
        p = t % 2
        for kc in range(8):
            P.mm(PJ0, PJ0[:, :], hT[p], hT[p][:, kc, :], wselb, wselb[:, kc, 0:512], start=(kc == 0), stop=(kc == 7))
        for kc in range(8):
            P.mm(PJ1, PJ1[:, :], hT[p], hT[p][:, kc, :], wselb, wselb[:, kc, 512:768], start=(kc == 0), stop=(kc == 7))

    def heads(t):
        p = t % 2
        cos_t = COST[:, t * 32:(t + 1) * 32]
        sin_t = SINT[:, t * 32:(t + 1) * 32]
        nsin_t = NSINT[:, t * 32:(t + 1) * 32]
        src4 = PJ0[:, 0:192].rearrange("p (a h f) -> p a h f", a=3, h=2)
        A4 = ropeA[:].rearrange("p (a h f) -> p a h f", a=3, h=2)
        B4 = ropeB[:].rearrange("p (a h f) -> p a h f", a=3, h=2)
        P.tt('dve', ropeA, A4, PJ0, src4, COST, cos_t.unsqueeze(1).unsqueeze(1).broadcast_to([128, 3, 2, 32]), ALU.mult)
        P.tt('dve', ropeB, B4[:, :, 0, :], PJ0, src4[:, :, 1, :], NSINT, nsin_t.unsqueeze(1).broadcast_to([128, 3, 32]), ALU.mult)
        P.tt('dve', ropeB, B4[:, :, 1, :], PJ0, src4[:, :, 0, :], SINT, sin_t.unsqueeze(1).broadcast_to([128, 3, 32]), ALU.mult)
        yield
        P.cp('act', vb[p], vb[p][:], PJ0, PJ0[:, 192:256])
        P.ts('dve', vdec[p], vdec[p][:], PJ0, PJ0[:, 192:256], kdec[:, 0:1], ALU.mult, extra_r=[kdec])
        P.act(gs[p], gs[p][:], PJ0, PJ0[:, 256:320], AF.Silu)
        P.cp('act', ub[p], ub[p][:], PJ0, PJ0[:, 320:384])
        P.op('dve', lambda e: e.memset(ssq[p][:], 0.0), w=[ssq[p]])
        P.op('dve', lambda e: e.memset(sskv[p][:], 0.0), w=[sskv[p]])
        P.act(junk64, junk64[:, 0:256], PJ1, PJ1[:, :], AF.Square, extra_w=[ssq[p]], accum_out=ssq[p][:])
        P.act(junk64, junk64[:, 0:128], PJ0, PJ0[:, 384:512], AF.Square, extra_w=[sskv[p]], accum_out=sskv[p][:])
        yield
        P.rstd(ssq[p], 256, None)
        yield
        P.rstd(sskv[p], 128, None)
        yield
        P.ts('dve', cqn[p], cqn[p][:], PJ1, PJ1[:, :], ssq[p][:, 0:1], ALU.mult, extra_r=[ssq[p]])
        P.ts('dve', ckvn[p], ckvn[p][:], PJ0, PJ0[:, 384:512], sskv[p][:, 0:1], ALU.mult, extra_r=[sskv[p]])
        P.tt('dve', rqk[p], rqk[p][:], ropeA, ropeA[:], ropeB, ropeB[:], ALU.add)
        yield

    def ret_chain(t):
        p = t % 2
        tok = slice(t * 128, (t + 1) * 128)
        s0 = ts_slot()
        P.tr(s0, s0[0:64, :], rqk[p], rqk[p][:, 0:64], ident)
        P.cp('act', QTs[p], QTs[p][:], s0, s0[0:64, :])
        yield
        P.tt('dve', QTd[p], QTd[p][:], QTs[p], QTs[p][:], qdec, qdec[:], ALU.mult)
        s1 = ts_slot()
        P.tr(s1, s1[0:64, :], rqk[p], rqk[p][:, 64:128], ident)
        P.cp('act', KTs[p], KTs[p][:], s1, s1[0:64, :])
        yield
        P.tt('dve', gs[p], gs[p][:], gs[p], gs[p][:], retg_t, retg_t[:], ALU.mult)
        P.mm(RS, RS[:, :], KTs[p], KTs[p][:], QTs[p], QTs[p][:])
        yield
        P.tt('dve', smask[p], smask[p][:], RS, RS[:, :], dmaskT, dmaskT[:], ALU.mult)
        yield
        P.mm(RO, RO[:, :], smask[p], smask[p][:], vb[p], vb[p][:], start=True, stop=False)
        P.mm(RO, RO[:, :], QTd[p], QTd[p][:], state_bf, state_bf[:], start=False, stop=True)
        P.mm(RKV, RKV[:, :], rqk[p], rqk[p][:, 64:128], vdec[p], vdec[p][:])
        yield
        P.op('dve', lambda e: e.scalar_tensor_tensor(out=state[:], in0=state[:], scalar=dcy[:, 0:1], in1=RKV[:, :],
                                                     op0=ALU.mult, op1=ALU.add), r=[state, dcy, RKV], w=[state])
        P.cp('dve', state_bf, state_bf[:], state, state[:])
        P.op('dve', lambda e: e.memset(ssr[p][:], 0.0), w=[ssr[p]])
        P.act(junk64, junk64[:, 0:64], RO, RO[:, :], AF.Square, extra_w=[ssr[p]], accum_out=ssr[p][:])
        yield
        P.rstd(ssr[p], 64, None)
        yield
        P.op('dve', lambda e: e.scalar_tensor_tensor(out=ro[p][:], in0=RO[:, :], scalar=ssr[p][:, 0:1], in1=gs[p][:],
                                                     op0=ALU.mult, op1=ALU.mult), r=[RO, ssr[p], gs[p]], w=[ro[p]])
        out_evs.append(P.dma('sp', hx[tok, 0:64], ro[p][:], r=[ro[p]]))
        yield

    def s5_chain(t):
        p = t % 2
        tok = slice(t * 128, (t + 1) * 128)
        s3 = ts_slot()
        P.tr(s3, s3[0:64, :], ub[p], ub[p][:], ident)
        P.cp('dve', UTs[p], UTs[p][:], s3, s3[0:64, :])
        yield
        P.mm(SA, SA[:, :], UTs[p], UTs[p][:], Bmat, Bmat[:])
        P.mm(SB, SB[:, :], UTs[p], UTs[p][:], Bswp, Bswp[:])
        yield
        P.tt('dve', T1, T1[:], SA, SA[:, :], EA, EA[:], ALU.mult)
        yield
        P.tt('dve', T2, T2[:], SB, SB[:, :], EB, EB[:], ALU.mult)
        yield
        P.tt('dve', Wb[p], Wb[p][:], T1, T1[:], T2, T2[:], ALU.add)
        yield
        for blk in range(4):
            P.mm(SA, SA[:, blk * 128:(blk + 1) * 128], Wb[p], Wb[p][:, blk * 128:(blk + 1) * 128], tri, tri[:])
        for blk in range(4):
            b2 = blk ^ 1
            P.mm(SB, SB[:, blk * 128:(blk + 1) * 128], Wb[p], Wb[p][:, b2 * 128:(b2 + 1) * 128], tri, tri[:])
        yield
        P.tt('dve', Zf, Zf[:].rearrange("p (a i) -> p a i", a=4), SA, SA[:, :].rearrange("p (a i) -> p a i", a=4),
             xprev, xprev[:].unsqueeze(2).broadcast_to([128, 4, 128]), ALU.add)
        yield
        P.tt('dve', Zsf, Zsf[:].rearrange("p (a i) -> p a i", a=4), SB, SB[:, :].rearrange("p (a i) -> p a i", a=4),
             xprevs, xprevs[:].unsqueeze(2).broadcast_to([128, 4, 128]), ALU.add)
        yield
        P.tt('dve', Zf, Zf[:], Zf, Zf[:], EA2, EA2[:], ALU.mult)
        yield
        P.tt('dve', Zsf, Zsf[:], Zsf, Zsf[:], EB2, EB2[:], ALU.mult)
        yield
        P.tt('dve', Xf, Xf[:], Zf, Zf[:], Zsf, Zsf[:], ALU.add)
        yield
        P.cp('act', Xb[p], Xb[p][:], Xf, Xf[:])
        X3 = Xf[:].rearrange("p (a i) -> p a i", a=4)
        P.cp('dve', xprev, xprev[:], Xf, X3[:, :, 127])
        xp3 = xprev[:].rearrange("p (a r) -> p a r", r=2)
        xs3 = xprevs[:].rearrange("p (a r) -> p a r", r=2)
        P.cp('dve', xprevs, xs3[:, :, 0], xprev, xp3[:, :, 1])
        P.cp('dve', xprevs, xs3[:, :, 1], xprev, xp3[:, :, 0])
        yield
        for blk in range(4):
            P.mm(SY, SY[:, :], Xb[p], Xb[p][:, blk * 128:(blk + 1) * 128], Cmat, Cmat[:, blk, :], start=(blk == 0), stop=False)
        P.mm(SY, SY[:, :], UTs[p], UTs[p][:], DDb, DDb[:], start=False, stop=True)
        yield
        P.act(g1, g1[:], SY, SY[:, :], AF.Square)
        yield
        P.ts('dve', g1, g1[:], g1, g1[:], 0.044715, ALU.mult, 1.0, ALU.add)
        P.tt('dve', g1, g1[:], g1, g1[:], SY, SY[:, :], ALU.mult)
        yield
        P.act(g2, g2[:], g1, g1[:], AF.Sigmoid, scale=1.5957691216057308)
        yield
        P.tt('dve', so[p], so[p][:], g2, g2[:], SY, SY[:, :], ALU.mult)
        out_evs.append(P.dma('sp', hx[tok, 64:128], so[p][:], r=[so[p]]))
        yield

    def mla_chain(t):
        p = t % 2
        tok = slice(t * 128, (t + 1) * 128)
        cos_t = COST[:, t * 32:(t + 1) * 32]
        sin_t = SINT[:, t * 32:(t + 1) * 32]
        nsin_t = NSINT[:, t * 32:(t + 1) * 32]
        s2 = ts_slot()
        P.tr(s2, s2[0:64, :], rqk[p], rqk[p][:, 128:192], ident)
        P.cp('act', KTR[t], KTR[t][:], s2, s2[0:64, :])
        yield
        for kc in range(2):
            s = ts_slot()
            P.tr(s, s[:, :], cqn[p], cqn[p][:, kc * 128:(kc + 1) * 128], ident)
            P.cp(evac_eng(), cqnT[p], cqnT[p][:, kc, :], s, s[:, :])
            yield
        s = ts_slot()
        P.tr(s, s[:, :], ckvn[p], ckvn[p][:], ident)
        P.cp(evac_eng(), ckvnT[p], ckvnT[p][:], s, s[:, :])
        yield
        for kc in range(2):
            P.mm(QTN, QTN[:, :], wuqb, wuqb[:, kc, 0:128], cqnT[p], cqnT[p][:, kc, :], start=(kc == 0), stop=(kc == 1))
        for kc in range(2):
            P.mm(QRP, QRP[:, :], cqnT[p], cqnT[p][:, kc, :], wuqb, wuqb[:, kc, 128:192], start=(kc == 0), stop=(kc == 1))
        P.mm(KTN, KTN[:, :], wukvb, wukvb[:, 0:128], ckvnT[p], ckvnT[p][:])
        P.mm(VP, VP[:, :], ckvnT[p], ckvnT[p][:], wukvb, wukvb[:, 128:256])
        yield
        P.act(QTNs[p], QTNs[p][:], QTN, QTN[:, :], AF.Copy, scale=SM_SCALE)
        q3 = QRP[:, :].rearrange("p (h f) -> p h f", h=2)
        a3 = qrA[:].rearrange("p (h f) -> p h f", h=2)
        b3 = qrB[:].rearrange("p (h f) -> p h f", h=2)
        P.tt('dve', qrA, a3, QRP, q3, COST, cos_t.unsqueeze(1).broadcast_to([128, 2, 32]), ALU.mult)
        P.tt('dve', qrB, b3[:, 0, :], QRP, q3[:, 1, :], NSINT, nsin_t, ALU.mult)
        P.tt('dve', qrB, b3[:, 1, :], QRP, q3[:, 0, :], SINT, sin_t, ALU.mult)
        P.cp('act', KTNP[t], KTNP[t][:], KTN, KTN[:, :])
        P.cp('act', VV[t], VV[t][:], VP, VP[:, :])
        yield
        P.tt('dve', qrb[p], qrb[p][:], qrA, qrA[:], qrB, qrB[:], ALU.add)
        yield
        s = ts_slot()
        P.tr(s, s[0:64, :], qrb[p], qrb[p][:], ident)
        P.act(QrTs[p], QrTs[p][:], s, s[0:64, :], AF.Copy, scale=SM_SCALE)
        yield
        Lk = (t + 1) * 128
        nkb = (Lk + 511) // 512
        for kb in range(nkb):
            n = min(512, Lk - kb * 512)
            sc = SC[kb % 2]
            kts = [KTNP[4 * kb + i_] for i_ in range(n // 128)]
            krs = [KTR[4 * kb + i_] for i_ in range(n // 128)]
            P.op('pe', lambda e: e.matmul(sc[:, 0:n], lhsT=QTNs[p][:], rhs=KTNP_t[:, kb * 512:kb * 512 + n],
                                          start=True, stop=False), r=[QTNs[p]] + kts, w=[sc])
            P.op('pe', lambda e: e.matmul(sc[:, 0:n], lhsT=QrTs[p][:], rhs=KTR_t[:, kb * 512:kb * 512 + n],
                                          start=False, stop=True), r=[QrTs[p]] + krs, w=[sc])
            yield
            srow = SROW[kb % 2]
            if kb == nkb - 1:
                if n > 128:
                    P.cp('act', srow, SROW_t[:, kb * 512:kb * 512 + n - 128], sc, sc[:, 0:n - 128])
                P.tt('dve', srow, SROW_t[:, Lk - 128:Lk], sc, sc[:, n - 128:n], cmask, cmask[:], ALU.add)
            else:
                P.cp(evac_eng(), srow, SROW_t[:, kb * 512:kb * 512 + n], sc, sc[:, 0:n])
            yield
        P.op('dve', lambda e: e.reduce_max(out=mrow[p][:], in_=SROW_t[:, 0:Lk], axis=AX.X), r=[SROW[0], SROW[1]], w=[mrow[p]])
        P.op('dve', lambda e: e.memset(acc4[p][:], 0.0), w=[acc4[p]])
        yield
        P.ts('dve', mrow[p], mrow[p][:], mrow[p], mrow[p][:], -1.0, ALU.mult)
        yield
        nch = (Lk + 2047) // 2048
        for ci in range(nch):
            c0 = ci * 2048
            c1 = min(Lk, c0 + 2048)
            P.op('act', lambda e: e.activation(out=PB[:, c0:c1], in_=SROW_t[:, c0:c1], func=AF.Exp,
                                               bias=mrow[p][:, 0:1], scale=1.0, accum_out=acc4[p][:, ci:ci + 1]),
                 r=[SROW[0], SROW[1], mrow[p]], w=[PB, acc4[p]])
            yield
        P.op('dve', lambda e: e.reduce_sum(out=rsum[p][:], in_=acc4[p][:, 0:nch], axis=AX.X), r=[acc4[p]], w=[rsum[p]])
        yield
        P.op('dve', lambda e: e.reciprocal(out=rsum[p][:], in_=rsum[p][:]), r=[rsum[p]], w=[rsum[p]])
        ng = (t + 1 + 7) // 8

        def tr_round(g):
            tbf = TBF[g % 2]
            pt = PT8[g % 2]
            blks = list(range(g * 8, min(t + 1, g * 8 + 8)))
            for blk in blks:
                P.tr(tbf, tbf[:, (blk % 8) * 128:(blk % 8 + 1) * 128], PB, PB[:, blk * 128:(blk + 1) * 128], ident)
            w_ = len(blks) * 128
            P.cp(evac_eng(), pt, pt[:, 0:w_], tbf, tbf[:, 0:w_])
            return blks, pt

        cur = tr_round(0)
        yield
        for g in range(ng):
            nxt = tr_round(g + 1) if g + 1 < ng else None
            blks, pt = cur
            for blk in blks:
                P.mm(PVO, PVO[:, :], pt, pt[:, (blk % 8) * 128:(blk % 8 + 1) * 128], VV[blk], VV[blk][:],
                     start=(blk == 0), stop=(blk == t))
            cur = nxt
            yield
        P.ts('dve', ao[p], ao[p][:], PVO, PVO[:, :], rsum[p][:, 0:1], ALU.mult, extra_r=[rsum[p]])
        out_evs.append(P.dma('sp', hx[tok, 128:256], ao[p][:], r=[ao[p]]))
        yield

    def drain(g):
        for _ in g:
            pass

    def interleave(gens):
        gens = [[g, 2 if i == 0 else 1] for i, g in enumerate(gens)]
        while gens:
            alive = []
            for g, n_ in gens:
                ok = True
                for _ in range(n_):
                    try:
                        next(g)
                    except StopIteration:
                        ok = False
                        break
                if ok:
                    alive.append([g, n_])
            gens = alive

    def step(g, n_=1):
        for _ in range(n_):
            try:
                next(g)
            except StopIteration:
                return False
        return True

    out_evs = []
    if ntiles > 0:
        drain(frontend(0))
        drain(projheads(0))
    for t in range(ntiles):
        mla = mla_chain(t)
        mla_alive = True
        others = [ret_chain(t), s5_chain(t)]
        if t + 1 < ntiles:
            others.append(frontend(t + 1))
        while others:
            if mla_alive:
                mla_alive = step(mla, 2)
            others = [g for g in others if step(g)]
        nxt = projheads(t + 1) if t + 1 < ntiles else None
        nxt_alive = nxt is not None
        while mla_alive or nxt_alive:
            if mla_alive:
                mla_alive = step(mla, 2)
            if nxt_alive:
                nxt_alive = step(nxt)
        if on_chunk is not None and (t + 1) % 8 == 0:
            on_chunk(t // 8, out_evs)
            out_evs = []
    P.barrier()
    st.close()
    P.es = old_es
    P.pre = {}


RET_GAMMA = [1.0 - 2.0 ** (-5.0 - h) for h in range(4)]


def consts_A(hg):
    g = RET_GAMMA[hg]
    lg = math.log1p(-(2.0 ** (-5.0 - hg)))
    i = np.arange(128, dtype=np.float64)
    diff = i[None, :] - i[:, None]
    dmaskT = np.where(diff >= 0, np.exp(lg * np.maximum(diff, 0.0)), 0.0) * 0.125
    tri = (diff >= 0).astype(np.float32)
    qdec = np.broadcast_to(np.exp(lg * (i + 1.0))[None, :], (64, 128))
    kdec = (np.exp(lg * (127.0 - i)) * 0.125)[:, None]
    dcy = np.full((64, 1), math.exp(lg * 128.0))
    cmask = np.where(i[None, :] <= i[:, None], 0.0, -1e30)
    inv = 10000.0 ** (-np.arange(0, 64, 2, dtype=np.float32) / 64.0)
    f = lambda a: np.ascontiguousarray(a, dtype=np.float32)
    return dict(dmaskT=f(dmaskT), tri=f(tri), qdec=f(qdec), kdec=f(kdec), dcy=f(dcy), cmask=f(cmask),
                jp1=f((i + 1.0)[:, None]), irow1=f(np.broadcast_to((i + 1.0)[None, :], (128, 128))),
                invf=f(inv[None, :]))


def chunkT(v, nch):
    return np.ascontiguousarray(v.reshape(nch, 128).T)


def inputs_A(inp, layer, xcur):
    f = lambda a: np.ascontiguousarray(a, dtype=np.float32)
    maps = []
    w_in = inp['w_in'][layer]
    for core in range(8):
        b, hg = core // 4, core % 4
        m = dict(consts_A(hg))
        m['x'] = f(xcur[b])
        m['cvec'] = f(inp['c'][b].reshape(128, 8))
        m['pos'] = np.ascontiguousarray(inp['positions'][b].reshape(NT, 128).T.astype(np.int32))
        m['adaw'] = f(inp['ada_w'][layer][:, 0:2048].reshape(128, 8, 2048))
        m['adab'] = chunkT(f(inp['ada_b'][layer][0:2048]), 16)
        m['gpre'] = chunkT(f(inp['norm_pre_mix'][layer]), 8)
        h64 = slice(hg * 64, (hg + 1) * 64)
        cols = np.concatenate([
            np.arange(0, 256)[h64], np.arange(256, 512)[h64], np.arange(1664, 1728),
            np.arange(512, 768)[h64], np.arange(768, 1024)[h64], np.arange(1024, 1280)[h64],
            np.arange(1536, 1664), np.arange(1280, 1536)])
        ws = w_in[:, cols]
        m['wsel'] = f(ws.reshape(8, 128, 768).transpose(1, 0, 2))
        m['retg'] = f(inp['ret_norm'][layer][h64][None, :])
        gs_ = slice(4 * hg, 4 * hg + 4)

        def tm(a):
            a = a.reshape(2, 1, 128)
            return f(np.broadcast_to(a, (2, 2, 128)).reshape(1, 512))

        def sm(a):
            return f(a.reshape(2, 128).T)

        are = inp['ssm_a_re'][layer][gs_]
        aim = inp['ssm_a_im'][layer][gs_]
        ldt = np.broadcast_to(inp['ssm_log_dt'][layer][gs_][:, None], (4, 64))
        m['are_tm'], m['aim_tm'], m['ldt_tm'] = tm(are), tm(aim), tm(ldt)
        m['are_sm'], m['aim_sm'], m['ldt_sm'] = sm(are), sm(aim), sm(ldt)
        BR = np.zeros((64, 2, 2, 2, 64), np.float32)
        BI = np.zeros((64, 2, 2, 2, 64), np.float32)
        CR = np.zeros((2, 64, 2, 64), np.float32)
        CI = np.zeros((2, 64, 2, 64), np.float32)
        for gl in range(4):
            gp_, g2_ = gl // 2, gl % 2
            br = inp['ssm_b_re'][layer][4 * hg + gl]
            bi = inp['ssm_b_im'][layer][4 * hg + gl]
            for ri in range(2):
                BR[gl * 16:(gl + 1) * 16, gp_, ri, g2_, :] = br.T
                BI[gl * 16:(gl + 1) * 16, gp_, ri, g2_, :] = bi.T
            cr = inp['ssm_c_re'][layer][4 * hg + gl]
            ci = inp['ssm_c_im'][layer][4 * hg + gl]
            CR[g2_, :, gp_, gl * 16:(gl + 1) * 16] = cr.T
            CI[g2_, :, gp_, gl * 16:(gl + 1) * 16] = ci.T
        m['BR'] = f(BR.reshape(64, 512))
        m['BI'] = f(BI.reshape(64, 512))
        m['CR'] = f(CR.reshape(128, 2, 64))
        m['CI'] = f(CI.reshape(128, 2, 64))
        DD = np.zeros((64, 64), np.float32)
        DD[np.arange(64), np.arange(64)] = inp['ssm_d'][layer][gs_].reshape(64)
        m['DD'] = DD
        m['qng'] = chunkT(f(inp['mla_q_norm'][layer]), 2)
        wq = inp['mla_w_uq'][layer][:, hg * 192:(hg + 1) * 192]
        m['wuq'] = f(wq.reshape(2, 128, 192).transpose(1, 0, 2))
        m['kvng'] = f(inp['mla_kv_norm'][layer][:, None])
        m['wukv'] = f(inp['mla_w_ukv'][layer][:, hg * 256:(hg + 1) * 256])
        maps.append(m)
    return maps


_CACHE = {}


TPC = 2048
GT = 4
NG = TPC // (GT * 128)


def decl_B(nc, sfx, moe):
    n_exp = N_EXP if moe else 1
    io = {}
    io['cvec'] = nc.dram_tensor('cvec' + sfx, [128, 8], F32, kind='ExternalInput').ap()
    io['adaw'] = nc.dram_tensor('adaw' + sfx, [128, 8, 4096], F32, kind='ExternalInput').ap()
    io['adab_row'] = nc.dram_tensor('adab_row' + sfx, [1, 4096], F32, kind='ExternalInput').ap()
    io['adab_col'] = nc.dram_tensor('adab_col' + sfx, [128, 32], F32, kind='ExternalInput').ap()
    io['gpost_m'] = nc.dram_tensor('gpost_m' + sfx, [1, D], F32, kind='ExternalInput').ap()
    io['gpre_f'] = nc.dram_tensor('gpre_f' + sfx, [128, 8], F32, kind='ExternalInput').ap()
    io['gpost_f'] = nc.dram_tensor('gpost_f' + sfx, [1, D], F32, kind='ExternalInput').ap()
    io['gluw_d'] = nc.dram_tensor('gluw' + sfx, [128, 2, 256], F32, kind='ExternalInput').ap()
    io['glub_d'] = nc.dram_tensor('glub' + sfx, [1, 256], F32, kind='ExternalInput').ap()
    io['ssmg_d'] = nc.dram_tensor('ssmg' + sfx, [1, 256], F32, kind='ExternalInput').ap()
    io['mlag_d'] = nc.dram_tensor('mlag' + sfx, [1, 512], F32, kind='ExternalInput').ap()
    io['wout_d'] = nc.dram_tensor('wout' + sfx, [128, 8, D], F32, kind='ExternalInput').ap()
    io['wg_d'] = nc.dram_tensor('wg' + sfx, [n_exp * NFC, 128, 1024], F32, kind='ExternalInput').ap()
    io['wu_d'] = nc.dram_tensor('wu' + sfx, [n_exp * NFC, 128, 1024], F32, kind='ExternalInput').ap()
    io['wd_d'] = nc.dram_tensor('wd' + sfx, [n_exp * NFC, 128, 1024], F32, kind='ExternalInput').ap()
    if moe:
        io['router_d'] = nc.dram_tensor('router' + sfx, [128, 8, 8], F32, kind='ExternalInput').ap()
    return io


def emit_B(P, nc, banks, io, x, hxall, HXALL, mixloc, out, rank256, moe, ngroups=NG, xdep=None, on_group=None):
    n_exp = N_EXP if moe else 1
    cvec = io['cvec']
    adaw = io['adaw']
    adab_row = io['adab_row']
    adab_col = io['adab_col']
    gpost_m = io['gpost_m']
    gpre_f = io['gpre_f']
    gpost_f = io['gpost_f']
    gluw_d = io['gluw_d']
    glub_d = io['glub_d']
    ssmg_d = io['ssmg_d']
    mlag_d = io['mlag_d']
    wout_d = io['wout_d']
    wg_d = io['wg_d']
    wu_d = io['wu_d']
    wd_d = io['wd_d']
    router_d = io.get('router_d')
    st = ExitStack()
    old_es = P.es
    P.es = st
    P.pre = {}
    b0 = banks[0][:, :].bitcast(BF16)
    TB = P.view(b0[:, 0:512], 'TB', parent=banks[0])
    ZP = P.view(banks[1][:, 0:256], 'ZP', parent=banks[1])
    LG = P.view(banks[1][:, 256:264], 'LG', parent=banks[1])
    MC = P.view(banks[1][:, 272:288], 'MC', parent=banks[1])
    YB = [P.view(banks[2][:, :], 'YA', parent=banks[2]), P.view(banks[3][:, :], 'YB', parent=banks[3])]
    GB = [P.view(banks[4][:, :], 'G0', parent=banks[4]), P.view(banks[5][:, :], 'G1', parent=banks[5])]
    UB = [P.view(banks[6][:, :], 'U0', parent=banks[6]), P.view(banks[7][:, :], 'U1', parent=banks[7])]

    ident = P.sb([128, 128], BF16, 'ident', keep=True)
    vecm = P.sb([128, D], F32, 'vecm', keep=True)
    vecf = P.sb([128, D], F32, 'vecf', keep=True)
    modc = P.sb([128, 16], F32, 'modc', keep=True)
    scale2 = P.sb([128, 8], F32, 'scale2', keep=True)
    woutb = P.sb([128, 8, D], BF16, 'woutb', keep=True)
    gluwb = P.sb([128, 2, 256], BF16, 'gluwb', keep=True)
    glub_t = P.sb([128, 256], F32, 'glub_t', keep=True)
    ssmg_t = P.sb([128, 256], F32, 'ssmg_t', keep=True)
    mlag_t = P.sb([128, 512], F32, 'mlag_t', keep=True)
    if moe:
        routerb = P.sb([128, 8, 8], BF16, 'routerb', keep=True)
    X1 = [P.sb([128, D], F32, 'X1_%d' % i, keep=True) for i in range(GT)]
    hT2 = P.sb([128, 8, GT * 128], BF16, 'hT2', keep=True)
    yacc = [P.sb([128, D], F32, 'yacc%d' % i, keep=True) for i in range(GT)]
    gatew = P.sb([128, GT, 8], F32, 'gatew', keep=True)

    tes = ExitStack()
    P.tes = tes
    make_ident(P)
    cv = P.sb([128, 8], F32, 'cv')
    cvb = P.sb([128, 8], BF16, 'cvb')
    cvbb = P.sb([128, 8, 128], BF16, 'cvbb')
    P.dma('sp', cv[:], cvec[:, :], w=[cv])
    P.act(cvb, cvb[:], cv, cv[:], AF.Silu)
    P.cp('dve', cvbb, cvbb[:], cvb, cvb[:].unsqueeze(2).broadcast_to([128, 8, 128]))
    abc = P.sb([128, 32], F32, 'abc')
    P.dma('sp', abc[:], adab_col[:, :], w=[abc])
    wts = [P.sb([128, 8, 1024], BF16, 'adw%d' % i) for i in range(2)]
    stager = make_stager(P)
    rowb = P.sb([128, D], F32, 'rowb')
    for ci in range(4):
        wt = wts[ci % 2]
        for kc in range(8):
            stager(wt, wt[:, kc, :], adaw[:, kc, ci * 1024:(ci + 1) * 1024])
        if ci in (0, 3):
            vec = vecm if ci == 0 else vecf
            gsrc = gpost_m if ci == 0 else gpost_f
            P.dma('sp', rowb[:], adab_row[0:1, ci * 1024:(ci + 1) * 1024].broadcast_to([128, 1024]), w=[rowb])
            for half in range(2):
                yb = YB[half]
                for kc in range(8):
                    P.mm(yb, yb[:, :], cvbb, cvbb[:, kc, :], wt, wt[:, kc, half * 512:(half + 1) * 512],
                         start=(kc == 0), stop=(kc == 7))
                P.tt('dve', vec, vec[:, half * 512:(half + 1) * 512], yb, yb[:, :], rowb, rowb[:, half * 512:(half + 1) * 512], ALU.add)
            P.dma('sp', rowb[:], gsrc[0:1, :].broadcast_to([128, 1024]), w=[rowb])
            P.tt('dve', vec, vec[:], vec, vec[:], rowb, rowb[:], ALU.mult)
        else:
            for cc in range(8):
                ch = (ci - 1) * 8 + cc
                for kc in range(8):
                    P.mm(MC, MC[:, ch:ch + 1], wt, wt[:, kc, cc * 128:(cc + 1) * 128], cvb, cvb[:, kc:kc + 1],
                         start=(kc == 0), stop=(kc == 7))
    P.tt('dve', modc, modc[:], MC, MC[:, 0:16], abc, abc[:, 8:24], ALU.add)
    gpf = P.sb([128, 8], F32, 'gpf')
    P.dma('sp', gpf[:], gpre_f[:, :], w=[gpf])
    P.ts('dve', scale2, scale2[:], modc, modc[:, 8:16], 1.0, ALU.add)
    P.tt('dve', scale2, scale2[:], scale2, scale2[:], gpf, gpf[:], ALU.mult)
    sh2 = P.view(modc[:, 0:8], 'sh2', parent=modc)
    for kc in range(8):
        stager(woutb, woutb[:, kc, :], wout_d[:, kc, :])
    P.dma('pool', gluwb[:], gluw_d[:, :, :], w=[gluwb])
    P.dma('sp', glub_t[:], glub_d[0:1, :].broadcast_to([128, 256]), w=[glub_t])
    P.dma('sp', ssmg_t[:], ssmg_d[0:1, :].broadcast_to([128, 256]), w=[ssmg_t])
    P.dma('sp', mlag_t[:], mlag_d[0:1, :].broadcast_to([128, 512]), w=[mlag_t])
    if moe:
        P.dma('pool', routerb[:], router_d[:, :, :], w=[routerb])
    P.barrier()
    tes.close()
    P.tes = None

    MIXLOC = P.view(mixloc, 'MIXLOC')
    for kk in range(2):
        src_ = hxall.rearrange("(a b) c -> a (b c)", b=32)[bass.ds(rank256 + kk * 128, 128), :]
        P.dma('sp', mixloc[kk].rearrange("r n c -> (r n) c").rearrange("(a b) c -> a (b c)", b=32), src_, r=[HXALL], w=[MIXLOC])
    ev_i = [0]

    def evac_eng():
        ev_i[0] += 1
        return 'act' if ev_i[0] % 2 == 0 else 'dve'

    for g in range(ngroups):
        ph = ExitStack()
        P.tes = ph
        junk = P.sb([128, D], BF16, 'junk')

        def p1_bufs(i):
            return dict(xt=P.sb([128, D], F32, 'xt%d' % i), mt=P.sb([128, D], F32, 'mt%d' % i),
                        hxt=P.sb([128, 4, 256], F32, 'hxt%d' % i), ysb=P.sb([128, 256], BF16, 'ysb%d' % i),
                        ysT=P.sb([128, 2, 128], BF16, 'ysT%d' % i), zz=P.sb([128, 256], F32, 'zz%d' % i),
                        s2=P.sb([128, 256], F32, 's2%d' % i), catb=P.sb([128, D], BF16, 'catb%d' % i),
                        catT=P.sb([128, 8, 128], BF16, 'catT%d' % i), tmp=P.sb([128, D], F32, 'tmp%d' % i),
                        xn2=P.sb([128, D], BF16, 'xn2%d' % i), ss=P.sb([128, 4], F32, 'ss%d' % i),
                        lg=P.sb([128, 8], F32, 'lg%d' % i), lg2=P.sb([128, 8], F32, 'lg2%d' % i),
                        mk1=P.sb([128, 8], F32, 'mk1%d' % i), mk2=P.sb([128, 8], F32, 'mk2%d' % i),
                        m12=P.sb([128, 4], F32, 'm12%d' % i))

        p1sets = [p1_bufs(0), p1_bufs(1)]

        def p1_tile(ti, B_, YK):
            xt, mt, hxt, ysb, ysT, zz, s2 = B_['xt'], B_['mt'], B_['hxt'], B_['ysb'], B_['ysT'], B_['zz'], B_['s2']
            catb, catT, tmp, xn2, ss = B_['catb'], B_['catT'], B_['tmp'], B_['xn2'], B_['ss']
            lg, lg2, mk1, mk2, m12 = B_['lg'], B_['lg2'], B_['mk1'], B_['mk2'], B_['m12']
            tok = slice((g * GT + ti) * 128, (g * GT + ti + 1) * 128)
            P.dma('sp', xt[:], x[tok, :], r=([xdep] if xdep is not None else []), w=[xt])
            row0 = (g * GT + ti) * 128
            P.dma('sp', hxt[:], mixloc[row0 // 1024, :, row0 % 1024:row0 % 1024 + 128, :].rearrange("r n c -> n r c"), r=[MIXLOC], w=[hxt])
            for (c0, w_, d0) in ((0, 64, 0), (64, 64, 256), (128, 128, 512)):
                P.cp('pool', mt, mt[:, d0:d0 + 4 * w_].rearrange("p (r c) -> p r c", r=4), hxt, hxt[:, :, c0:c0 + w_])
            yield
            P.cp('dve', ysb, ysb[:], mt, mt[:, 256:512])
            yield
            for kc in range(2):
                P.tr(TB, TB[:, kc * 128:(kc + 1) * 128], ysb, ysb[:, kc * 128:(kc + 1) * 128], ident)
            P.cp('act', ysT, ysT[:].rearrange("p a b -> p (a b)"), TB, TB[:, 0:256])
            yield
            for kc in range(2):
                P.mm(ZP, ZP[:, :], ysT, ysT[:, kc, :], gluwb, gluwb[:, kc, :], start=(kc == 0), stop=(kc == 1))
            P.tt('dve', zz, zz[:], ZP, ZP[:, :], glub_t, glub_t[:], ALU.add)
            yield
            P.act(zz, zz[:], zz, zz[:], AF.Sigmoid)
            P.op('pool', lambda e: e.memset(ss[:], 0.0), w=[ss])
            yield
            P.tt('dve', s2, s2[:], zz, zz[:], mt, mt[:, 256:512], ALU.mult)
            P.act(junk, junk[:, 0:512], mt, mt[:, 512:1024], AF.Square, extra_w=[ss], accum_out=ss[:, 1:2])
            yield
            P.act(junk, junk[:, 0:256], s2, s2[:], AF.Square, extra_w=[ss], accum_out=ss[:, 0:1])
            P.cp('act', catb, catb[:, 0:256], mt, mt[:, 0:256])
            yield
            P.ts('dve', ss, ss[:, 0:1], ss, ss[:, 0:1], 1.0 / 256, ALU.mult, EPS, ALU.add)
            P.ts('dve', ss, ss[:, 1:2], ss, ss[:, 1:2], 1.0 / 512, ALU.mult, EPS, ALU.add)
            yield
            P.act(ss, ss[:, 0:2], ss, ss[:, 0:2], AF.Sqrt)
            yield
            P.op('dve', lambda e: e.reciprocal(out=ss[:, 0:2], in_=ss[:, 0:2]), r=[ss], w=[ss])
            yield
            P.op('dve', lambda e: e.scalar_tensor_tensor(out=catb[:, 256:512], in0=s2[:], scalar=ss[:, 0:1], in1=ssmg_t[:],
                                                         op0=ALU.mult, op1=ALU.mult), r=[s2, ss, ssmg_t], w=[catb])
            P.op('dve', lambda e: e.scalar_tensor_tensor(out=catb[:, 512:1024], in0=mt[:, 512:1024], scalar=ss[:, 1:2], in1=mlag_t[:],
                                                         op0=ALU.mult, op1=ALU.mult), r=[mt, ss, mlag_t], w=[catb])
            yield
            for half in range(2):
                for c4 in range(4):
                    c = half * 4 + c4
                    P.tr(TB, TB[:, c4 * 128:(c4 + 1) * 128], catb, catb[:, c * 128:(c + 1) * 128], ident)
                P.cp(evac_eng(), catT, catT[:, half * 4:(half + 1) * 4, :].rearrange("p a b -> p (a b)"), TB, TB[:, :])
                yield
            for half in range(2):
                for kc in range(8):
                    P.mm(YK[half], YK[half][:, :], catT, catT[:, kc, :], woutb, woutb[:, kc, half * 512:(half + 1) * 512],
                         start=(kc == 0), stop=(kc == 7))
            P.op('pool', lambda e: e.memset(ss[:, 2:4], 0.0), w=[ss])
            yield
            for half in range(2):
                P.act(junk, junk[:, 0:512], YK[half], YK[half][:, :], AF.Square, extra_w=[ss], accum_out=ss[:, 2 + half:3 + half])
            yield
            P.tt('dve', ss, ss[:, 2:3], ss, ss[:, 2:3], ss, ss[:, 3:4], ALU.add)
            P.ts('dve', ss, ss[:, 2:3], ss, ss[:, 2:3], 1.0 / D, ALU.mult, EPS, ALU.add)
            yield
            P.act(ss, ss[:, 2:3], ss, ss[:, 2:3], AF.Sqrt)
            yield
            P.op('dve', lambda e: e.reciprocal(out=ss[:, 2:3], in_=ss[:, 2:3]), r=[ss], w=[ss])
            yield
            for half in range(2):
                hs = slice(half * 512, (half + 1) * 512)
                P.op('dve', lambda e: e.scalar_tensor_tensor(out=tmp[:, hs], in0=YK[half][:, :], scalar=ss[:, 2:3], in1=vecm[:, hs],
                                                             op0=ALU.mult, op1=ALU.mult), r=[YK[half], ss, vecm], w=[tmp])
            yield
            P.tt('dve', X1[ti], X1[ti][:], tmp, tmp[:], xt, xt[:], ALU.add)
            P.op('pool', lambda e: e.memset(ss[:, 3:4], 0.0), w=[ss])
            yield
            P.act(junk, junk[:], X1[ti], X1[ti][:], AF.Square, extra_w=[ss], accum_out=ss[:, 3:4])
            yield
            P.ts('dve', ss, ss[:, 3:4], ss, ss[:, 3:4], 1.0 / D, ALU.mult, EPS, ALU.add)
            yield
            P.act(ss, ss[:, 3:4], ss, ss[:, 3:4], AF.Sqrt)
            yield
            P.op('dve', lambda e: e.reciprocal(out=ss[:, 3:4], in_=ss[:, 3:4]), r=[ss], w=[ss])
            yield
            P.ts('dve', xn2, xn2[:], X1[ti], X1[ti][:], ss[:, 3:4], ALU.mult, extra_r=[ss])
            yield
            for half in range(2):
                for c4 in range(4):
                    c = half * 4 + c4
                    P.tr(TB, TB[:, c4 * 128:(c4 + 1) * 128], xn2, xn2[:, c * 128:(c + 1) * 128], ident)
                for c4 in range(4):
                    c = half * 4 + c4
                    if c4 % 2 == 0:
                        P.act(hT2, hT2[:, c, ti * 128:(ti + 1) * 128], TB, TB[:, c4 * 128:(c4 + 1) * 128], AF.Identity,
                              extra_r=[scale2, sh2], scale=scale2[:, c:c + 1], bias=sh2[:, c:c + 1])
                    else:
                        P.ts('dve', hT2, hT2[:, c, ti * 128:(ti + 1) * 128], TB, TB[:, c4 * 128:(c4 + 1) * 128],
                             scale2[:, c:c + 1], ALU.mult, sh2[:, c:c + 1], ALU.add, extra_r=[scale2, sh2])
                yield
            if moe:
                for kc in range(8):
                    P.mm(LG, LG[:, :], hT2, hT2[:, kc, ti * 128:(ti + 1) * 128], routerb, routerb[:, kc, :],
                         start=(kc == 0), stop=(kc == 7))
                P.cp('dve', lg, lg[:], LG, LG[:, :])
                yield
                P.op('dve', lambda e: e.reduce_max(out=m12[:, 0:1], in_=lg[:], axis=AX.X), r=[lg], w=[m12])
                yield
                P.ts('dve', mk1, mk1[:], lg, lg[:], m12[:, 0:1], ALU.is_equal, extra_r=[m12])
                yield
                P.op('dve', lambda e: e.scalar_tensor_tensor(out=lg2[:], in0=mk1[:], scalar=-1e30, in1=lg[:],
                                                             op0=ALU.mult, op1=ALU.add), r=[mk1, lg], w=[lg2])
                yield
                P.op('dve', lambda e: e.reduce_max(out=m12[:, 1:2], in_=lg2[:], axis=AX.X), r=[lg2], w=[m12])
                yield
                P.ts('dve', mk2, mk2[:], lg2, lg2[:], m12[:, 1:2], ALU.is_equal, extra_r=[m12])
                P.tt('dve', m12, m12[:, 2:3], m12, m12[:, 1:2], m12, m12[:, 0:1], ALU.subtract)
                yield
                P.act(m12, m12[:, 2:3], m12, m12[:, 2:3], AF.Exp)
                yield
                P.ts('dve', m12, m12[:, 3:4], m12, m12[:, 2:3], 1.0, ALU.add)
                yield
                P.op('dve', lambda e: e.reciprocal(out=m12[:, 3:4], in_=m12[:, 3:4]), r=[m12], w=[m12])
                yield
                P.tt('dve', m12, m12[:, 2:3], m12, m12[:, 2:3], m12, m12[:, 3:4], ALU.mult)
                P.ts('dve', mk1, mk1[:], mk1, mk1[:], m12[:, 3:4], ALU.mult, extra_r=[m12])
                yield
                P.op('dve', lambda e: e.scalar_tensor_tensor(out=gatew[:, ti, :], in0=mk2[:], scalar=m12[:, 2:3], in1=mk1[:],
                                                             op0=ALU.mult, op1=ALU.add), r=[mk2, m12, mk1], w=[gatew])
                yield

        for pair in range(GT // 2):
            gens = [p1_tile(2 * pair, p1sets[0], YB), p1_tile(2 * pair + 1, p1sets[1], GB)]
            while gens:
                alive = []
                for gen_ in gens:
                    try:
                        next(gen_)
                        alive.append(gen_)
                    except StopIteration:
                        pass
                gens = alive
        P.barrier()
        ph.close()
        ph = ExitStack()
        P.tes = ph
        actT = P.sb([128, NFC, GT * 128], BF16, 'actT')
        wdb = P.sb([128, NFC, D], BF16, 'wdb')
        stg = [P.sb([128, 1024], F32, 'stg%d' % i) for i in range(4)]
        wgb = [P.sb([128, 8, 128], BF16, 'wgb%d' % i) for i in range(2)]
        wub = [P.sb([128, 8, 128], BF16, 'wub%d' % i) for i in range(2)]
        gsil = [P.sb([128, GT * 128], F32, 'gsil%d' % i) for i in range(2)]
        ftmp = P.sb([128, D], F32, 'ftmp')
        fjunk = P.sb([128, D], BF16, 'fjunk')
        fss = P.sb([128, 1], F32, 'fss')
        st_i = [0]

        def load_cast(dst, dst_ap, src_ap, eng):
            sg = stg[st_i[0] % 4]
            st_i[0] += 1
            P.dma('sp', sg[:], src_ap, w=[sg])
            P.cp(eng, dst, dst_ap, sg, sg[:])

        def load_gu(idx):
            b_ = idx % 2
            load_cast(wgb[b_], wgb[b_][:].rearrange("p a b -> p (a b)"), wg_d[idx, :, :], 'dve')
            load_cast(wub[b_], wub[b_][:].rearrange("p a b -> p (a b)"), wu_d[idx, :, :], 'act')

        load_gu(0)
        for e_ in range(n_exp):
            for fc in range(NFC):
                b2 = fc % 2
                if e_ * NFC + fc + 1 < n_exp * NFC:
                    load_gu(e_ * NFC + fc + 1)
                load_cast(wdb, wdb[:, fc, :], wd_d[e_ * NFC + fc, :, :], 'pool')
                for kc in range(8):
                    P.mm(GB[b2], GB[b2][:, :], wgb[b2], wgb[b2][:, kc, :], hT2, hT2[:, kc, :], start=(kc == 0), stop=(kc == 7))
                for kc in range(8):
                    P.mm(UB[b2], UB[b2][:, :], wub[b2], wub[b2][:, kc, :], hT2, hT2[:, kc, :], start=(kc == 0), stop=(kc == 7))
                P.act(gsil[b2], gsil[b2][:], GB[b2], GB[b2][:, :], AF.Silu)
                P.tt('dve', actT, actT[:, fc, :], UB[b2], UB[b2][:, :], gsil[b2], gsil[b2][:], ALU.mult)
            for ti in range(GT):
                for half in range(2):
                    hs = slice(half * 512, (half + 1) * 512)
                    yb = YB[(ti * 2 + half) % 2]
                    for fc in range(NFC):
                        P.mm(yb, yb[:, :], actT, actT[:, fc, ti * 128:(ti + 1) * 128], wdb, wdb[:, fc, hs],
                             start=(fc == 0), stop=(fc == NFC - 1))
                    if not moe:
                        P.cp(evac_eng(), yacc[ti], yacc[ti][:, hs], yb, yb[:, :])
                    elif e_ == 0:
                        P.ts('dve', yacc[ti], yacc[ti][:, hs], yb, yb[:, :], gatew[:, ti, 0:1], ALU.mult, extra_r=[gatew])
                    else:
                        P.op('dve', lambda e: e.scalar_tensor_tensor(out=yacc[ti][:, hs], in0=yb[:, :], scalar=gatew[:, ti, e_:e_ + 1],
                                                                     in1=yacc[ti][:, hs], op0=ALU.mult, op1=ALU.add),
                             r=[yb, gatew, yacc[ti]], w=[yacc[ti]])
        for ti in range(GT):
            tok = slice((g * GT + ti) * 128, (g * GT + ti + 1) * 128)
            P.op('pool', lambda e: e.memset(fss[:], 0.0), w=[fss])
            P.act(fjunk, fjunk[:], yacc[ti], yacc[ti][:], AF.Square, extra_w=[fss], accum_out=fss[:, 0:1])
            P.rstd(fss, D, None)
            P.op('dve', lambda e: e.scalar_tensor_tensor(out=ftmp[:], in0=yacc[ti][:], scalar=fss[:, 0:1], in1=vecf[:],
                                                         op0=ALU.mult, op1=ALU.mult), r=[yacc[ti], fss, vecf], w=[ftmp])
            P.tt('dve', ftmp, ftmp[:], ftmp, ftmp[:], X1[ti], X1[ti][:], ALU.add)
            P.dma('sp', out[tok, :], ftmp[:], r=[ftmp])
        P.barrier()
        ph.close()
        P.tes = None
        if on_group is not None:
            on_group(g)
    st.close()
    P.es = old_es
    P.pre = {}


def inputs_B(inp, layer, xcur, mix):
    f = lambda a: np.ascontiguousarray(a, dtype=np.float32)
    moe = (layer % 2 == 1)
    j = layer // 2
    xf = xcur.reshape(-1, D)
    mf = mix.reshape(-1, D)
    aw = f(inp['ada_w'][layer][:, 2048:6144].reshape(128, 8, 4096))
    ab = inp['ada_b'][layer]
    shared = dict(
        adaw=aw, adab_row=f(ab[2048:6144][None, :]), adab_col=chunkT(f(ab[2048:6144]), 32),
        gpost_m=f(inp['norm_post_mix'][layer][None, :]), gpre_f=chunkT(f(inp['norm_pre_ffn'][layer]), 8),
        gpost_f=f(inp['norm_post_ffn'][layer][None, :]),
        gluw=f(inp['ssm_glu_w'][layer].reshape(2, 128, 256).transpose(1, 0, 2)),
        glub=f(inp['ssm_glu_b'][layer][None, :]), ssmg=f(inp['ssm_norm'][layer][None, :]),
        mlag=f(inp['mla_norm'][layer][None, :]),
        wout=f(inp['w_out'][layer].reshape(8, 128, D).transpose(1, 0, 2)))
    if moe:
        wg, wu, wd = inp['moe_w_gate'][j], inp['moe_w_up'][j], inp['moe_w_down'][j]
        shared['router'] = f(inp['moe_router'][j].reshape(8, 128, 8).transpose(1, 0, 2))
    else:
        wg, wu, wd = inp['ffn_w_gate'][j][None], inp['ffn_w_up'][j][None], inp['ffn_w_down'][j][None]
    ne = wg.shape[0]

    def gu(w):
        return f(w.reshape(ne, 8, 128, NFC, 128).transpose(0, 3, 2, 1, 4).reshape(ne * NFC, 128, 1024))

    shared['wg'] = gu(wg)
    shared['wu'] = gu(wu)
    shared['wd'] = f(wd.reshape(ne * NFC, 128, D))
    maps = []
    for core in range(8):
        b = core // 4
        m = dict(shared)
        m['x'] = f(xf[core * TPC:(core + 1) * TPC])
        m['mix'] = f(mf[core * TPC:(core + 1) * TPC])
        m['cvec'] = f(inp['c'][b].reshape(128, 8))
        maps.append(m)
    return maps


RG4 = [[0, 1, 2, 3], [4, 5, 6, 7]]


def build_fused(ntiles=NT, ngroups=NG):
    nc = bass.Bass("TRN2", target_bir_lowering=False)
    ioA = [decl_A(nc, '_a%d' % l) for l in range(2)]
    ioB = [decl_B(nc, '_b%d' % l, moe=(l == 1)) for l in range(2)]
    x = nc.dram_tensor("x", [L, D], F32, kind="ExternalInput").ap()
    xsl = nc.dram_tensor("xsl", [TPC, D], F32, kind="ExternalInput").ap()
    out = nc.dram_tensor("out", [TPC, D], F32, kind="ExternalOutput").ap()
    hx = [nc.dram_tensor("hx%d" % l, [L, 256], F32).ap() for l in range(2)]
    hxall = [nc.dram_tensor("hxall%d" % l, [8 * 4 * 1024, 256], F32).ap() for l in range(2)]
    mixloc = [nc.dram_tensor("mixloc%d" % l, [2, 4, 1024, 256], F32).ap() for l in range(2)]
    xs = nc.dram_tensor("xs", [TPC, D], F32).ap()
    xall = nc.dram_tensor("xall", [8 * 4 * 256, D], F32).ap()

    def xrow1(t):
        tok0 = t * 128
        r_, j_, i0 = tok0 // TPC, (tok0 % TPC) // 256, tok0 % 256
        return xall[(j_ * 4 + r_) * 256 + i0:(j_ * 4 + r_) * 256 + i0 + 128, :]

    with ExitStack() as es:
        P = Prog(nc, es)
        banks = [P.ps([128, 512], F32, 'bank%d' % i) for i in range(8)]
        rank256 = nc.sync.snap((nc.sync.partition_id() % 4) * 256, min_val=0, max_val=768)

        H = [P.view(hxall[l], 'HXALL%d' % l) for l in range(2)]
        XALL = P.view(xall, 'XALL')

        def hx_chunk(l):
            def f(k, evs):
                for ev in evs:
                    P._wait('pool', ev)
                P.coll('AllGather', hx[l][k * 1024:(k + 1) * 1024, :], H[l], hxall[l][k * 4096:(k + 1) * 4096, :], RG4)
            return f

        def xs_group(g):
            for j in (2 * g, 2 * g + 1):
                P.coll('AllGather', xs[j * 256:(j + 1) * 256, :], XALL, xall[j * 1024:(j + 1) * 1024, :], RG4)

        emit_A(P, nc, banks, ioA[0], lambda t: x[t * 128:(t + 1) * 128, :], hx[0], ntiles, on_chunk=hx_chunk(0))
        emit_B(P, nc, banks, ioB[0], xsl, hxall[0], H[0], mixloc[0], xs, rank256, False, ngroups, on_group=xs_group)
        emit_A(P, nc, banks, ioA[1], xrow1, hx[1], ntiles, xdep=XALL, on_chunk=hx_chunk(1))
        emit_B(P, nc, banks, ioB[1], xs, hxall[1], H[1], mixloc[1], out, rank256, True, ngroups)
        P.finish()
    return nc


def fused_inputs(inp):
    x = np.ascontiguousarray(inp['x'], dtype=np.float32)
    dummy = np.zeros((2, L, D), np.float32)
    maps = [dict() for _ in range(8)]
    for layer in range(2):
        ma = inputs_A(inp, layer, x)
        mb = inputs_B(inp, layer, x, dummy)
        for c in range(8):
            for k, v in ma[c].items():
                if k != 'x':
                    maps[c][k + '_a%d' % layer] = v
            for k, v in mb[c].items():
                if k not in ('x', 'mix'):
                    maps[c][k + '_b%d' % layer] = v
    xf = x.reshape(-1, D)
    for c in range(8):
        maps[c]['x'] = x[c // 4]
        maps[c]['xsl'] = np.ascontiguousarray(xf[c * TPC:(c + 1) * TPC])
    return maps


def kernel(**inputs):
    inp = {k: np.asarray(v) for k, v in inputs.items()}
    if 'F' not in _CACHE:
        _CACHE['F'] = build_fused()
    res = run_bass_kernel_spmd(_CACHE['F'], fused_inputs(inp), core_ids=list(range(8)))
    return np.concatenate([res.results[c]['out'] for c in range(8)], axis=0).reshape(2, L, D).astype(np.float32)
```

```python
import math
from contextlib import ExitStack

import numpy as np
import concourse.bass as bass
import concourse.mybir as mybir
from concourse.bass_utils import run_bass_kernel_spmd

F32 = mybir.dt.float32
BF16 = mybir.dt.bfloat16
I32 = mybir.dt.int32
ALU = mybir.AluOpType
AF = mybir.ActivationFunctionType
AX = mybir.AxisListType

D = 1024
L = 8192
NT = 64
EPS = 1e-6
TWO_PI = 2.0 * math.pi
D_FF = 3584
NFC = 28
N_EXP = 8


class T:
    def __init__(self, t, name, root=None):
        self.t = t
        self.name = name
        self.lastw = None
        self.readers = {}
        self.root = self if root is None else root.root
        self.excl = False

    def __getitem__(self, idx):
        return self.t[idx]


class Prog:
    def __init__(self, nc, es, ndma_sems=8):
        self.nc = nc
        self.es = es
        self.engs = {'pe': nc.tensor, 'act': nc.scalar, 'dve': nc.vector, 'pool': nc.gpsimd, 'sp': nc.sync}
        self.sem = {k: es.enter_context(nc.semaphore('s_' + k)) for k in self.engs}
        self.cnt = {k: 0 for k in self.engs}
        self.waited = {k: {} for k in self.engs}
        self.dma_sems = {}
        for q in ('sp', 'pool'):
            self.dma_sems[q] = [[es.enter_context(nc.semaphore('d_%s%d' % (q, i))), 0] for i in range(ndma_sems)]
        self.dma_i = {q: 0 for q in self.dma_sems}
        self.cc_sem = es.enter_context(nc.semaphore('cc_sem'))
        self.cc_n = 0
        self.nbuf = 0
        self.tes = None
        self.pre = {}

    def sb(self, shape, dt, name=None, keep=False):
        if keep and name in self.pre:
            assert list(self.pre[name].t.shape) == list(shape), name
            return self.pre[name]
        assert not (keep and self.tes is not None), "persistent buffer %s must be pre-allocated" % name
        self.nbuf += 1
        uname = (name or 'b') + '_%d' % self.nbuf
        es = self.es if (keep or self.tes is None) else self.tes
        t = T(es.enter_context(self.nc.sbuf_tensor(uname, shape, dt)), uname)
        if keep:
            self.pre[name] = t
        return t

    def ps(self, shape, dt, name=None):
        self.nbuf += 1
        name = name or ('p%d' % self.nbuf)
        t = T(self.es.enter_context(self.nc.psum_tensor(name, shape, dt)), name)
        t.excl = True
        return t

    def view(self, ap, name='v', parent=None):
        return T(ap, name, root=parent)

    def _wait(self, engname, ev):
        sem, val, src = ev
        if src == 'pe' and engname == 'pe':
            return
        key = id(sem)
        w = self.waited[engname]
        if w.get(key, 0) >= val:
            return
        w[key] = val
        self.engs[engname].wait_ge(sem, val)

    def _deps(self, engname, r, w):
        for b in r:
            b = b.root
            if b.lastw is not None:
                self._wait(engname, b.lastw)
        for b in w:
            b = b.root
            if b.lastw is not None:
                self._wait(engname, b.lastw)
            for ev in b.readers.values():
                self._wait(engname, ev)

    def _commit(self, ev, r, w):
        for b in r:
            b = b.root
            old = b.readers.get(id(ev[0]))
            if old is None or old[1] < ev[1]:
                b.readers[id(ev[0])] = ev
        for b in w:
            b = b.root
            b.lastw = ev
            b.readers = {}

    def op(self, engname, fn, r=(), w=()):
        xr = [b for b in r if b.root.excl]
        if xr:
            r = [b for b in r if not b.root.excl]
            w = list(w) + xr
        self._deps(engname, r, w)
        ins = fn(self.engs[engname])
        self.cnt[engname] += 1
        ins.then_inc(self.sem[engname], 1)
        ev = (self.sem[engname], self.cnt[engname], engname)
        self._commit(ev, r, w)
        return ev

    def dma(self, q, out, in_, r=(), w=(), **kw):
        if out.dtype != in_.dtype:
            q = 'pool'
        self._deps(q, r, w)
        slot = self.dma_sems[q][self.dma_i[q] % len(self.dma_sems[q])]
        self.dma_i[q] += 1
        sem, n = slot
        if n > 0:
            self._wait(q, (sem, 16 * n, 'dma'))
        slot[1] = n + 1
        self.engs[q].dma_start(out=out, in_=in_, **kw).then_inc(sem, 16)
        ev = (sem, 16 * (n + 1), 'dma')
        self._commit(ev, r, w)
        return ev

    def coll(self, kind, src_ap, dst, dst_ap, groups):
        self.nc.gpsimd.collective_compute(kind, ALU.bypass, replica_groups=groups, ins=[src_ap], outs=[dst_ap]).then_inc(self.cc_sem, 1)
        self.cc_n += 1
        ev = (self.cc_sem, self.cc_n, 'cc')
        self._commit(ev, [], [dst])
        return ev

    def barrier(self):
        for e in self.engs:
            for o in self.engs:
                if o != e and self.cnt[o] > 0:
                    self._wait(e, (self.sem[o], self.cnt[o], 'x'))
            for q in self.dma_sems:
                for sem, n in self.dma_sems[q]:
                    if n > 0:
                        self._wait(e, (sem, 16 * n, 'dma'))
            if self.cc_n > 0:
                self._wait(e, (self.cc_sem, self.cc_n, 'cc'))

    def finish(self):
        self.barrier()
        for q in self.dma_sems:
            for sem, n in self.dma_sems[q]:
                if n > 0:
                    self._wait('sp', (sem, 16 * n, 'dma'))

    def mm(self, out, oap, lhsT, lap, rhs, rap, start=True, stop=True):
        return self.op('pe', lambda e: e.matmul(oap, lhsT=lap, rhs=rap, start=start, stop=stop),
                       r=[lhsT, rhs], w=[out])

    def tr(self, out, oap, src, sap, ident):
        n = sap.shape[0]
        return self.op('pe', lambda e: e.transpose(oap, sap, ident[0:n, 0:n]), r=[src, ident], w=[out])

    def tt(self, eng, out, oap, a, aap, b, bap, op):
        return self.op(eng, lambda e: e.tensor_tensor(out=oap, in0=aap, in1=bap, op=op), r=[a, b], w=[out])

    def ts(self, eng, out, oap, a, aap, s1, op0, s2=None, op1=None, extra_r=()):
        if op1 is None:
            return self.op(eng, lambda e: e.tensor_scalar(out=oap, in0=aap, scalar1=s1, scalar2=None, op0=op0),
                           r=[a] + list(extra_r), w=[out])
        return self.op(eng, lambda e: e.tensor_scalar(out=oap, in0=aap, scalar1=s1, scalar2=s2, op0=op0, op1=op1),
                       r=[a] + list(extra_r), w=[out])

    def act(self, out, oap, a, aap, func, extra_r=(), extra_w=(), **kw):
        return self.op('act', lambda e: e.activation(out=oap, in_=aap, func=func, **kw),
                       r=[a] + list(extra_r), w=[out] + list(extra_w))

    def cp(self, eng, out, oap, a, aap):
        if eng == 'act':
            return self.act(out, oap, a, aap, AF.Copy)
        return self.op(eng, lambda e: e.tensor_copy(out=oap, in_=aap), r=[a], w=[out])

    def rstd(self, ss, n, tmp):
        self.ts('dve', ss, ss[:], ss, ss[:], 1.0 / n, ALU.mult, EPS, ALU.add)
        self.act(ss, ss[:], ss, ss[:], AF.Sqrt)
        self.op('dve', lambda e: e.reciprocal(out=ss[:], in_=ss[:]), r=[ss], w=[ss])

    def range_reduce(self, out, oap, src, sap, kf, kfap, ki, kiap, shift=0.0):
        self.ts('dve', kf, kfap, src, sap, 1.0 / TWO_PI, ALU.mult, shift / TWO_PI, ALU.add)
        self.cp('dve', ki, kiap, kf, kfap)
        self.cp('dve', kf, kfap, ki, kiap)
        self.op('dve', lambda e: e.scalar_tensor_tensor(out=oap, in0=kfap, scalar=-TWO_PI, in1=sap,
                                                        op0=ALU.mult, op1=ALU.add), r=[kf, src], w=[out])
        if shift != 0.0:
            self.ts('dve', out, oap, out, oap, shift, ALU.add)
        self.ts('dve', out, oap, out, oap, -math.pi, ALU.max, math.pi, ALU.min)


def make_stager(P, nstage=4, cols=1024):
    stg = [P.sb([128, cols], F32, 'stage%d' % i) for i in range(nstage)]
    cnt = [0]
    engs = ('dve', 'act', 'pool')

    def load(dst, dst_ap, src_ap):
        n = src_ap.shape[-1]
        sg = stg[cnt[0] % nstage]
        eng = engs[cnt[0] % 3]
        cnt[0] += 1
        P.dma('sp', sg[:, 0:n], src_ap, w=[sg])
        P.cp(eng, dst, dst_ap, sg, sg[:, 0:n])
    return load


def make_ident(P):
    identf = P.sb([128, 128], F32, 'identf')
    ident = P.sb([128, 128], BF16, 'ident', keep=True)
    P.op('pool', lambda e: e.memset(identf[:], 1.0), w=[identf])
    P.op('pool', lambda e: e.affine_select(out=identf[:], in_=identf[:], pattern=[[-1, 128]],
                                           compare_op=ALU.is_equal, fill=0.0, base=0, channel_multiplier=1),
         r=[identf], w=[identf])
    P.cp('dve', ident, ident[:], identf, identf[:])
    return ident, identf


def adaln_mod(P, nc, cvec, adaw, adab, ncol_chunks, modps, name, stager):
    cv = P.sb([128, 8], F32, name + '_cv')
    cvb = P.sb([128, 8], BF16, name + '_cvb')
    P.dma('sp', cv[:], cvec[:, :], w=[cv])
    P.act(cvb, cvb[:], cv, cv[:], AF.Silu)
    ncols = ncol_chunks * 128
    mod = P.sb([128, ncol_chunks], F32, name + '_mod', keep=True)
    ab = P.sb([128, ncol_chunks], F32, name + '_ab')
    P.dma('sp', ab[:], adab[:, :], w=[ab])
    cw = 1024
    wts = [P.sb([128, 8, cw], BF16, name + '_w%d' % i) for i in range(2)]
    for ci in range(ncols // cw):
        wt = wts[ci % 2]
        for kc in range(8):
            stager(wt, wt[:, kc, :], adaw[:, kc, ci * cw:(ci + 1) * cw])
        for cc in range(cw // 128):
            ch = ci * (cw // 128) + cc
            for kc in range(8):
                P.mm(modps, modps[:, ch:ch + 1], wt, wt[:, kc, cc * 128:(cc + 1) * 128], cvb, cvb[:, kc:kc + 1],
                     start=(kc == 0), stop=(kc == 7))
    P.tt('dve', mod, mod[:], modps, modps[:, 0:ncol_chunks], ab, ab[:], ALU.add)
    return mod


import os
STOP_AT = float(os.environ.get('STOP_AT', '99'))


def decl_A(nc, sfx):
    io = {}
    io['cvec'] = nc.dram_tensor('cvec' + sfx, [128, 8], F32, kind='ExternalInput').ap()
    io['pos'] = nc.dram_tensor('pos' + sfx, [128, NT], I32, kind='ExternalInput').ap()
    io['adaw'] = nc.dram_tensor('adaw' + sfx, [128, 8, 2048], F32, kind='ExternalInput').ap()
    io['adab'] = nc.dram_tensor('adab' + sfx, [128, 16], F32, kind='ExternalInput').ap()
    io['gpre'] = nc.dram_tensor('gpre' + sfx, [128, 8], F32, kind='ExternalInput').ap()
    io['wsel'] = nc.dram_tensor('wsel' + sfx, [128, 8, 768], F32, kind='ExternalInput').ap()
    io['retg'] = nc.dram_tensor('retg' + sfx, [1, 64], F32, kind='ExternalInput').ap()
    io['invf'] = nc.dram_tensor('invf' + sfx, [1, 32], F32, kind='ExternalInput').ap()
    io['dmaskT_d'] = nc.dram_tensor('dmaskT' + sfx, [128, 128], F32, kind='ExternalInput').ap()
    io['tri_d'] = nc.dram_tensor('tri' + sfx, [128, 128], F32, kind='ExternalInput').ap()
    io['qdec_d'] = nc.dram_tensor('qdec' + sfx, [64, 128], F32, kind='ExternalInput').ap()
    io['kdec_d'] = nc.dram_tensor('kdec' + sfx, [128, 1], F32, kind='ExternalInput').ap()
    io['dcy_d'] = nc.dram_tensor('dcy' + sfx, [64, 1], F32, kind='ExternalInput').ap()
    io['cmask_d'] = nc.dram_tensor('cmask' + sfx, [128, 128], F32, kind='ExternalInput').ap()
    io['jp1_d'] = nc.dram_tensor('jp1' + sfx, [128, 1], F32, kind='ExternalInput').ap()
    io['irow1_d'] = nc.dram_tensor('irow1' + sfx, [128, 128], F32, kind='ExternalInput').ap()
    io['are_tm_d'] = nc.dram_tensor('are_tm' + sfx, [1, 512], F32, kind='ExternalInput').ap()
    io['aim_tm_d'] = nc.dram_tensor('aim_tm' + sfx, [1, 512], F32, kind='ExternalInput').ap()
    io['ldt_tm_d'] = nc.dram_tensor('ldt_tm' + sfx, [1, 512], F32, kind='ExternalInput').ap()
    io['BR_d'] = nc.dram_tensor('BR' + sfx, [64, 512], F32, kind='ExternalInput').ap()
    io['BI_d'] = nc.dram_tensor('BI' + sfx, [64, 512], F32, kind='ExternalInput').ap()
    io['are_sm_d'] = nc.dram_tensor('are_sm' + sfx, [128, 2], F32, kind='ExternalInput').ap()
    io['aim_sm_d'] = nc.dram_tensor('aim_sm' + sfx, [128, 2], F32, kind='ExternalInput').ap()
    io['ldt_sm_d'] = nc.dram_tensor('ldt_sm' + sfx, [128, 2], F32, kind='ExternalInput').ap()
    io['CR_d'] = nc.dram_tensor('CR' + sfx, [128, 2, 64], F32, kind='ExternalInput').ap()
    io['CI_d'] = nc.dram_tensor('CI' + sfx, [128, 2, 64], F32, kind='ExternalInput').ap()
    io['DD_d'] = nc.dram_tensor('DD' + sfx, [64, 64], F32, kind='ExternalInput').ap()
    io['qng_d'] = nc.dram_tensor('qng' + sfx, [128, 2], F32, kind='ExternalInput').ap()
    io['wuq_d'] = nc.dram_tensor('wuq' + sfx, [128, 2, 192], F32, kind='ExternalInput').ap()
    io['kvng_d'] = nc.dram_tensor('kvng' + sfx, [128, 1], F32, kind='ExternalInput').ap()
    io['wukv_d'] = nc.dram_tensor('wukv' + sfx, [128, 256], F32, kind='ExternalInput').ap()
    return io


def emit_A(P, nc, banks, io, xrow, hx, ntiles=NT, xdep=None, on_chunk=None):
    cvec = io['cvec']
    pos = io['pos']
    adaw = io['adaw']
    adab = io['adab']
    gpre = io['gpre']
    wsel = io['wsel']
    retg = io['retg']
    invf = io['invf']
    dmaskT_d = io['dmaskT_d']
    tri_d = io['tri_d']
    qdec_d = io['qdec_d']
    kdec_d = io['kdec_d']
    dcy_d = io['dcy_d']
    cmask_d = io['cmask_d']
    jp1_d = io['jp1_d']
    irow1_d = io['irow1_d']
    are_tm_d = io['are_tm_d']
    aim_tm_d = io['aim_tm_d']
    ldt_tm_d = io['ldt_tm_d']
    BR_d = io['BR_d']
    BI_d = io['BI_d']
    are_sm_d = io['are_sm_d']
    aim_sm_d = io['aim_sm_d']
    ldt_sm_d = io['ldt_sm_d']
    CR_d = io['CR_d']
    CI_d = io['CI_d']
    DD_d = io['DD_d']
    qng_d = io['qng_d']
    wuq_d = io['wuq_d']
    kvng_d = io['kvng_d']
    wukv_d = io['wukv_d']
    st = ExitStack()
    old_es = P.es
    P.es = st
    P.pre = {}
    b7 = banks[7][:, :].bitcast(BF16)
    TB = [P.view(b7[:, 0:512], 'TB', parent=banks[7])]
    TS = [P.view(b7[:, 256 + 128 * k:384 + 128 * k], 'TS%d' % k, parent=banks[7]) for k in range(2)]
    PJ0 = P.view(banks[0][:, :], 'PJ0', parent=banks[0])
    PJ1 = P.view(banks[1][:, 0:256], 'PJ1', parent=banks[1])
    RS = P.view(banks[1][:, 256:384], 'RS', parent=banks[1])
    RO = P.view(banks[1][:, 384:448], 'RO', parent=banks[1])
    RKV = P.view(banks[1][0:64, 448:512], 'RKV', parent=banks[1])
    SA = P.view(banks[2][:, :], 'SA', parent=banks[2])
    SB = P.view(banks[3][:, :], 'SB', parent=banks[3])
    QTN = P.view(banks[4][:, 0:128], 'QTN', parent=banks[4])
    KTN = P.view(banks[4][:, 128:256], 'KTN', parent=banks[4])
    VP = P.view(banks[4][:, 256:384], 'VP', parent=banks[4])
    QRP = P.view(banks[4][:, 384:448], 'QRP', parent=banks[4])
    SY = P.view(banks[4][:, 448:512], 'SY', parent=banks[4])
    SC = [P.view(banks[5][:, :], 'SC0', parent=banks[5]), P.view(banks[6][:, :], 'SC1', parent=banks[6])]

    for nm, shp, dt_ in [('ident', [128, 128], BF16), ('ada_mod', [128, 16], F32), ('scale1', [128, 8], F32),
                         ('wselb', [128, 8, 768], BF16), ('retg_t', [128, 64], F32), ('dmaskT', [128, 128], F32),
                         ('tri', [128, 128], BF16), ('qdec', [64, 128], F32), ('kdec', [128, 1], F32),
                         ('dcy', [64, 1], F32), ('cmask', [128, 128], F32), ('SINT', [128, NT * 32], F32),
                         ('COST', [128, NT * 32], F32), ('NSINT', [128, NT * 32], F32), ('EA', [128, 512], F32),
                         ('EB', [128, 512], F32), ('Bmat', [64, 512], BF16), ('Bswp', [64, 512], BF16),
                         ('EA2', [128, 512], F32), ('EB2', [128, 512], F32), ('Cmat', [128, 4, 64], BF16),
                         ('DDb', [64, 64], BF16), ('xprev', [128, 4], F32), ('xprevs', [128, 4], F32),
                         ('wuqb', [128, 2, 192], BF16), ('wukvb', [128, 256], BF16)]:
        P.sb(shp, dt_, nm, keep=True)
    tes = ExitStack()
    P.tes = tes
    ident, identf = make_ident(P)

    stager = make_stager(P)
    mod = adaln_mod(P, nc, cvec, adaw, adab, 16, SA, 'ada', stager)
    gp = P.sb([128, 8], F32, 'gp')
    P.dma('sp', gp[:], gpre[:, :], w=[gp])
    scale1 = P.sb([128, 8], F32, 'scale1', keep=True)
    P.ts('dve', scale1, scale1[:], mod, mod[:, 8:16], 1.0, ALU.add)
    P.tt('dve', scale1, scale1[:], scale1, scale1[:], gp, gp[:], ALU.mult)
    shm = P.view(mod[:, 0:8], 'shm', parent=mod)

    wselb = P.sb([128, 8, 768], BF16, 'wselb', keep=True)
    for kc in range(8):
        stager(wselb, wselb[:, kc, :], wsel[:, kc, :])

    def load(name, src, shape, dt=F32, q='sp', bcast=None, keep=False):
        t = P.sb(shape, dt, name, keep=keep)
        P.dma(q, t[:], src if bcast is None else src.broadcast_to(bcast), w=[t])
        return t

    retg_t = load('retg_t', retg[0:1, :], [128, 64], bcast=[128, 64], keep=True)
    invf_t = load('invf_t', invf[0:1, :], [128, 32], bcast=[128, 32])
    dmaskT = load('dmaskT', dmaskT_d[:, :], [128, 128], keep=True)
    tri_f = load('tri_f', tri_d[:, :], [128, 128])
    tri = P.sb([128, 128], BF16, 'tri', keep=True)
    P.cp('dve', tri, tri[:], tri_f, tri_f[:])
    qdec = load('qdec', qdec_d[:, :], [64, 128], keep=True)
    kdec = load('kdec', kdec_d[:, :], [128, 1], keep=True)
    dcy = load('dcy', dcy_d[:, :], [64, 1], keep=True)
    cmask = load('cmask', cmask_d[:, :], [128, 128], keep=True)
    jp1 = load('jp1', jp1_d[:, :], [128, 1])
    irow1 = load('irow1', irow1_d[:, :], [128, 128])

    posi = load('posi', pos[:, :], [128, NT], I32)
    posf = P.sb([128, NT], F32, 'posf')
    P.cp('dve', posf, posf[:], posi, posi[:])
    NR = NT * 32
    ang = P.sb([128, NR], F32, 'ang')
    P.tt('dve', ang, ang[:].rearrange("p (t f) -> p t f", f=32), posf,
         posf[:].unsqueeze(2).broadcast_to([128, NT, 32]), invf_t,
         invf_t[:].unsqueeze(1).broadcast_to([128, NT, 32]), ALU.mult)
    SINT = P.sb([128, NR], F32, 'SINT', keep=True)
    COST = P.sb([128, NR], F32, 'COST', keep=True)
    NSINT = P.sb([128, NR], F32, 'NSINT', keep=True)
    rkf = P.sb([128, NR], F32, 'rkf')
    rki = P.sb([128, NR], I32, 'rki')
    P.range_reduce(SINT, SINT[:], ang, ang[:], rkf, rkf[:], rki, rki[:])
    P.act(SINT, SINT[:], SINT, SINT[:], AF.Sin)
    P.range_reduce(COST, COST[:], ang, ang[:], rkf, rkf[:], rki, rki[:], shift=math.pi / 2)
    P.act(COST, COST[:], COST, COST[:], AF.Sin)
    P.ts('dve', NSINT, NSINT[:], SINT, SINT[:], -1.0, ALU.mult)

    are_tm = load('are_tm', are_tm_d[0:1, :], [128, 512], bcast=[128, 512])
    aim_tm = load('aim_tm', aim_tm_d[0:1, :], [128, 512], bcast=[128, 512])
    dt_tm = load('dt_tm', ldt_tm_d[0:1, :], [128, 512], bcast=[128, 512])
    P.act(dt_tm, dt_tm[:], dt_tm, dt_tm[:], AF.Exp)
    rho = P.sb([128, 512], F32, 'rho')
    tht = P.sb([128, 512], F32, 'tht')
    P.tt('dve', rho, rho[:], are_tm, are_tm[:], dt_tm, dt_tm[:], ALU.mult)
    P.tt('dve', tht, tht[:], aim_tm, aim_tm[:], dt_tm, dt_tm[:], ALU.mult)
    s5a = P.sb([128, 512], F32, 's5a')
    s5b = P.sb([128, 512], F32, 's5b')
    s5c = P.sb([128, 512], F32, 's5c')
    s5k = P.sb([128, 512], F32, 's5k')
    s5i = P.sb([128, 512], I32, 's5i')
    EA = P.sb([128, 512], F32, 'EA', keep=True)
    EB = P.sb([128, 512], F32, 'EB', keep=True)
    njp1 = P.sb([128, 1], F32, 'njp1')
    P.ts('dve', njp1, njp1[:], jp1, jp1[:], -1.0, ALU.mult)
    P.ts('dve', s5a, s5a[:], rho, rho[:], njp1[:, 0:1], ALU.mult, extra_r=[njp1])
    P.act(s5a, s5a[:], s5a, s5a[:], AF.Exp)
    P.ts('dve', s5b, s5b[:], tht, tht[:], jp1[:, 0:1], ALU.mult, extra_r=[jp1])
    P.range_reduce(s5c, s5c[:], s5b, s5b[:], s5k, s5k[:], s5i, s5i[:], shift=math.pi / 2)
    P.act(s5c, s5c[:], s5c, s5c[:], AF.Sin)
    P.tt('dve', EA, EA[:], s5a, s5a[:], s5c, s5c[:], ALU.mult)
    P.range_reduce(s5c, s5c[:], s5b, s5b[:], s5k, s5k[:], s5i, s5i[:])
    P.act(s5c, s5c[:], s5c, s5c[:], AF.Sin)
    P.tt('dve', EB, EB[:], s5a, s5a[:], s5c, s5c[:], ALU.mult)
    P.ts('dve', EB, EB[:], EB, EB[:], -1.0, ALU.mult)
    Lre = P.sb([128, 512], F32, 'Lre')
    Lim = P.sb([128, 512], F32, 'Lim')
    P.act(s5a, s5a[:], rho, rho[:], AF.Exp)
    P.range_reduce(s5c, s5c[:], tht, tht[:], s5k, s5k[:], s5i, s5i[:], shift=math.pi / 2)
    P.act(s5c, s5c[:], s5c, s5c[:], AF.Sin)
    P.tt('dve', Lre, Lre[:], s5a, s5a[:], s5c, s5c[:], ALU.mult)
    P.range_reduce(s5c, s5c[:], tht, tht[:], s5k, s5k[:], s5i, s5i[:])
    P.act(s5c, s5c[:], s5c, s5c[:], AF.Sin)
    P.tt('dve', Lim, Lim[:], s5a, s5a[:], s5c, s5c[:], ALU.mult)
    P.ts('dve', Lre, Lre[:], Lre, Lre[:], -1.0, ALU.add)
    P.tt('dve', s5a, s5a[:], are_tm, are_tm[:], are_tm, are_tm[:], ALU.mult)
    P.tt('dve', s5b, s5b[:], aim_tm, aim_tm[:], aim_tm, aim_tm[:], ALU.mult)
    P.tt('dve', s5a, s5a[:], s5a, s5a[:], s5b, s5b[:], ALU.add)
    P.op('dve', lambda e: e.reciprocal(out=s5a[:], in_=s5a[:]), r=[s5a], w=[s5a])
    Fre = P.sb([128, 512], F32, 'Fre')
    Fim = P.sb([128, 512], F32, 'Fim')
    P.tt('dve', s5b, s5b[:], Lre, Lre[:], are_tm, are_tm[:], ALU.mult)
    P.tt('dve', s5c, s5c[:], Lim, Lim[:], aim_tm, aim_tm[:], ALU.mult)
    P.tt('dve', s5b, s5b[:], s5b, s5b[:], s5c, s5c[:], ALU.add)
    P.tt('dve', Fre, Fre[:], s5b, s5b[:], s5a, s5a[:], ALU.mult)
    P.tt('dve', s5b, s5b[:], Lim, Lim[:], are_tm, are_tm[:], ALU.mult)
    P.tt('dve', s5c, s5c[:], Lre, Lre[:], aim_tm, aim_tm[:], ALU.mult)
    P.tt('dve', s5b, s5b[:], s5b, s5b[:], s5c, s5c[:], ALU.subtract)
    P.tt('dve', Fim, Fim[:], s5b, s5b[:], s5a, s5a[:], ALU.mult)
    BRt = load('BRt', BR_d[:, :], [64, 512])
    BIt = load('BIt', BI_d[:, :], [64, 512])
    bre = P.sb([64, 512], F32, 'bre')
    bim = P.sb([64, 512], F32, 'bim')
    btmp = P.sb([64, 512], F32, 'btmp')
    P.tt('dve', bre, bre[:], Fre, Fre[0:64, :], BRt, BRt[:], ALU.mult)
    P.tt('dve', btmp, btmp[:], Fim, Fim[0:64, :], BIt, BIt[:], ALU.mult)
    P.tt('dve', bre, bre[:], bre, bre[:], btmp, btmp[:], ALU.subtract)
    P.tt('dve', bim, bim[:], Fre, Fre[0:64, :], BIt, BIt[:], ALU.mult)
    P.tt('dve', btmp, btmp[:], Fim, Fim[0:64, :], BRt, BRt[:], ALU.mult)
    P.tt('dve', bim, bim[:], bim, bim[:], btmp, btmp[:], ALU.add)
    Bmat = P.sb([64, 512], BF16, 'Bmat', keep=True)
    Bswp = P.sb([64, 512], BF16, 'Bswp', keep=True)

    def v4(t, rows=128):
        return t[0:rows, :].rearrange("p (a r q) -> p a r q", a=2, r=2)

    P.cp('dve', Bmat, v4(Bmat, 64)[:, :, 0, :], bre, v4(bre, 64)[:, :, 0, :])
    P.cp('dve', Bmat, v4(Bmat, 64)[:, :, 1, :], bim, v4(bim, 64)[:, :, 1, :])
    P.ts('dve', Bswp, v4(Bswp, 64)[:, :, 0, :], bim, v4(bim, 64)[:, :, 0, :], -1.0, ALU.mult)
    P.cp('dve', Bswp, v4(Bswp, 64)[:, :, 1, :], bre, v4(bre, 64)[:, :, 1, :])
    are_sm = load('are_sm', are_sm_d[:, :], [128, 2])
    aim_sm = load('aim_sm', aim_sm_d[:, :], [128, 2])
    dt_sm = load('dt_sm', ldt_sm_d[:, :], [128, 2])
    P.act(dt_sm, dt_sm[:], dt_sm, dt_sm[:], AF.Exp)
    rho_sm = P.sb([128, 2], F32, 'rho_sm')
    tht_sm = P.sb([128, 2], F32, 'tht_sm')
    P.tt('dve', rho_sm, rho_sm[:], are_sm, are_sm[:], dt_sm, dt_sm[:], ALU.mult)
    P.tt('dve', tht_sm, tht_sm[:], aim_sm, aim_sm[:], dt_sm, dt_sm[:], ALU.mult)
    EA2 = P.sb([128, 512], F32, 'EA2', keep=True)
    EB2 = P.sb([128, 512], F32, 'EB2', keep=True)
    pm = P.sb([128, 256], F32, 'pm')
    pa = P.sb([128, 256], F32, 'pa')
    pc = P.sb([128, 256], F32, 'pc')
    pk = P.sb([128, 256], F32, 'pk')
    pki = P.sb([128, 256], I32, 'pki')
    for gp_ in range(2):
        sl = slice(gp_ * 128, (gp_ + 1) * 128)
        P.ts('dve', pm, pm[:, sl], irow1, irow1[:], rho_sm[:, gp_:gp_ + 1], ALU.mult, extra_r=[rho_sm])
        P.ts('dve', pa, pa[:, sl], irow1, irow1[:], tht_sm[:, gp_:gp_ + 1], ALU.mult, extra_r=[tht_sm])
    P.act(pm, pm[:], pm, pm[:], AF.Exp)
    P.range_reduce(pc, pc[:], pa, pa[:], pk, pk[:], pki, pki[:], shift=math.pi / 2)
    P.act(pc, pc[:], pc, pc[:], AF.Sin)
    P.tt('dve', pc, pc[:], pc, pc[:], pm, pm[:], ALU.mult)
    pc3 = pc[:].rearrange("p (a i) -> p a i", a=2)
    P.cp('dve', EA2, v4(EA2)[:, :, 0, :], pc, pc3)
    P.cp('dve', EA2, v4(EA2)[:, :, 1, :], pc, pc3)
    P.range_reduce(pc, pc[:], pa, pa[:], pk, pk[:], pki, pki[:])
    P.act(pc, pc[:], pc, pc[:], AF.Sin)
    P.tt('dve', pc, pc[:], pc, pc[:], pm, pm[:], ALU.mult)
    P.ts('dve', EB2, v4(EB2)[:, :, 0, :], pc, pc3, -1.0, ALU.mult)
    P.cp('dve', EB2, v4(EB2)[:, :, 1, :], pc, pc3)
    CRt = load('CRt', CR_d[:, :, :], [128, 2, 64])
    CIt = load('CIt', CI_d[:, :, :], [128, 2, 64])
    Cmat = P.sb([128, 4, 64], BF16, 'Cmat', keep=True)
    for gp_ in range(2):
        P.cp('dve', Cmat, Cmat[:, gp_ * 2, :], CRt, CRt[:, gp_, :])
        P.ts('dve', Cmat, Cmat[:, gp_ * 2 + 1, :], CIt, CIt[:, gp_, :], -1.0, ALU.mult)
    DDt = load('DDt', DD_d[:, :], [64, 64])
    DDb = P.sb([64, 64], BF16, 'DDb', keep=True)
    P.cp('dve', DDb, DDb[:], DDt, DDt[:])
    xprev = P.sb([128, 4], F32, 'xprev', keep=True)
    xprevs = P.sb([128, 4], F32, 'xprevs', keep=True)
    P.op('pool', lambda e: e.memset(xprev[:], 0.0), w=[xprev])
    P.op('pool', lambda e: e.memset(xprevs[:], 0.0), w=[xprevs])

    qng = load('qng', qng_d[:, :], [128, 2])
    wuq_f = load('wuq_f', wuq_d[:, :, :], [128, 2, 192])
    wuqb = P.sb([128, 2, 192], BF16, 'wuqb', keep=True)
    for kc in range(2):
        P.ts('dve', wuqb, wuqb[:, kc, :], wuq_f, wuq_f[:, kc, :], qng[:, kc:kc + 1], ALU.mult, extra_r=[qng])
    kvng = load('kvng', kvng_d[:, :], [128, 1])
    wukv_f = load('wukv_f', wukv_d[:, :], [128, 256])
    wukvb = P.sb([128, 256], BF16, 'wukvb', keep=True)
    P.ts('dve', wukvb, wukvb[:], wukv_f, wukv_f[:], kvng[:, 0:1], ALU.mult, extra_r=[kvng])

    P.barrier()
    tes.close()
    P.tes = None
    KTNP_t = P.es.enter_context(nc.sbuf_tensor('KTNP_%d' % id(P.es), [128, L], BF16))
    KTR_t = P.es.enter_context(nc.sbuf_tensor('KTR_%d' % id(P.es), [64, L], BF16))
    VV_t = P.es.enter_context(nc.sbuf_tensor('VV_%d' % id(P.es), [128, NT, 128], BF16))
    KTNP = [P.view(KTNP_t[:, t * 128:(t + 1) * 128], 'ktn%d' % t) for t in range(NT)]
    KTR = [P.view(KTR_t[:, t * 128:(t + 1) * 128], 'ktr%d' % t) for t in range(NT)]
    VV = [P.view(VV_t[:, t, :], 'vv%d' % t) for t in range(NT)]
    SROW_t = P.es.enter_context(nc.sbuf_tensor('SROW_%d' % id(P.es), [128, L], F32))
    SROW = [P.view(SROW_t, 'srow0'), P.view(SROW_t, 'srow1')]
    PB = P.sb([128, L], BF16, 'PB')
    state = P.sb([64, 64], F32, 'state')
    state_bf = P.sb([64, 64], BF16, 'state_bf')
    P.op('pool', lambda e: e.memset(state[:], 0.0), w=[state])
    P.op('pool', lambda e: e.memset(state_bf[:], 0.0), w=[state_bf])

    def dbl(shape, dt, name):
        return [P.sb(shape, dt, name + '%d' % i) for i in range(2)]

    xt = dbl([128, D], F32, 'xt')
    xnb = dbl([128, D], BF16, 'xnb')
    junk = P.sb([128, D], BF16, 'junk')
    ssx = dbl([128, 1], F32, 'ssx')
    hT = dbl([128, 8, 128], BF16, 'hT')
    ropeA = P.sb([128, 192], F32, 'ropeA')
    ropeB = P.sb([128, 192], F32, 'ropeB')
    rqk = dbl([128, 192], BF16, 'rqk')
    QTs = dbl([64, 128], BF16, 'QTs')
    KTs = dbl([64, 128], BF16, 'KTs')
    QTd = dbl([64, 128], BF16, 'QTd')
    vb = dbl([128, 64], BF16, 'vb')
    vdec = dbl([128, 64], BF16, 'vdec')
    smask = dbl([128, 128], BF16, 'smask')
    ssr = dbl([128, 1], F32, 'ssr')
    gs = dbl([128, 64], F32, 'gs')
    ro = dbl([128, 64], F32, 'ro')
    junk64 = P.sb([128, 256], F32, 'junk64')
    ub = dbl([128, 64], BF16, 'ub')
    UTs = dbl([64, 128], BF16, 'UTs')
    T1 = P.sb([128, 512], F32, 'T1')
    T2 = P.sb([128, 512], F32, 'T2')
    Wb = dbl([128, 512], BF16, 'Wb')
    Zf = T1
    Zsf = T2
    Xf = P.sb([128, 512], F32, 'Xf')
    Xb = dbl([128, 512], BF16, 'Xb')
    g1 = P.sb([128, 64], F32, 'g1')
    g2 = P.sb([128, 64], F32, 'g2')
    so = dbl([128, 64], F32, 'so')
    ssq = dbl([128, 1], F32, 'ssq')
    sskv = dbl([128, 1], F32, 'sskv')
    cqn = dbl([128, 256], BF16, 'cqn')
    ckvn = dbl([128, 128], BF16, 'ckvn')
    cqnT = dbl([128, 2, 128], BF16, 'cqnT')
    ckvnT = dbl([128, 128], BF16, 'ckvnT')
    QTNs = dbl([128, 128], BF16, 'QTNs')
    qrA = P.sb([128, 64], F32, 'qrA')
    qrB = P.sb([128, 64], F32, 'qrB')
    qrb = dbl([128, 64], BF16, 'qrb')
    QrTs = dbl([64, 128], BF16, 'QrTs')
    mrow = dbl([128, 1], F32, 'mrow')
    acc4 = dbl([128, 4], F32, 'acc4')
    rsum = dbl([128, 1], F32, 'rsum')
    PT = dbl([128, 512], BF16, 'PT')
    ao = dbl([128, 128], F32, 'ao')
    SM_SCALE = 192.0 ** -0.5
    ts_i = [0]

    def ts_slot():
        s = TS[ts_i[0] % 2]
        ts_i[0] += 1
        return s

    tb_i = [0]
    ev_i = [0]

    def evac_eng():
        ev_i[0] += 1
        return 'act' if ev_i[0] % 2 == 0 else 'dve'

    PVO = P.view(banks[5][:, 0:128], 'PVO', parent=banks[5])
    TBF = [P.view(banks[7][:, :].bitcast(BF16), 'TBF0', parent=banks[7]), P.view(banks[6][:, :].bitcast(BF16), 'TBF1', parent=banks[6])]
    PT8 = dbl([128, 1024], BF16, 'PT8')

    def frontend(t):
        p = t % 2
        P.dma('sp', xt[p][:], xrow(t), r=([xdep] if xdep is not None else []), w=[xt[p]])
        P.op('dve', lambda e: e.memset(ssx[p][:], 0.0), w=[ssx[p]])
        P.act(junk, junk[:], xt[p], xt[p][:], AF.Square, extra_w=[ssx[p]], accum_out=ssx[p][:])
        yield
        P.rstd(ssx[p], D, None)
        P.ts('dve', xnb[p], xnb[p][:], xt[p], xt[p][:], ssx[p][:, 0:1], ALU.mult, extra_r=[ssx[p]])
        yield
        for half in range(2):
            tb = TB[0]
            for c4 in range(4):
                c = half * 4 + c4
                P.tr(tb, tb[:, c4 * 128:(c4 + 1) * 128], xnb[p], xnb[p][:, c * 128:(c + 1) * 128], ident)
            for c4 in range(4):
                c = half * 4 + c4
                if c4 % 2 == 0:
                    P.act(hT[p], hT[p][:, c, :], tb, tb[:, c4 * 128:(c4 + 1) * 128], AF.Identity,
                          extra_r=[scale1, shm], scale=scale1[:, c:c + 1], bias=shm[:, c:c + 1])
                else:
                    P.ts('dve', hT[p], hT[p][:, c, :], tb, tb[:, c4 * 128:(c4 + 1) * 128],
                         scale1[:, c:c + 1], ALU.mult, shm[:, c:c + 1], ALU.add, extra_r=[scale1, shm])
            yield

    def projheads(t):
        projection(t)
        yield
        for _ in heads(t):
            yield

    def projection(t):
        p = t % 2
        for kc in range(8):
            P.mm(PJ0, PJ0[:, :], hT[p], hT[p][:, kc, :], wselb, wselb[:, kc, 0:512], start=(kc == 0), stop=(kc == 7))
        for kc in range(8):
            P.mm(PJ1, PJ1[:, :], hT[p], hT[p][:, kc, :], wselb, wselb[:, kc, 512:768], start=(kc == 0), stop=(kc == 7))

    def heads(t):
        p = t % 2
        cos_t = COST[:, t * 32:(t + 1) * 32]
        sin_t = SINT[:, t * 32:(t + 1) * 32]
        nsin_t = NSINT[:, t * 32:(t + 1) * 32]
        src4 = PJ0[:, 0:192].rearrange("p (a h f) -> p a h f", a=3, h=2)
        A4 = ropeA[:].rearrange("p (a h f) -> p a h f", a=3, h=2)
        B4 = ropeB[:].rearrange("p (a h f) -> p a h f", a=3, h=2)
        P.tt('dve', ropeA, A4, PJ0, src4, COST, cos_t.unsqueeze(1).unsqueeze(1).broadcast_to([128, 3, 2, 32]), ALU.mult)
        P.tt('dve', ropeB, B4[:, :, 0, :], PJ0, src4[:, :, 1, :], NSINT, nsin_t.unsqueeze(1).broadcast_to([128, 3, 32]), ALU.mult)
        P.tt('dve', ropeB, B4[:, :, 1, :], PJ0, src4[:, :, 0, :], SINT, sin_t.unsqueeze(1).broadcast_to([128, 3, 32]), ALU.mult)
        yield
        P.cp('act', vb[p], vb[p][:], PJ0, PJ0[:, 192:256])
        P.ts('dve', vdec[p], vdec[p][:], PJ0, PJ0[:, 192:256], kdec[:, 0:1], ALU.mult, extra_r=[kdec])
        P.act(gs[p], gs[p][:], PJ0, PJ0[:, 256:320], AF.Silu)
        P.cp('act', ub[p], ub[p][:], PJ0, PJ0[:, 320:384])
        P.op('dve', lambda e: e.memset(ssq[p][:], 0.0), w=[ssq[p]])
        P.op('dve', lambda e: e.memset(sskv[p][:], 0.0), w=[sskv[p]])
        P.act(junk64, junk64[:, 0:256], PJ1, PJ1[:, :], AF.Square, extra_w=[ssq[p]], accum_out=ssq[p][:])
        P.act(junk64, junk64[:, 0:128], PJ0, PJ0[:, 384:512], AF.Square, extra_w=[sskv[p]], accum_out=sskv[p][:])
        yield
        P.rstd(ssq[p], 256, None)
        yield
        P.rstd(sskv[p], 128, None)
        yield
        P.ts('dve', cqn[p], cqn[p][:], PJ1, PJ1[:, :], ssq[p][:, 0:1], ALU.mult, extra_r=[ssq[p]])
        P.ts('dve', ckvn[p], ckvn[p][:], PJ0, PJ0[:, 384:512], sskv[p][:, 0:1], ALU.mult, extra_r=[sskv[p]])
        P.tt('dve', rqk[p], rqk[p][:], ropeA, ropeA[:], ropeB, ropeB[:], ALU.add)
        yield

    def ret_chain(t):
        p = t % 2
        tok = slice(t * 128, (t + 1) * 128)
        s0 = ts_slot()
        P.tr(s0, s0[0:64, :], rqk[p], rqk[p][:, 0:64], ident)
        P.cp('act', QTs[p], QTs[p][:], s0, s0[0:64, :])
        yield
        P.tt('dve', QTd[p], QTd[p][:], QTs[p], QTs[p][:], qdec, qdec[:], ALU.mult)
        s1 = ts_slot()
        P.tr(s1, s1[0:64, :], rqk[p], rqk[p][:, 64:128], ident)
        P.cp('act', KTs[p], KTs[p][:], s1, s1[0:64, :])
        yield
        P.tt('dve', gs[p], gs[p][:], gs[p], gs[p][:], retg_t, retg_t[:], ALU.mult)
        P.mm(RS, RS[:, :], KTs[p], KTs[p][:], QTs[p], QTs[p][:])
        yield
        P.tt('dve', smask[p], smask[p][:], RS, RS[:, :], dmaskT, dmaskT[:], ALU.mult)
        yield
        P.mm(RO, RO[:, :], smask[p], smask[p][:], vb[p], vb[p][:], start=True, stop=False)
        P.mm(RO, RO[:, :], QTd[p], QTd[p][:], state_bf, state_bf[:], start=False, stop=True)
        P.mm(RKV, RKV[:, :], rqk[p], rqk[p][:, 64:128], vdec[p], vdec[p][:])
        yield
        P.op('dve', lambda e: e.scalar_tensor_tensor(out=state[:], in0=state[:], scalar=dcy[:, 0:1], in1=RKV[:, :],
                                                     op0=ALU.mult, op1=ALU.add), r=[state, dcy, RKV], w=[state])
        P.cp('dve', state_bf, state_bf[:], state, state[:])
        P.op('dve', lambda e: e.memset(ssr[p][:], 0.0), w=[ssr[p]])
        P.act(junk64, junk64[:, 0:64], RO, RO[:, :], AF.Square, extra_w=[ssr[p]], accum_out=ssr[p][:])
        yield
        P.rstd(ssr[p], 64, None)
        yield
        P.op('dve', lambda e: e.scalar_tensor_tensor(out=ro[p][:], in0=RO[:, :], scalar=ssr[p][:, 0:1], in1=gs[p][:],
                                                     op0=ALU.mult, op1=ALU.mult), r=[RO, ssr[p], gs[p]], w=[ro[p]])
        out_evs.append(P.dma('sp', hx[tok, 0:64], ro[p][:], r=[ro[p]]))
        yield

    def s5_chain(t):
        p = t % 2
        tok = slice(t * 128, (t + 1) * 128)
        s3 = ts_slot()
        P.tr(s3, s3[0:64, :], ub[p], ub[p][:], ident)
        P.cp('dve', UTs[p], UTs[p][:], s3, s3[0:64, :])
        yield
        P.mm(SA, SA[:, :], UTs[p], UTs[p][:], Bmat, Bmat[:])
        P.mm(SB, SB[:, :], UTs[p], UTs[p][:], Bswp, Bswp[:])
        yield
        P.tt('dve', T1, T1[:], SA, SA[:, :], EA, EA[:], ALU.mult)
        yield
        P.tt('dve', T2, T2[:], SB, SB[:, :], EB, EB[:], ALU.mult)
        yield
        P.tt('dve', Wb[p], Wb[p][:], T1, T1[:], T2, T2[:], ALU.add)
        yield
        for blk in range(4):
            P.mm(SA, SA[:, blk * 128:(blk + 1) * 128], Wb[p], Wb[p][:, blk * 128:(blk + 1) * 128], tri, tri[:])
        for blk in range(4):
            b2 = blk ^ 1
            P.mm(SB, SB[:, blk * 128:(blk + 1) * 128], Wb[p], Wb[p][:, b2 * 128:(b2 + 1) * 128], tri, tri[:])
        yield
        P.tt('dve', Zf, Zf[:].rearrange("p (a i) -> p a i", a=4), SA, SA[:, :].rearrange("p (a i) -> p a i", a=4),
             xprev, xprev[:].unsqueeze(2).broadcast_to([128, 4, 128]), ALU.add)
        yield
        P.tt('dve', Zsf, Zsf[:].rearrange("p (a i) -> p a i", a=4), SB, SB[:, :].rearrange("p (a i) -> p a i", a=4),
             xprevs, xprevs[:].unsqueeze(2).broadcast_to([128, 4, 128]), ALU.add)
        yield
        P.tt('dve', Zf, Zf[:], Zf, Zf[:], EA2, EA2[:], ALU.mult)
        yield
        P.tt('dve', Zsf, Zsf[:], Zsf, Zsf[:], EB2, EB2[:], ALU.mult)
        yield
        P.tt('dve', Xf, Xf[:], Zf, Zf[:], Zsf, Zsf[:], ALU.add)
        yield
        P.cp('act', Xb[p], Xb[p][:], Xf, Xf[:])
        X3 = Xf[:].rearrange("p (a i) -> p a i", a=4)
        P.cp('dve', xprev, xprev[:], Xf, X3[:, :, 127])
        xp3 = xprev[:].rearrange("p (a r) -> p a r", r=2)
        xs3 = xprevs[:].rearrange("p (a r) -> p a r", r=2)
        P.cp('dve', xprevs, xs3[:, :, 0], xprev, xp3[:, :, 1])
        P.cp('dve', xprevs, xs3[:, :, 1], xprev, xp3[:, :, 0])
        yield
        for blk in range(4):
            P.mm(SY, SY[:, :], Xb[p], Xb[p][:, blk * 128:(blk + 1) * 128], Cmat, Cmat[:, blk, :], start=(blk == 0), stop=False)
        P.mm(SY, SY[:, :], UTs[p], UTs[p][:], DDb, DDb[:], start=False, stop=True)
        yield
        P.act(g1, g1[:], SY, SY[:, :], AF.Square)
        yield
        P.ts('dve', g1, g1[:], g1, g1[:], 0.044715, ALU.mult, 1.0, ALU.add)
        P.tt('dve', g1, g1[:], g1, g1[:], SY, SY[:, :], ALU.mult)
        yield
        P.act(g2, g2[:], g1, g1[:], AF.Sigmoid, scale=1.5957691216057308)
        yield
        P.tt('dve', so[p], so[p][:], g2, g2[:], SY, SY[:, :], ALU.mult)
        out_evs.append(P.dma('sp', hx[tok, 64:128], so[p][:], r=[so[p]]))
        yield

    def mla_chain(t):
        p = t % 2
        tok = slice(t * 128, (t + 1) * 128)
        cos_t = COST[:, t * 32:(t + 1) * 32]
        sin_t = SINT[:, t * 32:(t + 1) * 32]
        nsin_t = NSINT[:, t * 32:(t + 1) * 32]
        s2 = ts_slot()
        P.tr(s2, s2[0:64, :], rqk[p], rqk[p][:, 128:192], ident)
        P.cp('act', KTR[t], KTR[t][:], s2, s2[0:64, :])
        yield
        for kc in range(2):
            s = ts_slot()
            P.tr(s, s[:, :], cqn[p], cqn[p][:, kc * 128:(kc + 1) * 128], ident)
            P.cp(evac_eng(), cqnT[p], cqnT[p][:, kc, :], s, s[:, :])
            yield
        s = ts_slot()
        P.tr(s, s[:, :], ckvn[p], ckvn[p][:], ident)
        P.cp(evac_eng(), ckvnT[p], ckvnT[p][:], s, s[:, :])
        yield
        for kc in range(2):
            P.mm(QTN, QTN[:, :], wuqb, wuqb[:, kc, 0:128], cqnT[p], cqnT[p][:, kc, :], start=(kc == 0), stop=(kc == 1))
        for kc in range(2):
            P.mm(QRP, QRP[:, :], cqnT[p], cqnT[p][:, kc, :], wuqb, wuqb[:, kc, 128:192], start=(kc == 0), stop=(kc == 1))
        P.mm(KTN, KTN[:, :], wukvb, wukvb[:, 0:128], ckvnT[p], ckvnT[p][:])
        P.mm(VP, VP[:, :], ckvnT[p], ckvnT[p][:], wukvb, wukvb[:, 128:256])
        yield
        P.act(QTNs[p], QTNs[p][:], QTN, QTN[:, :], AF.Copy, scale=SM_SCALE)
        q3 = QRP[:, :].rearrange("p (h f) -> p h f", h=2)
        a3 = qrA[:].rearrange("p (h f) -> p h f", h=2)
        b3 = qrB[:].rearrange("p (h f) -> p h f", h=2)
        P.tt('dve', qrA, a3, QRP, q3, COST, cos_t.unsqueeze(1).broadcast_to([128, 2, 32]), ALU.mult)
        P.tt('dve', qrB, b3[:, 0, :], QRP, q3[:, 1, :], NSINT, nsin_t, ALU.mult)
        P.tt('dve', qrB, b3[:, 1, :], QRP, q3[:, 0, :], SINT, sin_t, ALU.mult)
        P.cp('act', KTNP[t], KTNP[t][:], KTN, KTN[:, :])
        P.cp('act', VV[t], VV[t][:], VP, VP[:, :])
        yield
        P.tt('dve', qrb[p], qrb[p][:], qrA, qrA[:], qrB, qrB[:], ALU.add)
        yield
        s = ts_slot()
        P.tr(s, s[0:64, :], qrb[p], qrb[p][:], ident)
        P.act(QrTs[p], QrTs[p][:], s, s[0:64, :], AF.Copy, scale=SM_SCALE)
        yield
        Lk = (t + 1) * 128
        nkb = (Lk + 511) // 512
        for kb in range(nkb):
            n = min(512, Lk - kb * 512)
            sc = SC[kb % 2]
            kts = [KTNP[4 * kb + i_] for i_ in range(n // 128)]
            krs = [KTR[4 * kb + i_] for i_ in range(n // 128)]
            P.op('pe', lambda e: e.matmul(sc[:, 0:n], lhsT=QTNs[p][:], rhs=KTNP_t[:, kb * 512:kb * 512 + n],
                                          start=True, stop=False), r=[QTNs[p]] + kts, w=[sc])
            P.op('pe', lambda e: e.matmul(sc[:, 0:n], lhsT=QrTs[p][:], rhs=KTR_t[:, kb * 512:kb * 512 + n],
                                          start=False, stop=True), r=[QrTs[p]] + krs, w=[sc])
            yield
            srow = SROW[kb % 2]
            if kb == nkb - 1:
                if n > 128:
                    P.cp('act', srow, SROW_t[:, kb * 512:kb * 512 + n - 128], sc, sc[:, 0:n - 128])
                P.tt('dve', srow, SROW_t[:, Lk - 128:Lk], sc, sc[:, n - 128:n], cmask, cmask[:], ALU.add)
            else:
                P.cp(evac_eng(), srow, SROW_t[:, kb * 512:kb * 512 + n], sc, sc[:, 0:n])
            yield
        P.op('dve', lambda e: e.reduce_max(out=mrow[p][:], in_=SROW_t[:, 0:Lk], axis=AX.X), r=[SROW[0], SROW[1]], w=[mrow[p]])
        P.op('dve', lambda e: e.memset(acc4[p][:], 0.0), w=[acc4[p]])
        yield
        P.ts('dve', mrow[p], mrow[p][:], mrow[p], mrow[p][:], -1.0, ALU.mult)
        yield
        nch = (Lk + 2047) // 2048
        for ci in range(nch):
            c0 = ci * 2048
            c1 = min(Lk, c0 + 2048)
            P.op('act', lambda e: e.activation(out=PB[:, c0:c1], in_=SROW_t[:, c0:c1], func=AF.Exp,
                                               bias=mrow[p][:, 0:1], scale=1.0, accum_out=acc4[p][:, ci:ci + 1]),
                 r=[SROW[0], SROW[1], mrow[p]], w=[PB, acc4[p]])
            yield
        P.op('dve', lambda e: e.reduce_sum(out=rsum[p][:], in_=acc4[p][:, 0:nch], axis=AX.X), r=[acc4[p]], w=[rsum[p]])
        yield
        P.op('dve', lambda e: e.reciprocal(out=rsum[p][:], in_=rsum[p][:]), r=[rsum[p]], w=[rsum[p]])
        ng = (t + 1 + 7) // 8

        def tr_round(g):
            tbf = TBF[g % 2]
            pt = PT8[g % 2]
            blks = list(range(g * 8, min(t + 1, g * 8 + 8)))
            for blk in blks:
                P.tr(tbf, tbf[:, (blk % 8) * 128:(blk % 8 + 1) * 128], PB, PB[:, blk * 128:(blk + 1) * 128], ident)
            w_ = len(blks) * 128
            P.cp(evac_eng(), pt, pt[:, 0:w_], tbf, tbf[:, 0:w_])
            return blks, pt

        cur = tr_round(0)
        yield
        for g in range(ng):
            nxt = tr_round(g + 1) if g + 1 < ng else None
            blks, pt = cur
            for blk in blks:
                P.mm(PVO, PVO[:, :], pt, pt[:, (blk % 8) * 128:(blk % 8 + 1) * 128], VV[blk], VV[blk][:],
                     start=(blk == 0), stop=(blk == t))
            cur = nxt
            yield
        P.ts('dve', ao[p], ao[p][:], PVO, PVO[:, :], rsum[p][:, 0:1], ALU.mult, extra_r=[rsum[p]])
        out_evs.append(P.dma('sp', hx[tok, 128:256], ao[p][:], r=[ao[p]]))
        yield

    def drain(g):
        for _ in g:
            pass

    def interleave(gens):
        gens = [[g, 2 if i == 0 else 1] for i, g in enumerate(gens)]
        while gens:
            alive = []
            for g, n_ in gens:
                ok = True
                for _ in range(n_):
                    try:
                        next(g)
                    except StopIteration:
                        ok = False
                        break
                if ok:
                    alive.append([g, n_])
            gens = alive

    def step(g, n_=1):
        for _ in range(n_):
            try:
                next(g)
            except StopIteration:
                return False
        return True

    out_evs = []
    if ntiles > 0:
        drain(frontend(0))
        drain(projheads(0))
    for t in range(ntiles):
        mla = mla_chain(t)
        mla_alive = True
        others = [ret_chain(t), s5_chain(t)]
        if t + 1 < ntiles:
            others.append(frontend(t + 1))
        while others:
            if mla_alive:
                mla_alive = step(mla, 2)
            others = [g for g in others if step(g)]
        nxt = projheads(t + 1) if t + 1 < ntiles else None
        nxt_alive = nxt is not None
        while mla_alive or nxt_alive:
            if mla_alive:
                mla_alive = step(mla, 2)
            if nxt_alive:
                nxt_alive = step(nxt)
        if on_chunk is not None and (t + 1) % 8 == 0:
            on_chunk(t // 8, out_evs)
            out_evs = []
    P.barrier()
    st.close()
    P.es = old_es
    P.pre = {}


RET_GAMMA = [1.0 - 2.0 ** (-5.0 - h) for h in range(4)]


def consts_A(hg):
    g = RET_GAMMA[hg]
    lg = math.log1p(-(2.0 ** (-5.0 - hg)))
    i = np.arange(128, dtype=np.float64)
    diff = i[None, :] - i[:, None]
    dmaskT = np.where(diff >= 0, np.exp(lg * np.maximum(diff, 0.0)), 0.0) * 0.125
    tri = (diff >= 0).astype(np.float32)
    qdec = np.broadcast_to(np.exp(lg * (i + 1.0))[None, :], (64, 128))
    kdec = (np.exp(lg * (127.0 - i)) * 0.125)[:, None]
    dcy = np.full((64, 1), math.exp(lg * 128.0))
    cmask = np.where(i[None, :] <= i[:, None], 0.0, -1e30)
    inv = 10000.0 ** (-np.arange(0, 64, 2, dtype=np.float32) / 64.0)
    f = lambda a: np.ascontiguousarray(a, dtype=np.float32)
    return dict(dmaskT=f(dmaskT), tri=f(tri), qdec=f(qdec), kdec=f(kdec), dcy=f(dcy), cmask=f(cmask),
                jp1=f((i + 1.0)[:, None]), irow1=f(np.broadcast_to((i + 1.0)[None, :], (128, 128))),
                invf=f(inv[None, :]))


def chunkT(v, nch):
    return np.ascontiguousarray(v.reshape(nch, 128).T)


def inputs_A(inp, layer, xcur):
    f = lambda a: np.ascontiguousarray(a, dtype=np.float32)
    maps = []
    w_in = inp['w_in'][layer]
    for core in range(8):
        b, hg = core // 4, core % 4
        m = dict(consts_A(hg))
        m['x'] = f(xcur[b])
        m['cvec'] = f(inp['c'][b].reshape(128, 8))
        m['pos'] = np.ascontiguousarray(inp['positions'][b].reshape(NT, 128).T.astype(np.int32))
        m['adaw'] = f(inp['ada_w'][layer][:, 0:2048].reshape(128, 8, 2048))
        m['adab'] = chunkT(f(inp['ada_b'][layer][0:2048]), 16)
        m['gpre'] = chunkT(f(inp['norm_pre_mix'][layer]), 8)
        h64 = slice(hg * 64, (hg + 1) * 64)
        cols = np.concatenate([
            np.arange(0, 256)[h64], np.arange(256, 512)[h64], np.arange(1664, 1728),
            np.arange(512, 768)[h64], np.arange(768, 1024)[h64], np.arange(1024, 1280)[h64],
            np.arange(1536, 1664), np.arange(1280, 1536)])
        ws = w_in[:, cols]
        m['wsel'] = f(ws.reshape(8, 128, 768).transpose(1, 0, 2))
        m['retg'] = f(inp['ret_norm'][layer][h64][None, :])
        gs_ = slice(4 * hg, 4 * hg + 4)

        def tm(a):
            a = a.reshape(2, 1, 128)
            return f(np.broadcast_to(a, (2, 2, 128)).reshape(1, 512))

        def sm(a):
            return f(a.reshape(2, 128).T)

        are = inp['ssm_a_re'][layer][gs_]
        aim = inp['ssm_a_im'][layer][gs_]
        ldt = np.broadcast_to(inp['ssm_log_dt'][layer][gs_][:, None], (4, 64))
        m['are_tm'], m['aim_tm'], m['ldt_tm'] = tm(are), tm(aim), tm(ldt)
        m['are_sm'], m['aim_sm'], m['ldt_sm'] = sm(are), sm(aim), sm(ldt)
        BR = np.zeros((64, 2, 2, 2, 64), np.float32)
        BI = np.zeros((64, 2, 2, 2, 64), np.float32)
        CR = np.zeros((2, 64, 2, 64), np.float32)
        CI = np.zeros((2, 64, 2, 64), np.float32)
        for gl in range(4):
            gp_, g2_ = gl // 2, gl % 2
            br = inp['ssm_b_re'][layer][4 * hg + gl]
            bi = inp['ssm_b_im'][layer][4 * hg + gl]
            for ri in range(2):
                BR[gl * 16:(gl + 1) * 16, gp_, ri, g2_, :] = br.T
                BI[gl * 16:(gl + 1) * 16, gp_, ri, g2_, :] = bi.T
            cr = inp['ssm_c_re'][layer][4 * hg + gl]
            ci = inp['ssm_c_im'][layer][4 * hg + gl]
            CR[g2_, :, gp_, gl * 16:(gl + 1) * 16] = cr.T
            CI[g2_, :, gp_, gl * 16:(gl + 1) * 16] = ci.T
        m['BR'] = f(BR.reshape(64, 512))
        m['BI'] = f(BI.reshape(64, 512))
        m['CR'] = f(CR.reshape(128, 2, 64))
        m['CI'] = f(CI.reshape(128, 2, 64))
        DD = np.zeros((64, 64), np.float32)
        DD[np.arange(64), np.arange(64)] = inp['ssm_d'][layer][gs_].reshape(64)
        m['DD'] = DD
        m['qng'] = chunkT(f(inp['mla_q_norm'][layer]), 2)
        wq = inp['mla_w_uq'][layer][:, hg * 192:(hg + 1) * 192]
        m['wuq'] = f(wq.reshape(2, 128, 192).transpose(1, 0, 2))
        m['kvng'] = f(inp['mla_kv_norm'][layer][:, None])
        m['wukv'] = f(inp['mla_w_ukv'][layer][:, hg * 256:(hg + 1) * 256])
        maps.append(m)
    return maps


_CACHE = {}


TPC = 2048
GT = 4
NG = TPC // (GT * 128)


def decl_B(nc, sfx, moe):
    n_exp = N_EXP if moe else 1
    io = {}
    io['cvec'] = nc.dram_tensor('cvec' + sfx, [128, 8], F32, kind='ExternalInput').ap()
    io['adaw'] = nc.dram_tensor('adaw' + sfx, [128, 8, 4096], F32, kind='ExternalInput').ap()
    io['adab_row'] = nc.dram_tensor('adab_row' + sfx, [1, 4096], F32, kind='ExternalInput').ap()
    io['adab_col'] = nc.dram_tensor('adab_col' + sfx, [128, 32], F32, kind='ExternalInput').ap()
    io['gpost_m'] = nc.dram_tensor('gpost_m' + sfx, [1, D], F32, kind='ExternalInput').ap()
    io['gpre_f'] = nc.dram_tensor('gpre_f' + sfx, [128, 8], F32, kind='ExternalInput').ap()
    io['gpost_f'] = nc.dram_tensor('gpost_f' + sfx, [1, D], F32, kind='ExternalInput').ap()
    io['gluw_d'] = nc.dram_tensor('gluw' + sfx, [128, 2, 256], F32, kind='ExternalInput').ap()
    io['glub_d'] = nc.dram_tensor('glub' + sfx, [1, 256], F32, kind='ExternalInput').ap()
    io['ssmg_d'] = nc.dram_tensor('ssmg' + sfx, [1, 256], F32, kind='ExternalInput').ap()
    io['mlag_d'] = nc.dram_tensor('mlag' + sfx, [1, 512], F32, kind='ExternalInput').ap()
    io['wout_d'] = nc.dram_tensor('wout' + sfx, [128, 8, D], F32, kind='ExternalInput').ap()
    io['wg_d'] = nc.dram_tensor('wg' + sfx, [n_exp * NFC, 128, 1024], F32, kind='ExternalInput').ap()
    io['wu_d'] = nc.dram_tensor('wu' + sfx, [n_exp * NFC, 128, 1024], F32, kind='ExternalInput').ap()
    io['wd_d'] = nc.dram_tensor('wd' + sfx, [n_exp * NFC, 128, 1024], F32, kind='ExternalInput').ap()
    if moe:
        io['router_d'] = nc.dram_tensor('router' + sfx, [128, 8, 8], F32, kind='ExternalInput').ap()
    return io


def emit_B(P, nc, banks, io, x, hxall, HXALL, mixloc, out, rank256, moe, ngroups=NG, xdep=None, on_group=None):
    n_exp = N_EXP if moe else 1
    cvec = io['cvec']
    adaw = io['adaw']
    adab_row = io['adab_row']
    adab_col = io['adab_col']
    gpost_m = io['gpost_m']
    gpre_f = io['gpre_f']
    gpost_f = io['gpost_f']
    gluw_d = io['gluw_d']
    glub_d = io['glub_d']
    ssmg_d = io['ssmg_d']
    mlag_d = io['mlag_d']
    wout_d = io['wout_d']
    wg_d = io['wg_d']
    wu_d = io['wu_d']
    wd_d = io['wd_d']
    router_d = io.get('router_d')
    st = ExitStack()
    old_es = P.es
    P.es = st
    P.pre = {}
    b0 = banks[0][:, :].bitcast(BF16)
    TB = P.view(b0[:, 0:512], 'TB', parent=banks[0])
    ZP = P.view(banks[1][:, 0:256], 'ZP', parent=banks[1])
    LG = P.view(banks[1][:, 256:264], 'LG', parent=banks[1])
    MC = P.view(banks[1][:, 272:288], 'MC', parent=banks[1])
    YB = [P.view(banks[2][:, :], 'YA', parent=banks[2]), P.view(banks[3][:, :], 'YB', parent=banks[3])]
    GB = [P.view(banks[4][:, :], 'G0', parent=banks[4]), P.view(banks[5][:, :], 'G1', parent=banks[5])]
    UB = [P.view(banks[6][:, :], 'U0', parent=banks[6]), P.view(banks[7][:, :], 'U1', parent=banks[7])]

    ident = P.sb([128, 128], BF16, 'ident', keep=True)
    vecm = P.sb([128, D], F32, 'vecm', keep=True)
    vecf = P.sb([128, D], F32, 'vecf', keep=True)
    modc = P.sb([128, 16], F32, 'modc', keep=True)
    scale2 = P.sb([128, 8], F32, 'scale2', keep=True)
    woutb = P.sb([128, 8, D], BF16, 'woutb', keep=True)
    gluwb = P.sb([128, 2, 256], BF16, 'gluwb', keep=True)
    glub_t = P.sb([128, 256], F32, 'glub_t', keep=True)
    ssmg_t = P.sb([128, 256], F32, 'ssmg_t', keep=True)
    mlag_t = P.sb([128, 512], F32, 'mlag_t', keep=True)
    if moe:
        routerb = P.sb([128, 8, 8], BF16, 'routerb', keep=True)
    X1 = [P.sb([128, D], F32, 'X1_%d' % i, keep=True) for i in range(GT)]
    hT2 = P.sb([128, 8, GT * 128], BF16, 'hT2', keep=True)
    yacc = [P.sb([128, D], F32, 'yacc%d' % i, keep=True) for i in range(GT)]
    gatew = P.sb([128, GT, 8], F32, 'gatew', keep=True)

    tes = ExitStack()
    P.tes = tes
    make_ident(P)
    cv = P.sb([128, 8], F32, 'cv')
    cvb = P.sb([128, 8], BF16, 'cvb')
    cvbb = P.sb([128, 8, 128], BF16, 'cvbb')
    P.dma('sp', cv[:], cvec[:, :], w=[cv])
    P.act(cvb, cvb[:], cv, cv[:], AF.Silu)
    P.cp('dve', cvbb, cvbb[:], cvb, cvb[:].unsqueeze(2).broadcast_to([128, 8, 128]))
    abc = P.sb([128, 32], F32, 'abc')
    P.dma('sp', abc[:], adab_col[:, :], w=[abc])
    wts = [P.sb([128, 8, 1024], BF16, 'adw%d' % i) for i in range(2)]
    stager = make_stager(P)
    rowb = P.sb([128, D], F32, 'rowb')
    for ci in range(4):
        wt = wts[ci % 2]
        for kc in range(8):
            stager(wt, wt[:, kc, :], adaw[:, kc, ci * 1024:(ci + 1) * 1024])
        if ci in (0, 3):
            vec = vecm if ci == 0 else vecf
            gsrc = gpost_m if ci == 0 else gpost_f
            P.dma('sp', rowb[:], adab_row[0:1, ci * 1024:(ci + 1) * 1024].broadcast_to([128, 1024]), w=[rowb])
            for half in range(2):
                yb = YB[half]
                for kc in range(8):
                    P.mm(yb, yb[:, :], cvbb, cvbb[:, kc, :], wt, wt[:, kc, half * 512:(half + 1) * 512],
                         start=(kc == 0), stop=(kc == 7))
                P.tt('dve', vec, vec[:, half * 512:(half + 1) * 512], yb, yb[:, :], rowb, rowb[:, half * 512:(half + 1) * 512], ALU.add)
            P.dma('sp', rowb[:], gsrc[0:1, :].broadcast_to([128, 1024]), w=[rowb])
            P.tt('dve', vec, vec[:], vec, vec[:], rowb, rowb[:], ALU.mult)
        else:
            for cc in range(8):
                ch = (ci - 1) * 8 + cc
                for kc in range(8):
                    P.mm(MC, MC[:, ch:ch + 1], wt, wt[:, kc, cc * 128:(cc + 1) * 128], cvb, cvb[:, kc:kc + 1],
                         start=(kc == 0), stop=(kc == 7))
    P.tt('dve', modc, modc[:], MC, MC[:, 0:16], abc, abc[:, 8:24], ALU.add)
    gpf = P.sb([128, 8], F32, 'gpf')
    P.dma('sp', gpf[:], gpre_f[:, :], w=[gpf])
    P.ts('dve', scale2, scale2[:], modc, modc[:, 8:16], 1.0, ALU.add)
    P.tt('dve', scale2, scale2[:], scale2, scale2[:], gpf, gpf[:], ALU.mult)
    sh2 = P.view(modc[:, 0:8], 'sh2', parent=modc)
    for kc in range(8):
        stager(woutb, woutb[:, kc, :], wout_d[:, kc, :])
    P.dma('pool', gluwb[:], gluw_d[:, :, :], w=[gluwb])
    P.dma('sp', glub_t[:], glub_d[0:1, :].broadcast_to([128, 256]), w=[glub_t])
    P.dma('sp', ssmg_t[:], ssmg_d[0:1, :].broadcast_to([128, 256]), w=[ssmg_t])
    P.dma('sp', mlag_t[:], mlag_d[0:1, :].broadcast_to([128, 512]), w=[mlag_t])
    if moe:
        P.dma('pool', routerb[:], router_d[:, :, :], w=[routerb])
    P.barrier()
    tes.close()
    P.tes = None

    MIXLOC = P.view(mixloc, 'MIXLOC')
    for kk in range(2):
        src_ = hxall.rearrange("(a b) c -> a (b c)", b=32)[bass.ds(rank256 + kk * 128, 128), :]
        P.dma('sp', mixloc[kk].rearrange("r n c -> (r n) c").rearrange("(a b) c -> a (b c)", b=32), src_, r=[HXALL], w=[MIXLOC])
    ev_i = [0]

    def evac_eng():
        ev_i[0] += 1
        return 'act' if ev_i[0] % 2 == 0 else 'dve'

    for g in range(ngroups):
        ph = ExitStack()
        P.tes = ph
        junk = P.sb([128, D], BF16, 'junk')

        def p1_bufs(i):
            return dict(xt=P.sb([128, D], F32, 'xt%d' % i), mt=P.sb([128, D], F32, 'mt%d' % i),
                        hxt=P.sb([128, 4, 256], F32, 'hxt%d' % i), ysb=P.sb([128, 256], BF16, 'ysb%d' % i),
                        ysT=P.sb([128, 2, 128], BF16, 'ysT%d' % i), zz=P.sb([128, 256], F32, 'zz%d' % i),
                        s2=P.sb([128, 256], F32, 's2%d' % i), catb=P.sb([128, D], BF16, 'catb%d' % i),
                        catT=P.sb([128, 8, 128], BF16, 'catT%d' % i), tmp=P.sb([128, D], F32, 'tmp%d' % i),
                        xn2=P.sb([128, D], BF16, 'xn2%d' % i), ss=P.sb([128, 4], F32, 'ss%d' % i),
                        lg=P.sb([128, 8], F32, 'lg%d' % i), lg2=P.sb([128, 8], F32, 'lg2%d' % i),
                        mk1=P.sb([128, 8], F32, 'mk1%d' % i), mk2=P.sb([128, 8], F32, 'mk2%d' % i),
                        m12=P.sb([128, 4], F32, 'm12%d' % i))

        p1sets = [p1_bufs(0), p1_bufs(1)]

        def p1_tile(ti, B_, YK):
            xt, mt, hxt, ysb, ysT, zz, s2 = B_['xt'], B_['mt'], B_['hxt'], B_['ysb'], B_['ysT'], B_['zz'], B_['s2']
            catb, catT, tmp, xn2, ss = B_['catb'], B_['catT'], B_['tmp'], B_['xn2'], B_['ss']
            lg, lg2, mk1, mk2, m12 = B_['lg'], B_['lg2'], B_['mk1'], B_['mk2'], B_['m12']
            tok = slice((g * GT + ti) * 128, (g * GT + ti + 1) * 128)
            P.dma('sp', xt[:], x[tok, :], r=([xdep] if xdep is not None else []), w=[xt])
            row0 = (g * GT + ti) * 128
            P.dma('sp', hxt[:], mixloc[row0 // 1024, :, row0 % 1024:row0 % 1024 + 128, :].rearrange("r n c -> n r c"), r=[MIXLOC], w=[hxt])
            for (c0, w_, d0) in ((0, 64, 0), (64, 64, 256), (128, 128, 512)):
                P.cp('pool', mt, mt[:, d0:d0 + 4 * w_].rearrange("p (r c) -> p r c", r=4), hxt, hxt[:, :, c0:c0 + w_])
            yield
            P.cp('dve', ysb, ysb[:], mt, mt[:, 256:512])
            yield
            for kc in range(2):
                P.tr(TB, TB[:, kc * 128:(kc + 1) * 128], ysb, ysb[:, kc * 128:(kc + 1) * 128], ident)
            P.cp('act', ysT, ysT[:].rearrange("p a b -> p (a b)"), TB, TB[:, 0:256])
            yield
            for kc in range(2):
                P.mm(ZP, ZP[:, :], ysT, ysT[:, kc, :], gluwb, gluwb[:, kc, :], start=(kc == 0), stop=(kc == 1))
            P.tt('dve', zz, zz[:], ZP, ZP[:, :], glub_t, glub_t[:], ALU.add)
            yield
            P.act(zz, zz[:], zz, zz[:], AF.Sigmoid)
            P.op('pool', lambda e: e.memset(ss[:], 0.0), w=[ss])
            yield
            P.tt('dve', s2, s2[:], zz, zz[:], mt, mt[:, 256:512], ALU.mult)
            P.act(junk, junk[:, 0:512], mt, mt[:, 512:1024], AF.Square, extra_w=[ss], accum_out=ss[:, 1:2])
            yield
            P.act(junk, junk[:, 0:256], s2, s2[:], AF.Square, extra_w=[ss], accum_out=ss[:, 0:1])
            P.cp('act', catb, catb[:, 0:256], mt, mt[:, 0:256])
            yield
            P.ts('dve', ss, ss[:, 0:1], ss, ss[:, 0:1], 1.0 / 256, ALU.mult, EPS, ALU.add)
            P.ts('dve', ss, ss[:, 1:2], ss, ss[:, 1:2], 1.0 / 512, ALU.mult, EPS, ALU.add)
            yield
            P.act(ss, ss[:, 0:2], ss, ss[:, 0:2], AF.Sqrt)
            yield
            P.op('dve', lambda e: e.reciprocal(out=ss[:, 0:2], in_=ss[:, 0:2]), r=[ss], w=[ss])
            yield
            P.op('dve', lambda e: e.scalar_tensor_tensor(out=catb[:, 256:512], in0=s2[:], scalar=ss[:, 0:1], in1=ssmg_t[:],
                                                         op0=ALU.mult, op1=ALU.mult), r=[s2, ss, ssmg_t], w=[catb])
            P.op('dve', lambda e: e.scalar_tensor_tensor(out=catb[:, 512:1024], in0=mt[:, 512:1024], scalar=ss[:, 1:2], in1=mlag_t[:],
                                                         op0=ALU.mult, op1=ALU.mult), r=[mt, ss, mlag_t], w=[catb])
            yield
            for half in range(2):
                for c4 in range(4):
                    c = half * 4 + c4
                    P.tr(TB, TB[:, c4 * 128:(c4 + 1) * 128], catb, catb[:, c * 128:(c + 1) * 128], ident)
                P.cp(evac_eng(), catT, catT[:, half * 4:(half + 1) * 4, :].rearrange("p a b -> p (a b)"), TB, TB[:, :])
                yield
            for half in range(2):
                for kc in range(8):
                    P.mm(YK[half], YK[half][:, :], catT, catT[:, kc, :], woutb, woutb[:, kc, half * 512:(half + 1) * 512],
                         start=(kc == 0), stop=(kc == 7))
            P.op('pool', lambda e: e.memset(ss[:, 2:4], 0.0), w=[ss])
            yield
            for half in range(2):
                P.act(junk, junk[:, 0:512], YK[half], YK[half][:, :], AF.Square, extra_w=[ss], accum_out=ss[:, 2 + half:3 + half])
            yield
            P.tt('dve', ss, ss[:, 2:3], ss, ss[:, 2:3], ss, ss[:, 3:4], ALU.add)
            P.ts('dve', ss, ss[:, 2:3], ss, ss[:, 2:3], 1.0 / D, ALU.mult, EPS, ALU.add)
            yield
            P.act(ss, ss[:, 2:3], ss, ss[:, 2:3], AF.Sqrt)
            yield
            P.op('dve', lambda e: e.reciprocal(out=ss[:, 2:3], in_=ss[:, 2:3]), r=[ss], w=[ss])
            yield
            for half in range(2):
                hs = slice(half * 512, (half + 1) * 512)
                P.op('dve', lambda e: e.scalar_tensor_tensor(out=tmp[:, hs], in0=YK[half][:, :], scalar=ss[:, 2:3], in1=vecm[:, hs],
                                                             op0=ALU.mult, op1=ALU.mult), r=[YK[half], ss, vecm], w=[tmp])
            yield
            P.tt('dve', X1[ti], X1[ti][:], tmp, tmp[:], xt, xt[:], ALU.add)
            P.op('pool', lambda e: e.memset(ss[:, 3:4], 0.0), w=[ss])
            yield
            P.act(junk, junk[:], X1[ti], X1[ti][:], AF.Square, extra_w=[ss], accum_out=ss[:, 3:4])
            yield
            P.ts('dve', ss, ss[:, 3:4], ss, ss[:, 3:4], 1.0 / D, ALU.mult, EPS, ALU.add)
            yield
            P.act(ss, ss[:, 3:4], ss, ss[:, 3:4], AF.Sqrt)
            yield
            P.op('dve', lambda e: e.reciprocal(out=ss[:, 3:4], in_=ss[:, 3:4]), r=[ss], w=[ss])
            yield
            P.ts('dve', xn2, xn2[:], X1[ti], X1[ti][:], ss[:, 3:4], ALU.mult, extra_r=[ss])
            yield
            for half in range(2):
                for c4 in range(4):
                    c = half * 4 + c4
                    P.tr(TB, TB[:, c4 * 128:(c4 + 1) * 128], xn2, xn2[:, c * 128:(c + 1) * 128], ident)
                for c4 in range(4):
                    c = half * 4 + c4
                    if c4 % 2 == 0:
                        P.act(hT2, hT2[:, c, ti * 128:(ti + 1) * 128], TB, TB[:, c4 * 128:(c4 + 1) * 128], AF.Identity,
                              extra_r=[scale2, sh2], scale=scale2[:, c:c + 1], bias=sh2[:, c:c + 1])
                    else:
                        P.ts('dve', hT2, hT2[:, c, ti * 128:(ti + 1) * 128], TB, TB[:, c4 * 128:(c4 + 1) * 128],
                             scale2[:, c:c + 1], ALU.mult, sh2[:, c:c + 1], ALU.add, extra_r=[scale2, sh2])
                yield
            if moe:
                for kc in range(8):
                    P.mm(LG, LG[:, :], hT2, hT2[:, kc, ti * 128:(ti + 1) * 128], routerb, routerb[:, kc, :],
                         start=(kc == 0), stop=(kc == 7))
                P.cp('dve', lg, lg[:], LG, LG[:, :])
                yield
                P.op('dve', lambda e: e.reduce_max(out=m12[:, 0:1], in_=lg[:], axis=AX.X), r=[lg], w=[m12])
                yield
                P.ts('dve', mk1, mk1[:], lg, lg[:], m12[:, 0:1], ALU.is_equal, extra_r=[m12])
                yield
                P.op('dve', lambda e: e.scalar_tensor_tensor(out=lg2[:], in0=mk1[:], scalar=-1e30, in1=lg[:],
                                                             op0=ALU.mult, op1=ALU.add), r=[mk1, lg], w=[lg2])
                yield
                P.op('dve', lambda e: e.reduce_max(out=m12[:, 1:2], in_=lg2[:], axis=AX.X), r=[lg2], w=[m12])
                yield
                P.ts('dve', mk2, mk2[:], lg2, lg2[:], m12[:, 1:2], ALU.is_equal, extra_r=[m12])
                P.tt('dve', m12, m12[:, 2:3], m12, m12[:, 1:2], m12, m12[:, 0:1], ALU.subtract)
                yield
                P.act(m12, m12[:, 2:3], m12, m12[:, 2:3], AF.Exp)
                yield
                P.ts('dve', m12, m12[:, 3:4], m12, m12[:, 2:3], 1.0, ALU.add)
                yield
                P.op('dve', lambda e: e.reciprocal(out=m12[:, 3:4], in_=m12[:, 3:4]), r=[m12], w=[m12])
                yield
                P.tt('dve', m12, m12[:, 2:3], m12, m12[:, 2:3], m12, m12[:, 3:4], ALU.mult)
                P.ts('dve', mk1, mk1[:], mk1, mk1[:], m12[:, 3:4], ALU.mult, extra_r=[m12])
                yield
                P.op('dve', lambda e: e.scalar_tensor_tensor(out=gatew[:, ti, :], in0=mk2[:], scalar=m12[:, 2:3], in1=mk1[:],
                                                             op0=ALU.mult, op1=ALU.add), r=[mk2, m12, mk1], w=[gatew])
                yield

        for pair in range(GT // 2):
            gens = [p1_tile(2 * pair, p1sets[0], YB), p1_tile(2 * pair + 1, p1sets[1], GB)]
            while gens:
                alive = []
                for gen_ in gens:
                    try:
                        next(gen_)
                        alive.append(gen_)
                    except StopIteration:
                        pass
                gens = alive
        P.barrier()
        ph.close()
        ph = ExitStack()
        P.tes = ph
        actT = P.sb([128, NFC, GT * 128], BF16, 'actT')
        wdb = P.sb([128, NFC, D], BF16, 'wdb')
        stg = [P.sb([128, 1024], F32, 'stg%d' % i) for i in range(4)]
        stgd = [P.sb([128, 1024], F32, 'stgd%d' % i) for i in range(2)]
        NWB = 3
        wgb = [P.sb([128, 8, 128], BF16, 'wgb%d' % i) for i in range(NWB)]
        wub = [P.sb([128, 8, 128], BF16, 'wub%d' % i) for i in range(NWB)]
        gsil = [P.sb([128, GT * 128], F32, 'gsil%d' % i) for i in range(2)]
        ftmp = P.sb([128, D], F32, 'ftmp')
        fjunk = P.sb([128, D], BF16, 'fjunk')
        fss = P.sb([128, 1], F32, 'fss')
        st_i = [0]

        sd_i = [0]

        def load_cast(dst, dst_ap, src_ap, eng):
            if eng == 'pool':
                sg = stgd[sd_i[0] % 2]
                sd_i[0] += 1
            else:
                sg = stg[st_i[0] % 4]
                st_i[0] += 1
            P.dma('sp', sg[:], src_ap, w=[sg])
            P.cp(eng, dst, dst_ap, sg, sg[:])

        def load_gu(idx):
            b_ = idx % NWB
            load_cast(wgb[b_], wgb[b_][:].rearrange("p a b -> p (a b)"), wg_d[idx, :, :], 'dve')
            load_cast(wub[b_], wub[b_][:].rearrange("p a b -> p (a b)"), wu_d[idx, :, :], 'act')

        load_gu(0)
        load_gu(1)
        for e_ in range(n_exp):
            for fc in range(NFC):
                b2 = fc % 2
                wb = (e_ * NFC + fc) % NWB
                if e_ * NFC + fc + 2 < n_exp * NFC:
                    load_gu(e_ * NFC + fc + 2)
                load_cast(wdb, wdb[:, fc, :], wd_d[e_ * NFC + fc, :, :], 'pool')
                for kc in range(8):
                    P.mm(GB[b2], GB[b2][:, :], wgb[wb], wgb[wb][:, kc, :], hT2, hT2[:, kc, :], start=(kc == 0), stop=(kc == 7))
                for kc in range(8):
                    P.mm(UB[b2], UB[b2][:, :], wub[wb], wub[wb][:, kc, :], hT2, hT2[:, kc, :], start=(kc == 0), stop=(kc == 7))
                P.act(gsil[b2], gsil[b2][:], GB[b2], GB[b2][:, :], AF.Silu)
                P.tt('dve', actT, actT[:, fc, :], UB[b2], UB[b2][:, :], gsil[b2], gsil[b2][:], ALU.mult)
            for ti in range(GT):
                for half in range(2):
                    hs = slice(half * 512, (half + 1) * 512)
                    yb = YB[(ti * 2 + half) % 2]
                    for fc in range(NFC):
                        P.mm(yb, yb[:, :], actT, actT[:, fc, ti * 128:(ti + 1) * 128], wdb, wdb[:, fc, hs],
                             start=(fc == 0), stop=(fc == NFC - 1))
                    if not moe:
                        P.cp(evac_eng(), yacc[ti], yacc[ti][:, hs], yb, yb[:, :])
                    elif e_ == 0:
                        P.ts('dve', yacc[ti], yacc[ti][:, hs], yb, yb[:, :], gatew[:, ti, 0:1], ALU.mult, extra_r=[gatew])
                    else:
                        P.op('dve', lambda e: e.scalar_tensor_tensor(out=yacc[ti][:, hs], in0=yb[:, :], scalar=gatew[:, ti, e_:e_ + 1],
                                                                     in1=yacc[ti][:, hs], op0=ALU.mult, op1=ALU.add),
                             r=[yb, gatew, yacc[ti]], w=[yacc[ti]])
        for ti in range(GT):
            tok = slice((g * GT + ti) * 128, (g * GT + ti + 1) * 128)
            P.op('pool', lambda e: e.memset(fss[:], 0.0), w=[fss])
            P.act(fjunk, fjunk[:], yacc[ti], yacc[ti][:], AF.Square, extra_w=[fss], accum_out=fss[:, 0:1])
            P.rstd(fss, D, None)
            P.op('dve', lambda e: e.scalar_tensor_tensor(out=ftmp[:], in0=yacc[ti][:], scalar=fss[:, 0:1], in1=vecf[:],
                                                         op0=ALU.mult, op1=ALU.mult), r=[yacc[ti], fss, vecf], w=[ftmp])
            P.tt('dve', ftmp, ftmp[:], ftmp, ftmp[:], X1[ti], X1[ti][:], ALU.add)
            P.dma('sp', out[tok, :], ftmp[:], r=[ftmp])
        P.barrier()
        ph.close()
        P.tes = None
        if on_group is not None:
            on_group(g)
    st.close()
    P.es = old_es
    P.pre = {}


def inputs_B(inp, layer, xcur, mix):
    f = lambda a: np.ascontiguousarray(a, dtype=np.float32)
    moe = (layer % 2 == 1)
    j = layer // 2
    xf = xcur.reshape(-1, D)
    mf = mix.reshape(-1, D)
    aw = f(inp['ada_w'][layer][:, 2048:6144].reshape(128, 8, 4096))
    ab = inp['ada_b'][layer]
    shared = dict(
        adaw=aw, adab_row=f(ab[2048:6144][None, :]), adab_col=chunkT(f(ab[2048:6144]), 32),
        gpost_m=f(inp['norm_post_mix'][layer][None, :]), gpre_f=chunkT(f(inp['norm_pre_ffn'][layer]), 8),
        gpost_f=f(inp['norm_post_ffn'][layer][None, :]),
        gluw=f(inp['ssm_glu_w'][layer].reshape(2, 128, 256).transpose(1, 0, 2)),
        glub=f(inp['ssm_glu_b'][layer][None, :]), ssmg=f(inp['ssm_norm'][layer][None, :]),
        mlag=f(inp['mla_norm'][layer][None, :]),
        wout=f(inp['w_out'][layer].reshape(8, 128, D).transpose(1, 0, 2)))
    if moe:
        wg, wu, wd = inp['moe_w_gate'][j], inp['moe_w_up'][j], inp['moe_w_down'][j]
        shared['router'] = f(inp['moe_router'][j].reshape(8, 128, 8).transpose(1, 0, 2))
    else:
        wg, wu, wd = inp['ffn_w_gate'][j][None], inp['ffn_w_up'][j][None], inp['ffn_w_down'][j][None]
    ne = wg.shape[0]

    def gu(w):
        return f(w.reshape(ne, 8, 128, NFC, 128).transpose(0, 3, 2, 1, 4).reshape(ne * NFC, 128, 1024))

    shared['wg'] = gu(wg)
    shared['wu'] = gu(wu)
    shared['wd'] = f(wd.reshape(ne * NFC, 128, D))
    maps = []
    for core in range(8):
        b = core // 4
        m = dict(shared)
        m['x'] = f(xf[core * TPC:(core + 1) * TPC])
        m['mix'] = f(mf[core * TPC:(core + 1) * TPC])
        m['cvec'] = f(inp['c'][b].reshape(128, 8))
        maps.append(m)
    return maps


RG4 = [[0, 1, 2, 3], [4, 5, 6, 7]]


def build_fused(ntiles=NT, ngroups=NG):
    nc = bass.Bass("TRN2", target_bir_lowering=False)
    ioA = [decl_A(nc, '_a%d' % l) for l in range(2)]
    ioB = [decl_B(nc, '_b%d' % l, moe=(l == 1)) for l in range(2)]
    x = nc.dram_tensor("x", [L, D], F32, kind="ExternalInput").ap()
    xsl = nc.dram_tensor("xsl", [TPC, D], F32, kind="ExternalInput").ap()
    out = nc.dram_tensor("out", [TPC, D], F32, kind="ExternalOutput").ap()
    hx = [nc.dram_tensor("hx%d" % l, [L, 256], F32).ap() for l in range(2)]
    hxall = [nc.dram_tensor("hxall%d" % l, [8 * 4 * 1024, 256], F32).ap() for l in range(2)]
    mixloc = [nc.dram_tensor("mixloc%d" % l, [2, 4, 1024, 256], F32).ap() for l in range(2)]
    xs = nc.dram_tensor("xs", [TPC, D], F32).ap()
    xall = nc.dram_tensor("xall", [8 * 4 * 256, D], F32).ap()

    def xrow1(t):
        tok0 = t * 128
        r_, j_, i0 = tok0 // TPC, (tok0 % TPC) // 256, tok0 % 256
        return xall[(j_ * 4 + r_) * 256 + i0:(j_ * 4 + r_) * 256 + i0 + 128, :]

    with ExitStack() as es:
        P = Prog(nc, es)
        banks = [P.ps([128, 512], F32, 'bank%d' % i) for i in range(8)]
        rank256 = nc.sync.snap((nc.sync.partition_id() % 4) * 256, min_val=0, max_val=768)

        H = [P.view(hxall[l], 'HXALL%d' % l) for l in range(2)]
        XALL = P.view(xall, 'XALL')

        def hx_chunk(l):
            def f(k, evs):
                for ev in evs:
                    P._wait('pool', ev)
                P.coll('AllGather', hx[l][k * 1024:(k + 1) * 1024, :], H[l], hxall[l][k * 4096:(k + 1) * 4096, :], RG4)
            return f

        def xs_group(g):
            for j in (2 * g, 2 * g + 1):
                P.coll('AllGather', xs[j * 256:(j + 1) * 256, :], XALL, xall[j * 1024:(j + 1) * 1024, :], RG4)

        emit_A(P, nc, banks, ioA[0], lambda t: x[t * 128:(t + 1) * 128, :], hx[0], ntiles, on_chunk=hx_chunk(0))
        emit_B(P, nc, banks, ioB[0], xsl, hxall[0], H[0], mixloc[0], xs, rank256, False, ngroups, on_group=xs_group)
        emit_A(P, nc, banks, ioA[1], xrow1, hx[1], ntiles, xdep=XALL, on_chunk=hx_chunk(1))
        emit_B(P, nc, banks, ioB[1], xs, hxall[1], H[1], mixloc[1], out, rank256, True, ngroups)
        P.finish()
    return nc


def fused_inputs(inp):
    x = np.ascontiguousarray(inp['x'], dtype=np.float32)
    dummy = np.zeros((2, L, D), np.float32)
    maps = [dict() for _ in range(8)]
    for layer in range(2):
        ma = inputs_A(inp, layer, x)
        mb = inputs_B(inp, layer, x, dummy)
        for c in range(8):
            for k, v in ma[c].items():
                if k != 'x':
                    maps[c][k + '_a%d' % layer] = v
            for k, v in mb[c].items():
                if k not in ('x', 'mix'):
                    maps[c][k + '_b%d' % layer] = v
    xf = x.reshape(-1, D)
    for c in range(8):
        maps[c]['x'] = x[c // 4]
        maps[c]['xsl'] = np.ascontiguousarray(xf[c * TPC:(c + 1) * TPC])
    return maps


def kernel(**inputs):
    inp = {k: np.asarray(v) for k, v in inputs.items()}
    if 'F' not in _CACHE:
        _CACHE['F'] = build_fused()
    res = run_bass_kernel_spmd(_CACHE['F'], fused_inputs(inp), core_ids=list(range(8)))
    return np.concatenate([res.results[c]['out'] for c in range(8)], axis=0).reshape(2, L, D).astype(np.float32)
```

```python
import math
from contextlib import ExitStack

import numpy as np
import concourse.bass as bass
import concourse.mybir as mybir
from concourse.bass_utils import run_bass_kernel_spmd

F32 = mybir.dt.float32
BF16 = mybir.dt.bfloat16
I32 = mybir.dt.int32
ALU = mybir.AluOpType
AF = mybir.ActivationFunctionType
AX = mybir.AxisListType

D = 1024
L = 8192
NT = 64
EPS = 1e-6
TWO_PI = 2.0 * math.pi
D_FF = 3584
NFC = 28
N_EXP = 8


class T:
    def __init__(self, t, name, root=None):
        self.t = t
        self.name = name
        self.lastw = None
        self.readers = {}
        self.root = self if root is None else root.root
        self.excl = False

    def __getitem__(self, idx):
        return self.t[idx]


class Prog:
    def __init__(self, nc, es, ndma_sems=8):
        self.nc = nc
        self.es = es
        self.engs = {'pe': nc.tensor, 'act': nc.scalar, 'dve': nc.vector, 'pool': nc.gpsimd, 'sp': nc.sync}
        self.sem = {k: es.enter_context(nc.semaphore('s_' + k)) for k in self.engs}
        self.cnt = {k: 0 for k in self.engs}
        self.waited = {k: {} for k in self.engs}
        self.dma_sems = {}
        for q in ('sp', 'pool'):
            self.dma_sems[q] = [[es.enter_context(nc.semaphore('d_%s%d' % (q, i))), 0] for i in range(ndma_sems)]
        self.dma_i = {q: 0 for q in self.dma_sems}
        self.cc_sem = es.enter_context(nc.semaphore('cc_sem'))
        self.cc_n = 0
        self.nbuf = 0
        self.tes = None
        self.pre = {}

    def sb(self, shape, dt, name=None, keep=False):
        if keep and name in self.pre:
            assert list(self.pre[name].t.shape) == list(shape), name
            return self.pre[name]
        assert not (keep and self.tes is not None), "persistent buffer %s must be pre-allocated" % name
        self.nbuf += 1
        uname = (name or 'b') + '_%d' % self.nbuf
        es = self.es if (keep or self.tes is None) else self.tes
        t = T(es.enter_context(self.nc.sbuf_tensor(uname, shape, dt)), uname)
        if keep:
            self.pre[name] = t
        return t

    def ps(self, shape, dt, name=None):
        self.nbuf += 1
        name = name or ('p%d' % self.nbuf)
        t = T(self.es.enter_context(self.nc.psum_tensor(name, shape, dt)), name)
        t.excl = True
        return t

    def view(self, ap, name='v', parent=None):
        return T(ap, name, root=parent)

    def _wait(self, engname, ev):
        sem, val, src = ev
        if src == 'pe' and engname == 'pe':
            return
        key = id(sem)
        w = self.waited[engname]
        if w.get(key, 0) >= val:
            return
        w[key] = val
        self.engs[engname].wait_ge(sem, val)

    def _deps(self, engname, r, w):
        for b in r:
            b = b.root
            if b.lastw is not None:
                self._wait(engname, b.lastw)
        for b in w:
            b = b.root
            if b.lastw is not None:
                self._wait(engname, b.lastw)
            for ev in b.readers.values():
                self._wait(engname, ev)

    def _commit(self, ev, r, w):
        for b in r:
            b = b.root
            old = b.readers.get(id(ev[0]))
            if old is None or old[1] < ev[1]:
                b.readers[id(ev[0])] = ev
        for b in w:
            b = b.root
            b.lastw = ev
            b.readers = {}

    def op(self, engname, fn, r=(), w=()):
        xr = [b for b in r if b.root.excl]
        if xr:
            r = [b for b in r if not b.root.excl]
            w = list(w) + xr
        self._deps(engname, r, w)
        ins = fn(self.engs[engname])
        self.cnt[engname] += 1
        ins.then_inc(self.sem[engname], 1)
        ev = (self.sem[engname], self.cnt[engname], engname)
        self._commit(ev, r, w)
        return ev

    def dma(self, q, out, in_, r=(), w=(), **kw):
        if out.dtype != in_.dtype:
            q = 'pool'
        self._deps(q, r, w)
        slot = self.dma_sems[q][self.dma_i[q] % len(self.dma_sems[q])]
        self.dma_i[q] += 1
        sem, n = slot
        if n > 0:
            self._wait(q, (sem, 16 * n, 'dma'))
        slot[1] = n + 1
        self.engs[q].dma_start(out=out, in_=in_, **kw).then_inc(sem, 16)
        ev = (sem, 16 * (n + 1), 'dma')
        self._commit(ev, r, w)
        return ev

    def coll(self, kind, src_ap, dst, dst_ap, groups):
        self.nc.gpsimd.collective_compute(kind, ALU.bypass, replica_groups=groups, ins=[src_ap], outs=[dst_ap]).then_inc(self.cc_sem, 1)
        self.cc_n += 1
        ev = (self.cc_sem, self.cc_n, 'cc')
        self._commit(ev, [], [dst])
        return ev

    def barrier(self):
        for e in self.engs:
            for o in self.engs:
                if o != e and self.cnt[o] > 0:
                    self._wait(e, (self.sem[o], self.cnt[o], 'x'))
            for q in self.dma_sems:
                for sem, n in self.dma_sems[q]:
                    if n > 0:
                        self._wait(e, (sem, 16 * n, 'dma'))
            if self.cc_n > 0:
                self._wait(e, (self.cc_sem, self.cc_n, 'cc'))

    def finish(self):
        self.barrier()
        for q in self.dma_sems:
            for sem, n in self.dma_sems[q]:
                if n > 0:
                    self._wait('sp', (sem, 16 * n, 'dma'))

    def mm(self, out, oap, lhsT, lap, rhs, rap, start=True, stop=True):
        return self.op('pe', lambda e: e.matmul(oap, lhsT=lap, rhs=rap, start=start, stop=stop),
                       r=[lhsT, rhs], w=[out])

    def tr(self, out, oap, src, sap, ident):
        n = sap.shape[0]
        return self.op('pe', lambda e: e.transpose(oap, sap, ident[0:n, 0:n]), r=[src, ident], w=[out])

    def tt(self, eng, out, oap, a, aap, b, bap, op):
        return self.op(eng, lambda e: e.tensor_tensor(out=oap, in0=aap, in1=bap, op=op), r=[a, b], w=[out])

    def ts(self, eng, out, oap, a, aap, s1, op0, s2=None, op1=None, extra_r=()):
        if op1 is None:
            return self.op(eng, lambda e: e.tensor_scalar(out=oap, in0=aap, scalar1=s1, scalar2=None, op0=op0),
                           r=[a] + list(extra_r), w=[out])
        return self.op(eng, lambda e: e.tensor_scalar(out=oap, in0=aap, scalar1=s1, scalar2=s2, op0=op0, op1=op1),
                       r=[a] + list(extra_r), w=[out])

    def act(self, out, oap, a, aap, func, extra_r=(), extra_w=(), **kw):
        return self.op('act', lambda e: e.activation(out=oap, in_=aap, func=func, **kw),
                       r=[a] + list(extra_r), w=[out] + list(extra_w))

    def cp(self, eng, out, oap, a, aap):
        if eng == 'act':
            return self.act(out, oap, a, aap, AF.Copy)
        return self.op(eng, lambda e: e.tensor_copy(out=oap, in_=aap), r=[a], w=[out])

    def rstd(self, ss, n, tmp):
        self.ts('dve', ss, ss[:], ss, ss[:], 1.0 / n, ALU.mult, EPS, ALU.add)
        self.act(ss, ss[:], ss, ss[:], AF.Sqrt)
        self.op('dve', lambda e: e.reciprocal(out=ss[:], in_=ss[:]), r=[ss], w=[ss])

    def range_reduce(self, out, oap, src, sap, kf, kfap, ki, kiap, shift=0.0):
        self.ts('dve', kf, kfap, src, sap, 1.0 / TWO_PI, ALU.mult, shift / TWO_PI, ALU.add)
        self.cp('dve', ki, kiap, kf, kfap)
        self.cp('dve', kf, kfap, ki, kiap)
        self.op('dve', lambda e: e.scalar_tensor_tensor(out=oap, in0=kfap, scalar=-TWO_PI, in1=sap,
                                                        op0=ALU.mult, op1=ALU.add), r=[kf, src], w=[out])
        if shift != 0.0:
            self.ts('dve', out, oap, out, oap, shift, ALU.add)
        self.ts('dve', out, oap, out, oap, -math.pi, ALU.max, math.pi, ALU.min)


def make_stager(P, nstage=4, cols=1024):
    stg = [P.sb([128, cols], F32, 'stage%d' % i) for i in range(nstage)]
    cnt = [0]
    engs = ('dve', 'act', 'pool')

    def load(dst, dst_ap, src_ap):
        n = src_ap.shape[-1]
        sg = stg[cnt[0] % nstage]
        eng = engs[cnt[0] % 3]
        cnt[0] += 1
        P.dma('sp', sg[:, 0:n], src_ap, w=[sg])
        P.cp(eng, dst, dst_ap, sg, sg[:, 0:n])
    return load


def make_ident(P):
    identf = P.sb([128, 128], F32, 'identf')
    ident = P.sb([128, 128], BF16, 'ident', keep=True)
    P.op('pool', lambda e: e.memset(identf[:], 1.0), w=[identf])
    P.op('pool', lambda e: e.affine_select(out=identf[:], in_=identf[:], pattern=[[-1, 128]],
                                           compare_op=ALU.is_equal, fill=0.0, base=0, channel_multiplier=1),
         r=[identf], w=[identf])
    P.cp('dve', ident, ident[:], identf, identf[:])
    return ident, identf


def adaln_mod(P, nc, cvec, adaw, adab, ncol_chunks, modps, name, stager):
    cv = P.sb([128, 8], F32, name + '_cv')
    cvb = P.sb([128, 8], BF16, name + '_cvb')
    P.dma('sp', cv[:], cvec[:, :], w=[cv])
    P.act(cvb, cvb[:], cv, cv[:], AF.Silu)
    ncols = ncol_chunks * 128
    mod = P.sb([128, ncol_chunks], F32, name + '_mod', keep=True)
    ab = P.sb([128, ncol_chunks], F32, name + '_ab')
    P.dma('sp', ab[:], adab[:, :], w=[ab])
    cw = 1024
    wts = [P.sb([128, 8, cw], BF16, name + '_w%d' % i) for i in range(2)]
    for ci in range(ncols // cw):
        wt = wts[ci % 2]
        for kc in range(8):
            stager(wt, wt[:, kc, :], adaw[:, kc, ci * cw:(ci + 1) * cw])
        for cc in range(cw // 128):
            ch = ci * (cw // 128) + cc
            for kc in range(8):
                P.mm(modps, modps[:, ch:ch + 1], wt, wt[:, kc, cc * 128:(cc + 1) * 128], cvb, cvb[:, kc:kc + 1],
                     start=(kc == 0), stop=(kc == 7))
    P.tt('dve', mod, mod[:], modps, modps[:, 0:ncol_chunks], ab, ab[:], ALU.add)
    return mod


import os
STOP_AT = float(os.environ.get('STOP_AT', '99'))


def decl_A(nc, sfx):
    io = {}
    io['cvec'] = nc.dram_tensor('cvec' + sfx, [128, 8], F32, kind='ExternalInput').ap()
    io['pos'] = nc.dram_tensor('pos' + sfx, [128, NT], I32, kind='ExternalInput').ap()
    io['adaw'] = nc.dram_tensor('adaw' + sfx, [128, 8, 2048], F32, kind='ExternalInput').ap()
    io['adab'] = nc.dram_tensor('adab' + sfx, [128, 16], F32, kind='ExternalInput').ap()
    io['gpre'] = nc.dram_tensor('gpre' + sfx, [128, 8], F32, kind='ExternalInput').ap()
    io['wsel'] = nc.dram_tensor('wsel' + sfx, [128, 8, 768], F32, kind='ExternalInput').ap()
    io['retg'] = nc.dram_tensor('retg' + sfx, [1, 64], F32, kind='ExternalInput').ap()
    io['invf'] = nc.dram_tensor('invf' + sfx, [1, 32], F32, kind='ExternalInput').ap()
    io['dmaskT_d'] = nc.dram_tensor('dmaskT' + sfx, [128, 128], F32, kind='ExternalInput').ap()
    io['tri_d'] = nc.dram_tensor('tri' + sfx, [128, 128], F32, kind='ExternalInput').ap()
    io['qdec_d'] = nc.dram_tensor('qdec' + sfx, [64, 128], F32, kind='ExternalInput').ap()
    io['kdec_d'] = nc.dram_tensor('kdec' + sfx, [128, 1], F32, kind='ExternalInput').ap()
    io['dcy_d'] = nc.dram_tensor('dcy' + sfx, [64, 1], F32, kind='ExternalInput').ap()
    io['cmask_d'] = nc.dram_tensor('cmask' + sfx, [128, 128], F32, kind='ExternalInput').ap()
    io['jp1_d'] = nc.dram_tensor('jp1' + sfx, [128, 1], F32, kind='ExternalInput').ap()
    io['irow1_d'] = nc.dram_tensor('irow1' + sfx, [128, 128], F32, kind='ExternalInput').ap()
    io['are_tm_d'] = nc.dram_tensor('are_tm' + sfx, [1, 512], F32, kind='ExternalInput').ap()
    io['aim_tm_d'] = nc.dram_tensor('aim_tm' + sfx, [1, 512], F32, kind='ExternalInput').ap()
    io['ldt_tm_d'] = nc.dram_tensor('ldt_tm' + sfx, [1, 512], F32, kind='ExternalInput').ap()
    io['BR_d'] = nc.dram_tensor('BR' + sfx, [64, 512], F32, kind='ExternalInput').ap()
    io['BI_d'] = nc.dram_tensor('BI' + sfx, [64, 512], F32, kind='ExternalInput').ap()
    io['are_sm_d'] = nc.dram_tensor('are_sm' + sfx, [128, 2], F32, kind='ExternalInput').ap()
    io['aim_sm_d'] = nc.dram_tensor('aim_sm' + sfx, [128, 2], F32, kind='ExternalInput').ap()
    io['ldt_sm_d'] = nc.dram_tensor('ldt_sm' + sfx, [128, 2], F32, kind='ExternalInput').ap()
    io['CR_d'] = nc.dram_tensor('CR' + sfx, [128, 2, 64], F32, kind='ExternalInput').ap()
    io['CI_d'] = nc.dram_tensor('CI' + sfx, [128, 2, 64], F32, kind='ExternalInput').ap()
    io['DD_d'] = nc.dram_tensor('DD' + sfx, [64, 64], F32, kind='ExternalInput').ap()
    io['qng_d'] = nc.dram_tensor('qng' + sfx, [128, 2], F32, kind='ExternalInput').ap()
    io['wuq_d'] = nc.dram_tensor('wuq' + sfx, [128, 2, 192], F32, kind='ExternalInput').ap()
    io['kvng_d'] = nc.dram_tensor('kvng' + sfx, [128, 1], F32, kind='ExternalInput').ap()
    io['wukv_d'] = nc.dram_tensor('wukv' + sfx, [128, 256], F32, kind='ExternalInput').ap()
    return io


def emit_A(P, nc, banks, io, xrow, hx, ntiles=NT, xdep=None, on_chunk=None):
    cvec = io['cvec']
    pos = io['pos']
    adaw = io['adaw']
    adab = io['adab']
    gpre = io['gpre']
    wsel = io['wsel']
    retg = io['retg']
    invf = io['invf']
    dmaskT_d = io['dmaskT_d']
    tri_d = io['tri_d']
    qdec_d = io['qdec_d']
    kdec_d = io['kdec_d']
    dcy_d = io['dcy_d']
    cmask_d = io['cmask_d']
    jp1_d = io['jp1_d']
    irow1_d = io['irow1_d']
    are_tm_d = io['are_tm_d']
    aim_tm_d = io['aim_tm_d']
    ldt_tm_d = io['ldt_tm_d']
    BR_d = io['BR_d']
    BI_d = io['BI_d']
    are_sm_d = io['are_sm_d']
    aim_sm_d = io['aim_sm_d']
    ldt_sm_d = io['ldt_sm_d']
    CR_d = io['CR_d']
    CI_d = io['CI_d']
    DD_d = io['DD_d']
    qng_d = io['qng_d']
    wuq_d = io['wuq_d']
    kvng_d = io['kvng_d']
    wukv_d = io['wukv_d']
    st = ExitStack()
    old_es = P.es
    P.es = st
    P.pre = {}
    b7 = banks[7][:, :].bitcast(BF16)
    TB = [P.view(b7[:, 0:512], 'TB', parent=banks[7])]
    TS = [P.view(b7[:, 256 + 128 * k:384 + 128 * k], 'TS%d' % k, parent=banks[7]) for k in range(2)]
    PJ0 = P.view(banks[0][:, :], 'PJ0', parent=banks[0])
    PJ1 = P.view(banks[1][:, 0:256], 'PJ1', parent=banks[1])
    RS = P.view(banks[1][:, 256:384], 'RS', parent=banks[1])
    RO = P.view(banks[1][:, 384:448], 'RO', parent=banks[1])
    RKV = P.view(banks[1][0:64, 448:512], 'RKV', parent=banks[1])
    SA = P.view(banks[2][:, :], 'SA', parent=banks[2])
    SB = P.view(banks[3][:, :], 'SB', parent=banks[3])
    QTN = P.view(banks[4][:, 0:128], 'QTN', parent=banks[4])
    KTN = P.view(banks[4][:, 128:256], 'KTN', parent=banks[4])
    VP = P.view(banks[4][:, 256:384], 'VP', parent=banks[4])
    QRP = P.view(banks[4][:, 384:448], 'QRP', parent=banks[4])
    SY = P.view(banks[4][:, 448:512], 'SY', parent=banks[4])
    SC = [P.view(banks[5][:, :], 'SC0', parent=banks[5]), P.view(banks[6][:, :], 'SC1', parent=banks[6])]

    for nm, shp, dt_ in [('ident', [128, 128], BF16), ('ada_mod', [128, 16], F32), ('scale1', [128, 8], F32),
                         ('wselb', [128, 8, 768], BF16), ('retg_t', [128, 64], F32), ('dmaskT', [128, 128], F32),
                         ('tri', [128, 128], BF16), ('qdec', [64, 128], F32), ('kdec', [128, 1], F32),
                         ('dcy', [64, 1], F32), ('cmask', [128, 128], F32), ('SINT', [128, NT * 32], F32),
                         ('COST', [128, NT * 32], F32), ('NSINT', [128, NT * 32], F32), ('EA', [128, 512], F32),
                         ('EB', [128, 512], F32), ('Bmat', [64, 512], BF16), ('Bswp', [64, 512], BF16),
                         ('EA2', [128, 512], F32), ('EB2', [128, 512], F32), ('Cmat', [128, 4, 64], BF16),
                         ('DDb', [64, 64], BF16), ('xprev', [128, 4], F32), ('xprevs', [128, 4], F32),
                         ('wuqb', [128, 2, 192], BF16), ('wukvb', [128, 256], BF16)]:
        P.sb(shp, dt_, nm, keep=True)
    tes = ExitStack()
    P.tes = tes
    ident, identf = make_ident(P)

    stager = make_stager(P)
    mod = adaln_mod(P, nc, cvec, adaw, adab, 16, SA, 'ada', stager)
    gp = P.sb([128, 8], F32, 'gp')
    P.dma('sp', gp[:], gpre[:, :], w=[gp])
    scale1 = P.sb([128, 8], F32, 'scale1', keep=True)
    P.ts('dve', scale1, scale1[:], mod, mod[:, 8:16], 1.0, ALU.add)
    P.tt('dve', scale1, scale1[:], scale1, scale1[:], gp, gp[:], ALU.mult)
    shm = P.view(mod[:, 0:8], 'shm', parent=mod)

    wselb = P.sb([128, 8, 768], BF16, 'wselb', keep=True)
    for kc in range(8):
        stager(wselb, wselb[:, kc, :], wsel[:, kc, :])

    def load(name, src, shape, dt=F32, q='sp', bcast=None, keep=False):
        t = P.sb(shape, dt, name, keep=keep)
        P.dma(q, t[:], src if bcast is None else src.broadcast_to(bcast), w=[t])
        return t

    retg_t = load('retg_t', retg[0:1, :], [128, 64], bcast=[128, 64], keep=True)
    invf_t = load('invf_t', invf[0:1, :], [128, 32], bcast=[128, 32])
    dmaskT = load('dmaskT', dmaskT_d[:, :], [128, 128], keep=True)
    tri_f = load('tri_f', tri_d[:, :], [128, 128])
    tri = P.sb([128, 128], BF16, 'tri', keep=True)
    P.cp('dve', tri, tri[:], tri_f, tri_f[:])
    qdec = load('qdec', qdec_d[:, :], [64, 128], keep=True)
    kdec = load('kdec', kdec_d[:, :], [128, 1], keep=True)
    dcy = load('dcy', dcy_d[:, :], [64, 1], keep=True)
    cmask = load('cmask', cmask_d[:, :], [128, 128], keep=True)
    jp1 = load('jp1', jp1_d[:, :], [128, 1])
    irow1 = load('irow1', irow1_d[:, :], [128, 128])

    posi = load('posi', pos[:, :], [128, NT], I32)
    posf = P.sb([128, NT], F32, 'posf')
    P.cp('dve', posf, posf[:], posi, posi[:])
    NR = NT * 32
    ang = P.sb([128, NR], F32, 'ang')
    P.tt('dve', ang, ang[:].rearrange("p (t f) -> p t f", f=32), posf,
         posf[:].unsqueeze(2).broadcast_to([128, NT, 32]), invf_t,
         invf_t[:].unsqueeze(1).broadcast_to([128, NT, 32]), ALU.mult)
    SINT = P.sb([128, NR], F32, 'SINT', keep=True)
    COST = P.sb([128, NR], F32, 'COST', keep=True)
    NSINT = P.sb([128, NR], F32, 'NSINT', keep=True)
    rkf = P.sb([128, NR], F32, 'rkf')
    rki = P.sb([128, NR], I32, 'rki')
    P.range_reduce(SINT, SINT[:], ang, ang[:], rkf, rkf[:], rki, rki[:])
    P.act(SINT, SINT[:], SINT, SINT[:], AF.Sin)
    P.range_reduce(COST, COST[:], ang, ang[:], rkf, rkf[:], rki, rki[:], shift=math.pi / 2)
    P.act(COST, COST[:], COST, COST[:], AF.Sin)
    P.ts('dve', NSINT, NSINT[:], SINT, SINT[:], -1.0, ALU.mult)

    are_tm = load('are_tm', are_tm_d[0:1, :], [128, 512], bcast=[128, 512])
    aim_tm = load('aim_tm', aim_tm_d[0:1, :], [128, 512], bcast=[128, 512])
    dt_tm = load('dt_tm', ldt_tm_d[0:1, :], [128, 512], bcast=[128, 512])
    P.act(dt_tm, dt_tm[:], dt_tm, dt_tm[:], AF.Exp)
    rho = P.sb([128, 512], F32, 'rho')
    tht = P.sb([128, 512], F32, 'tht')
    P.tt('dve', rho, rho[:], are_tm, are_tm[:], dt_tm, dt_tm[:], ALU.mult)
    P.tt('dve', tht, tht[:], aim_tm, aim_tm[:], dt_tm, dt_tm[:], ALU.mult)
    s5a = P.sb([128, 512], F32, 's5a')
    s5b = P.sb([128, 512], F32, 's5b')
    s5c = P.sb([128, 512], F32, 's5c')
    s5k = P.sb([128, 512], F32, 's5k')
    s5i = P.sb([128, 512], I32, 's5i')
    EA = P.sb([128, 512], F32, 'EA', keep=True)
    EB = P.sb([128, 512], F32, 'EB', keep=True)
    njp1 = P.sb([128, 1], F32, 'njp1')
    P.ts('dve', njp1, njp1[:], jp1, jp1[:], -1.0, ALU.mult)
    P.ts('dve', s5a, s5a[:], rho, rho[:], njp1[:, 0:1], ALU.mult, extra_r=[njp1])
    P.act(s5a, s5a[:], s5a, s5a[:], AF.Exp)
    P.ts('dve', s5b, s5b[:], tht, tht[:], jp1[:, 0:1], ALU.mult, extra_r=[jp1])
    P.range_reduce(s5c, s5c[:], s5b, s5b[:], s5k, s5k[:], s5i, s5i[:], shift=math.pi / 2)
    P.act(s5c, s5c[:], s5c, s5c[:], AF.Sin)
    P.tt('dve', EA, EA[:], s5a, s5a[:], s5c, s5c[:], ALU.mult)
    P.range_reduce(s5c, s5c[:], s5b, s5b[:], s5k, s5k[:], s5i, s5i[:])
    P.act(s5c, s5c[:], s5c, s5c[:], AF.Sin)
    P.tt('dve', EB, EB[:], s5a, s5a[:], s5c, s5c[:], ALU.mult)
    P.ts('dve', EB, EB[:], EB, EB[:], -1.0, ALU.mult)
    Lre = P.sb([128, 512], F32, 'Lre')
    Lim = P.sb([128, 512], F32, 'Lim')
    P.act(s5a, s5a[:], rho, rho[:], AF.Exp)
    P.range_reduce(s5c, s5c[:], tht, tht[:], s5k, s5k[:], s5i, s5i[:], shift=math.pi / 2)
    P.act(s5c, s5c[:], s5c, s5c[:], AF.Sin)
    P.tt('dve', Lre, Lre[:], s5a, s5a[:], s5c, s5c[:], ALU.mult)
    P.range_reduce(s5c, s5c[:], tht, tht[:], s5k, s5k[:], s5i, s5i[:])
    P.act(s5c, s5c[:], s5c, s5c[:], AF.Sin)
    P.tt('dve', Lim, Lim[:], s5a, s5a[:], s5c, s5c[:], ALU.mult)
    P.ts('dve', Lre, Lre[:], Lre, Lre[:], -1.0, ALU.add)
    P.tt('dve', s5a, s5a[:], are_tm, are_tm[:], are_tm, are_tm[:], ALU.mult)
    P.tt('dve', s5b, s5b[:], aim_tm, aim_tm[:], aim_tm, aim_tm[:], ALU.mult)
    P.tt('dve', s5a, s5a[:], s5a, s5a[:], s5b, s5b[:], ALU.add)
    P.op('dve', lambda e: e.reciprocal(out=s5a[:], in_=s5a[:]), r=[s5a], w=[s5a])
    Fre = P.sb([128, 512], F32, 'Fre')
    Fim = P.sb([128, 512], F32, 'Fim')
    P.tt('dve', s5b, s5b[:], Lre, Lre[:], are_tm, are_tm[:], ALU.mult)
    P.tt('dve', s5c, s5c[:], Lim, Lim[:], aim_tm, aim_tm[:], ALU.mult)
    P.tt('dve', s5b, s5b[:], s5b, s5b[:], s5c, s5c[:], ALU.add)
    P.tt('dve', Fre, Fre[:], s5b, s5b[:], s5a, s5a[:], ALU.mult)
    P.tt('dve', s5b, s5b[:], Lim, Lim[:], are_tm, are_tm[:], ALU.mult)
    P.tt('dve', s5c, s5c[:], Lre, Lre[:], aim_tm, aim_tm[:], ALU.mult)
    P.tt('dve', s5b, s5b[:], s5b, s5b[:], s5c, s5c[:], ALU.subtract)
    P.tt('dve', Fim, Fim[:], s5b, s5b[:], s5a, s5a[:], ALU.mult)
    BRt = load('BRt', BR_d[:, :], [64, 512])
    BIt = load('BIt', BI_d[:, :], [64, 512])
    bre = P.sb([64, 512], F32, 'bre')
    bim = P.sb([64, 512], F32, 'bim')
    btmp = P.sb([64, 512], F32, 'btmp')
    P.tt('dve', bre, bre[:], Fre, Fre[0:64, :], BRt, BRt[:], ALU.mult)
    P.tt('dve', btmp, btmp[:], Fim, Fim[0:64, :], BIt, BIt[:], ALU.mult)
    P.tt('dve', bre, bre[:], bre, bre[:], btmp, btmp[:], ALU.subtract)
    P.tt('dve', bim, bim[:], Fre, Fre[0:64, :], BIt, BIt[:], ALU.mult)
    P.tt('dve', btmp, btmp[:], Fim, Fim[0:64, :], BRt, BRt[:], ALU.mult)
    P.tt('dve', bim, bim[:], bim, bim[:], btmp, btmp[:], ALU.add)
    Bmat = P.sb([64, 512], BF16, 'Bmat', keep=True)
    Bswp = P.sb([64, 512], BF16, 'Bswp', keep=True)

    def v4(t, rows=128):
        return t[0:rows, :].rearrange("p (a r q) -> p a r q", a=2, r=2)

    P.cp('dve', Bmat, v4(Bmat, 64)[:, :, 0, :], bre, v4(bre, 64)[:, :, 0, :])
    P.cp('dve', Bmat, v4(Bmat, 64)[:, :, 1, :], bim, v4(bim, 64)[:, :, 1, :])
    P.ts('dve', Bswp, v4(Bswp, 64)[:, :, 0, :], bim, v4(bim, 64)[:, :, 0, :], -1.0, ALU.mult)
    P.cp('dve', Bswp, v4(Bswp, 64)[:, :, 1, :], bre, v4(bre, 64)[:, :, 1, :])
    are_sm = load('are_sm', are_sm_d[:, :], [128, 2])
    aim_sm = load('aim_sm', aim_sm_d[:, :], [128, 2])
    dt_sm = load('dt_sm', ldt_sm_d[:, :], [128, 2])
    P.act(dt_sm, dt_sm[:], dt_sm, dt_sm[:], AF.Exp)
    rho_sm = P.sb([128, 2], F32, 'rho_sm')
    tht_sm = P.sb([128, 2], F32, 'tht_sm')
    P.tt('dve', rho_sm, rho_sm[:], are_sm, are_sm[:], dt_sm, dt_sm[:], ALU.mult)
    P.tt('dve', tht_sm, tht_sm[:], aim_sm, aim_sm[:], dt_sm, dt_sm[:], ALU.mult)
    EA2 = P.sb([128, 512], F32, 'EA2', keep=True)
    EB2 = P.sb([128, 512], F32, 'EB2', keep=True)
    pm = P.sb([128, 256], F32, 'pm')
    pa = P.sb([128, 256], F32, 'pa')
    pc = P.sb([128, 256], F32, 'pc')
    pk = P.sb([128, 256], F32, 'pk')
    pki = P.sb([128, 256], I32, 'pki')
    for gp_ in range(2):
        sl = slice(gp_ * 128, (gp_ + 1) * 128)
        P.ts('dve', pm, pm[:, sl], irow1, irow1[:], rho_sm[:, gp_:gp_ + 1], ALU.mult, extra_r=[rho_sm])
        P.ts('dve', pa, pa[:, sl], irow1, irow1[:], tht_sm[:, gp_:gp_ + 1], ALU.mult, extra_r=[tht_sm])
    P.act(pm, pm[:], pm, pm[:], AF.Exp)
    P.range_reduce(pc, pc[:], pa, pa[:], pk, pk[:], pki, pki[:], shift=math.pi / 2)
    P.act(pc, pc[:], pc, pc[:], AF.Sin)
    P.tt('dve', pc, pc[:], pc, pc[:], pm, pm[:], ALU.mult)
    pc3 = pc[:].rearrange("p (a i) -> p a i", a=2)
    P.cp('dve', EA2, v4(EA2)[:, :, 0, :], pc, pc3)
    P.cp('dve', EA2, v4(EA2)[:, :, 1, :], pc, pc3)
    P.range_reduce(pc, pc[:], pa, pa[:], pk, pk[:], pki, pki[:])
    P.act(pc, pc[:], pc, pc[:], AF.Sin)
    P.tt('dve', pc, pc[:], pc, pc[:], pm, pm[:], ALU.mult)
    P.ts('dve', EB2, v4(EB2)[:, :, 0, :], pc, pc3, -1.0, ALU.mult)
    P.cp('dve', EB2, v4(EB2)[:, :, 1, :], pc, pc3)
    CRt = load('CRt', CR_d[:, :, :], [128, 2, 64])
    CIt = load('CIt', CI_d[:, :, :], [128, 2, 64])
    Cmat = P.sb([128, 4, 64], BF16, 'Cmat', keep=True)
    for gp_ in range(2):
        P.cp('dve', Cmat, Cmat[:, gp_ * 2, :], CRt, CRt[:, gp_, :])
        P.ts('dve', Cmat, Cmat[:, gp_ * 2 + 1, :], CIt, CIt[:, gp_, :], -1.0, ALU.mult)
    DDt = load('DDt', DD_d[:, :], [64, 64])
    DDb = P.sb([64, 64], BF16, 'DDb', keep=True)
    P.cp('dve', DDb, DDb[:], DDt, DDt[:])
    xprev = P.sb([128, 4], F32, 'xprev', keep=True)
    xprevs = P.sb([128, 4], F32, 'xprevs', keep=True)
    P.op('pool', lambda e: e.memset(xprev[:], 0.0), w=[xprev])
    P.op('pool', lambda e: e.memset(xprevs[:], 0.0), w=[xprevs])

    qng = load('qng', qng_d[:, :], [128, 2])
    wuq_f = load('wuq_f', wuq_d[:, :, :], [128, 2, 192])
    wuqb = P.sb([128, 2, 192], BF16, 'wuqb', keep=True)
    for kc in range(2):
        P.ts('dve', wuqb, wuqb[:, kc, :], wuq_f, wuq_f[:, kc, :], qng[:, kc:kc + 1], ALU.mult, extra_r=[qng])
    kvng = load('kvng', kvng_d[:, :], [128, 1])
    wukv_f = load('wukv_f', wukv_d[:, :], [128, 256])
    wukvb = P.sb([128, 256], BF16, 'wukvb', keep=True)
    P.ts('dve', wukvb, wukvb[:], wukv_f, wukv_f[:], kvng[:, 0:1], ALU.mult, extra_r=[kvng])

    P.barrier()
    tes.close()
    P.tes = None
    KTNP_t = P.es.enter_context(nc.sbuf_tensor('KTNP_%d' % id(P.es), [128, L], BF16))
    KTR_t = P.es.enter_context(nc.sbuf_tensor('KTR_%d' % id(P.es), [64, L], BF16))
    VV_t = P.es.enter_context(nc.sbuf_tensor('VV_%d' % id(P.es), [128, NT, 128], BF16))
    KTNP = [P.view(KTNP_t[:, t * 128:(t + 1) * 128], 'ktn%d' % t) for t in range(NT)]
    KTR = [P.view(KTR_t[:, t * 128:(t + 1) * 128], 'ktr%d' % t) for t in range(NT)]
    VV = [P.view(VV_t[:, t, :], 'vv%d' % t) for t in range(NT)]
    SROW_t = P.es.enter_context(nc.sbuf_tensor('SROW_%d' % id(P.es), [128, L], F32))
    SROW = [P.view(SROW_t, 'srow0'), P.view(SROW_t, 'srow1')]
    PB = P.sb([128, L], BF16, 'PB')
    PBv = [P.view(PB[:, c * 2048:(c + 1) * 2048], 'PBv%d' % c) for c in range(4)]
    state = P.sb([64, 64], F32, 'state')
    state_bf = P.sb([64, 64], BF16, 'state_bf')
    P.op('pool', lambda e: e.memset(state[:], 0.0), w=[state])
    P.op('pool', lambda e: e.memset(state_bf[:], 0.0), w=[state_bf])

    def dbl(shape, dt, name):
        return [P.sb(shape, dt, name + '%d' % i) for i in range(2)]

    xt = dbl([128, D], F32, 'xt')
    xnb = dbl([128, D], BF16, 'xnb')
    junk = P.sb([128, D], BF16, 'junk')
    ssx = dbl([128, 1], F32, 'ssx')
    hT = dbl([128, 8, 128], BF16, 'hT')
    ropeA = P.sb([128, 192], F32, 'ropeA')
    ropeB = P.sb([128, 192], F32, 'ropeB')
    rqk = dbl([128, 192], BF16, 'rqk')
    QTs = dbl([64, 128], BF16, 'QTs')
    KTs = dbl([64, 128], BF16, 'KTs')
    QTd = dbl([64, 128], BF16, 'QTd')
    vb = dbl([128, 64], BF16, 'vb')
    vdec = dbl([128, 64], BF16, 'vdec')
    smask = dbl([128, 128], BF16, 'smask')
    ssr = dbl([128, 1], F32, 'ssr')
    gs = dbl([128, 64], F32, 'gs')
    ro = dbl([128, 64], F32, 'ro')
    junk64 = P.sb([128, 256], F32, 'junk64')
    ub = dbl([128, 64], BF16, 'ub')
    UTs = dbl([64, 128], BF16, 'UTs')
    T1 = P.sb([128, 512], F32, 'T1')
    T2 = P.sb([128, 512], F32, 'T2')
    Wb = dbl([128, 512], BF16, 'Wb')
    Zf = T1
    Zsf = T2
    Xf = P.sb([128, 512], F32, 'Xf')
    Xb = dbl([128, 512], BF16, 'Xb')
    g1 = P.sb([128, 64], F32, 'g1')
    g2 = P.sb([128, 64], F32, 'g2')
    so = dbl([128, 64], F32, 'so')
    ssq = dbl([128, 1], F32, 'ssq')
    sskv = dbl([128, 1], F32, 'sskv')
    cqn = dbl([128, 256], BF16, 'cqn')
    ckvn = dbl([128, 128], BF16, 'ckvn')
    cqnT = dbl([128, 2, 128], BF16, 'cqnT')
    ckvnT = dbl([128, 128], BF16, 'ckvnT')
    QTNs = dbl([128, 128], BF16, 'QTNs')
    qrA = P.sb([128, 64], F32, 'qrA')
    qrB = P.sb([128, 64], F32, 'qrB')
    qrb = dbl([128, 64], BF16, 'qrb')
    QrTs = dbl([64, 128], BF16, 'QrTs')
    mrow = dbl([128, 1], F32, 'mrow')
    acc4 = dbl([128, 4], F32, 'acc4')
    rsum = dbl([128, 1], F32, 'rsum')
    PT = dbl([128, 512], BF16, 'PT')
    ao = dbl([128, 128], F32, 'ao')
    SM_SCALE = 192.0 ** -0.5
    ts_i = [0]

    def ts_slot():
        s = TS[ts_i[0] % 2]
        ts_i[0] += 1
        return s

    tb_i = [0]
    ev_i = [0]

    def evac_eng():
        ev_i[0] += 1
        return 'act' if ev_i[0] % 2 == 0 else 'dve'

    PVO = P.view(banks[5][:, 0:128], 'PVO', parent=banks[5])
    TBF = [P.view(banks[7][:, :].bitcast(BF16), 'TBF0', parent=banks[7]), P.view(banks[6][:, :].bitcast(BF16), 'TBF1', parent=banks[6])]
    PT8 = dbl([128, 1024], BF16, 'PT8')

    def frontend(t):
        p = t % 2
        P.dma('sp', xt[p][:], xrow(t), r=([xdep] if xdep is not None else []), w=[xt[p]])
        P.op('dve', lambda e: e.memset(ssx[p][:], 0.0), w=[ssx[p]])
        P.act(junk, junk[:], xt[p], xt[p][:], AF.Square, extra_w=[ssx[p]], accum_out=ssx[p][:])
        yield
        P.rstd(ssx[p], D, None)
        P.ts('dve', xnb[p], xnb[p][:], xt[p], xt[p][:], ssx[p][:, 0:1], ALU.mult, extra_r=[ssx[p]])
        yield
        for half in range(2):
            tb = TB[0]
            for c4 in range(4):
                c = half * 4 + c4
                P.tr(tb, tb[:, c4 * 128:(c4 + 1) * 128], xnb[p], xnb[p][:, c * 128:(c + 1) * 128], ident)
            for c4 in range(4):
                c = half * 4 + c4
                if c4 % 2 == 0:
                    P.act(hT[p], hT[p][:, c, :], tb, tb[:, c4 * 128:(c4 + 1) * 128], AF.Identity,
                          extra_r=[scale1, shm], scale=scale1[:, c:c + 1], bias=shm[:, c:c + 1])
                else:
                    P.ts('dve', hT[p], hT[p][:, c, :], tb, tb[:, c4 * 128:(c4 + 1) * 128],
                         scale1[:, c:c + 1], ALU.mult, shm[:, c:c + 1], ALU.add, extra_r=[scale1, shm])
            yield

    def projheads(t):
        projection(t)
        yield
        for _ in heads(t):
            yield

    def projection(t):
        p = t % 2
        for kc in range(8):
            P.mm(PJ0, PJ0[:, :], hT[p], hT[p][:, kc, :], wselb, wselb[:, kc, 0:512], start=(kc == 0), stop=(kc == 7))
        for kc in range(8):
            P.mm(PJ1, PJ1[:, :], hT[p], hT[p][:, kc, :], wselb, wselb[:, kc, 512:768], start=(kc == 0), stop=(kc == 7))

    def heads(t):
        p = t % 2
        cos_t = COST[:, t * 32:(t + 1) * 32]
        sin_t = SINT[:, t * 32:(t + 1) * 32]
        nsin_t = NSINT[:, t * 32:(t + 1) * 32]
        src4 = PJ0[:, 0:192].rearrange("p (a h f) -> p a h f", a=3, h=2)
        A4 = ropeA[:].rearrange("p (a h f) -> p a h f", a=3, h=2)
        B4 = ropeB[:].rearrange("p (a h f) -> p a h f", a=3, h=2)
        P.tt('dve', ropeA, A4, PJ0, src4, COST, cos_t.unsqueeze(1).unsqueeze(1).broadcast_to([128, 3, 2, 32]), ALU.mult)
        P.tt('dve', ropeB, B4[:, :, 0, :], PJ0, src4[:, :, 1, :], NSINT, nsin_t.unsqueeze(1).broadcast_to([128, 3, 32]), ALU.mult)
        P.tt('dve', ropeB, B4[:, :, 1, :], PJ0, src4[:, :, 0, :], SINT, sin_t.unsqueeze(1).broadcast_to([128, 3, 32]), ALU.mult)
        yield
        P.cp('act', vb[p], vb[p][:], PJ0, PJ0[:, 192:256])
        P.ts('dve', vdec[p], vdec[p][:], PJ0, PJ0[:, 192:256], kdec[:, 0:1], ALU.mult, extra_r=[kdec])
        P.act(gs[p], gs[p][:], PJ0, PJ0[:, 256:320], AF.Silu)
        P.cp('act', ub[p], ub[p][:], PJ0, PJ0[:, 320:384])
        P.op('dve', lambda e: e.memset(ssq[p][:], 0.0), w=[ssq[p]])
        P.op('dve', lambda e: e.memset(sskv[p][:], 0.0), w=[sskv[p]])
        P.act(junk64, junk64[:, 0:256], PJ1, PJ1[:, :], AF.Square, extra_w=[ssq[p]], accum_out=ssq[p][:])
        P.act(junk64, junk64[:, 0:128], PJ0, PJ0[:, 384:512], AF.Square, extra_w=[sskv[p]], accum_out=sskv[p][:])
        yield
        P.rstd(ssq[p], 256, None)
        yield
        P.rstd(sskv[p], 128, None)
        yield
        P.ts('dve', cqn[p], cqn[p][:], PJ1, PJ1[:, :], ssq[p][:, 0:1], ALU.mult, extra_r=[ssq[p]])
        P.ts('dve', ckvn[p], ckvn[p][:], PJ0, PJ0[:, 384:512], sskv[p][:, 0:1], ALU.mult, extra_r=[sskv[p]])
        P.tt('dve', rqk[p], rqk[p][:], ropeA, ropeA[:], ropeB, ropeB[:], ALU.add)
        yield

    def ret_chain(t):
        p = t % 2
        tok = slice(t * 128, (t + 1) * 128)
        s0 = ts_slot()
        P.tr(s0, s0[0:64, :], rqk[p], rqk[p][:, 0:64], ident)
        P.cp('act', QTs[p], QTs[p][:], s0, s0[0:64, :])
        yield
        P.tt('dve', QTd[p], QTd[p][:], QTs[p], QTs[p][:], qdec, qdec[:], ALU.mult)
        s1 = ts_slot()
        P.tr(s1, s1[0:64, :], rqk[p], rqk[p][:, 64:128], ident)
        P.cp('act', KTs[p], KTs[p][:], s1, s1[0:64, :])
        yield
        P.tt('dve', gs[p], gs[p][:], gs[p], gs[p][:], retg_t, retg_t[:], ALU.mult)
        P.mm(RS, RS[:, :], KTs[p], KTs[p][:], QTs[p], QTs[p][:])
        yield
        P.tt('dve', smask[p], smask[p][:], RS, RS[:, :], dmaskT, dmaskT[:], ALU.mult)
        yield
        P.mm(RO, RO[:, :], smask[p], smask[p][:], vb[p], vb[p][:], start=True, stop=False)
        P.mm(RO, RO[:, :], QTd[p], QTd[p][:], state_bf, state_bf[:], start=False, stop=True)
        P.mm(RKV, RKV[:, :], rqk[p], rqk[p][:, 64:128], vdec[p], vdec[p][:])
        yield
        P.op('dve', lambda e: e.scalar_tensor_tensor(out=state[:], in0=state[:], scalar=dcy[:, 0:1], in1=RKV[:, :],
                                                     op0=ALU.mult, op1=ALU.add), r=[state, dcy, RKV], w=[state])
        P.cp('dve', state_bf, state_bf[:], state, state[:])
        P.op('dve', lambda e: e.memset(ssr[p][:], 0.0), w=[ssr[p]])
        P.act(junk64, junk64[:, 0:64], RO, RO[:, :], AF.Square, extra_w=[ssr[p]], accum_out=ssr[p][:])
        yield
        P.rstd(ssr[p], 64, None)
        yield
        P.op('dve', lambda e: e.scalar_tensor_tensor(out=ro[p][:], in0=RO[:, :], scalar=ssr[p][:, 0:1], in1=gs[p][:],
                                                     op0=ALU.mult, op1=ALU.mult), r=[RO, ssr[p], gs[p]], w=[ro[p]])
        out_evs.append(P.dma('sp', hx[tok, 0:64], ro[p][:], r=[ro[p]]))
        yield

    def s5_chain(t):
        p = t % 2
        tok = slice(t * 128, (t + 1) * 128)
        s3 = ts_slot()
        P.tr(s3, s3[0:64, :], ub[p], ub[p][:], ident)
        P.cp('dve', UTs[p], UTs[p][:], s3, s3[0:64, :])
        yield
        P.mm(SA, SA[:, :], UTs[p], UTs[p][:], Bmat, Bmat[:])
        P.mm(SB, SB[:, :], UTs[p], UTs[p][:], Bswp, Bswp[:])
        yield
        P.tt('dve', T1, T1[:], SA, SA[:, :], EA, EA[:], ALU.mult)
        yield
        P.tt('dve', T2, T2[:], SB, SB[:, :], EB, EB[:], ALU.mult)
        yield
        P.tt('dve', Wb[p], Wb[p][:], T1, T1[:], T2, T2[:], ALU.add)
        yield
        for blk in range(4):
            P.mm(SA, SA[:, blk * 128:(blk + 1) * 128], Wb[p], Wb[p][:, blk * 128:(blk + 1) * 128], tri, tri[:])
        for blk in range(4):
            b2 = blk ^ 1
            P.mm(SB, SB[:, blk * 128:(blk + 1) * 128], Wb[p], Wb[p][:, b2 * 128:(b2 + 1) * 128], tri, tri[:])
        yield
        P.tt('dve', Zf, Zf[:].rearrange("p (a i) -> p a i", a=4), SA, SA[:, :].rearrange("p (a i) -> p a i", a=4),
             xprev, xprev[:].unsqueeze(2).broadcast_to([128, 4, 128]), ALU.add)
        yield
        P.tt('dve', Zsf, Zsf[:].rearrange("p (a i) -> p a i", a=4), SB, SB[:, :].rearrange("p (a i) -> p a i", a=4),
             xprevs, xprevs[:].unsqueeze(2).broadcast_to([128, 4, 128]), ALU.add)
        yield
        P.tt('dve', Zf, Zf[:], Zf, Zf[:], EA2, EA2[:], ALU.mult)
        yield
        P.tt('dve', Zsf, Zsf[:], Zsf, Zsf[:], EB2, EB2[:], ALU.mult)
        yield
        P.tt('dve', Xf, Xf[:], Zf, Zf[:], Zsf, Zsf[:], ALU.add)
        yield
        P.cp('act', Xb[p], Xb[p][:], Xf, Xf[:])
        X3 = Xf[:].rearrange("p (a i) -> p a i", a=4)
        P.cp('dve', xprev, xprev[:], Xf, X3[:, :, 127])
        xp3 = xprev[:].rearrange("p (a r) -> p a r", r=2)
        xs3 = xprevs[:].rearrange("p (a r) -> p a r", r=2)
        P.cp('dve', xprevs, xs3[:, :, 0], xprev, xp3[:, :, 1])
        P.cp('dve', xprevs, xs3[:, :, 1], xprev, xp3[:, :, 0])
        yield
        for blk in range(4):
            P.mm(SY, SY[:, :], Xb[p], Xb[p][:, blk * 128:(blk + 1) * 128], Cmat, Cmat[:, blk, :], start=(blk == 0), stop=False)
        P.mm(SY, SY[:, :], UTs[p], UTs[p][:], DDb, DDb[:], start=False, stop=True)
        yield
        P.act(g1, g1[:], SY, SY[:, :], AF.Square)
        yield
        P.ts('dve', g1, g1[:], g1, g1[:], 0.044715, ALU.mult, 1.0, ALU.add)
        P.tt('dve', g1, g1[:], g1, g1[:], SY, SY[:, :], ALU.mult)
        yield
        P.act(g2, g2[:], g1, g1[:], AF.Sigmoid, scale=1.5957691216057308)
        yield
        P.tt('dve', so[p], so[p][:], g2, g2[:], SY, SY[:, :], ALU.mult)
        out_evs.append(P.dma('sp', hx[tok, 64:128], so[p][:], r=[so[p]]))
        yield

    def mla_chain(t):
        p = t % 2
        tok = slice(t * 128, (t + 1) * 128)
        cos_t = COST[:, t * 32:(t + 1) * 32]
        sin_t = SINT[:, t * 32:(t + 1) * 32]
        nsin_t = NSINT[:, t * 32:(t + 1) * 32]
        s2 = ts_slot()
        P.tr(s2, s2[0:64, :], rqk[p], rqk[p][:, 128:192], ident)
        P.cp('act', KTR[t], KTR[t][:], s2, s2[0:64, :])
        yield
        for kc in range(2):
            s = ts_slot()
            P.tr(s, s[:, :], cqn[p], cqn[p][:, kc * 128:(kc + 1) * 128], ident)
            P.cp(evac_eng(), cqnT[p], cqnT[p][:, kc, :], s, s[:, :])
            yield
        s = ts_slot()
        P.tr(s, s[:, :], ckvn[p], ckvn[p][:], ident)
        P.cp(evac_eng(), ckvnT[p], ckvnT[p][:], s, s[:, :])
        yield
        for kc in range(2):
            P.mm(QTN, QTN[:, :], wuqb, wuqb[:, kc, 0:128], cqnT[p], cqnT[p][:, kc, :], start=(kc == 0), stop=(kc == 1))
        for kc in range(2):
            P.mm(QRP, QRP[:, :], cqnT[p], cqnT[p][:, kc, :], wuqb, wuqb[:, kc, 128:192], start=(kc == 0), stop=(kc == 1))
        P.mm(KTN, KTN[:, :], wukvb, wukvb[:, 0:128], ckvnT[p], ckvnT[p][:])
        P.mm(VP, VP[:, :], ckvnT[p], ckvnT[p][:], wukvb, wukvb[:, 128:256])
        yield
        P.act(QTNs[p], QTNs[p][:], QTN, QTN[:, :], AF.Copy, scale=SM_SCALE)
        q3 = QRP[:, :].rearrange("p (h f) -> p h f", h=2)
        a3 = qrA[:].rearrange("p (h f) -> p h f", h=2)
        b3 = qrB[:].rearrange("p (h f) -> p h f", h=2)
        P.tt('dve', qrA, a3, QRP, q3, COST, cos_t.unsqueeze(1).broadcast_to([128, 2, 32]), ALU.mult)
        P.tt('dve', qrB, b3[:, 0, :], QRP, q3[:, 1, :], NSINT, nsin_t, ALU.mult)
        P.tt('dve', qrB, b3[:, 1, :], QRP, q3[:, 0, :], SINT, sin_t, ALU.mult)
        P.cp('act', KTNP[t], KTNP[t][:], KTN, KTN[:, :])
        P.cp('act', VV[t], VV[t][:], VP, VP[:, :])
        yield
        P.tt('dve', qrb[p], qrb[p][:], qrA, qrA[:], qrB, qrB[:], ALU.add)
        yield
        s = ts_slot()
        P.tr(s, s[0:64, :], qrb[p], qrb[p][:], ident)
        P.act(QrTs[p], QrTs[p][:], s, s[0:64, :], AF.Copy, scale=SM_SCALE)
        yield
        Lk = (t + 1) * 128
        nkb = (Lk + 511) // 512
        for kb in range(nkb):
            n = min(512, Lk - kb * 512)
            sc = SC[kb % 2]
            kts = [KTNP[4 * kb + i_] for i_ in range(n // 128)]
            krs = [KTR[4 * kb + i_] for i_ in range(n // 128)]
            P.op('pe', lambda e: e.matmul(sc[:, 0:n], lhsT=QTNs[p][:], rhs=KTNP_t[:, kb * 512:kb * 512 + n],
                                          start=True, stop=False), r=[QTNs[p]] + kts, w=[sc])
            P.op('pe', lambda e: e.matmul(sc[:, 0:n], lhsT=QrTs[p][:], rhs=KTR_t[:, kb * 512:kb * 512 + n],
                                          start=False, stop=True), r=[QrTs[p]] + krs, w=[sc])
            yield
            srow = SROW[kb % 2]
            if kb == nkb - 1:
                if n > 128:
                    P.cp('act', srow, SROW_t[:, kb * 512:kb * 512 + n - 128], sc, sc[:, 0:n - 128])
                P.tt('dve', srow, SROW_t[:, Lk - 128:Lk], sc, sc[:, n - 128:n], cmask, cmask[:], ALU.add)
            else:
                P.cp(evac_eng(), srow, SROW_t[:, kb * 512:kb * 512 + n], sc, sc[:, 0:n])
            yield
        P.op('dve', lambda e: e.reduce_max(out=mrow[p][:], in_=SROW_t[:, 0:Lk], axis=AX.X), r=[SROW[0], SROW[1]], w=[mrow[p]])
        P.op('dve', lambda e: e.memset(acc4[p][:], 0.0), w=[acc4[p]])
        yield
        P.ts('dve', mrow[p], mrow[p][:], mrow[p], mrow[p][:], -1.0, ALU.mult)
        yield
        nch = (Lk + 2047) // 2048
        for ci in range(nch):
            c0 = ci * 2048
            c1 = min(Lk, c0 + 2048)
            P.op('act', lambda e: e.activation(out=PB[:, c0:c1], in_=SROW_t[:, c0:c1], func=AF.Exp,
                                               bias=mrow[p][:, 0:1], scale=1.0, accum_out=acc4[p][:, ci:ci + 1]),
                 r=[SROW[0], SROW[1], mrow[p]], w=[PBv[ci], acc4[p]])
            yield
        P.op('dve', lambda e: e.reduce_sum(out=rsum[p][:], in_=acc4[p][:, 0:nch], axis=AX.X), r=[acc4[p]], w=[rsum[p]])
        yield
        P.op('dve', lambda e: e.reciprocal(out=rsum[p][:], in_=rsum[p][:]), r=[rsum[p]], w=[rsum[p]])
        ng = (t + 1 + 7) // 8

        def tr_round(g):
            tbf = TBF[g % 2]
            pt = PT8[g % 2]
            blks = list(range(g * 8, min(t + 1, g * 8 + 8)))
            for blk in blks:
                P.tr(tbf, tbf[:, (blk % 8) * 128:(blk % 8 + 1) * 128], PBv[blk // 16], PB[:, blk * 128:(blk + 1) * 128], ident)
            w_ = len(blks) * 128
            P.cp(evac_eng(), pt, pt[:, 0:w_], tbf, tbf[:, 0:w_])
            return blks, pt

        cur = tr_round(0)
        yield
        for g in range(ng):
            nxt = tr_round(g + 1) if g + 1 < ng else None
            blks, pt = cur
            for blk in blks:
                P.mm(PVO, PVO[:, :], pt, pt[:, (blk % 8) * 128:(blk % 8 + 1) * 128], VV[blk], VV[blk][:],
                     start=(blk == 0), stop=(blk == t))
            cur = nxt
            yield
        P.ts('dve', ao[p], ao[p][:], PVO, PVO[:, :], rsum[p][:, 0:1], ALU.mult, extra_r=[rsum[p]])
        out_evs.append(P.dma('sp', hx[tok, 128:256], ao[p][:], r=[ao[p]]))
        yield

    def drain(g):
        for _ in g:
            pass

    def interleave(gens):
        gens = [[g, 2 if i == 0 else 1] for i, g in enumerate(gens)]
        while gens:
            alive = []
            for g, n_ in gens:
                ok = True
                for _ in range(n_):
                    try:
                        next(g)
                    except StopIteration:
                        ok = False
                        break
                if ok:
                    alive.append([g, n_])
            gens = alive

    def step(g, n_=1):
        for _ in range(n_):
            try:
                next(g)
            except StopIteration:
                return False
        return True

    out_evs = []
    if ntiles > 0:
        drain(frontend(0))
        drain(projheads(0))
    for t in range(ntiles):
        mla = mla_chain(t)
        mla_alive = True
        others = [ret_chain(t), s5_chain(t)]
        if t + 1 < ntiles:
            others.append(frontend(t + 1))
        while others:
            if mla_alive:
                mla_alive = step(mla, 2)
            others = [g for g in others if step(g)]
        nxt = projheads(t + 1) if t + 1 < ntiles else None
        nxt_alive = nxt is not None
        while mla_alive or nxt_alive:
            if mla_alive:
                mla_alive = step(mla, 2)
            if nxt_alive:
                nxt_alive = step(nxt)
        if on_chunk is not None and (t + 1) % 8 == 0:
            on_chunk(t // 8, out_evs)
            out_evs = []
    P.barrier()
    st.close()
    P.es = old_es
    P.pre = {}


RET_GAMMA = [1.0 - 2.0 ** (-5.0 - h) for h in range(4)]


def consts_A(hg):
    g = RET_GAMMA[hg]
    lg = math.log1p(-(2.0 ** (-5.0 - hg)))
    i = np.arange(128, dtype=np.float64)
    diff = i[None, :] - i[:, None]
    dmaskT = np.where(diff >= 0, np.exp(lg * np.maximum(diff, 0.0)), 0.0) * 0.125
    tri = (diff >= 0).astype(np.float32)
    qdec = np.broadcast_to(np.exp(lg * (i + 1.0))[None, :], (64, 128))
    kdec = (np.exp(lg * (127.0 - i)) * 0.125)[:, None]
    dcy = np.full((64, 1), math.exp(lg * 128.0))
    cmask = np.where(i[None, :] <= i[:, None], 0.0, -1e30)
    inv = 10000.0 ** (-np.arange(0, 64, 2, dtype=np.float32) / 64.0)
    f = lambda a: np.ascontiguousarray(a, dtype=np.float32)
    return dict(dmaskT=f(dmaskT), tri=f(tri), qdec=f(qdec), kdec=f(kdec), dcy=f(dcy), cmask=f(cmask),
                jp1=f((i + 1.0)[:, None]), irow1=f(np.broadcast_to((i + 1.0)[None, :], (128, 128))),
                invf=f(inv[None, :]))


def chunkT(v, nch):
    return np.ascontiguousarray(v.reshape(nch, 128).T)


def inputs_A(inp, layer, xcur):
    f = lambda a: np.ascontiguousarray(a, dtype=np.float32)
    maps = []
    w_in = inp['w_in'][layer]
    for core in range(8):
        b, hg = core // 4, core % 4
        m = dict(consts_A(hg))
        m['x'] = f(xcur[b])
        m['cvec'] = f(inp['c'][b].reshape(128, 8))
        m['pos'] = np.ascontiguousarray(inp['positions'][b].reshape(NT, 128).T.astype(np.int32))
        m['adaw'] = f(inp['ada_w'][layer][:, 0:2048].reshape(128, 8, 2048))
        m['adab'] = chunkT(f(inp['ada_b'][layer][0:2048]), 16)
        m['gpre'] = chunkT(f(inp['norm_pre_mix'][layer]), 8)
        h64 = slice(hg * 64, (hg + 1) * 64)
        cols = np.concatenate([
            np.arange(0, 256)[h64], np.arange(256, 512)[h64], np.arange(1664, 1728),
            np.arange(512, 768)[h64], np.arange(768, 1024)[h64], np.arange(1024, 1280)[h64],
            np.arange(1536, 1664), np.arange(1280, 1536)])
        ws = w_in[:, cols]
        m['wsel'] = f(ws.reshape(8, 128, 768).transpose(1, 0, 2))
        m['retg'] = f(inp['ret_norm'][layer][h64][None, :])
        gs_ = slice(4 * hg, 4 * hg + 4)

        def tm(a):
            a = a.reshape(2, 1, 128)
            return f(np.broadcast_to(a, (2, 2, 128)).reshape(1, 512))

        def sm(a):
            return f(a.reshape(2, 128).T)

        are = inp['ssm_a_re'][layer][gs_]
        aim = inp['ssm_a_im'][layer][gs_]
        ldt = np.broadcast_to(inp['ssm_log_dt'][layer][gs_][:, None], (4, 64))
        m['are_tm'], m['aim_tm'], m['ldt_tm'] = tm(are), tm(aim), tm(ldt)
        m['are_sm'], m['aim_sm'], m['ldt_sm'] = sm(are), sm(aim), sm(ldt)
        BR = np.zeros((64, 2, 2, 2, 64), np.float32)
        BI = np.zeros((64, 2, 2, 2, 64), np.float32)
        CR = np.zeros((2, 64, 2, 64), np.float32)
        CI = np.zeros((2, 64, 2, 64), np.float32)
        for gl in range(4):
            gp_, g2_ = gl // 2, gl % 2
            br = inp['ssm_b_re'][layer][4 * hg + gl]
            bi = inp['ssm_b_im'][layer][4 * hg + gl]
            for ri in range(2):
                BR[gl * 16:(gl + 1) * 16, gp_, ri, g2_, :] = br.T
                BI[gl * 16:(gl + 1) * 16, gp_, ri, g2_, :] = bi.T
            cr = inp['ssm_c_re'][layer][4 * hg + gl]
            ci = inp['ssm_c_im'][layer][4 * hg + gl]
            CR[g2_, :, gp_, gl * 16:(gl + 1) * 16] = cr.T
            CI[g2_, :, gp_, gl * 16:(gl + 1) * 16] = ci.T
        m['BR'] = f(BR.reshape(64, 512))
        m['BI'] = f(BI.reshape(64, 512))
        m['CR'] = f(CR.reshape(128, 2, 64))
        m['CI'] = f(CI.reshape(128, 2, 64))
        DD = np.zeros((64, 64), np.float32)
        DD[np.arange(64), np.arange(64)] = inp['ssm_d'][layer][gs_].reshape(64)
        m['DD'] = DD
        m['qng'] = chunkT(f(inp['mla_q_norm'][layer]), 2)
        wq = inp['mla_w_uq'][layer][:, hg * 192:(hg + 1) * 192]
        m['wuq'] = f(wq.reshape(2, 128, 192).transpose(1, 0, 2))
        m['kvng'] = f(inp['mla_kv_norm'][layer][:, None])
        m['wukv'] = f(inp['mla_w_ukv'][layer][:, hg * 256:(hg + 1) * 256])
        maps.append(m)
    return maps


_CACHE = {}


TPC = 2048
GT = 4
NG = TPC // (GT * 128)


def decl_B(nc, sfx, moe):
    n_exp = N_EXP if moe else 1
    io = {}
    io['cvec'] = nc.dram_tensor('cvec' + sfx, [128, 8], F32, kind='ExternalInput').ap()
    io['adaw'] = nc.dram_tensor('adaw' + sfx, [128, 8, 4096], F32, kind='ExternalInput').ap()
    io['adab_row'] = nc.dram_tensor('adab_row' + sfx, [1, 4096], F32, kind='ExternalInput').ap()
    io['adab_col'] = nc.dram_tensor('adab_col' + sfx, [128, 32], F32, kind='ExternalInput').ap()
    io['gpost_m'] = nc.dram_tensor('gpost_m' + sfx, [1, D], F32, kind='ExternalInput').ap()
    io['gpre_f'] = nc.dram_tensor('gpre_f' + sfx, [128, 8], F32, kind='ExternalInput').ap()
    io['gpost_f'] = nc.dram_tensor('gpost_f' + sfx, [1, D], F32, kind='ExternalInput').ap()
    io['gluw_d'] = nc.dram_tensor('gluw' + sfx, [128, 2, 256], F32, kind='ExternalInput').ap()
    io['glub_d'] = nc.dram_tensor('glub' + sfx, [1, 256], F32, kind='ExternalInput').ap()
    io['ssmg_d'] = nc.dram_tensor('ssmg' + sfx, [1, 256], F32, kind='ExternalInput').ap()
    io['mlag_d'] = nc.dram_tensor('mlag' + sfx, [1, 512], F32, kind='ExternalInput').ap()
    io['wout_d'] = nc.dram_tensor('wout' + sfx, [128, 8, D], F32, kind='ExternalInput').ap()
    io['wg_d'] = nc.dram_tensor('wg' + sfx, [n_exp * NFC, 128, 1024], F32, kind='ExternalInput').ap()
    io['wu_d'] = nc.dram_tensor('wu' + sfx, [n_exp * NFC, 128, 1024], F32, kind='ExternalInput').ap()
    io['wd_d'] = nc.dram_tensor('wd' + sfx, [n_exp * NFC, 128, 1024], F32, kind='ExternalInput').ap()
    if moe:
        io['router_d'] = nc.dram_tensor('router' + sfx, [128, 8, 8], F32, kind='ExternalInput').ap()
    return io


def emit_B(P, nc, banks, io, x, hxall, HXALL, mixloc, out, rank256, moe, ngroups=NG, xdep=None, on_group=None):
    n_exp = N_EXP if moe else 1
    cvec = io['cvec']
    adaw = io['adaw']
    adab_row = io['adab_row']
    adab_col = io['adab_col']
    gpost_m = io['gpost_m']
    gpre_f = io['gpre_f']
    gpost_f = io['gpost_f']
    gluw_d = io['gluw_d']
    glub_d = io['glub_d']
    ssmg_d = io['ssmg_d']
    mlag_d = io['mlag_d']
    wout_d = io['wout_d']
    wg_d = io['wg_d']
    wu_d = io['wu_d']
    wd_d = io['wd_d']
    router_d = io.get('router_d')
    st = ExitStack()
    old_es = P.es
    P.es = st
    P.pre = {}
    b0 = banks[0][:, :].bitcast(BF16)
    TB = P.view(b0[:, 0:512], 'TB', parent=banks[0])
    ZP = P.view(banks[1][:, 0:256], 'ZP', parent=banks[1])
    LG = P.view(banks[1][:, 256:264], 'LG', parent=banks[1])
    MC = P.view(banks[1][:, 272:288], 'MC', parent=banks[1])
    YB = [P.view(banks[2][:, :], 'YA', parent=banks[2]), P.view(banks[3][:, :], 'YB', parent=banks[3])]
    GB = [P.view(banks[4][:, :], 'G0', parent=banks[4]), P.view(banks[5][:, :], 'G1', parent=banks[5])]
    UB = [P.view(banks[6][:, :], 'U0', parent=banks[6]), P.view(banks[7][:, :], 'U1', parent=banks[7])]

    ident = P.sb([128, 128], BF16, 'ident', keep=True)
    vecm = P.sb([128, D], F32, 'vecm', keep=True)
    vecf = P.sb([128, D], F32, 'vecf', keep=True)
    modc = P.sb([128, 16], F32, 'modc', keep=True)
    scale2 = P.sb([128, 8], F32, 'scale2', keep=True)
    woutb = P.sb([128, 8, D], BF16, 'woutb', keep=True)
    gluwb = P.sb([128, 2, 256], BF16, 'gluwb', keep=True)
    glub_t = P.sb([128, 256], F32, 'glub_t', keep=True)
    ssmg_t = P.sb([128, 256], F32, 'ssmg_t', keep=True)
    mlag_t = P.sb([128, 512], F32, 'mlag_t', keep=True)
    if moe:
        routerb = P.sb([128, 8, 8], BF16, 'routerb', keep=True)
    X1 = [P.sb([128, D], F32, 'X1_%d' % i, keep=True) for i in range(GT)]
    hT2 = P.sb([128, 8, GT * 128], BF16, 'hT2', keep=True)
    yacc = [P.sb([128, D], F32, 'yacc%d' % i, keep=True) for i in range(GT)]
    gatew = P.sb([128, GT, 8], F32, 'gatew', keep=True)

    tes = ExitStack()
    P.tes = tes
    make_ident(P)
    cv = P.sb([128, 8], F32, 'cv')
    cvb = P.sb([128, 8], BF16, 'cvb')
    cvbb = P.sb([128, 8, 128], BF16, 'cvbb')
    P.dma('sp', cv[:], cvec[:, :], w=[cv])
    P.act(cvb, cvb[:], cv, cv[:], AF.Silu)
    P.cp('dve', cvbb, cvbb[:], cvb, cvb[:].unsqueeze(2).broadcast_to([128, 8, 128]))
    abc = P.sb([128, 32], F32, 'abc')
    P.dma('sp', abc[:], adab_col[:, :], w=[abc])
    wts = [P.sb([128, 8, 1024], BF16, 'adw%d' % i) for i in range(2)]
    stager = make_stager(P)
    rowb = P.sb([128, D], F32, 'rowb')
    for ci in range(4):
        wt = wts[ci % 2]
        for kc in range(8):
            stager(wt, wt[:, kc, :], adaw[:, kc, ci * 1024:(ci + 1) * 1024])
        if ci in (0, 3):
            vec = vecm if ci == 0 else vecf
            gsrc = gpost_m if ci == 0 else gpost_f
            P.dma('sp', rowb[:], adab_row[0:1, ci * 1024:(ci + 1) * 1024].broadcast_to([128, 1024]), w=[rowb])
            for half in range(2):
                yb = YB[half]
                for kc in range(8):
                    P.mm(yb, yb[:, :], cvbb, cvbb[:, kc, :], wt, wt[:, kc, half * 512:(half + 1) * 512],
                         start=(kc == 0), stop=(kc == 7))
                P.tt('dve', vec, vec[:, half * 512:(half + 1) * 512], yb, yb[:, :], rowb, rowb[:, half * 512:(half + 1) * 512], ALU.add)
            P.dma('sp', rowb[:], gsrc[0:1, :].broadcast_to([128, 1024]), w=[rowb])
            P.tt('dve', vec, vec[:], vec, vec[:], rowb, rowb[:], ALU.mult)
        else:
            for cc in range(8):
                ch = (ci - 1) * 8 + cc
                for kc in range(8):
                    P.mm(MC, MC[:, ch:ch + 1], wt, wt[:, kc, cc * 128:(cc + 1) * 128], cvb, cvb[:, kc:kc + 1],
                         start=(kc == 0), stop=(kc == 7))
    P.tt('dve', modc, modc[:], MC, MC[:, 0:16], abc, abc[:, 8:24], ALU.add)
    gpf = P.sb([128, 8], F32, 'gpf')
    P.dma('sp', gpf[:], gpre_f[:, :], w=[gpf])
    P.ts('dve', scale2, scale2[:], modc, modc[:, 8:16], 1.0, ALU.add)
    P.tt('dve', scale2, scale2[:], scale2, scale2[:], gpf, gpf[:], ALU.mult)
    sh2 = P.view(modc[:, 0:8], 'sh2', parent=modc)
    for kc in range(8):
        stager(woutb, woutb[:, kc, :], wout_d[:, kc, :])
    P.dma('pool', gluwb[:], gluw_d[:, :, :], w=[gluwb])
    P.dma('sp', glub_t[:], glub_d[0:1, :].broadcast_to([128, 256]), w=[glub_t])
    P.dma('sp', ssmg_t[:], ssmg_d[0:1, :].broadcast_to([128, 256]), w=[ssmg_t])
    P.dma('sp', mlag_t[:], mlag_d[0:1, :].broadcast_to([128, 512]), w=[mlag_t])
    if moe:
        P.dma('pool', routerb[:], router_d[:, :, :], w=[routerb])
    P.barrier()
    tes.close()
    P.tes = None

    MIXLOC = P.view(mixloc, 'MIXLOC')
    for kk in range(2):
        src_ = hxall.rearrange("(a b) c -> a (b c)", b=32)[bass.ds(rank256 + kk * 128, 128), :]
        P.dma('sp', mixloc[kk].rearrange("r n c -> (r n) c").rearrange("(a b) c -> a (b c)", b=32), src_, r=[HXALL], w=[MIXLOC])
    ev_i = [0]

    def evac_eng():
        ev_i[0] += 1
        return 'act' if ev_i[0] % 2 == 0 else 'dve'

    for g in range(ngroups):
        ph = ExitStack()
        P.tes = ph
        junk = P.sb([128, D], BF16, 'junk')

        def p1_bufs(i):
            return dict(xt=P.sb([128, D], F32, 'xt%d' % i), mt=P.sb([128, D], F32, 'mt%d' % i),
                        hxt=P.sb([128, 4, 256], F32, 'hxt%d' % i), ysb=P.sb([128, 256], BF16, 'ysb%d' % i),
                        ysT=P.sb([128, 2, 128], BF16, 'ysT%d' % i), zz=P.sb([128, 256], F32, 'zz%d' % i),
                        s2=P.sb([128, 256], F32, 's2%d' % i), catb=P.sb([128, D], BF16, 'catb%d' % i),
                        catT=P.sb([128, 8, 128], BF16, 'catT%d' % i), tmp=P.sb([128, D], F32, 'tmp%d' % i),
                        xn2=P.sb([128, D], BF16, 'xn2%d' % i), ss=P.sb([128, 4], F32, 'ss%d' % i),
                        lg=P.sb([128, 8], F32, 'lg%d' % i), lg2=P.sb([128, 8], F32, 'lg2%d' % i),
                        mk1=P.sb([128, 8], F32, 'mk1%d' % i), mk2=P.sb([128, 8], F32, 'mk2%d' % i),
                        m12=P.sb([128, 4], F32, 'm12%d' % i))

        p1sets = [p1_bufs(0), p1_bufs(1)]

        def p1_tile(ti, B_, YK):
            xt, mt, hxt, ysb, ysT, zz, s2 = B_['xt'], B_['mt'], B_['hxt'], B_['ysb'], B_['ysT'], B_['zz'], B_['s2']
            catb, catT, tmp, xn2, ss = B_['catb'], B_['catT'], B_['tmp'], B_['xn2'], B_['ss']
            lg, lg2, mk1, mk2, m12 = B_['lg'], B_['lg2'], B_['mk1'], B_['mk2'], B_['m12']
            tok = slice((g * GT + ti) * 128, (g * GT + ti + 1) * 128)
            P.dma('sp', xt[:], x[tok, :], r=([xdep] if xdep is not None else []), w=[xt])
            row0 = (g * GT + ti) * 128
            P.dma('sp', hxt[:], mixloc[row0 // 1024, :, row0 % 1024:row0 % 1024 + 128, :].rearrange("r n c -> n r c"), r=[MIXLOC], w=[hxt])
            for (c0, w_, d0) in ((0, 64, 0), (64, 64, 256), (128, 128, 512)):
                P.cp('pool', mt, mt[:, d0:d0 + 4 * w_].rearrange("p (r c) -> p r c", r=4), hxt, hxt[:, :, c0:c0 + w_])
            yield
            P.cp('dve', ysb, ysb[:], mt, mt[:, 256:512])
            yield
            for kc in range(2):
                P.tr(TB, TB[:, kc * 128:(kc + 1) * 128], ysb, ysb[:, kc * 128:(kc + 1) * 128], ident)
            P.cp('act', ysT, ysT[:].rearrange("p a b -> p (a b)"), TB, TB[:, 0:256])
            yield
            for kc in range(2):
                P.mm(ZP, ZP[:, :], ysT, ysT[:, kc, :], gluwb, gluwb[:, kc, :], start=(kc == 0), stop=(kc == 1))
            P.tt('dve', zz, zz[:], ZP, ZP[:, :], glub_t, glub_t[:], ALU.add)
            yield
            P.act(zz, zz[:], zz, zz[:], AF.Sigmoid)
            P.op('pool', lambda e: e.memset(ss[:], 0.0), w=[ss])
            yield
            P.tt('dve', s2, s2[:], zz, zz[:], mt, mt[:, 256:512], ALU.mult)
            P.act(junk, junk[:, 0:512], mt, mt[:, 512:1024], AF.Square, extra_w=[ss], accum_out=ss[:, 1:2])
            yield
            P.act(junk, junk[:, 0:256], s2, s2[:], AF.Square, extra_w=[ss], accum_out=ss[:, 0:1])
            P.cp('act', catb, catb[:, 0:256], mt, mt[:, 0:256])
            yield
            P.ts('dve', ss, ss[:, 0:1], ss, ss[:, 0:1], 1.0 / 256, ALU.mult, EPS, ALU.add)
            P.ts('dve', ss, ss[:, 1:2], ss, ss[:, 1:2], 1.0 / 512, ALU.mult, EPS, ALU.add)
            yield
            P.act(ss, ss[:, 0:2], ss, ss[:, 0:2], AF.Sqrt)
            yield
            P.op('dve', lambda e: e.reciprocal(out=ss[:, 0:2], in_=ss[:, 0:2]), r=[ss], w=[ss])
            yield
            P.op('dve', lambda e: e.scalar_tensor_tensor(out=catb[:, 256:512], in0=s2[:], scalar=ss[:, 0:1], in1=ssmg_t[:],
                                                         op0=ALU.mult, op1=ALU.mult), r=[s2, ss, ssmg_t], w=[catb])
            P.op('dve', lambda e: e.scalar_tensor_tensor(out=catb[:, 512:1024], in0=mt[:, 512:1024], scalar=ss[:, 1:2], in1=mlag_t[:],
                                                         op0=ALU.mult, op1=ALU.mult), r=[mt, ss, mlag_t], w=[catb])
            yield
            for half in range(2):
                for c4 in range(4):
                    c = half * 4 + c4
                    P.tr(TB, TB[:, c4 * 128:(c4 + 1) * 128], catb, catb[:, c * 128:(c + 1) * 128], ident)
                P.cp(evac_eng(), catT, catT[:, half * 4:(half + 1) * 4, :].rearrange("p a b -> p (a b)"), TB, TB[:, :])
                yield
            for half in range(2):
                for kc in range(8):
                    P.mm(YK[half], YK[half][:, :], catT, catT[:, kc, :], woutb, woutb[:, kc, half * 512:(half + 1) * 512],
                         start=(kc == 0), stop=(kc == 7))
            P.op('pool', lambda e: e.memset(ss[:, 2:4], 0.0), w=[ss])
            yield
            for half in range(2):
                P.act(junk, junk[:, 0:512], YK[half], YK[half][:, :], AF.Square, extra_w=[ss], accum_out=ss[:, 2 + half:3 + half])
            yield
            P.tt('dve', ss, ss[:, 2:3], ss, ss[:, 2:3], ss, ss[:, 3:4], ALU.add)
            P.ts('dve', ss, ss[:, 2:3], ss, ss[:, 2:3], 1.0 / D, ALU.mult, EPS, ALU.add)
            yield
            P.act(ss, ss[:, 2:3], ss, ss[:, 2:3], AF.Sqrt)
            yield
            P.op('dve', lambda e: e.reciprocal(out=ss[:, 2:3], in_=ss[:, 2:3]), r=[ss], w=[ss])
            yield
            for half in range(2):
                hs = slice(half * 512, (half + 1) * 512)
                P.op('dve', lambda e: e.scalar_tensor_tensor(out=tmp[:, hs], in0=YK[half][:, :], scalar=ss[:, 2:3], in1=vecm[:, hs],
                                                             op0=ALU.mult, op1=ALU.mult), r=[YK[half], ss, vecm], w=[tmp])
            yield
            P.tt('dve', X1[ti], X1[ti][:], tmp, tmp[:], xt, xt[:], ALU.add)
            P.op('pool', lambda e: e.memset(ss[:, 3:4], 0.0), w=[ss])
            yield
            P.act(junk, junk[:], X1[ti], X1[ti][:], AF.Square, extra_w=[ss], accum_out=ss[:, 3:4])
            yield
            P.ts('dve', ss, ss[:, 3:4], ss, ss[:, 3:4], 1.0 / D, ALU.mult, EPS, ALU.add)
            yield
            P.act(ss, ss[:, 3:4], ss, ss[:, 3:4], AF.Sqrt)
            yield
            P.op('dve', lambda e: e.reciprocal(out=ss[:, 3:4], in_=ss[:, 3:4]), r=[ss], w=[ss])
            yield
            P.ts('dve', xn2, xn2[:], X1[ti], X1[ti][:], ss[:, 3:4], ALU.mult, extra_r=[ss])
            yield
            for half in range(2):
                for c4 in range(4):
                    c = half * 4 + c4
                    P.tr(TB, TB[:, c4 * 128:(c4 + 1) * 128], xn2, xn2[:, c * 128:(c + 1) * 128], ident)
                for c4 in range(4):
                    c = half * 4 + c4
                    if c4 % 2 == 0:
                        P.act(hT2, hT2[:, c, ti * 128:(ti + 1) * 128], TB, TB[:, c4 * 128:(c4 + 1) * 128], AF.Identity,
                              extra_r=[scale2, sh2], scale=scale2[:, c:c + 1], bias=sh2[:, c:c + 1])
                    else:
                        P.ts('dve', hT2, hT2[:, c, ti * 128:(ti + 1) * 128], TB, TB[:, c4 * 128:(c4 + 1) * 128],
                             scale2[:, c:c + 1], ALU.mult, sh2[:, c:c + 1], ALU.add, extra_r=[scale2, sh2])
                yield
            if moe:
                for kc in range(8):
                    P.mm(LG, LG[:, :], hT2, hT2[:, kc, ti * 128:(ti + 1) * 128], routerb, routerb[:, kc, :],
                         start=(kc == 0), stop=(kc == 7))
                P.cp('dve', lg, lg[:], LG, LG[:, :])
                yield
                P.op('dve', lambda e: e.reduce_max(out=m12[:, 0:1], in_=lg[:], axis=AX.X), r=[lg], w=[m12])
                yield
                P.ts('dve', mk1, mk1[:], lg, lg[:], m12[:, 0:1], ALU.is_equal, extra_r=[m12])
                yield
                P.op('dve', lambda e: e.scalar_tensor_tensor(out=lg2[:], in0=mk1[:], scalar=-1e30, in1=lg[:],
                                                             op0=ALU.mult, op1=ALU.add), r=[mk1, lg], w=[lg2])
                yield
                P.op('dve', lambda e: e.reduce_max(out=m12[:, 1:2], in_=lg2[:], axis=AX.X), r=[lg2], w=[m12])
                yield
                P.ts('dve', mk2, mk2[:], lg2, lg2[:], m12[:, 1:2], ALU.is_equal, extra_r=[m12])
                P.tt('dve', m12, m12[:, 2:3], m12, m12[:, 1:2], m12, m12[:, 0:1], ALU.subtract)
                yield
                P.act(m12, m12[:, 2:3], m12, m12[:, 2:3], AF.Exp)
                yield
                P.ts('dve', m12, m12[:, 3:4], m12, m12[:, 2:3], 1.0, ALU.add)
                yield
                P.op('dve', lambda e: e.reciprocal(out=m12[:, 3:4], in_=m12[:, 3:4]), r=[m12], w=[m12])
                yield
                P.tt('dve', m12, m12[:, 2:3], m12, m12[:, 2:3], m12, m12[:, 3:4], ALU.mult)
                P.ts('dve', mk1, mk1[:], mk1, mk1[:], m12[:, 3:4], ALU.mult, extra_r=[m12])
                yield
                P.op('dve', lambda e: e.scalar_tensor_tensor(out=gatew[:, ti, :], in0=mk2[:], scalar=m12[:, 2:3], in1=mk1[:],
                                                             op0=ALU.mult, op1=ALU.add), r=[mk2, m12, mk1], w=[gatew])
                yield

        for pair in range(GT // 2):
            gens = [p1_tile(2 * pair, p1sets[0], YB), p1_tile(2 * pair + 1, p1sets[1], GB)]
            while gens:
                alive = []
                for gen_ in gens:
                    try:
                        next(gen_)
                        alive.append(gen_)
                    except StopIteration:
                        pass
                gens = alive
        P.barrier()
        ph.close()
        ph = ExitStack()
        P.tes = ph
        actT = P.sb([128, NFC, GT * 128], BF16, 'actT')
        wdb = P.sb([128, NFC, D], BF16, 'wdb')
        stg = [P.sb([128, 1024], F32, 'stg%d' % i) for i in range(4)]
        stgd = [P.sb([128, 1024], F32, 'stgd%d' % i) for i in range(2)]
        NWB = 3
        wgb = [P.sb([128, 8, 128], BF16, 'wgb%d' % i) for i in range(NWB)]
        wub = [P.sb([128, 8, 128], BF16, 'wub%d' % i) for i in range(NWB)]
        gsil = [P.sb([128, GT * 128], F32, 'gsil%d' % i) for i in range(2)]
        ftmp = P.sb([128, D], F32, 'ftmp')
        fjunk = P.sb([128, D], BF16, 'fjunk')
        fss = P.sb([128, 1], F32, 'fss')
        st_i = [0]

        sd_i = [0]

        def load_cast(dst, dst_ap, src_ap, eng):
            if eng == 'pool':
                sg = stgd[sd_i[0] % 2]
                sd_i[0] += 1
            else:
                sg = stg[st_i[0] % 4]
                st_i[0] += 1
            P.dma('sp', sg[:], src_ap, w=[sg])
            P.cp(eng, dst, dst_ap, sg, sg[:])

        def load_gu(idx):
            b_ = idx % NWB
            load_cast(wgb[b_], wgb[b_][:].rearrange("p a b -> p (a b)"), wg_d[idx, :, :], 'dve')
            load_cast(wub[b_], wub[b_][:].rearrange("p a b -> p (a b)"), wu_d[idx, :, :], 'act')

        load_gu(0)
        load_gu(1)
        for e_ in range(n_exp):
            for fc in range(NFC):
                b2 = fc % 2
                wb = (e_ * NFC + fc) % NWB
                if e_ * NFC + fc + 2 < n_exp * NFC:
                    load_gu(e_ * NFC + fc + 2)
                load_cast(wdb, wdb[:, fc, :], wd_d[e_ * NFC + fc, :, :], 'pool')
                for kc in range(8):
                    P.mm(GB[b2], GB[b2][:, :], wgb[wb], wgb[wb][:, kc, :], hT2, hT2[:, kc, :], start=(kc == 0), stop=(kc == 7))
                for kc in range(8):
                    P.mm(UB[b2], UB[b2][:, :], wub[wb], wub[wb][:, kc, :], hT2, hT2[:, kc, :], start=(kc == 0), stop=(kc == 7))
                P.act(gsil[b2], gsil[b2][:], GB[b2], GB[b2][:, :], AF.Silu)
                P.tt('dve', actT, actT[:, fc, :], UB[b2], UB[b2][:, :], gsil[b2], gsil[b2][:], ALU.mult)
            for ti in range(GT):
                for half in range(2):
                    hs = slice(half * 512, (half + 1) * 512)
                    yb = YB[(ti * 2 + half) % 2]
                    for fc in range(NFC):
                        P.mm(yb, yb[:, :], actT, actT[:, fc, ti * 128:(ti + 1) * 128], wdb, wdb[:, fc, hs],
                             start=(fc == 0), stop=(fc == NFC - 1))
                    if not moe:
                        P.cp(evac_eng(), yacc[ti], yacc[ti][:, hs], yb, yb[:, :])
                    elif e_ == 0:
                        P.ts('dve', yacc[ti], yacc[ti][:, hs], yb, yb[:, :], gatew[:, ti, 0:1], ALU.mult, extra_r=[gatew])
                    else:
                        P.op('dve', lambda e: e.scalar_tensor_tensor(out=yacc[ti][:, hs], in0=yb[:, :], scalar=gatew[:, ti, e_:e_ + 1],
                                                                     in1=yacc[ti][:, hs], op0=ALU.mult, op1=ALU.add),
                             r=[yb, gatew, yacc[ti]], w=[yacc[ti]])
        for ti in range(GT):
            tok = slice((g * GT + ti) * 128, (g * GT + ti + 1) * 128)
            P.op('pool', lambda e: e.memset(fss[:], 0.0), w=[fss])
            P.act(fjunk, fjunk[:], yacc[ti], yacc[ti][:], AF.Square, extra_w=[fss], accum_out=fss[:, 0:1])
            P.rstd(fss, D, None)
            P.op('dve', lambda e: e.scalar_tensor_tensor(out=ftmp[:], in0=yacc[ti][:], scalar=fss[:, 0:1], in1=vecf[:],
                                                         op0=ALU.mult, op1=ALU.mult), r=[yacc[ti], fss, vecf], w=[ftmp])
            P.tt('dve', ftmp, ftmp[:], ftmp, ftmp[:], X1[ti], X1[ti][:], ALU.add)
            P.dma('sp', out[tok, :], ftmp[:], r=[ftmp])
        P.barrier()
        ph.close()
        P.tes = None
        if on_group is not None:
            on_group(g)
    st.close()
    P.es = old_es
    P.pre = {}


def inputs_B(inp, layer, xcur, mix):
    f = lambda a: np.ascontiguousarray(a, dtype=np.float32)
    moe = (layer % 2 == 1)
    j = layer // 2
    xf = xcur.reshape(-1, D)
    mf = mix.reshape(-1, D)
    aw = f(inp['ada_w'][layer][:, 2048:6144].reshape(128, 8, 4096))
    ab = inp['ada_b'][layer]
    shared = dict(
        adaw=aw, adab_row=f(ab[2048:6144][None, :]), adab_col=chunkT(f(ab[2048:6144]), 32),
        gpost_m=f(inp['norm_post_mix'][layer][None, :]), gpre_f=chunkT(f(inp['norm_pre_ffn'][layer]), 8),
        gpost_f=f(inp['norm_post_ffn'][layer][None, :]),
        gluw=f(inp['ssm_glu_w'][layer].reshape(2, 128, 256).transpose(1, 0, 2)),
        glub=f(inp['ssm_glu_b'][layer][None, :]), ssmg=f(inp['ssm_norm'][layer][None, :]),
        mlag=f(inp['mla_norm'][layer][None, :]),
        wout=f(inp['w_out'][layer].reshape(8, 128, D).transpose(1, 0, 2)))
    if moe:
        wg, wu, wd = inp['moe_w_gate'][j], inp['moe_w_up'][j], inp['moe_w_down'][j]
        shared['router'] = f(inp['moe_router'][j].reshape(8, 128, 8).transpose(1, 0, 2))
    else:
        wg, wu, wd = inp['ffn_w_gate'][j][None], inp['ffn_w_up'][j][None], inp['ffn_w_down'][j][None]
    ne = wg.shape[0]

    def gu(w):
        return f(w.reshape(ne, 8, 128, NFC, 128).transpose(0, 3, 2, 1, 4).reshape(ne * NFC, 128, 1024))

    shared['wg'] = gu(wg)
    shared['wu'] = gu(wu)
    shared['wd'] = f(wd.reshape(ne * NFC, 128, D))
    maps = []
    for core in range(8):
        b = core // 4
        m = dict(shared)
        m['x'] = f(xf[core * TPC:(core + 1) * TPC])
        m['mix'] = f(mf[core * TPC:(core + 1) * TPC])
        m['cvec'] = f(inp['c'][b].reshape(128, 8))
        maps.append(m)
    return maps


RG4 = [[0, 1, 2, 3], [4, 5, 6, 7]]


def build_fused(ntiles=NT, ngroups=NG):
    nc = bass.Bass("TRN2", target_bir_lowering=False)
    ioA = [decl_A(nc, '_a%d' % l) for l in range(2)]
    ioB = [decl_B(nc, '_b%d' % l, moe=(l == 1)) for l in range(2)]
    x = nc.dram_tensor("x", [L, D], F32, kind="ExternalInput").ap()
    xsl = nc.dram_tensor("xsl", [TPC, D], F32, kind="ExternalInput").ap()
    out = nc.dram_tensor("out", [TPC, D], F32, kind="ExternalOutput").ap()
    hx = [nc.dram_tensor("hx%d" % l, [L, 256], F32).ap() for l in range(2)]
    hxall = [nc.dram_tensor("hxall%d" % l, [8 * 4 * 1024, 256], F32).ap() for l in range(2)]
    mixloc = [nc.dram_tensor("mixloc%d" % l, [2, 4, 1024, 256], F32).ap() for l in range(2)]
    xs = nc.dram_tensor("xs", [TPC, D], F32).ap()
    xall = nc.dram_tensor("xall", [8 * 4 * 256, D], F32).ap()

    def xrow1(t):
        tok0 = t * 128
        r_, j_, i0 = tok0 // TPC, (tok0 % TPC) // 256, tok0 % 256
        return xall[(j_ * 4 + r_) * 256 + i0:(j_ * 4 + r_) * 256 + i0 + 128, :]

    with ExitStack() as es:
        P = Prog(nc, es)
        banks = [P.ps([128, 512], F32, 'bank%d' % i) for i in range(8)]
        rank256 = nc.sync.snap((nc.sync.partition_id() % 4) * 256, min_val=0, max_val=768)

        H = [P.view(hxall[l], 'HXALL%d' % l) for l in range(2)]
        XALL = P.view(xall, 'XALL')

        def hx_chunk(l):
            def f(k, evs):
                for ev in evs:
                    P._wait('pool', ev)
                P.coll('AllGather', hx[l][k * 1024:(k + 1) * 1024, :], H[l], hxall[l][k * 4096:(k + 1) * 4096, :], RG4)
            return f

        def xs_group(g):
            for j in (2 * g, 2 * g + 1):
                P.coll('AllGather', xs[j * 256:(j + 1) * 256, :], XALL, xall[j * 1024:(j + 1) * 1024, :], RG4)

        emit_A(P, nc, banks, ioA[0], lambda t: x[t * 128:(t + 1) * 128, :], hx[0], ntiles, on_chunk=hx_chunk(0))
        emit_B(P, nc, banks, ioB[0], xsl, hxall[0], H[0], mixloc[0], xs, rank256, False, ngroups, on_group=xs_group)
        emit_A(P, nc, banks, ioA[1], xrow1, hx[1], ntiles, xdep=XALL, on_chunk=hx_chunk(1))
        emit_B(P, nc, banks, ioB[1], xs, hxall[1], H[1], mixloc[1], out, rank256, True, ngroups)
        P.finish()
    return nc


def fused_inputs(inp):
    x = np.ascontiguousarray(inp['x'], dtype=np.float32)
    dummy = np.zeros((2, L, D), np.float32)
    maps = [dict() for _ in range(8)]
    for layer in range(2):
        ma = inputs_A(inp, layer, x)
        mb = inputs_B(inp, layer, x, dummy)
        for c in range(8):
            for k, v in ma[c].items():
                if k != 'x':
                    maps[c][k + '_a%d' % layer] = v
            for k, v in mb[c].items():
                if k not in ('x', 'mix'):
                    maps[c][k + '_b%d' % layer] = v
    xf = x.reshape(-1, D)
    for c in range(8):
        maps[c]['x'] = x[c // 4]
        maps[c]['xsl'] = np.ascontiguousarray(xf[c * TPC:(c + 1) * TPC])
    return maps


def kernel(**inputs):
    inp = {k: np.asarray(v) for k, v in inputs.items()}
    if 'F' not in _CACHE:
        _CACHE['F'] = build_fused()
    res = run_bass_kernel_spmd(_CACHE['F'], fused_inputs(inp), core_ids=list(range(8)))
    return np.concatenate([res.results[c]['out'] for c in range(8)], axis=0).reshape(2, L, D).astype(np.float32)
```

```python
import math
from contextlib import ExitStack

import numpy as np
import concourse.bass as bass
import concourse.mybir as mybir
from concourse.bass_utils import run_bass_kernel_spmd

F32 = mybir.dt.float32
BF16 = mybir.dt.bfloat16
I32 = mybir.dt.int32
ALU = mybir.AluOpType
AF = mybir.ActivationFunctionType
AX = mybir.AxisListType

D = 1024
L = 8192
NT = 64
EPS = 1e-6
TWO_PI = 2.0 * math.pi
D_FF = 3584
NFC = 28
N_EXP = 8


class T:
    def __init__(self, t, name, root=None):
        self.t = t
        self.name = name
        self.lastw = None
        self.readers = {}
        self.root = self if root is None else root.root
        self.excl = False

    def __getitem__(self, idx):
        return self.t[idx]


class Prog:
    def __init__(self, nc, es, ndma_sems=8):
        self.nc = nc
        self.es = es
        self.engs = {'pe': nc.tensor, 'act': nc.scalar, 'dve': nc.vector, 'pool': nc.gpsimd, 'sp': nc.sync}
        self.sem = {k: es.enter_context(nc.semaphore('s_' + k)) for k in self.engs}
        self.cnt = {k: 0 for k in self.engs}
        self.waited = {k: {} for k in self.engs}
        self.dma_sems = {}
        for q in ('sp', 'pool'):
            self.dma_sems[q] = [[es.enter_context(nc.semaphore('d_%s%d' % (q, i))), 0] for i in range(ndma_sems)]
        self.dma_i = {q: 0 for q in self.dma_sems}
        self.cc_sem = es.enter_context(nc.semaphore('cc_sem'))
        self.cc_n = 0
        self.nbuf = 0
        self.tes = None
        self.pre = {}

    def sb(self, shape, dt, name=None, keep=False):
        if keep and name in self.pre:
            assert list(self.pre[name].t.shape) == list(shape), name
            return self.pre[name]
        assert not (keep and self.tes is not None), "persistent buffer %s must be pre-allocated" % name
        self.nbuf += 1
        uname = (name or 'b') + '_%d' % self.nbuf
        es = self.es if (keep or self.tes is None) else self.tes
        t = T(es.enter_context(self.nc.sbuf_tensor(uname, shape, dt)), uname)
        if keep:
            self.pre[name] = t
        return t

    def ps(self, shape, dt, name=None):
        self.nbuf += 1
        name = name or ('p%d' % self.nbuf)
        t = T(self.es.enter_context(self.nc.psum_tensor(name, shape, dt)), name)
        t.excl = True
        return t

    def view(self, ap, name='v', parent=None):
        return T(ap, name, root=parent)

    def _wait(self, engname, ev):
        sem, val, src = ev
        if src == 'pe' and engname == 'pe':
            return
        key = id(sem)
        w = self.waited[engname]
        if w.get(key, 0) >= val:
            return
        w[key] = val
        self.engs[engname].wait_ge(sem, val)

    def _deps(self, engname, r, w):
        for b in r:
            b = b.root
            if b.lastw is not None:
                self._wait(engname, b.lastw)
        for b in w:
            b = b.root
            if b.lastw is not None:
                self._wait(engname, b.lastw)
            for ev in b.readers.values():
                self._wait(engname, ev)

    def _commit(self, ev, r, w):
        for b in r:
            b = b.root
            old = b.readers.get(id(ev[0]))
            if old is None or old[1] < ev[1]:
                b.readers[id(ev[0])] = ev
        for b in w:
            b = b.root
            b.lastw = ev
            b.readers = {}

    def op(self, engname, fn, r=(), w=()):
        xr = [b for b in r if b.root.excl]
        if xr:
            r = [b for b in r if not b.root.excl]
            w = list(w) + xr
        self._deps(engname, r, w)
        ins = fn(self.engs[engname])
        self.cnt[engname] += 1
        ins.then_inc(self.sem[engname], 1)
        ev = (self.sem[engname], self.cnt[engname], engname)
        self._commit(ev, r, w)
        return ev

    def dma(self, q, out, in_, r=(), w=(), **kw):
        if out.dtype != in_.dtype:
            q = 'pool'
        self._deps(q, r, w)
        slot = self.dma_sems[q][self.dma_i[q] % len(self.dma_sems[q])]
        self.dma_i[q] += 1
        sem, n = slot
        if n > 0:
            self._wait(q, (sem, 16 * n, 'dma'))
        slot[1] = n + 1
        self.engs[q].dma_start(out=out, in_=in_, **kw).then_inc(sem, 16)
        ev = (sem, 16 * (n + 1), 'dma')
        self._commit(ev, r, w)
        return ev

    def coll(self, kind, src_ap, dst, dst_ap, groups):
        self.nc.gpsimd.collective_compute(kind, ALU.bypass, replica_groups=groups, ins=[src_ap], outs=[dst_ap]).then_inc(self.cc_sem, 1)
        self.cc_n += 1
        ev = (self.cc_sem, self.cc_n, 'cc')
        self._commit(ev, [], [dst])
        return ev

    def barrier(self):
        for e in self.engs:
            for o in self.engs:
                if o != e and self.cnt[o] > 0:
                    self._wait(e, (self.sem[o], self.cnt[o], 'x'))
            for q in self.dma_sems:
                for sem, n in self.dma_sems[q]:
                    if n > 0:
                        self._wait(e, (sem, 16 * n, 'dma'))
            if self.cc_n > 0:
                self._wait(e, (self.cc_sem, self.cc_n, 'cc'))

    def finish(self):
        self.barrier()
        for q in self.dma_sems:
            for sem, n in self.dma_sems[q]:
                if n > 0:
                    self._wait('sp', (sem, 16 * n, 'dma'))

    def mm(self, out, oap, lhsT, lap, rhs, rap, start=True, stop=True):
        return self.op('pe', lambda e: e.matmul(oap, lhsT=lap, rhs=rap, start=start, stop=stop),
                       r=[lhsT, rhs], w=[out])

    def tr(self, out, oap, src, sap, ident):
        n = sap.shape[0]
        return self.op('pe', lambda e: e.transpose(oap, sap, ident[0:n, 0:n]), r=[src, ident], w=[out])

    def tt(self, eng, out, oap, a, aap, b, bap, op):
        return self.op(eng, lambda e: e.tensor_tensor(out=oap, in0=aap, in1=bap, op=op), r=[a, b], w=[out])

    def ts(self, eng, out, oap, a, aap, s1, op0, s2=None, op1=None, extra_r=()):
        if op1 is None:
            return self.op(eng, lambda e: e.tensor_scalar(out=oap, in0=aap, scalar1=s1, scalar2=None, op0=op0),
                           r=[a] + list(extra_r), w=[out])
        return self.op(eng, lambda e: e.tensor_scalar(out=oap, in0=aap, scalar1=s1, scalar2=s2, op0=op0, op1=op1),
                       r=[a] + list(extra_r), w=[out])

    def act(self, out, oap, a, aap, func, extra_r=(), extra_w=(), **kw):
        return self.op('act', lambda e: e.activation(out=oap, in_=aap, func=func, **kw),
                       r=[a] + list(extra_r), w=[out] + list(extra_w))

    def cp(self, eng, out, oap, a, aap):
        if eng == 'act':
            return self.act(out, oap, a, aap, AF.Copy)
        return self.op(eng, lambda e: e.tensor_copy(out=oap, in_=aap), r=[a], w=[out])

    def rstd(self, ss, n, tmp):
        self.ts('dve', ss, ss[:], ss, ss[:], 1.0 / n, ALU.mult, EPS, ALU.add)
        self.act(ss, ss[:], ss, ss[:], AF.Sqrt)
        self.op('dve', lambda e: e.reciprocal(out=ss[:], in_=ss[:]), r=[ss], w=[ss])

    def range_reduce(self, out, oap, src, sap, kf, kfap, ki, kiap, shift=0.0):
        self.ts('dve', kf, kfap, src, sap, 1.0 / TWO_PI, ALU.mult, shift / TWO_PI, ALU.add)
        self.cp('dve', ki, kiap, kf, kfap)
        self.cp('dve', kf, kfap, ki, kiap)
        self.op('dve', lambda e: e.scalar_tensor_tensor(out=oap, in0=kfap, scalar=-TWO_PI, in1=sap,
                                                        op0=ALU.mult, op1=ALU.add), r=[kf, src], w=[out])
        if shift != 0.0:
            self.ts('dve', out, oap, out, oap, shift, ALU.add)
        self.ts('dve', out, oap, out, oap, -math.pi, ALU.max, math.pi, ALU.min)


def make_stager(P, nstage=4, cols=1024):
    stg = [P.sb([128, cols], F32, 'stage%d' % i) for i in range(nstage)]
    cnt = [0]
    engs = ('dve', 'act', 'pool')

    def load(dst, dst_ap, src_ap):
        n = src_ap.shape[-1]
        sg = stg[cnt[0] % nstage]
        eng = engs[cnt[0] % 3]
        cnt[0] += 1
        P.dma('sp', sg[:, 0:n], src_ap, w=[sg])
        P.cp(eng, dst, dst_ap, sg, sg[:, 0:n])
    return load


def make_ident(P):
    identf = P.sb([128, 128], F32, 'identf')
    ident = P.sb([128, 128], BF16, 'ident', keep=True)
    P.op('pool', lambda e: e.memset(identf[:], 1.0), w=[identf])
    P.op('pool', lambda e: e.affine_select(out=identf[:], in_=identf[:], pattern=[[-1, 128]],
                                           compare_op=ALU.is_equal, fill=0.0, base=0, channel_multiplier=1),
         r=[identf], w=[identf])
    P.cp('dve', ident, ident[:], identf, identf[:])
    return ident, identf


def adaln_mod(P, nc, cvec, adaw, adab, ncol_chunks, modps, name, stager):
    cv = P.sb([128, 8], F32, name + '_cv')
    cvb = P.sb([128, 8], BF16, name + '_cvb')
    P.dma('sp', cv[:], cvec[:, :], w=[cv])
    P.act(cvb, cvb[:], cv, cv[:], AF.Silu)
    ncols = ncol_chunks * 128
    mod = P.sb([128, ncol_chunks], F32, name + '_mod', keep=True)
    ab = P.sb([128, ncol_chunks], F32, name + '_ab')
    P.dma('sp', ab[:], adab[:, :], w=[ab])
    cw = 1024
    wts = [P.sb([128, 8, cw], BF16, name + '_w%d' % i) for i in range(2)]
    for ci in range(ncols // cw):
        wt = wts[ci % 2]
        for kc in range(8):
            stager(wt, wt[:, kc, :], adaw[:, kc, ci * cw:(ci + 1) * cw])
        for cc in range(cw // 128):
            ch = ci * (cw // 128) + cc
            for kc in range(8):
                P.mm(modps, modps[:, ch:ch + 1], wt, wt[:, kc, cc * 128:(cc + 1) * 128], cvb, cvb[:, kc:kc + 1],
                     start=(kc == 0), stop=(kc == 7))
    P.tt('dve', mod, mod[:], modps, modps[:, 0:ncol_chunks], ab, ab[:], ALU.add)
    return mod


import os
STOP_AT = float(os.environ.get('STOP_AT', '99'))


def decl_A(nc, sfx):
    io = {}
    io['cvec'] = nc.dram_tensor('cvec' + sfx, [128, 8], F32, kind='ExternalInput').ap()
    io['pos'] = nc.dram_tensor('pos' + sfx, [128, NT], I32, kind='ExternalInput').ap()
    io['adaw'] = nc.dram_tensor('adaw' + sfx, [128, 8, 2048], F32, kind='ExternalInput').ap()
    io['adab'] = nc.dram_tensor('adab' + sfx, [128, 16], F32, kind='ExternalInput').ap()
    io['gpre'] = nc.dram_tensor('gpre' + sfx, [128, 8], F32, kind='ExternalInput').ap()
    io['wsel'] = nc.dram_tensor('wsel' + sfx, [128, 8, 768], F32, kind='ExternalInput').ap()
    io['retg'] = nc.dram_tensor('retg' + sfx, [1, 64], F32, kind='ExternalInput').ap()
    io['invf'] = nc.dram_tensor('invf' + sfx, [1, 32], F32, kind='ExternalInput').ap()
    io['dmaskT_d'] = nc.dram_tensor('dmaskT' + sfx, [128, 128], F32, kind='ExternalInput').ap()
    io['tri_d'] = nc.dram_tensor('tri' + sfx, [128, 128], F32, kind='ExternalInput').ap()
    io['qdec_d'] = nc.dram_tensor('qdec' + sfx, [64, 128], F32, kind='ExternalInput').ap()
    io['kdec_d'] = nc.dram_tensor('kdec' + sfx, [128, 1], F32, kind='ExternalInput').ap()
    io['dcy_d'] = nc.dram_tensor('dcy' + sfx, [64, 1], F32, kind='ExternalInput').ap()
    io['cmask_d'] = nc.dram_tensor('cmask' + sfx, [128, 128], F32, kind='ExternalInput').ap()
    io['jp1_d'] = nc.dram_tensor('jp1' + sfx, [128, 1], F32, kind='ExternalInput').ap()
    io['irow1_d'] = nc.dram_tensor('irow1' + sfx, [128, 128], F32, kind='ExternalInput').ap()
    io['are_tm_d'] = nc.dram_tensor('are_tm' + sfx, [1, 512], F32, kind='ExternalInput').ap()
    io['aim_tm_d'] = nc.dram_tensor('aim_tm' + sfx, [1, 512], F32, kind='ExternalInput').ap()
    io['ldt_tm_d'] = nc.dram_tensor('ldt_tm' + sfx, [1, 512], F32, kind='ExternalInput').ap()
    io['BR_d'] = nc.dram_tensor('BR' + sfx, [64, 512], F32, kind='ExternalInput').ap()
    io['BI_d'] = nc.dram_tensor('BI' + sfx, [64, 512], F32, kind='ExternalInput').ap()
    io['are_sm_d'] = nc.dram_tensor('are_sm' + sfx, [128, 2], F32, kind='ExternalInput').ap()
    io['aim_sm_d'] = nc.dram_tensor('aim_sm' + sfx, [128, 2], F32, kind='ExternalInput').ap()
    io['ldt_sm_d'] = nc.dram_tensor('ldt_sm' + sfx, [128, 2], F32, kind='ExternalInput').ap()
    io['CR_d'] = nc.dram_tensor('CR' + sfx, [128, 2, 64], F32, kind='ExternalInput').ap()
    io['CI_d'] = nc.dram_tensor('CI' + sfx, [128, 2, 64], F32, kind='ExternalInput').ap()
    io['DD_d'] = nc.dram_tensor('DD' + sfx, [64, 64], F32, kind='ExternalInput').ap()
    io['qng_d'] = nc.dram_tensor('qng' + sfx, [128, 2], F32, kind='ExternalInput').ap()
    io['wuq_d'] = nc.dram_tensor('wuq' + sfx, [128, 2, 192], F32, kind='ExternalInput').ap()
    io['kvng_d'] = nc.dram_tensor('kvng' + sfx, [128, 1], F32, kind='ExternalInput').ap()
    io['wukv_d'] = nc.dram_tensor('wukv' + sfx, [128, 256], F32, kind='ExternalInput').ap()
    return io


def emit_A(P, nc, banks, io, xrow, hx, ntiles=NT, xdep=None, on_chunk=None):
    cvec = io['cvec']
    pos = io['pos']
    adaw = io['adaw']
    adab = io['adab']
    gpre = io['gpre']
    wsel = io['wsel']
    retg = io['retg']
    invf = io['invf']
    dmaskT_d = io['dmaskT_d']
    tri_d = io['tri_d']
    qdec_d = io['qdec_d']
    kdec_d = io['kdec_d']
    dcy_d = io['dcy_d']
    cmask_d = io['cmask_d']
    jp1_d = io['jp1_d']
    irow1_d = io['irow1_d']
    are_tm_d = io['are_tm_d']
    aim_tm_d = io['aim_tm_d']
    ldt_tm_d = io['ldt_tm_d']
    BR_d = io['BR_d']
    BI_d = io['BI_d']
    are_sm_d = io['are_sm_d']
    aim_sm_d = io['aim_sm_d']
    ldt_sm_d = io['ldt_sm_d']
    CR_d = io['CR_d']
    CI_d = io['CI_d']
    DD_d = io['DD_d']
    qng_d = io['qng_d']
    wuq_d = io['wuq_d']
    kvng_d = io['kvng_d']
    wukv_d = io['wukv_d']
    st = ExitStack()
    old_es = P.es
    P.es = st
    P.pre = {}
    b7 = banks[7][:, :].bitcast(BF16)
    TB = [P.view(b7[:, 0:512], 'TB', parent=banks[7])]
    TS = [P.view(b7[:, 256 + 128 * k:384 + 128 * k], 'TS%d' % k, parent=banks[7]) for k in range(2)]
    PJ0 = P.view(banks[0][:, :], 'PJ0', parent=banks[0])
    PJ1 = P.view(banks[1][:, 0:256], 'PJ1', parent=banks[1])
    RS = P.view(banks[1][:, 256:384], 'RS', parent=banks[1])
    RO = P.view(banks[1][:, 384:448], 'RO', parent=banks[1])
    RKV = P.view(banks[1][0:64, 448:512], 'RKV', parent=banks[1])
    SA = P.view(banks[2][:, :], 'SA', parent=banks[2])
    SB = P.view(banks[3][:, :], 'SB', parent=banks[3])
    QTN = P.view(banks[4][:, 0:128], 'QTN', parent=banks[4])
    KTN = P.view(banks[4][:, 128:256], 'KTN', parent=banks[4])
    VP = P.view(banks[4][:, 256:384], 'VP', parent=banks[4])
    QRP = P.view(banks[4][:, 384:448], 'QRP', parent=banks[4])
    SY = P.view(banks[4][:, 448:512], 'SY', parent=banks[4])
    SC = [P.view(banks[5][:, :], 'SC0', parent=banks[5]), P.view(banks[6][:, :], 'SC1', parent=banks[6])]

    for nm, shp, dt_ in [('ident', [128, 128], BF16), ('ada_mod', [128, 16], F32), ('scale1', [128, 8], F32),
                         ('wselb', [128, 8, 768], BF16), ('retg_t', [128, 64], F32), ('dmaskT', [128, 128], F32),
                         ('tri', [128, 128], BF16), ('qdec', [64, 128], F32), ('kdec', [128, 1], F32),
                         ('dcy', [64, 1], F32), ('cmask', [128, 128], F32), ('SINT', [128, NT * 32], F32),
                         ('COST', [128, NT * 32], F32), ('NSINT', [128, NT * 32], F32), ('EA', [128, 512], F32),
                         ('EB', [128, 512], F32), ('Bmat', [64, 512], BF16), ('Bswp', [64, 512], BF16),
                         ('EA2', [128, 512], F32), ('EB2', [128, 512], F32), ('Cmat', [128, 4, 64], BF16),
                         ('DDb', [64, 64], BF16), ('xprev', [128, 4], F32), ('xprevs', [128, 4], F32),
                         ('wuqb', [128, 2, 192], BF16), ('wukvb', [128, 256], BF16)]:
        P.sb(shp, dt_, nm, keep=True)
    tes = ExitStack()
    P.tes = tes
    ident, identf = make_ident(P)

    stager = make_stager(P)
    mod = adaln_mod(P, nc, cvec, adaw, adab, 16, SA, 'ada', stager)
    gp = P.sb([128, 8], F32, 'gp')
    P.dma('sp', gp[:], gpre[:, :], w=[gp])
    scale1 = P.sb([128, 8], F32, 'scale1', keep=True)
    P.ts('dve', scale1, scale1[:], mod, mod[:, 8:16], 1.0, ALU.add)
    P.tt('dve', scale1, scale1[:], scale1, scale1[:], gp, gp[:], ALU.mult)
    shm = P.view(mod[:, 0:8], 'shm', parent=mod)

    wselb = P.sb([128, 8, 768], BF16, 'wselb', keep=True)
    for kc in range(8):
        stager(wselb, wselb[:, kc, :], wsel[:, kc, :])

    def load(name, src, shape, dt=F32, q='sp', bcast=None, keep=False):
        t = P.sb(shape, dt, name, keep=keep)
        P.dma(q, t[:], src if bcast is None else src.broadcast_to(bcast), w=[t])
        return t

    retg_t = load('retg_t', retg[0:1, :], [128, 64], bcast=[128, 64], keep=True)
    invf_t = load('invf_t', invf[0:1, :], [128, 32], bcast=[128, 32])
    dmaskT = load('dmaskT', dmaskT_d[:, :], [128, 128], keep=True)
    tri_f = load('tri_f', tri_d[:, :], [128, 128])
    tri = P.sb([128, 128], BF16, 'tri', keep=True)
    P.cp('dve', tri, tri[:], tri_f, tri_f[:])
    qdec = load('qdec', qdec_d[:, :], [64, 128], keep=True)
    kdec = load('kdec', kdec_d[:, :], [128, 1], keep=True)
    dcy = load('dcy', dcy_d[:, :], [64, 1], keep=True)
    cmask = load('cmask', cmask_d[:, :], [128, 128], keep=True)
    jp1 = load('jp1', jp1_d[:, :], [128, 1])
    irow1 = load('irow1', irow1_d[:, :], [128, 128])

    posi = load('posi', pos[:, :], [128, NT], I32)
    posf = P.sb([128, NT], F32, 'posf')
    P.cp('dve', posf, posf[:], posi, posi[:])
    NR = NT * 32
    ang = P.sb([128, NR], F32, 'ang')
    P.tt('dve', ang, ang[:].rearrange("p (t f) -> p t f", f=32), posf,
         posf[:].unsqueeze(2).broadcast_to([128, NT, 32]), invf_t,
         invf_t[:].unsqueeze(1).broadcast_to([128, NT, 32]), ALU.mult)
    SINT = P.sb([128, NR], F32, 'SINT', keep=True)
    COST = P.sb([128, NR], F32, 'COST', keep=True)
    NSINT = P.sb([128, NR], F32, 'NSINT', keep=True)
    rkf = P.sb([128, NR], F32, 'rkf')
    rki = P.sb([128, NR], I32, 'rki')
    P.range_reduce(SINT, SINT[:], ang, ang[:], rkf, rkf[:], rki, rki[:])
    P.act(SINT, SINT[:], SINT, SINT[:], AF.Sin)
    P.range_reduce(COST, COST[:], ang, ang[:], rkf, rkf[:], rki, rki[:], shift=math.pi / 2)
    P.act(COST, COST[:], COST, COST[:], AF.Sin)
    P.ts('dve', NSINT, NSINT[:], SINT, SINT[:], -1.0, ALU.mult)

    are_tm = load('are_tm', are_tm_d[0:1, :], [128, 512], bcast=[128, 512])
    aim_tm = load('aim_tm', aim_tm_d[0:1, :], [128, 512], bcast=[128, 512])
    dt_tm = load('dt_tm', ldt_tm_d[0:1, :], [128, 512], bcast=[128, 512])
    P.act(dt_tm, dt_tm[:], dt_tm, dt_tm[:], AF.Exp)
    rho = P.sb([128, 512], F32, 'rho')
    tht = P.sb([128, 512], F32, 'tht')
    P.tt('dve', rho, rho[:], are_tm, are_tm[:], dt_tm, dt_tm[:], ALU.mult)
    P.tt('dve', tht, tht[:], aim_tm, aim_tm[:], dt_tm, dt_tm[:], ALU.mult)
    s5a = P.sb([128, 512], F32, 's5a')
    s5b = P.sb([128, 512], F32, 's5b')
    s5c = P.sb([128, 512], F32, 's5c')
    s5k = P.sb([128, 512], F32, 's5k')
    s5i = P.sb([128, 512], I32, 's5i')
    EA = P.sb([128, 512], F32, 'EA', keep=True)
    EB = P.sb([128, 512], F32, 'EB', keep=True)
    njp1 = P.sb([128, 1], F32, 'njp1')
    P.ts('dve', njp1, njp1[:], jp1, jp1[:], -1.0, ALU.mult)
    P.ts('dve', s5a, s5a[:], rho, rho[:], njp1[:, 0:1], ALU.mult, extra_r=[njp1])
    P.act(s5a, s5a[:], s5a, s5a[:], AF.Exp)
    P.ts('dve', s5b, s5b[:], tht, tht[:], jp1[:, 0:1], ALU.mult, extra_r=[jp1])
    P.range_reduce(s5c, s5c[:], s5b, s5b[:], s5k, s5k[:], s5i, s5i[:], shift=math.pi / 2)
    P.act(s5c, s5c[:], s5c, s5c[:], AF.Sin)
    P.tt('dve', EA, EA[:], s5a, s5a[:], s5c, s5c[:], ALU.mult)
    P.range_reduce(s5c, s5c[:], s5b, s5b[:], s5k, s5k[:], s5i, s5i[:])
    P.act(s5c, s5c[:], s5c, s5c[:], AF.Sin)
    P.tt('dve', EB, EB[:], s5a, s5a[:], s5c, s5c[:], ALU.mult)
    P.ts('dve', EB, EB[:], EB, EB[:], -1.0, ALU.mult)
    Lre = P.sb([128, 512], F32, 'Lre')
    Lim = P.sb([128, 512], F32, 'Lim')
    P.act(s5a, s5a[:], rho, rho[:], AF.Exp)
    P.range_reduce(s5c, s5c[:], tht, tht[:], s5k, s5k[:], s5i, s5i[:], shift=math.pi / 2)
    P.act(s5c, s5c[:], s5c, s5c[:], AF.Sin)
    P.tt('dve', Lre, Lre[:], s5a, s5a[:], s5c, s5c[:], ALU.mult)
    P.range_reduce(s5c, s5c[:], tht, tht[:], s5k, s5k[:], s5i, s5i[:])
    P.act(s5c, s5c[:], s5c, s5c[:], AF.Sin)
    P.tt('dve', Lim, Lim[:], s5a, s5a[:], s5c, s5c[:], ALU.mult)
    P.ts('dve', Lre, Lre[:], Lre, Lre[:], -1.0, ALU.add)
    P.tt('dve', s5a, s5a[:], are_tm, are_tm[:], are_tm, are_tm[:], ALU.mult)
    P.tt('dve', s5b, s5b[:], aim_tm, aim_tm[:], aim_tm, aim_tm[:], ALU.mult)
    P.tt('dve', s5a, s5a[:], s5a, s5a[:], s5b, s5b[:], ALU.add)
    P.op('dve', lambda e: e.reciprocal(out=s5a[:], in_=s5a[:]), r=[s5a], w=[s5a])
    Fre = P.sb([128, 512], F32, 'Fre')
    Fim = P.sb([128, 512], F32, 'Fim')
    P.tt('dve', s5b, s5b[:], Lre, Lre[:], are_tm, are_tm[:], ALU.mult)
    P.tt('dve', s5c, s5c[:], Lim, Lim[:], aim_tm, aim_tm[:], ALU.mult)
    P.tt('dve', s5b, s5b[:], s5b, s5b[:], s5c, s5c[:], ALU.add)
    P.tt('dve', Fre, Fre[:], s5b, s5b[:], s5a, s5a[:], ALU.mult)
    P.tt('dve', s5b, s5b[:], Lim, Lim[:], are_tm, are_tm[:], ALU.mult)
    P.tt('dve', s5c, s5c[:], Lre, Lre[:], aim_tm, aim_tm[:], ALU.mult)
    P.tt('dve', s5b, s5b[:], s5b, s5b[:], s5c, s5c[:], ALU.subtract)
    P.tt('dve', Fim, Fim[:], s5b, s5b[:], s5a, s5a[:], ALU.mult)
    BRt = load('BRt', BR_d[:, :], [64, 512])
    BIt = load('BIt', BI_d[:, :], [64, 512])
    bre = P.sb([64, 512], F32, 'bre')
    bim = P.sb([64, 512], F32, 'bim')
    btmp = P.sb([64, 512], F32, 'btmp')
    P.tt('dve', bre, bre[:], Fre, Fre[0:64, :], BRt, BRt[:], ALU.mult)
    P.tt('dve', btmp, btmp[:], Fim, Fim[0:64, :], BIt, BIt[:], ALU.mult)
    P.tt('dve', bre, bre[:], bre, bre[:], btmp, btmp[:], ALU.subtract)
    P.tt('dve', bim, bim[:], Fre, Fre[0:64, :], BIt, BIt[:], ALU.mult)
    P.tt('dve', btmp, btmp[:], Fim, Fim[0:64, :], BRt, BRt[:], ALU.mult)
    P.tt('dve', bim, bim[:], bim, bim[:], btmp, btmp[:], ALU.add)
    Bmat = P.sb([64, 512], BF16, 'Bmat', keep=True)
    Bswp = P.sb([64, 512], BF16, 'Bswp', keep=True)

    def v4(t, rows=128):
        return t[0:rows, :].rearrange("p (a r q) -> p a r q", a=2, r=2)

    P.cp('dve', Bmat, v4(Bmat, 64)[:, :, 0, :], bre, v4(bre, 64)[:, :, 0, :])
    P.cp('dve', Bmat, v4(Bmat, 64)[:, :, 1, :], bim, v4(bim, 64)[:, :, 1, :])
    P.ts('dve', Bswp, v4(Bswp, 64)[:, :, 0, :], bim, v4(bim, 64)[:, :, 0, :], -1.0, ALU.mult)
    P.cp('dve', Bswp, v4(Bswp, 64)[:, :, 1, :], bre, v4(bre, 64)[:, :, 1, :])
    are_sm = load('are_sm', are_sm_d[:, :], [128, 2])
    aim_sm = load('aim_sm', aim_sm_d[:, :], [128, 2])
    dt_sm = load('dt_sm', ldt_sm_d[:, :], [128, 2])
    P.act(dt_sm, dt_sm[:], dt_sm, dt_sm[:], AF.Exp)
    rho_sm = P.sb([128, 2], F32, 'rho_sm')
    tht_sm = P.sb([128, 2], F32, 'tht_sm')
    P.tt('dve', rho_sm, rho_sm[:], are_sm, are_sm[:], dt_sm, dt_sm[:], ALU.mult)
    P.tt('dve', tht_sm, tht_sm[:], aim_sm, aim_sm[:], dt_sm, dt_sm[:], ALU.mult)
    EA2 = P.sb([128, 512], F32, 'EA2', keep=True)
    EB2 = P.sb([128, 512], F32, 'EB2', keep=True)
    pm = P.sb([128, 256], F32, 'pm')
    pa = P.sb([128, 256], F32, 'pa')
    pc = P.sb([128, 256], F32, 'pc')
    pk = P.sb([128, 256], F32, 'pk')
    pki = P.sb([128, 256], I32, 'pki')
    for gp_ in range(2):
        sl = slice(gp_ * 128, (gp_ + 1) * 128)
        P.ts('dve', pm, pm[:, sl], irow1, irow1[:], rho_sm[:, gp_:gp_ + 1], ALU.mult, extra_r=[rho_sm])
        P.ts('dve', pa, pa[:, sl], irow1, irow1[:], tht_sm[:, gp_:gp_ + 1], ALU.mult, extra_r=[tht_sm])
    P.act(pm, pm[:], pm, pm[:], AF.Exp)
    P.range_reduce(pc, pc[:], pa, pa[:], pk, pk[:], pki, pki[:], shift=math.pi / 2)
    P.act(pc, pc[:], pc, pc[:], AF.Sin)
    P.tt('dve', pc, pc[:], pc, pc[:], pm, pm[:], ALU.mult)
    pc3 = pc[:].rearrange("p (a i) -> p a i", a=2)
    P.cp('dve', EA2, v4(EA2)[:, :, 0, :], pc, pc3)
    P.cp('dve', EA2, v4(EA2)[:, :, 1, :], pc, pc3)
    P.range_reduce(pc, pc[:], pa, pa[:], pk, pk[:], pki, pki[:])
    P.act(pc, pc[:], pc, pc[:], AF.Sin)
    P.tt('dve', pc, pc[:], pc, pc[:], pm, pm[:], ALU.mult)
    P.ts('dve', EB2, v4(EB2)[:, :, 0, :], pc, pc3, -1.0, ALU.mult)
    P.cp('dve', EB2, v4(EB2)[:, :, 1, :], pc, pc3)
    CRt = load('CRt', CR_d[:, :, :], [128, 2, 64])
    CIt = load('CIt', CI_d[:, :, :], [128, 2, 64])
    Cmat = P.sb([128, 4, 64], BF16, 'Cmat', keep=True)
    for gp_ in range(2):
        P.cp('dve', Cmat, Cmat[:, gp_ * 2, :], CRt, CRt[:, gp_, :])
        P.ts('dve', Cmat, Cmat[:, gp_ * 2 + 1, :], CIt, CIt[:, gp_, :], -1.0, ALU.mult)
    DDt = load('DDt', DD_d[:, :], [64, 64])
    DDb = P.sb([64, 64], BF16, 'DDb', keep=True)
    P.cp('dve', DDb, DDb[:], DDt, DDt[:])
    xprev = P.sb([128, 4], F32, 'xprev', keep=True)
    xprevs = P.sb([128, 4], F32, 'xprevs', keep=True)
    P.op('pool', lambda e: e.memset(xprev[:], 0.0), w=[xprev])
    P.op('pool', lambda e: e.memset(xprevs[:], 0.0), w=[xprevs])

    qng = load('qng', qng_d[:, :], [128, 2])
    wuq_f = load('wuq_f', wuq_d[:, :, :], [128, 2, 192])
    wuqb = P.sb([128, 2, 192], BF16, 'wuqb', keep=True)
    for kc in range(2):
        P.ts('dve', wuqb, wuqb[:, kc, :], wuq_f, wuq_f[:, kc, :], qng[:, kc:kc + 1], ALU.mult, extra_r=[qng])
    kvng = load('kvng', kvng_d[:, :], [128, 1])
    wukv_f = load('wukv_f', wukv_d[:, :], [128, 256])
    wukvb = P.sb([128, 256], BF16, 'wukvb', keep=True)
    P.ts('dve', wukvb, wukvb[:], wukv_f, wukv_f[:], kvng[:, 0:1], ALU.mult, extra_r=[kvng])

    P.barrier()
    tes.close()
    P.tes = None
    KTNP_t = P.es.enter_context(nc.sbuf_tensor('KTNP_%d' % id(P.es), [128, L], BF16))
    KTR_t = P.es.enter_context(nc.sbuf_tensor('KTR_%d' % id(P.es), [64, L], BF16))
    VV_t = P.es.enter_context(nc.sbuf_tensor('VV_%d' % id(P.es), [128, NT, 128], BF16))
    KTNP = [P.view(KTNP_t[:, t * 128:(t + 1) * 128], 'ktn%d' % t) for t in range(NT)]
    KTR = [P.view(KTR_t[:, t * 128:(t + 1) * 128], 'ktr%d' % t) for t in range(NT)]
    VV = [P.view(VV_t[:, t, :], 'vv%d' % t) for t in range(NT)]
    SROW_t = P.es.enter_context(nc.sbuf_tensor('SROW_%d' % id(P.es), [128, L], F32))
    SROW = [P.view(SROW_t, 'srow0'), P.view(SROW_t, 'srow1')]
    PB = P.sb([128, L], BF16, 'PB')
    state = P.sb([64, 64], F32, 'state')
    state_bf = P.sb([64, 64], BF16, 'state_bf')
    P.op('pool', lambda e: e.memset(state[:], 0.0), w=[state])
    P.op('pool', lambda e: e.memset(state_bf[:], 0.0), w=[state_bf])

    def dbl(shape, dt, name):
        return [P.sb(shape, dt, name + '%d' % i) for i in range(2)]

    xt = dbl([128, D], F32, 'xt')
    xnb = dbl([128, D], BF16, 'xnb')
    junk = P.sb([128, D], BF16, 'junk')
    ssx = dbl([128, 1], F32, 'ssx')
    hT = dbl([128, 8, 128], BF16, 'hT')
    ropeA = P.sb([128, 192], F32, 'ropeA')
    ropeB = P.sb([128, 192], F32, 'ropeB')
    rqk = dbl([128, 192], BF16, 'rqk')
    QTs = dbl([64, 128], BF16, 'QTs')
    KTs = dbl([64, 128], BF16, 'KTs')
    QTd = dbl([64, 128], BF16, 'QTd')
    vb = dbl([128, 64], BF16, 'vb')
    vdec = dbl([128, 64], BF16, 'vdec')
    smask = dbl([128, 128], BF16, 'smask')
    ssr = dbl([128, 1], F32, 'ssr')
    gs = dbl([128, 64], F32, 'gs')
    ro = dbl([128, 64], F32, 'ro')
    junk64 = P.sb([128, 256], F32, 'junk64')
    ub = dbl([128, 64], BF16, 'ub')
    UTs = dbl([64, 128], BF16, 'UTs')
    T1 = P.sb([128, 512], F32, 'T1')
    T2 = P.sb([128, 512], F32, 'T2')
    Wb = dbl([128, 512], BF16, 'Wb')
    Zf = T1
    Zsf = T2
    Xf = P.sb([128, 512], F32, 'Xf')
    Xb = dbl([128, 512], BF16, 'Xb')
    g1 = P.sb([128, 64], F32, 'g1')
    g2 = P.sb([128, 64], F32, 'g2')
    so = dbl([128, 64], F32, 'so')
    ssq = dbl([128, 1], F32, 'ssq')
    sskv = dbl([128, 1], F32, 'sskv')
    cqn = dbl([128, 256], BF16, 'cqn')
    ckvn = dbl([128, 128], BF16, 'ckvn')
    cqnT = dbl([128, 2, 128], BF16, 'cqnT')
    ckvnT = dbl([128, 128], BF16, 'ckvnT')
    QTNs = dbl([128, 128], BF16, 'QTNs')
    qrA = P.sb([128, 64], F32, 'qrA')
    qrB = P.sb([128, 64], F32, 'qrB')
    qrb = dbl([128, 64], BF16, 'qrb')
    QrTs = dbl([64, 128], BF16, 'QrTs')
    mrow = dbl([128, 1], F32, 'mrow')
    acc4 = dbl([128, 4], F32, 'acc4')
    rsum = dbl([128, 1], F32, 'rsum')
    PT = dbl([128, 512], BF16, 'PT')
    ao = dbl([128, 128], F32, 'ao')
    SM_SCALE = 192.0 ** -0.5
    ts_i = [0]

    def ts_slot():
        s = TS[ts_i[0] % 2]
        ts_i[0] += 1
        return s

    tb_i = [0]
    ev_i = [0]

    def evac_eng():
        ev_i[0] += 1
        return 'act' if ev_i[0] % 2 == 0 else 'dve'

    PVO = P.view(banks[5][:, 0:128], 'PVO', parent=banks[5])
    TBF = [P.view(banks[7][:, :].bitcast(BF16), 'TBF0', parent=banks[7]), P.view(banks[6][:, :].bitcast(BF16), 'TBF1', parent=banks[6])]
    PT8 = dbl([128, 1024], BF16, 'PT8')

    def frontend(t):
        p = t % 2
        P.dma('sp', xt[p][:], xrow(t), r=([xdep] if xdep is not None else []), w=[xt[p]])
        P.op('dve', lambda e: e.memset(ssx[p][:], 0.0), w=[ssx[p]])
        P.act(junk, junk[:], xt[p], xt[p][:], AF.Square, extra_w=[ssx[p]], accum_out=ssx[p][:])
        yield
        P.rstd(ssx[p], D, None)
        P.ts('dve', xnb[p], xnb[p][:], xt[p], xt[p][:], ssx[p][:, 0:1], ALU.mult, extra_r=[ssx[p]])
        yield
        for half in range(2):
            tb = TB[0]
            for c4 in range(4):
                c = half * 4 + c4
                P.tr(tb, tb[:, c4 * 128:(c4 + 1) * 128], xnb[p], xnb[p][:, c * 128:(c + 1) * 128], ident)
            for c4 in range(4):
                c = half * 4 + c4
                if c4 % 2 == 0:
                    P.act(hT[p], hT[p][:, c, :], tb, tb[:, c4 * 128:(c4 + 1) * 128], AF.Identity,
                          extra_r=[scale1, shm], scale=scale1[:, c:c + 1], bias=shm[:, c:c + 1])
                else:
                    P.ts('dve', hT[p], hT[p][:, c, :], tb, tb[:, c4 * 128:(c4 + 1) * 128],
                         scale1[:, c:c + 1], ALU.mult, shm[:, c:c + 1], ALU.add, extra_r=[scale1, shm])
            yield

    def projheads(t):
        projection(t)
        yield
        for _ in heads(t):
            yield

    def projection(t):
        p = t % 2
        for kc in range(8):
            P.mm(PJ0, PJ0[:, :], hT[p], hT[p][:, kc, :], wselb, wselb[:, kc, 0:512], start=(kc == 0), stop=(kc == 7))
        for kc in range(8):
            P.mm(PJ1, PJ1[:, :], hT[p], hT[p][:, kc, :], wselb, wselb[:, kc, 512:768], start=(kc == 0), stop=(kc == 7))

    def heads(t):
        p = t % 2
        cos_t = COST[:, t * 32:(t + 1) * 32]
        sin_t = SINT[:, t * 32:(t + 1) * 32]
        nsin_t = NSINT[:, t * 32:(t + 1) * 32]
        src4 = PJ0[:, 0:192].rearrange("p (a h f) -> p a h f", a=3, h=2)
        A4 = ropeA[:].rearrange("p (a h f) -> p a h f", a=3, h=2)
        B4 = ropeB[:].rearrange("p (a h f) -> p a h f", a=3, h=2)
        P.tt('dve', ropeA, A4, PJ0, src4, COST, cos_t.unsqueeze(1).unsqueeze(1).broadcast_to([128, 3, 2, 32]), ALU.mult)
        P.tt('dve', ropeB, B4[:, :, 0, :], PJ0, src4[:, :, 1, :], NSINT, nsin_t.unsqueeze(1).broadcast_to([128, 3, 32]), ALU.mult)
        P.tt('dve', ropeB, B4[:, :, 1, :], PJ0, src4[:, :, 0, :], SINT, sin_t.unsqueeze(1).broadcast_to([128, 3, 32]), ALU.mult)
        yield
        P.cp('act', vb[p], vb[p][:], PJ0, PJ0[:, 192:256])
        P.ts('dve', vdec[p], vdec[p][:], PJ0, PJ0[:, 192:256], kdec[:, 0:1], ALU.mult, extra_r=[kdec])
        P.act(gs[p], gs[p][:], PJ0, PJ0[:, 256:320], AF.Silu)
        P.cp('act', ub[p], ub[p][:], PJ0, PJ0[:, 320:384])
        P.op('dve', lambda e: e.memset(ssq[p][:], 0.0), w=[ssq[p]])
        P.op('dve', lambda e: e.memset(sskv[p][:], 0.0), w=[sskv[p]])
        P.act(junk64, junk64[:, 0:256], PJ1, PJ1[:, :], AF.Square, extra_w=[ssq[p]], accum_out=ssq[p][:])
        P.act(junk64, junk64[:, 0:128], PJ0, PJ0[:, 384:512], AF.Square, extra_w=[sskv[p]], accum_out=sskv[p][:])
        yield
        P.rstd(ssq[p], 256, None)
        yield
        P.rstd(sskv[p], 128, None)
        yield
        P.ts('dve', cqn[p], cqn[p][:], PJ1, PJ1[:, :], ssq[p][:, 0:1], ALU.mult, extra_r=[ssq[p]])
        P.ts('dve', ckvn[p], ckvn[p][:], PJ0, PJ0[:, 384:512], sskv[p][:, 0:1], ALU.mult, extra_r=[sskv[p]])
        P.tt('dve', rqk[p], rqk[p][:], ropeA, ropeA[:], ropeB, ropeB[:], ALU.add)
        yield

    def ret_chain(t):
        p = t % 2
        tok = slice(t * 128, (t + 1) * 128)
        s0 = ts_slot()
        P.tr(s0, s0[0:64, :], rqk[p], rqk[p][:, 0:64], ident)
        P.cp('act', QTs[p], QTs[p][:], s0, s0[0:64, :])
        yield
        P.tt('dve', QTd[p], QTd[p][:], QTs[p], QTs[p][:], qdec, qdec[:], ALU.mult)
        s1 = ts_slot()
        P.tr(s1, s1[0:64, :], rqk[p], rqk[p][:, 64:128], ident)
        P.cp('act', KTs[p], KTs[p][:], s1, s1[0:64, :])
        yield
        P.tt('dve', gs[p], gs[p][:], gs[p], gs[p][:], retg_t, retg_t[:], ALU.mult)
        P.mm(RS, RS[:, :], KTs[p], KTs[p][:], QTs[p], QTs[p][:])
        yield
        P.tt('dve', smask[p], smask[p][:], RS, RS[:, :], dmaskT, dmaskT[:], ALU.mult)
        yield
        P.mm(RO, RO[:, :], smask[p], smask[p][:], vb[p], vb[p][:], start=True, stop=False)
        P.mm(RO, RO[:, :], QTd[p], QTd[p][:], state_bf, state_bf[:], start=False, stop=True)
        P.mm(RKV, RKV[:, :], rqk[p], rqk[p][:, 64:128], vdec[p], vdec[p][:])
        yield
        P.op('dve', lambda e: e.scalar_tensor_tensor(out=state[:], in0=state[:], scalar=dcy[:, 0:1], in1=RKV[:, :],
                                                     op0=ALU.mult, op1=ALU.add), r=[state, dcy, RKV], w=[state])
        P.cp('dve', state_bf, state_bf[:], state, state[:])
        P.op('dve', lambda e: e.memset(ssr[p][:], 0.0), w=[ssr[p]])
        P.act(junk64, junk64[:, 0:64], RO, RO[:, :], AF.Square, extra_w=[ssr[p]], accum_out=ssr[p][:])
        yield
        P.rstd(ssr[p], 64, None)
        yield
        P.op('dve', lambda e: e.scalar_tensor_tensor(out=ro[p][:], in0=RO[:, :], scalar=ssr[p][:, 0:1], in1=gs[p][:],
                                                     op0=ALU.mult, op1=ALU.mult), r=[RO, ssr[p], gs[p]], w=[ro[p]])
        out_evs.append(P.dma('sp', hx[tok, 0:64], ro[p][:], r=[ro[p]]))
        yield

    def s5_chain(t):
        p = t % 2
        tok = slice(t * 128, (t + 1) * 128)
        s3 = ts_slot()
        P.tr(s3, s3[0:64, :], ub[p], ub[p][:], ident)
        P.cp('dve', UTs[p], UTs[p][:], s3, s3[0:64, :])
        yield
        P.mm(SA, SA[:, :], UTs[p], UTs[p][:], Bmat, Bmat[:])
        P.mm(SB, SB[:, :], UTs[p], UTs[p][:], Bswp, Bswp[:])
        yield
        P.tt('dve', T1, T1[:], SA, SA[:, :], EA, EA[:], ALU.mult)
        yield
        P.tt('dve', T2, T2[:], SB, SB[:, :], EB, EB[:], ALU.mult)
        yield
        P.tt('dve', Wb[p], Wb[p][:], T1, T1[:], T2, T2[:], ALU.add)
        yield
        for blk in range(4):
            P.mm(SA, SA[:, blk * 128:(blk + 1) * 128], Wb[p], Wb[p][:, blk * 128:(blk + 1) * 128], tri, tri[:])
        for blk in range(4):
            b2 = blk ^ 1
            P.mm(SB, SB[:, blk * 128:(blk + 1) * 128], Wb[p], Wb[p][:, b2 * 128:(b2 + 1) * 128], tri, tri[:])
        yield
        P.tt('dve', Zf, Zf[:].rearrange("p (a i) -> p a i", a=4), SA, SA[:, :].rearrange("p (a i) -> p a i", a=4),
             xprev, xprev[:].unsqueeze(2).broadcast_to([128, 4, 128]), ALU.add)
        yield
        P.tt('dve', Zsf, Zsf[:].rearrange("p (a i) -> p a i", a=4), SB, SB[:, :].rearrange("p (a i) -> p a i", a=4),
             xprevs, xprevs[:].unsqueeze(2).broadcast_to([128, 4, 128]), ALU.add)
        yield
        P.tt('dve', Zf, Zf[:], Zf, Zf[:], EA2, EA2[:], ALU.mult)
        yield
        P.tt('dve', Zsf, Zsf[:], Zsf, Zsf[:], EB2, EB2[:], ALU.mult)
        yield
        P.tt('dve', Xf, Xf[:], Zf, Zf[:], Zsf, Zsf[:], ALU.add)
        yield
        P.cp('act', Xb[p], Xb[p][:], Xf, Xf[:])
        X3 = Xf[:].rearrange("p (a i) -> p a i", a=4)
        P.cp('dve', xprev, xprev[:], Xf, X3[:, :, 127])
        xp3 = xprev[:].rearrange("p (a r) -> p a r", r=2)
        xs3 = xprevs[:].rearrange("p (a r) -> p a r", r=2)
        P.cp('dve', xprevs, xs3[:, :, 0], xprev, xp3[:, :, 1])
        P.cp('dve', xprevs, xs3[:, :, 1], xprev, xp3[:, :, 0])
        yield
        for blk in range(4):
            P.mm(SY, SY[:, :], Xb[p], Xb[p][:, blk * 128:(blk + 1) * 128], Cmat, Cmat[:, blk, :], start=(blk == 0), stop=False)
        P.mm(SY, SY[:, :], UTs[p], UTs[p][:], DDb, DDb[:], start=False, stop=True)
        yield
        P.act(g1, g1[:], SY, SY[:, :], AF.Square)
        yield
        P.ts('dve', g1, g1[:], g1, g1[:], 0.044715, ALU.mult, 1.0, ALU.add)
        P.tt('dve', g1, g1[:], g1, g1[:], SY, SY[:, :], ALU.mult)
        yield
        P.act(g2, g2[:], g1, g1[:], AF.Sigmoid, scale=1.5957691216057308)
        yield
        P.tt('dve', so[p], so[p][:], g2, g2[:], SY, SY[:, :], ALU.mult)
        out_evs.append(P.dma('sp', hx[tok, 64:128], so[p][:], r=[so[p]]))
        yield

    def mla_chain(t):
        p = t % 2
        tok = slice(t * 128, (t + 1) * 128)
        cos_t = COST[:, t * 32:(t + 1) * 32]
        sin_t = SINT[:, t * 32:(t + 1) * 32]
        nsin_t = NSINT[:, t * 32:(t + 1) * 32]
        s2 = ts_slot()
        P.tr(s2, s2[0:64, :], rqk[p], rqk[p][:, 128:192], ident)
        P.cp('act', KTR[t], KTR[t][:], s2, s2[0:64, :])
        yield
        for kc in range(2):
            s = ts_slot()
            P.tr(s, s[:, :], cqn[p], cqn[p][:, kc * 128:(kc + 1) * 128], ident)
            P.cp(evac_eng(), cqnT[p], cqnT[p][:, kc, :], s, s[:, :])
            yield
        s = ts_slot()
        P.tr(s, s[:, :], ckvn[p], ckvn[p][:], ident)
        P.cp(evac_eng(), ckvnT[p], ckvnT[p][:], s, s[:, :])
        yield
        for kc in range(2):
            P.mm(QTN, QTN[:, :], wuqb, wuqb[:, kc, 0:128], cqnT[p], cqnT[p][:, kc, :], start=(kc == 0), stop=(kc == 1))
        for kc in range(2):
            P.mm(QRP, QRP[:, :], cqnT[p], cqnT[p][:, kc, :], wuqb, wuqb[:, kc, 128:192], start=(kc == 0), stop=(kc == 1))
        P.mm(KTN, KTN[:, :], wukvb, wukvb[:, 0:128], ckvnT[p], ckvnT[p][:])
        P.mm(VP, VP[:, :], ckvnT[p], ckvnT[p][:], wukvb, wukvb[:, 128:256])
        yield
        P.act(QTNs[p], QTNs[p][:], QTN, QTN[:, :], AF.Copy, scale=SM_SCALE)
        q3 = QRP[:, :].rearrange("p (h f) -> p h f", h=2)
        a3 = qrA[:].rearrange("p (h f) -> p h f", h=2)
        b3 = qrB[:].rearrange("p (h f) -> p h f", h=2)
        P.tt('dve', qrA, a3, QRP, q3, COST, cos_t.unsqueeze(1).broadcast_to([128, 2, 32]), ALU.mult)
        P.tt('dve', qrB, b3[:, 0, :], QRP, q3[:, 1, :], NSINT, nsin_t, ALU.mult)
        P.tt('dve', qrB, b3[:, 1, :], QRP, q3[:, 0, :], SINT, sin_t, ALU.mult)
        P.cp('act', KTNP[t], KTNP[t][:], KTN, KTN[:, :])
        P.cp('act', VV[t], VV[t][:], VP, VP[:, :])
        yield
        P.tt('dve', qrb[p], qrb[p][:], qrA, qrA[:], qrB, qrB[:], ALU.add)
        yield
        s = ts_slot()
        P.tr(s, s[0:64, :], qrb[p], qrb[p][:], ident)
        P.act(QrTs[p], QrTs[p][:], s, s[0:64, :], AF.Copy, scale=SM_SCALE)
        yield
        Lk = (t + 1) * 128
        nkb = (Lk + 511) // 512
        for kb in range(nkb):
            n = min(512, Lk - kb * 512)
            sc = SC[kb % 2]
            kts = [KTNP[4 * kb + i_] for i_ in range(n // 128)]
            krs = [KTR[4 * kb + i_] for i_ in range(n // 128)]
            P.op('pe', lambda e: e.matmul(sc[:, 0:n], lhsT=QTNs[p][:], rhs=KTNP_t[:, kb * 512:kb * 512 + n],
                                          start=True, stop=False), r=[QTNs[p]] + kts, w=[sc])
            P.op('pe', lambda e: e.matmul(sc[:, 0:n], lhsT=QrTs[p][:], rhs=KTR_t[:, kb * 512:kb * 512 + n],
                                          start=False, stop=True), r=[QrTs[p]] + krs, w=[sc])
            yield
            srow = SROW[kb % 2]
            if kb == nkb - 1:
                if n > 128:
                    P.cp('act', srow, SROW_t[:, kb * 512:kb * 512 + n - 128], sc, sc[:, 0:n - 128])
                P.tt('dve', srow, SROW_t[:, Lk - 128:Lk], sc, sc[:, n - 128:n], cmask, cmask[:], ALU.add)
            else:
                P.cp(evac_eng(), srow, SROW_t[:, kb * 512:kb * 512 + n], sc, sc[:, 0:n])
            yield
        P.op('dve', lambda e: e.reduce_max(out=mrow[p][:], in_=SROW_t[:, 0:Lk], axis=AX.X), r=[SROW[0], SROW[1]], w=[mrow[p]])
        P.op('dve', lambda e: e.memset(acc4[p][:], 0.0), w=[acc4[p]])
        yield
        P.ts('dve', mrow[p], mrow[p][:], mrow[p], mrow[p][:], -1.0, ALU.mult)
        yield
        nch = (Lk + 2047) // 2048
        for ci in range(nch):
            c0 = ci * 2048
            c1 = min(Lk, c0 + 2048)
            P.op('act', lambda e: e.activation(out=PB[:, c0:c1], in_=SROW_t[:, c0:c1], func=AF.Exp,
                                               bias=mrow[p][:, 0:1], scale=1.0, accum_out=acc4[p][:, ci:ci + 1]),
                 r=[SROW[0], SROW[1], mrow[p]], w=[PB, acc4[p]])
            yield
        P.op('dve', lambda e: e.reduce_sum(out=rsum[p][:], in_=acc4[p][:, 0:nch], axis=AX.X), r=[acc4[p]], w=[rsum[p]])
        yield
        P.op('dve', lambda e: e.reciprocal(out=rsum[p][:], in_=rsum[p][:]), r=[rsum[p]], w=[rsum[p]])
        ng = (t + 1 + 7) // 8

        def tr_round(g):
            tbf = TBF[g % 2]
            pt = PT8[g % 2]
            blks = list(range(g * 8, min(t + 1, g * 8 + 8)))
            for blk in blks:
                P.tr(tbf, tbf[:, (blk % 8) * 128:(blk % 8 + 1) * 128], PB, PB[:, blk * 128:(blk + 1) * 128], ident)
            w_ = len(blks) * 128
            P.cp(evac_eng(), pt, pt[:, 0:w_], tbf, tbf[:, 0:w_])
            return blks, pt

        cur = tr_round(0)
        yield
        for g in range(ng):
            nxt = tr_round(g + 1) if g + 1 < ng else None
            blks, pt = cur
            for blk in blks:
                P.mm(PVO, PVO[:, :], pt, pt[:, (blk % 8) * 128:(blk % 8 + 1) * 128], VV[blk], VV[blk][:],
                     start=(blk == 0), stop=(blk == t))
            cur = nxt
            yield
        P.ts('dve', ao[p], ao[p][:], PVO, PVO[:, :], rsum[p][:, 0:1], ALU.mult, extra_r=[rsum[p]])
        out_evs.append(P.dma('sp', hx[tok, 128:256], ao[p][:], r=[ao[p]]))
        yield

    def drain(g):
        for _ in g:
            pass

    def interleave(gens):
        gens = [[g, 2 if i == 0 else 1] for i, g in enumerate(gens)]
        while gens:
            alive = []
            for g, n_ in gens:
                ok = True
                for _ in range(n_):
                    try:
                        next(g)
                    except StopIteration:
                        ok = False
                        break
                if ok:
                    alive.append([g, n_])
            gens = alive

    def step(g, n_=1):
        for _ in range(n_):
            try:
                next(g)
            except StopIteration:
                return False
        return True

    out_evs = []
    if ntiles > 0:
        drain(frontend(0))
        drain(projheads(0))
    for t in range(ntiles):
        mla = mla_chain(t)
        mla_alive = True
        others = [ret_chain(t), s5_chain(t)]
        if t + 1 < ntiles:
            others.append(frontend(t + 1))
        while others:
            if mla_alive:
                mla_alive = step(mla, 2)
            others = [g for g in others if step(g)]
        nxt = projheads(t + 1) if t + 1 < ntiles else None
        nxt_alive = nxt is not None
        while mla_alive or nxt_alive:
            if mla_alive:
                mla_alive = step(mla, 2)
            if nxt_alive:
                nxt_alive = step(nxt)
        if on_chunk is not None and (t + 1) % 8 == 0:
            on_chunk(t // 8, out_evs)
            out_evs = []
    P.barrier()
    st.close()
    P.es = old_es
    P.pre = {}


RET_GAMMA = [1.0 - 2.0 ** (-5.0 - h) for h in range(4)]


def consts_A(hg):
    g = RET_GAMMA[hg]
    lg = math.log1p(-(2.0 ** (-5.0 - hg)))
    i = np.arange(128, dtype=np.float64)
    diff = i[None, :] - i[:, None]
    dmaskT = np.where(diff >= 0, np.exp(lg * np.maximum(diff, 0.0)), 0.0) * 0.125
    tri = (diff >= 0).astype(np.float32)
    qdec = np.broadcast_to(np.exp(lg * (i + 1.0))[None, :], (64, 128))
    kdec = (np.exp(lg * (127.0 - i)) * 0.125)[:, None]
    dcy = np.full((64, 1), math.exp(lg * 128.0))
    cmask = np.where(i[None, :] <= i[:, None], 0.0, -1e30)
    inv = 10000.0 ** (-np.arange(0, 64, 2, dtype=np.float32) / 64.0)
    f = lambda a: np.ascontiguousarray(a, dtype=np.float32)
    return dict(dmaskT=f(dmaskT), tri=f(tri), qdec=f(qdec), kdec=f(kdec), dcy=f(dcy), cmask=f(cmask),
                jp1=f((i + 1.0)[:, None]), irow1=f(np.broadcast_to((i + 1.0)[None, :], (128, 128))),
                invf=f(inv[None, :]))


def chunkT(v, nch):
    return np.ascontiguousarray(v.reshape(nch, 128).T)


def inputs_A(inp, layer, xcur):
    f = lambda a: np.ascontiguousarray(a, dtype=np.float32)
    maps = []
    w_in = inp['w_in'][layer]
    for core in range(8):
        b, hg = core // 4, core % 4
        m = dict(consts_A(hg))
        m['x'] = f(xcur[b])
        m['cvec'] = f(inp['c'][b].reshape(128, 8))
        m['pos'] = np.ascontiguousarray(inp['positions'][b].reshape(NT, 128).T.astype(np.int32))
        m['adaw'] = f(inp['ada_w'][layer][:, 0:2048].reshape(128, 8, 2048))
        m['adab'] = chunkT(f(inp['ada_b'][layer][0:2048]), 16)
        m['gpre'] = chunkT(f(inp['norm_pre_mix'][layer]), 8)
        h64 = slice(hg * 64, (hg + 1) * 64)
        cols = np.concatenate([
            np.arange(0, 256)[h64], np.arange(256, 512)[h64], np.arange(1664, 1728),
            np.arange(512, 768)[h64], np.arange(768, 1024)[h64], np.arange(1024, 1280)[h64],
            np.arange(1536, 1664), np.arange(1280, 1536)])
        ws = w_in[:, cols]
        m['wsel'] = f(ws.reshape(8, 128, 768).transpose(1, 0, 2))
        m['retg'] = f(inp['ret_norm'][layer][h64][None, :])
        gs_ = slice(4 * hg, 4 * hg + 4)

        def tm(a):
            a = a.reshape(2, 1, 128)
            return f(np.broadcast_to(a, (2, 2, 128)).reshape(1, 512))

        def sm(a):
            return f(a.reshape(2, 128).T)

        are = inp['ssm_a_re'][layer][gs_]
        aim = inp['ssm_a_im'][layer][gs_]
        ldt = np.broadcast_to(inp['ssm_log_dt'][layer][gs_][:, None], (4, 64))
        m['are_tm'], m['aim_tm'], m['ldt_tm'] = tm(are), tm(aim), tm(ldt)
        m['are_sm'], m['aim_sm'], m['ldt_sm'] = sm(are), sm(aim), sm(ldt)
        BR = np.zeros((64, 2, 2, 2, 64), np.float32)
        BI = np.zeros((64, 2, 2, 2, 64), np.float32)
        CR = np.zeros((2, 64, 2, 64), np.float32)
        CI = np.zeros((2, 64, 2, 64), np.float32)
        for gl in range(4):
            gp_, g2_ = gl // 2, gl % 2
            br = inp['ssm_b_re'][layer][4 * hg + gl]
            bi = inp['ssm_b_im'][layer][4 * hg + gl]
            for ri in range(2):
                BR[gl * 16:(gl + 1) * 16, gp_, ri, g2_, :] = br.T
                BI[gl * 16:(gl + 1) * 16, gp_, ri, g2_, :] = bi.T
            cr = inp['ssm_c_re'][layer][4 * hg + gl]
            ci = inp['ssm_c_im'][layer][4 * hg + gl]
            CR[g2_, :, gp_, gl * 16:(gl + 1) * 16] = cr.T
            CI[g2_, :, gp_, gl * 16:(gl + 1) * 16] = ci.T
        m['BR'] = f(BR.reshape(64, 512))
        m['BI'] = f(BI.reshape(64, 512))
        m['CR'] = f(CR.reshape(128, 2, 64))
        m['CI'] = f(CI.reshape(128, 2, 64))
        DD = np.zeros((64, 64), np.float32)
        DD[np.arange(64), np.arange(64)] = inp['ssm_d'][layer][gs_].reshape(64)
        m['DD'] = DD
        m['qng'] = chunkT(f(inp['mla_q_norm'][layer]), 2)
        wq = inp['mla_w_uq'][layer][:, hg * 192:(hg + 1) * 192]
        m['wuq'] = f(wq.reshape(2, 128, 192).transpose(1, 0, 2))
        m['kvng'] = f(inp['mla_kv_norm'][layer][:, None])
        m['wukv'] = f(inp['mla_w_ukv'][layer][:, hg * 256:(hg + 1) * 256])
        maps.append(m)
    return maps


_CACHE = {}


TPC = 2048
GT = 4
NG = TPC // (GT * 128)


def decl_B(nc, sfx, moe):
    n_exp = N_EXP if moe else 1
    io = {}
    io['cvec'] = nc.dram_tensor('cvec' + sfx, [128, 8], F32, kind='ExternalInput').ap()
    io['adaw'] = nc.dram_tensor('adaw' + sfx, [128, 8, 4096], F32, kind='ExternalInput').ap()
    io['adab_row'] = nc.dram_tensor('adab_row' + sfx, [1, 4096], F32, kind='ExternalInput').ap()
    io['adab_col'] = nc.dram_tensor('adab_col' + sfx, [128, 32], F32, kind='ExternalInput').ap()
    io['gpost_m'] = nc.dram_tensor('gpost_m' + sfx, [1, D], F32, kind='ExternalInput').ap()
    io['gpre_f'] = nc.dram_tensor('gpre_f' + sfx, [128, 8], F32, kind='ExternalInput').ap()
    io['gpost_f'] = nc.dram_tensor('gpost_f' + sfx, [1, D], F32, kind='ExternalInput').ap()
    io['gluw_d'] = nc.dram_tensor('gluw' + sfx, [128, 2, 256], F32, kind='ExternalInput').ap()
    io['glub_d'] = nc.dram_tensor('glub' + sfx, [1, 256], F32, kind='ExternalInput').ap()
    io['ssmg_d'] = nc.dram_tensor('ssmg' + sfx, [1, 256], F32, kind='ExternalInput').ap()
    io['mlag_d'] = nc.dram_tensor('mlag' + sfx, [1, 512], F32, kind='ExternalInput').ap()
    io['wout_d'] = nc.dram_tensor('wout' + sfx, [128, 8, D], F32, kind='ExternalInput').ap()
    io['wg_d'] = nc.dram_tensor('wg' + sfx, [n_exp * NFC, 128, 1024], F32, kind='ExternalInput').ap()
    io['wu_d'] = nc.dram_tensor('wu' + sfx, [n_exp * NFC, 128, 1024], F32, kind='ExternalInput').ap()
    io['wd_d'] = nc.dram_tensor('wd' + sfx, [n_exp * NFC, 128, 1024], F32, kind='ExternalInput').ap()
    if moe:
        io['router_d'] = nc.dram_tensor('router' + sfx, [128, 8, 8], F32, kind='ExternalInput').ap()
    return io


def emit_B(P, nc, banks, io, x, hxall, HXALL, mixloc, out, rank256, moe, ngroups=NG, xdep=None, on_group=None):
    n_exp = N_EXP if moe else 1
    cvec = io['cvec']
    adaw = io['adaw']
    adab_row = io['adab_row']
    adab_col = io['adab_col']
    gpost_m = io['gpost_m']
    gpre_f = io['gpre_f']
    gpost_f = io['gpost_f']
    gluw_d = io['gluw_d']
    glub_d = io['glub_d']
    ssmg_d = io['ssmg_d']
    mlag_d = io['mlag_d']
    wout_d = io['wout_d']
    wg_d = io['wg_d']
    wu_d = io['wu_d']
    wd_d = io['wd_d']
    router_d = io.get('router_d')
    st = ExitStack()
    old_es = P.es
    P.es = st
    P.pre = {}
    b0 = banks[0][:, :].bitcast(BF16)
    TB = P.view(b0[:, 0:512], 'TB', parent=banks[0])
    ZP = P.view(banks[1][:, 0:256], 'ZP', parent=banks[1])
    LG = P.view(banks[1][:, 256:264], 'LG', parent=banks[1])
    MC = P.view(banks[1][:, 272:288], 'MC', parent=banks[1])
    YB = [P.view(banks[2][:, :], 'YA', parent=banks[2]), P.view(banks[3][:, :], 'YB', parent=banks[3])]
    GB = [P.view(banks[4][:, :], 'G0', parent=banks[4]), P.view(banks[5][:, :], 'G1', parent=banks[5])]
    UB = [P.view(banks[6][:, :], 'U0', parent=banks[6]), P.view(banks[7][:, :], 'U1', parent=banks[7])]

    ident = P.sb([128, 128], BF16, 'ident', keep=True)
    vecm = P.sb([128, D], F32, 'vecm', keep=True)
    vecf = P.sb([128, D], F32, 'vecf', keep=True)
    modc = P.sb([128, 16], F32, 'modc', keep=True)
    scale2 = P.sb([128, 8], F32, 'scale2', keep=True)
    woutb = P.sb([128, 8, D], BF16, 'woutb', keep=True)
    gluwb = P.sb([128, 2, 256], BF16, 'gluwb', keep=True)
    glub_t = P.sb([128, 256], F32, 'glub_t', keep=True)
    ssmg_t = P.sb([128, 256], F32, 'ssmg_t', keep=True)
    mlag_t = P.sb([128, 512], F32, 'mlag_t', keep=True)
    if moe:
        routerb = P.sb([128, 8, 8], BF16, 'routerb', keep=True)
    X1 = [P.sb([128, D], F32, 'X1_%d' % i, keep=True) for i in range(GT)]
    hT2 = P.sb([128, 8, GT * 128], BF16, 'hT2', keep=True)
    yacc = [P.sb([128, D], F32, 'yacc%d' % i, keep=True) for i in range(GT)]
    gatew = P.sb([128, GT, 8], F32, 'gatew', keep=True)

    tes = ExitStack()
    P.tes = tes
    make_ident(P)
    cv = P.sb([128, 8], F32, 'cv')
    cvb = P.sb([128, 8], BF16, 'cvb')
    cvbb = P.sb([128, 8, 128], BF16, 'cvbb')
    P.dma('sp', cv[:], cvec[:, :], w=[cv])
    P.act(cvb, cvb[:], cv, cv[:], AF.Silu)
    P.cp('dve', cvbb, cvbb[:], cvb, cvb[:].unsqueeze(2).broadcast_to([128, 8, 128]))
    abc = P.sb([128, 32], F32, 'abc')
    P.dma('sp', abc[:], adab_col[:, :], w=[abc])
    wts = [P.sb([128, 8, 1024], BF16, 'adw%d' % i) for i in range(2)]
    stager = make_stager(P)
    rowb = P.sb([128, D], F32, 'rowb')
    for ci in range(4):
        wt = wts[ci % 2]
        for kc in range(8):
            stager(wt, wt[:, kc, :], adaw[:, kc, ci * 1024:(ci + 1) * 1024])
        if ci in (0, 3):
            vec = vecm if ci == 0 else vecf
            gsrc = gpost_m if ci == 0 else gpost_f
            P.dma('sp', rowb[:], adab_row[0:1, ci * 1024:(ci + 1) * 1024].broadcast_to([128, 1024]), w=[rowb])
            for half in range(2):
                yb = YB[half]
                for kc in range(8):
                    P.mm(yb, yb[:, :], cvbb, cvbb[:, kc, :], wt, wt[:, kc, half * 512:(half + 1) * 512],
                         start=(kc == 0), stop=(kc == 7))
                P.tt('dve', vec, vec[:, half * 512:(half + 1) * 512], yb, yb[:, :], rowb, rowb[:, half * 512:(half + 1) * 512], ALU.add)
            P.dma('sp', rowb[:], gsrc[0:1, :].broadcast_to([128, 1024]), w=[rowb])
            P.tt('dve', vec, vec[:], vec, vec[:], rowb, rowb[:], ALU.mult)
        else:
            for cc in range(8):
                ch = (ci - 1) * 8 + cc
                for kc in range(8):
                    P.mm(MC, MC[:, ch:ch + 1], wt, wt[:, kc, cc * 128:(cc + 1) * 128], cvb, cvb[:, kc:kc + 1],
                         start=(kc == 0), stop=(kc == 7))
    P.tt('dve', modc, modc[:], MC, MC[:, 0:16], abc, abc[:, 8:24], ALU.add)
    gpf = P.sb([128, 8], F32, 'gpf')
    P.dma('sp', gpf[:], gpre_f[:, :], w=[gpf])
    P.ts('dve', scale2, scale2[:], modc, modc[:, 8:16], 1.0, ALU.add)
    P.tt('dve', scale2, scale2[:], scale2, scale2[:], gpf, gpf[:], ALU.mult)
    sh2 = P.view(modc[:, 0:8], 'sh2', parent=modc)
    for kc in range(8):
        stager(woutb, woutb[:, kc, :], wout_d[:, kc, :])
    P.dma('pool', gluwb[:], gluw_d[:, :, :], w=[gluwb])
    P.dma('sp', glub_t[:], glub_d[0:1, :].broadcast_to([128, 256]), w=[glub_t])
    P.dma('sp', ssmg_t[:], ssmg_d[0:1, :].broadcast_to([128, 256]), w=[ssmg_t])
    P.dma('sp', mlag_t[:], mlag_d[0:1, :].broadcast_to([128, 512]), w=[mlag_t])
    if moe:
        P.dma('pool', routerb[:], router_d[:, :, :], w=[routerb])
    P.barrier()
    tes.close()
    P.tes = None

    MIXLOC = P.view(mixloc, 'MIXLOC')
    for kk in range(2):
        src_ = hxall.rearrange("(a b) c -> a (b c)", b=32)[bass.ds(rank256 + kk * 128, 128), :]
        P.dma('sp', mixloc[kk].rearrange("r n c -> (r n) c").rearrange("(a b) c -> a (b c)", b=32), src_, r=[HXALL], w=[MIXLOC])
    ev_i = [0]

    def evac_eng():
        ev_i[0] += 1
        return 'act' if ev_i[0] % 2 == 0 else 'dve'

    for g in range(ngroups):
        ph = ExitStack()
        P.tes = ph
        junk = P.sb([128, D], BF16, 'junk')

        def p1_bufs(i):
            return dict(xt=P.sb([128, D], F32, 'xt%d' % i), mt=P.sb([128, D], F32, 'mt%d' % i),
                        hxt=P.sb([128, 4, 256], F32, 'hxt%d' % i), ysb=P.sb([128, 256], BF16, 'ysb%d' % i),
                        ysT=P.sb([128, 2, 128], BF16, 'ysT%d' % i), zz=P.sb([128, 256], F32, 'zz%d' % i),
                        s2=P.sb([128, 256], F32, 's2%d' % i), catb=P.sb([128, D], BF16, 'catb%d' % i),
                        catT=P.sb([128, 8, 128], BF16, 'catT%d' % i), tmp=P.sb([128, D], F32, 'tmp%d' % i),
                        xn2=P.sb([128, D], BF16, 'xn2%d' % i), ss=P.sb([128, 4], F32, 'ss%d' % i),
                        lg=P.sb([128, 8], F32, 'lg%d' % i), lg2=P.sb([128, 8], F32, 'lg2%d' % i),
                        mk1=P.sb([128, 8], F32, 'mk1%d' % i), mk2=P.sb([128, 8], F32, 'mk2%d' % i),
                        m12=P.sb([128, 4], F32, 'm12%d' % i))

        p1sets = [p1_bufs(0), p1_bufs(1)]

        def p1_tile(ti, B_, YK):
            xt, mt, hxt, ysb, ysT, zz, s2 = B_['xt'], B_['mt'], B_['hxt'], B_['ysb'], B_['ysT'], B_['zz'], B_['s2']
            catb, catT, tmp, xn2, ss = B_['catb'], B_['catT'], B_['tmp'], B_['xn2'], B_['ss']
            lg, lg2, mk1, mk2, m12 = B_['lg'], B_['lg2'], B_['mk1'], B_['mk2'], B_['m12']
            tok = slice((g * GT + ti) * 128, (g * GT + ti + 1) * 128)
            P.dma('sp', xt[:], x[tok, :], r=([xdep] if xdep is not None else []), w=[xt])
            row0 = (g * GT + ti) * 128
            P.dma('sp', hxt[:], mixloc[row0 // 1024, :, row0 % 1024:row0 % 1024 + 128, :].rearrange("r n c -> n r c"), r=[MIXLOC], w=[hxt])
            for (c0, w_, d0) in ((0, 64, 0), (64, 64, 256), (128, 128, 512)):
                P.cp('pool', mt, mt[:, d0:d0 + 4 * w_].rearrange("p (r c) -> p r c", r=4), hxt, hxt[:, :, c0:c0 + w_])
            yield
            P.cp('dve', ysb, ysb[:], mt, mt[:, 256:512])
            yield
            for kc in range(2):
                P.tr(TB, TB[:, kc * 128:(kc + 1) * 128], ysb, ysb[:, kc * 128:(kc + 1) * 128], ident)
            P.cp('act', ysT, ysT[:].rearrange("p a b -> p (a b)"), TB, TB[:, 0:256])
            yield
            for kc in range(2):
                P.mm(ZP, ZP[:, :], ysT, ysT[:, kc, :], gluwb, gluwb[:, kc, :], start=(kc == 0), stop=(kc == 1))
            P.tt('dve', zz, zz[:], ZP, ZP[:, :], glub_t, glub_t[:], ALU.add)
            yield
            P.act(zz, zz[:], zz, zz[:], AF.Sigmoid)
            P.op('pool', lambda e: e.memset(ss[:], 0.0), w=[ss])
            yield
            P.tt('dve', s2, s2[:], zz, zz[:], mt, mt[:, 256:512], ALU.mult)
            P.act(junk, junk[:, 0:512], mt, mt[:, 512:1024], AF.Square, extra_w=[ss], accum_out=ss[:, 1:2])
            yield
            P.act(junk, junk[:, 0:256], s2, s2[:], AF.Square, extra_w=[ss], accum_out=ss[:, 0:1])
            P.cp('act', catb, catb[:, 0:256], mt, mt[:, 0:256])
            yield
            P.ts('dve', ss, ss[:, 0:1], ss, ss[:, 0:1], 1.0 / 256, ALU.mult, EPS, ALU.add)
            P.ts('dve', ss, ss[:, 1:2], ss, ss[:, 1:2], 1.0 / 512, ALU.mult, EPS, ALU.add)
            yield
            P.act(ss, ss[:, 0:2], ss, ss[:, 0:2], AF.Sqrt)
            yield
            P.op('dve', lambda e: e.reciprocal(out=ss[:, 0:2], in_=ss[:, 0:2]), r=[ss], w=[ss])
            yield
            P.op('dve', lambda e: e.scalar_tensor_tensor(out=catb[:, 256:512], in0=s2[:], scalar=ss[:, 0:1], in1=ssmg_t[:],
                                                         op0=ALU.mult, op1=ALU.mult), r=[s2, ss, ssmg_t], w=[catb])
            P.op('dve', lambda e: e.scalar_tensor_tensor(out=catb[:, 512:1024], in0=mt[:, 512:1024], scalar=ss[:, 1:2], in1=mlag_t[:],
                                                         op0=ALU.mult, op1=ALU.mult), r=[mt, ss, mlag_t], w=[catb])
            yield
            for half in range(2):
                for c4 in range(4):
                    c = half * 4 + c4
                    P.tr(TB, TB[:, c4 * 128:(c4 + 1) * 128], catb, catb[:, c * 128:(c + 1) * 128], ident)
                P.cp(evac_eng(), catT, catT[:, half * 4:(half + 1) * 4, :].rearrange("p a b -> p (a b)"), TB, TB[:, :])
                yield
            for half in range(2):
                for kc in range(8):
                    P.mm(YK[half], YK[half][:, :], catT, catT[:, kc, :], woutb, woutb[:, kc, half * 512:(half + 1) * 512],
                         start=(kc == 0), stop=(kc == 7))
            P.op('pool', lambda e: e.memset(ss[:, 2:4], 0.0), w=[ss])
            yield
            for half in range(2):
                P.act(junk, junk[:, 0:512], YK[half], YK[half][:, :], AF.Square, extra_w=[ss], accum_out=ss[:, 2 + half:3 + half])
            yield
            P.tt('dve', ss, ss[:, 2:3], ss, ss[:, 2:3], ss, ss[:, 3:4], ALU.add)
            P.ts('dve', ss, ss[:, 2:3], ss, ss[:, 2:3], 1.0 / D, ALU.mult, EPS, ALU.add)
            yield
            P.act(ss, ss[:, 2:3], ss, ss[:, 2:3], AF.Sqrt)
            yield
            P.op('dve', lambda e: e.reciprocal(out=ss[:, 2:3], in_=ss[:, 2:3]), r=[ss], w=[ss])
            yield
            for half in range(2):
                hs = slice(half * 512, (half + 1) * 512)
                P.op('dve', lambda e: e.scalar_tensor_tensor(out=tmp[:, hs], in0=YK[half][:, :], scalar=ss[:, 2:3], in1=vecm[:, hs],
                                                             op0=ALU.mult, op1=ALU.mult), r=[YK[half], ss, vecm], w=[tmp])
            yield
            P.tt('dve', X1[ti], X1[ti][:], tmp, tmp[:], xt, xt[:], ALU.add)
            P.op('pool', lambda e: e.memset(ss[:, 3:4], 0.0), w=[ss])
            yield
            P.act(junk, junk[:], X1[ti], X1[ti][:], AF.Square, extra_w=[ss], accum_out=ss[:, 3:4])
            yield
            P.ts('dve', ss, ss[:, 3:4], ss, ss[:, 3:4], 1.0 / D, ALU.mult, EPS, ALU.add)
            yield
            P.act(ss, ss[:, 3:4], ss, ss[:, 3:4], AF.Sqrt)
            yield
            P.op('dve', lambda e: e.reciprocal(out=ss[:, 3:4], in_=ss[:, 3:4]), r=[ss], w=[ss])
            yield
            P.ts('dve', xn2, xn2[:], X1[ti], X1[ti][:], ss[:, 3:4], ALU.mult, extra_r=[ss])
            yield
            for half in range(2):
                for c4 in range(4):
                    c = half * 4 + c4
                    P.tr(TB, TB[:, c4 * 128:(c4 + 1) * 128], xn2, xn2[:, c * 128:(c + 1) * 128], ident)
                for c4 in range(4):
                    c = half * 4 + c4
                    if c4 % 2 == 0:
                        P.act(hT2, hT2[:, c, ti * 128:(ti + 1) * 128], TB, TB[:, c4 * 128:(c4 + 1) * 128], AF.Identity,
                              extra_r=[scale2, sh2], scale=scale2[:, c:c + 1], bias=sh2[:, c:c + 1])
                    else:
                        P.ts('dve', hT2, hT2[:, c, ti * 128:(ti + 1) * 128], TB, TB[:, c4 * 128:(c4 + 1) * 128],
                             scale2[:, c:c + 1], ALU.mult, sh2[:, c:c + 1], ALU.add, extra_r=[scale2, sh2])
                yield
            if moe:
                for kc in range(8):
                    P.mm(LG, LG[:, :], hT2, hT2[:, kc, ti * 128:(ti + 1) * 128], routerb, routerb[:, kc, :],
                         start=(kc == 0), stop=(kc == 7))
                P.cp('dve', lg, lg[:], LG, LG[:, :])
                yield
                P.op('dve', lambda e: e.reduce_max(out=m12[:, 0:1], in_=lg[:], axis=AX.X), r=[lg], w=[m12])
                yield
                P.ts('dve', mk1, mk1[:], lg, lg[:], m12[:, 0:1], ALU.is_equal, extra_r=[m12])
                yield
                P.op('dve', lambda e: e.scalar_tensor_tensor(out=lg2[:], in0=mk1[:], scalar=-1e30, in1=lg[:],
                                                             op0=ALU.mult, op1=ALU.add), r=[mk1, lg], w=[lg2])
                yield
                P.op('dve', lambda e: e.reduce_max(out=m12[:, 1:2], in_=lg2[:], axis=AX.X), r=[lg2], w=[m12])
                yield
                P.ts('dve', mk2, mk2[:], lg2, lg2[:], m12[:, 1:2], ALU.is_equal, extra_r=[m12])
                P.tt('dve', m12, m12[:, 2:3], m12, m12[:, 1:2], m12, m12[:, 0:1], ALU.subtract)
                yield
                P.act(m12, m12[:, 2:3], m12, m12[:, 2:3], AF.Exp)
                yield
                P.ts('dve', m12, m12[:, 3:4], m12, m12[:, 2:3], 1.0, ALU.add)
                yield
                P.op('dve', lambda e: e.reciprocal(out=m12[:, 3:4], in_=m12[:, 3:4]), r=[m12], w=[m12])
                yield
                P.tt('dve', m12, m12[:, 2:3], m12, m12[:, 2:3], m12, m12[:, 3:4], ALU.mult)
                P.ts('dve', mk1, mk1[:], mk1, mk1[:], m12[:, 3:4], ALU.mult, extra_r=[m12])
                yield
                P.op('dve', lambda e: e.scalar_tensor_tensor(out=gatew[:, ti, :], in0=mk2[:], scalar=m12[:, 2:3], in1=mk1[:],
                                                             op0=ALU.mult, op1=ALU.add), r=[mk2, m12, mk1], w=[gatew])
                yield

        for pair in range(GT // 2):
            gens = [p1_tile(2 * pair, p1sets[0], YB), p1_tile(2 * pair + 1, p1sets[1], GB)]
            while gens:
                alive = []
                for gen_ in gens:
                    try:
                        next(gen_)
                        alive.append(gen_)
                    except StopIteration:
                        pass
                gens = alive
        P.barrier()
        ph.close()
        ph = ExitStack()
        P.tes = ph
        actT = P.sb([128, NFC, GT * 128], BF16, 'actT')
        wdb = P.sb([128, NFC, D], BF16, 'wdb')
        NSTG = 5
        stg = [P.sb([128, 1024], F32, 'stg%d' % i) for i in range(NSTG)]
        stgd = [P.sb([128, 1024], F32, 'stgd%d' % i) for i in range(2)]
        NWB = 4
        wgb = [P.sb([128, 8, 128], BF16, 'wgb%d' % i) for i in range(NWB)]
        wub = [P.sb([128, 8, 128], BF16, 'wub%d' % i) for i in range(NWB)]
        gsil = [P.sb([128, GT * 128], F32, 'gsil%d' % i) for i in range(2)]
        ftmp = P.sb([128, D], F32, 'ftmp')
        fjunk = P.sb([128, D], BF16, 'fjunk')
        fss = P.sb([128, 1], F32, 'fss')
        st_i = [0]

        sd_i = [0]

        def load_cast(dst, dst_ap, src_ap, eng):
            if eng == 'pool':
                sg = stgd[sd_i[0] % 2]
                sd_i[0] += 1
            else:
                sg = stg[st_i[0] % NSTG]
                st_i[0] += 1
            P.dma('sp', sg[:], src_ap, w=[sg])
            P.cp(eng, dst, dst_ap, sg, sg[:])

        def load_gu(idx):
            b_ = idx % NWB
            load_cast(wgb[b_], wgb[b_][:].rearrange("p a b -> p (a b)"), wg_d[idx, :, :], 'dve')
            load_cast(wub[b_], wub[b_][:].rearrange("p a b -> p (a b)"), wu_d[idx, :, :], 'act')

        load_gu(0)
        load_gu(1)
        load_gu(2)
        for e_ in range(n_exp):
            for fc in range(NFC):
                b2 = fc % 2
                wb = (e_ * NFC + fc) % NWB
                if e_ * NFC + fc + 3 < n_exp * NFC:
                    load_gu(e_ * NFC + fc + 3)
                load_cast(wdb, wdb[:, fc, :], wd_d[e_ * NFC + fc, :, :], 'pool')
                for kc in range(8):
                    P.mm(GB[b2], GB[b2][:, :], wgb[wb], wgb[wb][:, kc, :], hT2, hT2[:, kc, :], start=(kc == 0), stop=(kc == 7))
                for kc in range(8):
                    P.mm(UB[b2], UB[b2][:, :], wub[wb], wub[wb][:, kc, :], hT2, hT2[:, kc, :], start=(kc == 0), stop=(kc == 7))
                P.act(gsil[b2], gsil[b2][:], GB[b2], GB[b2][:, :], AF.Silu)
                P.tt('dve', actT, actT[:, fc, :], UB[b2], UB[b2][:, :], gsil[b2], gsil[b2][:], ALU.mult)
            for ti in range(GT):
                for half in range(2):
                    hs = slice(half * 512, (half + 1) * 512)
                    yb = YB[(ti * 2 + half) % 2]
                    for fc in range(NFC):
                        P.mm(yb, yb[:, :], actT, actT[:, fc, ti * 128:(ti + 1) * 128], wdb, wdb[:, fc, hs],
                             start=(fc == 0), stop=(fc == NFC - 1))
                    if not moe:
                        P.cp(evac_eng(), yacc[ti], yacc[ti][:, hs], yb, yb[:, :])
                    elif e_ == 0:
                        P.ts('dve', yacc[ti], yacc[ti][:, hs], yb, yb[:, :], gatew[:, ti, 0:1], ALU.mult, extra_r=[gatew])
                    else:
                        P.op('dve', lambda e: e.scalar_tensor_tensor(out=yacc[ti][:, hs], in0=yb[:, :], scalar=gatew[:, ti, e_:e_ + 1],
                                                                     in1=yacc[ti][:, hs], op0=ALU.mult, op1=ALU.add),
                             r=[yb, gatew, yacc[ti]], w=[yacc[ti]])
        for ti in range(GT):
            tok = slice((g * GT + ti) * 128, (g * GT + ti + 1) * 128)
            P.op('pool', lambda e: e.memset(fss[:], 0.0), w=[fss])
            P.act(fjunk, fjunk[:], yacc[ti], yacc[ti][:], AF.Square, extra_w=[fss], accum_out=fss[:, 0:1])
            P.rstd(fss, D, None)
            P.op('dve', lambda e: e.scalar_tensor_tensor(out=ftmp[:], in0=yacc[ti][:], scalar=fss[:, 0:1], in1=vecf[:],
                                                         op0=ALU.mult, op1=ALU.mult), r=[yacc[ti], fss, vecf], w=[ftmp])
            P.tt('dve', ftmp, ftmp[:], ftmp, ftmp[:], X1[ti], X1[ti][:], ALU.add)
            P.dma('sp', out[tok, :], ftmp[:], r=[ftmp])
        P.barrier()
        ph.close()
        P.tes = None
        if on_group is not None:
            on_group(g)
    st.close()
    P.es = old_es
    P.pre = {}


def inputs_B(inp, layer, xcur, mix):
    f = lambda a: np.ascontiguousarray(a, dtype=np.float32)
    moe = (layer % 2 == 1)
    j = layer // 2
    xf = xcur.reshape(-1, D)
    mf = mix.reshape(-1, D)
    aw = f(inp['ada_w'][layer][:, 2048:6144].reshape(128, 8, 4096))
    ab = inp['ada_b'][layer]
    shared = dict(
        adaw=aw, adab_row=f(ab[2048:6144][None, :]), adab_col=chunkT(f(ab[2048:6144]), 32),
        gpost_m=f(inp['norm_post_mix'][layer][None, :]), gpre_f=chunkT(f(inp['norm_pre_ffn'][layer]), 8),
        gpost_f=f(inp['norm_post_ffn'][layer][None, :]),
        gluw=f(inp['ssm_glu_w'][layer].reshape(2, 128, 256).transpose(1, 0, 2)),
        glub=f(inp['ssm_glu_b'][layer][None, :]), ssmg=f(inp['ssm_norm'][layer][None, :]),
        mlag=f(inp['mla_norm'][layer][None, :]),
        wout=f(inp['w_out'][layer].reshape(8, 128, D).transpose(1, 0, 2)))
    if moe:
        wg, wu, wd = inp['moe_w_gate'][j], inp['moe_w_up'][j], inp['moe_w_down'][j]
        shared['router'] = f(inp['moe_router'][j].reshape(8, 128, 8).transpose(1, 0, 2))
    else:
        wg, wu, wd = inp['ffn_w_gate'][j][None], inp['ffn_w_up'][j][None], inp['ffn_w_down'][j][None]
    ne = wg.shape[0]

    def gu(w):
        return f(w.reshape(ne, 8, 128, NFC, 128).transpose(0, 3, 2, 1, 4).reshape(ne * NFC, 128, 1024))

    shared['wg'] = gu(wg)
    shared['wu'] = gu(wu)
    shared['wd'] = f(wd.reshape(ne * NFC, 128, D))
    maps = []
    for core in range(8):
        b = core // 4
        m = dict(shared)
        m['x'] = f(xf[core * TPC:(core + 1) * TPC])
        m['mix'] = f(mf[core * TPC:(core + 1) * TPC])
        m['cvec'] = f(inp['c'][b].reshape(128, 8))
        maps.append(m)
    return maps


RG4 = [[0, 1, 2, 3], [4, 5, 6, 7]]


def build_fused(ntiles=NT, ngroups=NG):
    nc = bass.Bass("TRN2", target_bir_lowering=False)
    ioA = [decl_A(nc, '_a%d' % l) for l in range(2)]
    ioB = [decl_B(nc, '_b%d' % l, moe=(l == 1)) for l in range(2)]
    x = nc.dram_tensor("x", [L, D], F32, kind="ExternalInput").ap()
    xsl = nc.dram_tensor("xsl", [TPC, D], F32, kind="ExternalInput").ap()
    out = nc.dram_tensor("out", [TPC, D], F32, kind="ExternalOutput").ap()
    hx = [nc.dram_tensor("hx%d" % l, [L, 256], F32).ap() for l in range(2)]
    hxall = [nc.dram_tensor("hxall%d" % l, [8 * 4 * 1024, 256], F32).ap() for l in range(2)]
    mixloc = [nc.dram_tensor("mixloc%d" % l, [2, 4, 1024, 256], F32).ap() for l in range(2)]
    xs = nc.dram_tensor("xs", [TPC, D], F32).ap()
    xall = nc.dram_tensor("xall", [8 * 4 * 256, D], F32).ap()

    def xrow1(t):
        tok0 = t * 128
        r_, j_, i0 = tok0 // TPC, (tok0 % TPC) // 256, tok0 % 256
        return xall[(j_ * 4 + r_) * 256 + i0:(j_ * 4 + r_) * 256 + i0 + 128, :]

    with ExitStack() as es:
        P = Prog(nc, es)
        banks = [P.ps([128, 512], F32, 'bank%d' % i) for i in range(8)]
        rank256 = nc.sync.snap((nc.sync.partition_id() % 4) * 256, min_val=0, max_val=768)

        H = [P.view(hxall[l], 'HXALL%d' % l) for l in range(2)]
        XALL = P.view(xall, 'XALL')

        def hx_chunk(l):
            def f(k, evs):
                for ev in evs:
                    P._wait('pool', ev)
                P.coll('AllGather', hx[l][k * 1024:(k + 1) * 1024, :], H[l], hxall[l][k * 4096:(k + 1) * 4096, :], RG4)
            return f

        def xs_group(g):
            for j in (2 * g, 2 * g + 1):
                P.coll('AllGather', xs[j * 256:(j + 1) * 256, :], XALL, xall[j * 1024:(j + 1) * 1024, :], RG4)

        emit_A(P, nc, banks, ioA[0], lambda t: x[t * 128:(t + 1) * 128, :], hx[0], ntiles, on_chunk=hx_chunk(0))
        emit_B(P, nc, banks, ioB[0], xsl, hxall[0], H[0], mixloc[0], xs, rank256, False, ngroups, on_group=xs_group)
        emit_A(P, nc, banks, ioA[1], xrow1, hx[1], ntiles, xdep=XALL, on_chunk=hx_chunk(1))
        emit_B(P, nc, banks, ioB[1], xs, hxall[1], H[1], mixloc[1], out, rank256, True, ngroups)
        P.finish()
    return nc


def fused_inputs(inp):
    x = np.ascontiguousarray(inp['x'], dtype=np.float32)
    dummy = np.zeros((2, L, D), np.float32)
    maps = [dict() for _ in range(8)]
    for layer in range(2):
        ma = inputs_A(inp, layer, x)
        mb = inputs_B(inp, layer, x, dummy)
        for c in range(8):
            for k, v in ma[c].items():
                if k != 'x':
                    maps[c][k + '_a%d' % layer] = v
            for k, v in mb[c].items():
                if k not in ('x', 'mix'):
                    maps[c][k + '_b%d' % layer] = v
    xf = x.reshape(-1, D)
    for c in range(8):
        maps[c]['x'] = x[c // 4]
        maps[c]['xsl'] = np.ascontiguousarray(xf[c * TPC:(c + 1) * TPC])
    return maps


def kernel(**inputs):
    inp = {k: np.asarray(v) for k, v in inputs.items()}
    if 'F' not in _CACHE:
        _CACHE['F'] = build_fused()
    res = run_bass_kernel_spmd(_CACHE['F'], fused_inputs(inp), core_ids=list(range(8)))
    return np.concatenate([res.results[c]['out'] for c in range(8)], axis=0).reshape(2, L, D).astype(np.float32)
```
